# Optimizing a Trainium2 kernel written in Bass

```python
import numpy as np
import jax
import jax.numpy as jnp
from jax import lax

D_MODEL = 1024
BATCH = 16
SEQ = 2048
DEPTH = 2

EPS = 1e-6
NEG_BIG = -1e30
TINY = 1e-30
D_FF = ((8 * D_MODEL // 3 + 127) // 128) * 128
N_SUB = 3
N_BRANCH = 3

ML_WIDTH = D_MODEL
ML_HEADS = 4
ML_HD = ML_WIDTH // ML_HEADS
ML_CONV = 4
ML_CHUNK = 64

RW_WIDTH = D_MODEL
RW_HEAD = 64
RW_HEADS = RW_WIDTH // RW_HEAD
RW_DECAY_LORA = 64
RW_AAA_LORA = 64
RW_MV_LORA = 32
RW_GATE_LORA = 128
RW_LN_EPS = 64e-5
RW_SPLITS = (RW_WIDTH, RW_WIDTH, RW_WIDTH, RW_DECAY_LORA, RW_AAA_LORA, RW_GATE_LORA)
RW_SHIFT_COLS = 3 * RW_WIDTH + RW_DECAY_LORA + RW_AAA_LORA + RW_GATE_LORA

HG_WIDTH = D_MODEL
HG_EXPAND = 128
HG_HEADS = HG_WIDTH // HG_EXPAND
HG_CHUNK = 32

IN_SPLITS = (ML_WIDTH, ML_WIDTH, ML_HEADS, ML_HEADS, RW_SHIFT_COLS,
             HG_WIDTH, HG_WIDTH, HG_WIDTH, HG_WIDTH, N_BRANCH * D_MODEL)
N_IN = sum(IN_SPLITS)

kernel_name = 'hybrid_mlstm_rwkv7_hgrn2_macaron_adaln'


def rms_norm(x, w, eps=EPS):
    xf = x.astype(jnp.float32)
    y = xf * lax.rsqrt(jnp.mean(xf * xf, axis=-1, keepdims=True) + eps)
    return (y * w).astype(x.dtype)


def head_norm(x, n_heads, w, b=None, eps=EPS, center=False):
    shp = x.shape
    xf = x.astype(jnp.float32).reshape(*shp[:-1], n_heads, shp[-1] // n_heads)
    if center:
        xf = xf - jnp.mean(xf, axis=-1, keepdims=True)
    xf = xf * lax.rsqrt(jnp.mean(xf * xf, axis=-1, keepdims=True) + eps)
    y = xf.reshape(shp) * w
    if b is not None:
        y = y + b
    return y.astype(x.dtype)


def modulate(x, w, shift, scale):
    return rms_norm(x, w) * (1.0 + scale) + shift


def swiglu(h, w_up, w_down):
    a, b = jnp.split(h @ w_up, 2, axis=-1)
    return (jax.nn.silu(a) * b) @ w_down


def split_cols(z, sizes):
    return jnp.split(z, np.cumsum(sizes)[:-1].tolist(), axis=-1)


def split_heads(x, n):
    B, T, W = x.shape
    return x.reshape(B, T, n, W // n).transpose(0, 2, 1, 3)


def merge_heads(y):
    B, H, T, d = y.shape
    return y.transpose(0, 2, 1, 3).reshape(B, T, H * d)


def to_chunks(x, L):
    B, H, T = x.shape[:3]
    return jnp.moveaxis(x.reshape(B, H, T // L, L, *x.shape[3:]), 2, 0)


def from_chunks(y):
    y = jnp.moveaxis(y, 0, 2)
    return y.reshape(y.shape[0], y.shape[1], -1, y.shape[-1])


def token_shift(z, mu):
    prev = jnp.pad(z, ((0, 0), (1, 0), (0, 0)))[:, :-1]
    return z + mu * (prev - z)


def causal_conv(x, w, b):
    K, C = w.shape
    y = lax.conv_general_dilated(x, w.astype(x.dtype)[:, None, :], window_strides=(1,),
                                 padding=((K - 1, 0),), dimension_numbers=('NWC', 'WIO', 'NWC'),
                                 feature_group_count=C)
    return y + b


def mlstm_chunkwise(q, k, v, log_i, log_f):
    f32 = jnp.float32
    B, H, T, dk = q.shape
    dv = v.shape[-1]
    L = ML_CHUNK
    q, k, v, log_i, log_f = (t.astype(f32) for t in (q, k, v, log_i, log_f))
    k = k * (dk ** -0.5)
    causal = jnp.tril(jnp.ones((L, L), dtype=bool))

    def step(carry, inp):
        C, n, m = carry
        qc, kc, vc, li, lf = inp
        b = jnp.cumsum(lf, axis=-1)
        log_d = jnp.where(causal, b[..., :, None] - b[..., None, :] + li[..., None, :], NEG_BIG)
        log_inter = b + m[..., None]
        m_t = jnp.maximum(log_inter, jnp.max(log_d, axis=-1))
        w_intra = jnp.exp(log_d - m_t[..., None])
        w_inter = jnp.exp(log_inter - m_t)
        s = jnp.einsum('bhtd,bhsd->bhts', qc, kc) * w_intra
        num = (w_inter[..., None] * jnp.einsum('bhtd,bhde->bhte', qc, C)
               + jnp.einsum('bhts,bhse->bhte', s, vc))
        den = w_inter * jnp.einsum('bhtd,bhd->bht', qc, n) + jnp.sum(s, axis=-1)
        h = num / jnp.maximum(jnp.abs(den), jnp.exp(-m_t))[..., None]
        b_end = b[..., -1]
        log_ws = b_end[..., None] - b + li
        m_new = jnp.maximum(b_end + m, jnp.max(log_ws, axis=-1))
        ws = jnp.exp(log_ws - m_new[..., None])
        decay = jnp.exp(b_end + m - m_new)
        kw = kc * ws[..., None]
        C = decay[..., None, None] * C + jnp.einsum('bhsd,bhse->bhde', kw, vc)
        n = decay[..., None] * n + jnp.sum(kw, axis=2)
        return (C, n, m_new), h

    init = (jnp.zeros((B, H, dk, dv), f32), jnp.zeros((B, H, dk), f32), jnp.zeros((B, H), f32))
    xs = (to_chunks(q, L), to_chunks(k, L), to_chunks(v, L), to_chunks(log_i, L), to_chunks(log_f, L))
    _, h = lax.scan(step, init, xs)
    return from_chunks(h)


def mlstm_branch(x_m, o_pre, i_pre, f_pre, conv_w, conv_b, wq, wk, i_b, f_b, norm_w, skip):
    xc = jax.nn.silu(causal_conv(x_m, conv_w, conv_b))
    xc_h = split_heads(xc, ML_HEADS)
    q = jnp.einsum('bhtd,hde->bhte', xc_h, wq)
    k = jnp.einsum('bhtd,hde->bhte', xc_h, wk)
    v = split_heads(x_m, ML_HEADS)
    log_i = jnp.swapaxes(i_pre + i_b, 1, 2)
    log_f = jax.nn.log_sigmoid(jnp.swapaxes(f_pre + f_b, 1, 2).astype(jnp.float32))
    h = merge_heads(mlstm_chunkwise(q, k, v, log_i, log_f)).astype(x_m.dtype)
    return head_norm(h * jax.nn.sigmoid(o_pre), ML_HEADS, norm_w, center=True) + skip * xc


def wkv7_scan(r, log_w, k, v, kk, a):
    T, B, H, N = r.shape

    def step(S, inp):
        r_t, lw_t, k_t, v_t, kk_t, a_t = inp
        sa = jnp.einsum('bhvk,bhk->bhv', S, -kk_t)
        S = (S * jnp.exp(lw_t)[:, :, None, :]
             + sa[..., None] * (kk_t * a_t)[:, :, None, :]
             + v_t[..., None] * k_t[:, :, None, :])
        return S, jnp.einsum('bhvk,bhk->bhv', S, r_t)

    _, y = lax.scan(step, jnp.zeros((B, H, N, N), jnp.float32), (r, log_w, k, v, kk, a))
    return y


def rwkv7_branch(feats, vres, v_first, mu, w0, w2, a0, a2, g2, k_k, k_a, r_k, ln_w, ln_b):
    B, T, _ = feats.shape
    f32 = jnp.float32
    r, k, v, w_lo, a_lo, g_lo = split_cols(token_shift(feats, mu), RW_SPLITS)
    w_raw = -jax.nn.softplus(-(w0 + jnp.tanh(w_lo) @ w2).astype(f32)) - 0.5
    log_w = -jnp.exp(w_raw)
    a = jax.nn.sigmoid(a0 + a_lo @ a2)
    g = jax.nn.sigmoid(g_lo) @ g2
    if vres is None:
        v_first = v
    else:
        v_lo, mu_v, v0, v2 = vres
        v = v + (v_first - v) * jax.nn.sigmoid(v0 + token_shift(v_lo, mu_v) @ v2)

    def hl(t):
        return t.reshape(B, T, RW_HEADS, RW_HEAD)

    kk = hl(k * k_k).astype(f32)
    kk = kk / jnp.maximum(jnp.sqrt(jnp.sum(kk * kk, axis=-1, keepdims=True)), 1e-12)
    k = k * (1.0 + (a - 1.0) * k_a)

    def tm(t):
        return jnp.moveaxis(hl(t).astype(f32), 1, 0)

    y = wkv7_scan(tm(r), tm(log_w), tm(k), tm(v), jnp.moveaxis(kk, 1, 0), tm(a))
    y = jnp.moveaxis(y, 0, 1).reshape(B, T, RW_WIDTH).astype(feats.dtype)
    y = head_norm(y, RW_HEADS, ln_w, ln_b, eps=RW_LN_EPS, center=True)
    bonus = (jnp.sum(hl(r) * hl(k) * r_k, axis=-1, keepdims=True) * hl(v)).reshape(B, T, RW_WIDTH)
    return (y + bonus) * g, v_first


def gla_chunkwise(q, k, v, log_f):
    f32 = jnp.float32
    B, H, T, dk = q.shape
    dv = v.shape[-1]
    L = HG_CHUNK
    q, k, v, log_f = (t.astype(f32) for t in (q, k, v, log_f))
    causal = jnp.tril(jnp.ones((L, L), dtype=bool))

    def step(S, inp):
        qc, kc, vc, gc = inp
        b = jnp.cumsum(gc, axis=2)
        inter = jnp.einsum('bhtd,bhde->bhte', qc * jnp.exp(b), S)
        rel = jnp.minimum(b[:, :, :, None, :] - b[:, :, None, :, :], 0.0)
        decay = jnp.where(causal[:, :, None], jnp.exp(rel), 0.0)
        A = jnp.einsum('bhtd,bhtsd,bhsd->bhts', qc, decay, kc)
        o = inter + jnp.einsum('bhts,bhse->bhte', A, vc)
        b_end = b[:, :, -1:, :]
        S = (jnp.exp(b_end[:, :, 0, :])[..., None] * S
             + jnp.einsum('bhsd,bhse->bhde', kc * jnp.exp(b_end - b), vc))
        return S, o

    xs = (to_chunks(q, L), to_chunks(k, L), to_chunks(v, L), to_chunks(log_f, L))
    _, o = lax.scan(step, jnp.zeros((B, H, dk, dv), f32), xs)
    return from_chunks(o)


def hgrn2_branch(q_pre, f_pre, i_in, g_pre, lb, norm_w):
    fp = f_pre.astype(jnp.float32)
    q = jax.nn.silu(q_pre)
    sig = jax.nn.sigmoid(fp)
    k = (1.0 - lb) * (1.0 - sig)
    log_f = jnp.log(jnp.maximum(lb + (1.0 - lb) * sig, TINY))
    o = gla_chunkwise(split_heads(q, HG_HEADS), split_heads(k, HG_HEADS),
                      split_heads(i_in, HG_HEADS), split_heads(log_f, HG_HEADS))
    o = merge_heads(o).astype(q_pre.dtype)
    return head_norm(o, HG_HEADS, norm_w) * jax.nn.sigmoid(g_pre)


def setup_inputs(seed: int = 0) -> dict:
    key = jax.random.key(seed)
    ks = iter(jax.random.split(key, 48))

    def nrm(shape, s=1.0):
        return s * jax.random.normal(next(ks), shape, jnp.float32)

    L, D, Lv = DEPTH, D_MODEL, DEPTH - 1
    return {
        'x': nrm((BATCH, SEQ, D)),
        'c': nrm((BATCH, D)),
        'norm_w': 1.0 + nrm((L, N_SUB, D), 0.02),
        'final_norm_w': 1.0 + nrm((D,), 0.02),
        'ada_w': nrm((L, D, N_SUB * 3 * D), 0.1 * D ** -0.5),
        'ada_b': nrm((L, N_SUB * 3 * D), 0.02),
        'ffn_up': nrm((L, 2, D, 2 * D_FF), D ** -0.5),
        'ffn_down': nrm((L, 2, D_FF, D), D_FF ** -0.5),
        'w_in': nrm((L, D, N_IN), D ** -0.5),
        'w_in_vres': nrm((Lv, D, RW_MV_LORA), D ** -0.5),
        'ml_conv_w': nrm((L, ML_CONV, ML_WIDTH), ML_CONV ** -0.5),
        'ml_conv_b': nrm((L, ML_WIDTH), 0.02),
        'ml_wq': nrm((L, ML_HEADS, ML_HD, ML_HD), ML_HD ** -0.5),
        'ml_wk': nrm((L, ML_HEADS, ML_HD, ML_HD), ML_HD ** -0.5),
        'ml_i_b': nrm((L, ML_HEADS), 0.1),
        'ml_f_b': jnp.linspace(3.0, 6.0, ML_HEADS)[None] + nrm((L, ML_HEADS), 0.1),
        'ml_norm_w': 1.0 + nrm((L, ML_WIDTH), 0.02),
        'ml_skip': 1.0 + nrm((L, ML_WIDTH), 0.02),
        'rw_mu': jax.random.uniform(next(ks), (L, RW_SHIFT_COLS), jnp.float32),
        'rw_mu_vres': jax.random.uniform(next(ks), (Lv, RW_MV_LORA), jnp.float32),
        'rw_w0': jnp.linspace(-6.5, -1.5, RW_WIDTH)[None] + nrm((L, RW_WIDTH), 0.1),
        'rw_w2': nrm((L, RW_DECAY_LORA, RW_WIDTH), 0.1),
        'rw_a0': nrm((L, RW_WIDTH), 0.1),
        'rw_a2': nrm((L, RW_AAA_LORA, RW_WIDTH), 0.1),
        'rw_v0': 1.0 + nrm((Lv, RW_WIDTH), 0.1),
        'rw_v2': nrm((Lv, RW_MV_LORA, RW_WIDTH), 0.1),
        'rw_g2': nrm((L, RW_GATE_LORA, RW_WIDTH), RW_GATE_LORA ** -0.5),
        'rw_k_k': 0.85 + nrm((L, RW_WIDTH), 0.02),
        'rw_k_a': 1.0 + nrm((L, RW_WIDTH), 0.02),
        'rw_r_k': nrm((L, RW_HEADS, RW_HEAD), 0.1),
        'rw_ln_w': 1.0 + nrm((L, RW_WIDTH), 0.02),
        'rw_ln_b': nrm((L, RW_WIDTH), 0.02),
        'hg_lb_logits': nrm((L, HG_WIDTH), 0.5),
        'hg_norm_w': 1.0 + nrm((L, HG_WIDTH), 0.02),
        'w_branch': nrm((L, N_BRANCH, D, D), D ** -0.5),
        'w_out': nrm((L, D, D), D ** -0.5),
    }


def reference(x, c, norm_w, final_norm_w, ada_w, ada_b, ffn_up, ffn_down, w_in, w_in_vres,
              ml_conv_w, ml_conv_b, ml_wq, ml_wk, ml_i_b, ml_f_b, ml_norm_w, ml_skip,
              rw_mu, rw_mu_vres, rw_w0, rw_w2, rw_a0, rw_a2, rw_v0, rw_v2, rw_g2,
              rw_k_k, rw_k_a, rw_r_k, rw_ln_w, rw_ln_b, hg_lb_logits, hg_norm_w,
              w_branch, w_out):
    B, T, D = x.shape
    lb_soft = jax.nn.softmax(hg_lb_logits.astype(jnp.float32), axis=0)
    lower_bounds = jnp.cumsum(lb_soft, axis=0) - lb_soft[0]
    cond = jax.nn.silu(c)
    v_first = None
    for l in range(DEPTH):
        mod = (cond @ ada_w[l] + ada_b[l]).reshape(B, N_SUB, 3, 1, D)

        h = modulate(x, norm_w[l, 0], mod[:, 0, 0], mod[:, 0, 1])
        x = x + 0.5 * (1.0 + mod[:, 0, 2]) * swiglu(h, ffn_up[l, 0], ffn_down[l, 0])

        h = modulate(x, norm_w[l, 1], mod[:, 1, 0], mod[:, 1, 1])
        if l == 0:
            z = h @ w_in[l]
            parts = split_cols(z, IN_SPLITS)
            vres = None
        else:
            z = h @ jnp.concatenate([w_in[l], w_in_vres[l - 1]], axis=1)
            parts = split_cols(z, IN_SPLITS + (RW_MV_LORA,))
            vres = (parts[10], rw_mu_vres[l - 1], rw_v0[l - 1], rw_v2[l - 1])
        ml_x, ml_o, ml_i, ml_f, rw_feats, hg_q, hg_f, hg_i, hg_g, gate_pre = parts[:10]

        y_ml = mlstm_branch(ml_x, ml_o, ml_i, ml_f, ml_conv_w[l], ml_conv_b[l], ml_wq[l], ml_wk[l],
                            ml_i_b[l], ml_f_b[l], ml_norm_w[l], ml_skip[l])
        y_rw, v_first = rwkv7_branch(rw_feats, vres, v_first, rw_mu[l], rw_w0[l], rw_w2[l],
                                     rw_a0[l], rw_a2[l], rw_g2[l], rw_k_k[l], rw_k_a[l],
                                     rw_r_k[l], rw_ln_w[l], rw_ln_b[l])
        y_hg = hgrn2_branch(hg_q, hg_f, hg_i, hg_g, lower_bounds[l], hg_norm_w[l])

        y_br = jnp.einsum('btnw,nwd->btnd', jnp.stack([y_ml, y_rw, y_hg], axis=2), w_branch[l])
        gates = jax.nn.sigmoid(gate_pre.reshape(B, T, N_BRANCH, D))
        mixed = jnp.sum(gates * y_br, axis=2) @ w_out[l]
        x = x + (1.0 + mod[:, 1, 2]) * mixed

        h = modulate(x, norm_w[l, 2], mod[:, 2, 0], mod[:, 2, 1])
        x = x + 0.5 * (1.0 + mod[:, 2, 2]) * swiglu(h, ffn_up[l, 1], ffn_down[l, 1])
    return rms_norm(x, final_norm_w)
```

```python
import contextlib
import numpy as np
import concourse.bass as bass
import concourse.mybir as mybir

F32 = mybir.dt.float32
BF16 = mybir.dt.bfloat16
ALU = mybir.AluOpType
AF = mybir.ActivationFunctionType
AX = mybir.AxisListType

ENGS = ("pe", "act", "dve", "pool", "sp")


class Tok:
    __slots__ = ("w", "r", "name")

    def __init__(self, name=""):
        self.w = None
        self.r = {}
        self.name = name


class V:
    __slots__ = ("ap", "toks")

    def __init__(self, ap, toks):
        self.ap = ap
        self.toks = toks

    def __getitem__(self, idx):
        return V(self.ap[idx], self.toks)

    def re(self, s, **kw):
        return V(self.ap.rearrange(s, **kw), self.toks)

    def bc(self, shape):
        return V(self.ap.broadcast_to(shape), self.toks)

    def un(self, axis):
        return V(self.ap.unsqueeze(axis), self.toks)

    def bitcast(self, dt):
        return V(self.ap.bitcast(dt), self.toks)

    @property
    def shape(self):
        return self.ap.shape


class Prog:
    def __init__(self, nc, es, n_dma_sems=40):
        self.nc = nc
        self.es = es
        self.q = {e: [] for e in ENGS}
        self.cnt = {e: 0 for e in ENGS}
        self.sem = {e: es.enter_context(nc.semaphore("s_" + e)) for e in ENGS}
        self.semobj = dict(self.sem)
        self.waited = {e: {} for e in ENGS}
        self.dsems = []
        for i in range(n_dma_sems):
            k = "d%d" % i
            self.semobj[k] = es.enter_context(nc.semaphore(k))
            self.dsems.append([k, 0, None])
        self.dnext = 0
        self.uid = 0

    def sb(self, shape, dt=F32, name=None):
        self.uid += 1
        name = name or ("t%d" % self.uid)
        t = self.es.enter_context(self.nc.sbuf_tensor(name, list(shape), dt))
        return V(t[:], (Tok(name),))

    def ps(self, shape, dt=F32, name=None):
        self.uid += 1
        name = name or ("p%d" % self.uid)
        shape = list(shape)
        n = 1
        for d_ in shape[1:]:
            n *= d_
        nb = (n + 511) // 512
        t = self.es.enter_context(self.nc.psum_tensor(name, [shape[0], nb * 512], dt))
        ap = t[:][:, 0:n]
        if len(shape) == 3:
            ap = ap.rearrange("p (a b) -> p a b", a=shape[1])
        elif len(shape) == 4:
            ap = ap.rearrange("p (a b c) -> p a b c", a=shape[1], b=shape[2])
        return V(ap, (Tok(name),))

    def dram(self, name, shape, dt=F32, kind="Internal"):
        t = self.nc.dram_tensor(name, list(shape), dt, kind=kind)
        return V(t.ap(), (Tok(name),))

    def sub(self, v, name=""):
        return V(v.ap, (Tok(name),))

    def _needs(self, reads, writes):
        needs = {}

        def need(k, val):
            if val > needs.get(k, 0):
                needs[k] = val
        for v in reads:
            for t in v.toks:
                if t.w is not None:
                    need(*t.w)
        for v in writes:
            for t in v.toks:
                if t.w is not None:
                    need(*t.w)
                for k, val in t.r.items():
                    need(k, val)
        return needs

    def issue(self, eng, fn, reads, writes, inc=True, pe_group=False):
        needs = self._needs(reads, writes)
        waits = []
        wd = self.waited[eng]
        for k, val in needs.items():
            if k == eng and eng == "pe":
                continue
            if wd.get(k, 0) < val:
                wd[k] = val
                waits.append((k, val))
        n = self.cnt[eng] + 1
        if inc:
            self.cnt[eng] = n
        self.q[eng].append((waits, fn, inc))
        for v in reads:
            for t in v.toks:
                if t.r.get(eng, 0) < n:
                    t.r[eng] = n
        for v in writes:
            for t in v.toks:
                t.w = (eng, n)
                t.r = {}
        return n

    def dma(self, out, in_, eng="sp", **kw):
        slot = self.dsems[self.dnext]
        self.dnext = (self.dnext + 1) % len(self.dsems)
        key, cur, _ = slot
        needs = self._needs([in_], [out])
        wd = self.waited[eng]
        waits = []
        if cur > 0:
            needs[key] = max(needs.get(key, 0), cur)
        for k, val in needs.items():
            if wd.get(k, 0) < val:
                wd[k] = val
                waits.append((k, val))
        newv = cur + 16
        slot[1] = newv
        oap, iap = out.ap, in_.ap

        def fn(e, oap=oap, iap=iap, kw=kw):
            return e.dma_start(out=oap, in_=iap, **kw)
        self.q[eng].append((waits, fn, ("dma", key)))
        for t in in_.toks:
            if t.r.get(key, 0) < newv:
                t.r[key] = newv
        for t in out.toks:
            t.w = (key, newv)
            t.r = {}

    def emit(self, final_waits=()):
        nc = self.nc
        engmap = {"pe": "tensor", "act": "scalar", "dve": "vector", "pool": "gpsimd", "sp": "sync"}
        with nc.Block() as block:
            for e in ENGS:
                lst = self.q[e]

                def body(eh, lst=lst, e=e):
                    for waits, fn, inc in lst:
                        for k, val in waits:
                            eh.wait_ge(self.semobj[k], val)
                        if fn is None:
                            continue
                        ins = fn(eh)
                        if inc is True:
                            ins.then_inc(self.semobj[e], 1)
                        elif inc:
                            ins.then_inc(self.semobj[inc[1]], 16)
                    if e == "sp":
                        for k, val in final_waits:
                            eh.wait_ge(self.semobj[k], val)
                getattr(block, engmap[e])(body)

    def mm(self, out, lhsT, rhs, start=True, stop=True):
        o, l, r = out.ap, lhsT.ap, rhs.ap

        def fn(e):
            return e.matmul(o, l, r, start=start, stop=stop)
        if stop:
            self.issue("pe", fn, [lhsT, rhs], [out], inc=True)
        else:
            self.issue("pe", fn, [lhsT, rhs], [], inc=False)
        return

    def mm_group(self, out, pairs):
        n = len(pairs)
        for i, (l, r) in enumerate(pairs):
            o, la, ra = out.ap, l.ap, r.ap
            st, sp_ = (i == 0), (i == n - 1)

            def fn(e, o=o, la=la, ra=ra, st=st, sp_=sp_):
                return e.matmul(o, la, ra, start=st, stop=sp_)
            if sp_ and n == 1:
                self.issue("pe", fn, [l, r], [out], inc=True)
            elif st:
                needs_v = V(out.ap, out.toks)
                self.issue_pe_first(fn, [l, r], needs_v)
            elif sp_:
                self.issue("pe", fn, [l, r], [out], inc=True)
            else:
                self.issue("pe", fn, [l, r], [], inc=False)

    def issue_pe_first(self, fn, reads, outv):
        eng = "pe"
        needs = self._needs(reads, [outv])
        waits = []
        wd = self.waited[eng]
        for k, val in needs.items():
            if k == eng:
                continue
            if wd.get(k, 0) < val:
                wd[k] = val
                waits.append((k, val))
        n = self.cnt[eng] + 1
        self.q[eng].append((waits, fn, False))
        for v in reads:
            for t in v.toks:
                if t.r.get(eng, 0) < n:
                    t.r[eng] = n

    def transpose(self, out, in_, ident):
        o, i, d = out.ap, in_.ap, ident.ap

        def fn(e):
            return e.transpose(o, i, d)
        self.issue("pe", fn, [in_, ident], [out])

    def act(self, out, in_, func, bias=None, scale=None, accum=None, eng="act"):
        o, i = out.ap, in_.ap
        reads = [in_]
        kw = {}
        if bias is not None:
            if isinstance(bias, V):
                reads.append(bias)
                kw["bias"] = bias.ap
            else:
                kw["bias"] = bias
        if scale is not None:
            if isinstance(scale, V):
                reads.append(scale)
                kw["scale"] = scale.ap
            else:
                kw["scale"] = scale
        writes = [out]
        if accum is not None:
            kw["accum_out"] = accum.ap
            writes.append(accum)

        def fn(e):
            return e.activation(o, i, func, **kw)
        self.issue("act", fn, reads, writes)

    def tt(self, out, a, b, op, eng="dve"):
        o, x, y = out.ap, a.ap, b.ap

        def fn(e):
            return e.tensor_tensor(o, x, y, op)
        self.issue(eng, fn, [a, b], [out])

    def ts(self, out, a, s1, op0, s2=None, op1=None, eng="dve", accum=None):
        o, x = out.ap, a.ap
        reads = [a]

        def cv(s):
            if isinstance(s, V):
                reads.append(s)
                return s.ap
            return s
        s1a = cv(s1)
        s2a = cv(s2)
        writes = [out]
        kw = {}
        if accum is not None:
            kw["accum_out"] = accum.ap
            writes.append(accum)

        def fn(e):
            if op1 is None:
                return e.tensor_scalar(o, x, s1a, None, op0, **kw)
            return e.tensor_scalar(o, x, s1a, s2a, op0, op1, **kw)
        self.issue(eng, fn, reads, writes)

    def stt(self, out, a, s, b, op0, op1, eng="dve"):
        o, x, y = out.ap, a.ap, b.ap
        reads = [a, b]
        if isinstance(s, V):
            reads.append(s)
            sa = s.ap
        else:
            sa = s

        def fn(e):
            return e.scalar_tensor_tensor(o, x, sa, y, op0, op1)
        self.issue(eng, fn, reads, [out])

    def copy(self, out, in_, eng="dve"):
        o, i = out.ap, in_.ap
        if eng == "act":
            def fn(e):
                return e.copy(o, i)
        else:
            def fn(e):
                return e.tensor_copy(o, i)
        self.issue(eng, fn, [in_], [out])

    def memset(self, out, val, eng="dve"):
        o = out.ap

        def fn(e):
            return e.memset(o, val)
        self.issue(eng, fn, [], [out])

    def recip(self, out, in_, eng="dve"):
        o, i = out.ap, in_.ap

        def fn(e):
            return e.reciprocal(o, i)
        self.issue(eng, fn, [in_], [out])

    def scan(self, out, d0, d1, init, op0, op1):
        o, a, b = out.ap, d0.ap, d1.ap
        reads = [d0, d1]
        if isinstance(init, V):
            reads.append(init)
            ia = init.ap
        else:
            ia = init

        def fn(e):
            return e.tensor_tensor_scan(o, a, b, ia, op0, op1)
        self.issue("dve", fn, reads, [out])

    def reduce(self, out, in_, op, axis=AX.X, eng="dve"):
        o, i = out.ap, in_.ap

        def fn(e):
            return e.tensor_reduce(o, i, axis, op)
        self.issue(eng, fn, [in_], [out])


def _prog_barrier(self):
    targets = {e: self.cnt[e] for e in ENGS if self.cnt[e] > 0}
    for s in self.dsems:
        if s[1] > 0:
            targets[s[0]] = s[1]
    for e in ENGS:
        wd = self.waited[e]
        waits = []
        for k, val in targets.items():
            if wd.get(k, 0) < val:
                wd[k] = val
                waits.append((k, val))
        if waits:
            self.q[e].append((waits, None, False))


@contextlib.contextmanager
def _prog_scope(self):
    old = self.es
    with contextlib.ExitStack() as es2:
        self.es = es2
        try:
            yield
        finally:
            self.barrier()
            self.es = old


Prog.barrier = _prog_barrier
Prog.scope = _prog_scope
D = 1024; T = 2048; NB = 2; TOK = NB * T; TB = 512; NTB = TOK // TB; KC = 8
DFF = 2816; NJ = 22; NIN = 12552
C_MLX, C_MLO, C_MLG = 0, 1024, 2048
C_RW = 2056; C_HG = 5384; C_GATE = 9480
EPS = 1e-6; RW_LN_EPS = 64e-5
LRW = 64; LHG = 64; LML = 128
WSC = 0.6065306597126334


def fm(v):
    v = np.asarray(v, np.float32).reshape(-1, 128)
    return np.ascontiguousarray(v.T)


def kxn(W):
    K, N = W.shape
    return np.ascontiguousarray(W.reshape(K // 128, 128, N).transpose(1, 0, 2))


class VecPack:
    def __init__(self):
        self.cols = []
        self.off = {}
        self.n = 0

    def add(self, name, arr):
        arr = np.asarray(arr, np.float32)
        assert arr.shape[0] == 128
        self.off[name] = (self.n, arr.shape[1])
        self.cols.append(arr)
        self.n += arr.shape[1]

    def build(self):
        return np.ascontiguousarray(np.concatenate(self.cols, axis=1))


def pack_vecs(inp):
    vp = VecPack()
    rep = lambda v: np.tile(np.asarray(v, np.float32)[None, :], (128, 1))
    for l in range(2):
        for s in range(3):
            vp.add("normw%d%d" % (l, s), fm(inp["norm_w"][l, s]))
        vp.add("adab%d" % l, fm(inp["ada_b"][l]))
        for tp in range(4):
            vp.add("convw%d%d" % (l, tp), fm(inp["ml_conv_w"][l, tp]))
        vp.add("convb%d" % l, fm(inp["ml_conv_b"][l]))
        vp.add("mlnw%d" % l, fm(inp["ml_norm_w"][l]))
        vp.add("mlskip%d" % l, fm(inp["ml_skip"][l]))
        vp.add("ib%d" % l, rep(inp["ml_i_b"][l]))
        vp.add("fb%d" % l, rep(inp["ml_f_b"][l]))
        vp.add("rwmu%d" % l, fm(inp["rw_mu"][l]))
        for nm in ("rw_w0", "rw_a0", "rw_k_k", "rw_k_a", "rw_ln_w", "rw_ln_b"):
            vp.add(nm + str(l), fm(inp[nm][l]))
        vp.add("rw_r_k%d" % l, fm(inp["rw_r_k"][l].reshape(-1)))
        vp.add("hglb%d" % l, fm(inp["hg_lb_logits"][l]))
        vp.add("hgnw%d" % l, fm(inp["hg_norm_w"][l]))
    vp.add("finw", fm(inp["final_norm_w"]))
    vp.add("rw_v0", fm(inp["rw_v0"][0]))
    mv = np.zeros((128, 1), np.float32)
    mv[:32, 0] = inp["rw_mu_vres"][0]
    vp.add("muvres", mv)
    return vp


def host_weights(inp):
    w = {}
    w["ada_w"] = np.ascontiguousarray(
        np.asarray(inp["ada_w"], np.float32).reshape(2, 8, 128, 18, 512).transpose(0, 3, 2, 1, 4))
    w["ffn_up"] = np.ascontiguousarray(
        np.asarray(inp["ffn_up"], np.float32).reshape(2, 2, 8, 128, 2 * DFF).transpose(0, 1, 3, 2, 4))
    w["ffn_down"] = np.ascontiguousarray(
        np.asarray(inp["ffn_down"], np.float32).reshape(2, 2, NJ, 128, D).transpose(0, 1, 3, 2, 4))
    w["w_in"] = np.ascontiguousarray(
        np.asarray(inp["w_in"], np.float32).reshape(2, 8, 128, NIN).transpose(0, 2, 1, 3))
    w["w_vres"] = np.ascontiguousarray(
        np.asarray(inp["w_in_vres"], np.float32).reshape(8, 128, 32).transpose(1, 0, 2))
    w["wq"] = np.ascontiguousarray(
        np.asarray(inp["ml_wq"], np.float32).reshape(2, 4, 2, 128, 256).transpose(0, 3, 1, 2, 4))
    w["wk"] = np.ascontiguousarray(
        np.asarray(inp["ml_wk"], np.float32).reshape(2, 4, 2, 128, 256).transpose(0, 3, 1, 2, 4))
    w["rw_wa2"] = np.ascontiguousarray(np.concatenate(
        [np.asarray(inp["rw_w2"], np.float32), np.asarray(inp["rw_a2"], np.float32)], axis=1))
    w["rw_g2"] = np.ascontiguousarray(np.asarray(inp["rw_g2"], np.float32))
    w["rw_v2"] = np.ascontiguousarray(np.asarray(inp["rw_v2"], np.float32)[0])
    w["w_branch"] = np.ascontiguousarray(
        np.asarray(inp["w_branch"], np.float32).reshape(2, 3, 8, 128, D).transpose(0, 3, 1, 2, 4))
    w["w_out"] = np.ascontiguousarray(
        np.asarray(inp["w_out"], np.float32).reshape(2, 8, 128, D).transpose(0, 2, 1, 3))
    return w


class DT:
    def __init__(self, P, name, shape, dt, tok_axis, kind="Internal", blk=TB):
        self.t = P.nc.dram_tensor(name, list(shape), dt, kind=kind)
        self.ap = self.t.ap()
        self.tok_axis = tok_axis
        self.blk = blk
        n = shape[tok_axis] // blk
        self.toks = [Tok("%s_%d" % (name, i)) for i in range(max(n, 1))]

    def v(self, t0, t1):
        i0, i1 = t0 // self.blk, (t1 - 1) // self.blk
        toks = tuple(self.toks[i0:i1 + 1])
        if self.tok_axis == 0:
            return V(self.ap[t0:t1], toks)
        return V(self.ap[:, t0:t1], toks)

    def all(self):
        return V(self.ap, tuple(self.toks))
class Ctx:
    pass


def build_program(vec_off, nvec, stop_after=None, debug_outs=(), only=None):
    nc = bass.Bass("TRN2", target_bir_lowering=False)
    es = contextlib.ExitStack()
    with es:
        P = Prog(nc, es)
        K = Ctx()
        K.P = P
        K.debug = {}

        def din(name, shape, dt=F32):
            return P.dram(name, shape, dt, kind="ExternalInput")
        K.xT_in = din("xT", [D, TOK])
        K.cT = din("cT", [128, KC, NB])
        K.vecs_d = din("vecs", [128, nvec])
        K.ada_w = din("ada_w", [2, 18, 128, 8, 512])
        K.ffn_up = din("ffn_up", [2, 2, 128, 8, 2 * DFF])
        K.ffn_down = din("ffn_down", [2, 2, 128, NJ, D])
        K.w_in = din("w_in", [2, 128, 8, NIN])
        K.w_vres = din("w_vres", [128, 8, 32])
        K.wq = din("wq", [2, 128, 4, 2, 256])
        K.wk = din("wk", [2, 128, 4, 2, 256])
        K.rw_wa2 = din("rw_wa2", [2, 128, D])
        K.rw_g2 = din("rw_g2", [2, 128, D])
        K.rw_v2 = din("rw_v2", [32, D])
        K.w_branch = din("w_branch", [2, 128, 3, 8, D])
        K.w_out = din("w_out", [2, 128, 8, D])
        K.outT = DT(P, "outT", [D, TOK], F32, 1, kind="ExternalOutput")

        def scr(name, shape, dt, tok_axis):
            kind = "ExternalOutput" if name in debug_outs else "Internal"
            return DT(P, name, shape, dt, tok_axis, kind=kind)
        K.xs = scr("xs", [D, TOK], F32, 1)
        K.act = scr("actT", [DFF, TOK], BF16, 1)
        K.hT = scr("hT", [D, TOK], BF16, 1)
        K.yT = [scr("yT%d" % i, [D, TOK], BF16, 1) for i in range(3)]
        K.ml_qT = scr("ml_qT", [D, TOK], F32, 1)
        K.ml_kT = scr("ml_kT", [D, TOK], F32, 1)
        K.ml_skx = scr("ml_skx", [D, TOK], F32, 1)
        K.ml_ktok = scr("ml_ktok", [TOK, D], F32, 0)
        K.ml_vo = scr("ml_vo", [TOK, 2056], F32, 0)
        K.hg_qT = scr("hg_qT", [D, TOK], F32, 1)
        K.hg_kT = scr("hg_kT", [D, TOK], F32, 1)
        K.hg_dec = scr("hg_dec", [D, TOK // LHG], F32, 1) if False else None
        K.hg_decT = DT(P, "hg_decT", [D, TOK // LHG], F32, 1, blk=TB // LHG)
        K.hg_vtok = scr("hg_vtok", [TOK, D], F32, 0)
        K.hg_khat = scr("hg_khat", [TOK, D], F32, 0)
        K.hg_gtok = scr("hg_gtok", [TOK, D], F32, 0)
        for nm in ("rw_kapT", "rw_rhoT", "rw_betT", "rw_ktlT", "rw_bonT", "rw_gT", "rw_vfT"):
            setattr(K, nm, scr(nm, [D, TOK], F32, 1))
        K.rw_gamT = DT(P, "rw_gamT", [D, TOK // LRW], F32, 1, blk=TB // LRW)
        for nm in ("rw_vtok", "rw_bhat", "rw_khat", "rw_ytok"):
            setattr(K, nm, scr(nm, [TOK, D], F32, 0))

        K.vecs = P.sb([128, nvec], F32, "vecs_sb")
        P.dma(K.vecs, K.vecs_d)

        def vec(name):
            o, n = vec_off[name]
            return K.vecs[:, o:o + n]
        K.vec = vec
        K.epsb = P.sb([128, 4], F32, "epsb")
        P.memset(K.epsb[:, 0:1], EPS)
        P.memset(K.epsb[:, 1:2], RW_LN_EPS)
        P.memset(K.epsb[:, 2:3], 1.0)
        P.memset(K.epsb[:, 3:4], 0.0)
        K.ones = P.sb([128, 128], F32, "ones")
        P.memset(K.ones, 1.0)
        K.ident = P.sb([128, 128], F32, "ident")
        P.memset(K.ident, 0.0)
        io = P.sb([128, 128], F32, "iota_f")
        ip = P.sb([128, 1], F32, "iota_p")
        io_i = P.sb([128, 128], mybir.dt.int32, "iota_fi")
        ip_i = P.sb([128, 1], mybir.dt.int32, "iota_pi")

        def iota(out, pattern, cm):
            o = out.ap

            def fn(e):
                return e.iota(o, pattern, base=0, channel_multiplier=cm)
            P.issue("pool", fn, [], [out])
        iota(io_i, [[1, 128]], 0)
        iota(ip_i, [[0, 1]], 1)
        P.copy(io, io_i)
        P.copy(ip, ip_i)
        K.iota_f = io
        K.iota_p = ip
        P.ts(K.ident, io, ip[:, 0:1], ALU.is_equal)
        K.tri = P.sb([128, 128], F32, "tri")
        P.ts(K.tri, io, ip[:, 0:1], ALU.is_ge)
        K.tris = P.sb([128, 128], F32, "tris")
        P.ts(K.tris, io, ip[:, 0:1], ALU.is_gt)
        K.lows = P.sb([128, 128], F32, "lows")
        P.ts(K.lows, io, ip[:, 0:1], ALU.is_lt)
        K.blk64 = P.sb([128, 128], F32, "blk64")
        P.memset(K.blk64, 0.0)
        P.memset(K.blk64[0:64, 0:64], 1.0)
        P.memset(K.blk64[64:128, 64:128], 1.0)
        K.rmask = P.sb([128, TB], F32, "rmask")
        P.memset(K.rmask, 1.0)
        P.memset(K.rmask.re("p (c l) -> p c l", l=64)[:, :, 0:1], 0.0)

        K.modT = [P.sb([128, 72, NB], F32, "modT%d" % l) for l in range(2)]
        K.sc1 = [[P.sb([128, 8, NB], F32) for s in range(3)] for l in range(2)]
        K.gt = [[P.sb([128, 8, NB], F32) for s in range(3)] for l in range(2)]
        stage_mod(K)
        stages = [
            ("copyx", lambda: stage_copyx(K)),
        ]
        for l in range(2):
            stages += [
                ("ffn%d0" % l, lambda l=l: stage_ffn(K, l, 0)),
                ("normmix%d" % l, lambda l=l: stage_normmix(K, l)),
                ("inml%d" % l, lambda l=l: stage_in_ml(K, l)),
                ("mlrec%d" % l, lambda l=l: stage_ml_rec(K, l)),
                ("inhg%d" % l, lambda l=l: stage_in_hg(K, l)),
                ("hgrec%d" % l, lambda l=l: stage_hg_rec(K, l)),
                ("inrw%d" % l, lambda l=l: stage_in_rw(K, l)),
                ("rwrec%d" % l, lambda l=l: stage_rw_rec(K, l)),
                ("mix%d" % l, lambda l=l: stage_mix(K, l)),
                ("ffn%d1" % l, lambda l=l: stage_ffn(K, l, 1)),
            ]
        stages.append(("final", lambda: stage_final(K)))
        for name, fn in stages:
            if only is not None and name not in only:
                continue
            fn()
            if stop_after == name:
                break
        P.barrier()
        fin = [(k, v) for k, v, _ in P.dsems if v > 0]
        P.emit(final_waits=fin)
    return nc


def stage_mod(K):
    P = K.P
    with P.scope():
        cT = P.sb([128, KC, NB])
        P.dma(cT, K.cT)
        cond = P.sb([128, KC, NB])
        P.act(cond, cT, AF.Silu)
        wb = [P.sb([128, 8, 512]) for _ in range(2)]
        for l in range(2):
            ps = P.ps([128, 72, NB])
            for pc in range(18):
                wt = wb[pc % 2]
                P.dma(wt, K.ada_w[l, pc])
                for jj in range(4):
                    j = pc * 4 + jj
                    P.mm_group(ps[:, j, :], [(wt[:, kc, jj * 128:(jj + 1) * 128], cond[:, kc, :]) for kc in range(KC)])
            P.tt(K.modT[l], ps, K.vec("adab%d" % l).un(2).bc([128, 72, NB]), ALU.add)
            for s in range(3):
                m = K.modT[l]
                nw = K.vec("normw%d%d" % (l, s))
                P.ts(K.sc1[l][s], m[:, s * 24 + 8:s * 24 + 16, :], 1.0, ALU.add)
                P.tt(K.sc1[l][s], K.sc1[l][s], nw.un(2).bc([128, 8, NB]), ALU.mult)
                f = 1.0 if s == 1 else 0.5
                P.ts(K.gt[l][s], m[:, s * 24 + 16:s * 24 + 24, :], 1.0, ALU.add, f, ALU.mult)


def stage_copyx(K):
    P = K.P
    for tb in range(NTB):
        P.dma(K.xs.v(tb * TB, (tb + 1) * TB), K.xT_in[:, tb * TB:(tb + 1) * TB])


def norm_block(K, xb, hb, l, s, b, sq, ssp, sd, rstd, hn, plain_w=None, out_f32=None):
    P = K.P
    P.act(sq, xb, AF.Square)
    P.mm_group(ssp, [(K.ones, sq[:, dc, :]) for dc in range(8)])
    P.act(sd, ssp, AF.Sqrt, bias=K.epsb[:, 0:1], scale=1.0 / D)
    P.recip(rstd, sd)
    P.tt(hn, xb, rstd.un(1).bc([128, 8, TB]), ALU.mult)
    if plain_w is not None:
        P.tt(out_f32, hn, plain_w.un(2).bc([128, 8, TB]), ALU.mult)
        return
    sh = K.modT[l][:, s * 24:s * 24 + 8, :]
    for dc in range(8):
        P.ts(hb[:, dc, :], hn[:, dc, :], K.sc1[l][s][:, dc, b:b + 1], ALU.mult, sh[:, dc, b:b + 1], ALU.add)


def alloc_norm_tmps(K):
    P = K.P
    if not hasattr(K, "epsb"):
        pass
    sq = P.sb([128, 8, TB])
    sd = P.sb([128, TB])
    return dict(sq=sq, ssp=P.ps([128, TB]), sd=sd, rstd=sd, hn=sq)


def xview(dt_, tb):
    return dt_.v(tb * TB, (tb + 1) * TB).re("(c p) t -> p c t", p=128)


def stage_ffn(K, l, f):
    P = K.P
    s = 0 if f == 0 else 2
    with P.scope():
        wup = P.sb([128, 8, 2 * DFF], BF16)
        for kc in range(8):
            P.dma(wup[:, kc, :].re("p (a n) -> p a n", a=4), K.ffn_up[l, f][:, kc, :].re("p (a n) -> p a n", a=4),
                  eng="pool")
        tm = alloc_norm_tmps(K)
        xbs = [P.sb([128, 8, TB]) for _ in range(2)]
        hbs = [P.sb([128, 8, TB], BF16)] * 2
        acts = [P.sb([128, NJ, TB], BF16) for _ in range(2)]
        sgs = [P.sb([128, TB]) for _ in range(2)]
        pas = [P.ps([128, TB]) for _ in range(2)]
        pbs = [P.ps([128, TB]) for _ in range(2)]
        for tb in range(NTB):
            b = tb // 4
            xb, hb, ab = xbs[tb % 2], hbs[tb % 2], acts[tb % 2]
            P.dma(xb, xview(K.xs, tb))
            norm_block(K, xb, hb, l, s, b, **tm)
            for j in range(NJ):
                pa, pb, sg = pas[j % 2], pbs[j % 2], sgs[j % 2]
                P.mm_group(pa, [(wup[:, kc, j * 128:(j + 1) * 128], hb[:, kc, :]) for kc in range(8)])
                P.mm_group(pb, [(wup[:, kc, DFF + j * 128:DFF + (j + 1) * 128], hb[:, kc, :]) for kc in range(8)])
                P.act(sg, pa, AF.Silu)
                P.tt(ab[:, j, :], sg, pb, ALU.mult)
            P.dma(xview(K.act, tb), ab)
    with P.scope():
        wdn = P.sb([128, NJ, D], BF16)
        for kc in range(NJ):
            P.dma(wdn[:, kc, :], K.ffn_down[l, f][:, kc, :], eng="pool")
        xbs = [P.sb([128, 8, TB]) for _ in range(2)]
        xos = [P.sb([128, 8, TB]) for _ in range(2)]
        abs_ = [P.sb([128, NJ, TB], BF16) for _ in range(2)]
        pos = [P.ps([128, TB]) for _ in range(2)]
        for tb in range(NTB):
            b = tb // 4
            xb, xo, ab = xbs[tb % 2], xos[tb % 2], abs_[tb % 2]
            P.dma(ab, xview(K.act, tb))
            P.dma(xb, xview(K.xs, tb))
            for m in range(8):
                po = pos[m % 2]
                P.mm_group(po, [(wdn[:, kc, m * 128:(m + 1) * 128], ab[:, kc, :]) for kc in range(NJ)])
                P.stt(xo[:, m, :], po, K.gt[l][s][:, m, b:b + 1], xb[:, m, :], ALU.mult, ALU.add)
            P.dma(xview(K.xs, tb), xo)


def stage_final(K):
    P = K.P
    with P.scope():
        tm = alloc_norm_tmps(K)
        xbs = [P.sb([128, 8, TB]) for _ in range(2)]
        obs = [P.sb([128, 8, TB]) for _ in range(2)]
        for tb in range(NTB):
            xb, ob = xbs[tb % 2], obs[tb % 2]
            P.dma(xb, xview(K.xs, tb))
            norm_block(K, xb, None, 0, 0, 0, plain_w=K.vec("finw"), out_f32=ob, **tm)
            P.dma(xview(K.outT, tb), ob)


def stage_normmix(K, l):
    P = K.P
    with P.scope():
        tm = alloc_norm_tmps(K)
        xbs = [P.sb([128, 8, TB]) for _ in range(2)]
        hbs = [P.sb([128, 8, TB], BF16) for _ in range(2)]
        for tb in range(NTB):
            xb, hb = xbs[tb % 2], hbs[tb % 2]
            P.dma(xb, xview(K.xs, tb))
            norm_block(K, xb, hb, l, 1, tb // 4, **tm)
            P.dma(xview(K.hT, tb), hb)
def load_w_cols(P, dst, src, c0, c1, eng="pool"):
    for kc in range(8):
        n = c1 - c0
        if n > 2048:
            h = (n // 2)
            P.dma(dst[:, kc, 0:h], src[:, kc, c0:c0 + h], eng=eng)
            P.dma(dst[:, kc, h:n], src[:, kc, c0 + h:c1], eng=eng)
        else:
            P.dma(dst[:, kc, :], src[:, kc, c0:c1], eng=eng)


def tmview(dt_, t0, n):
    return dt_.v(t0, t0 + n)


def stage_in_ml(K, l):
    P = K.P
    with P.scope():
        wml = P.sb([128, 8, 2056], BF16)
        load_w_cols(P, wml, K.w_in[l], 0, 2056)
        wq = P.sb([128, 4, 2, 256], BF16)
        wk = P.sb([128, 4, 2, 256], BF16)
        P.dma(wq, K.wq[l], eng="pool")
        P.dma(wk, K.wk[l], eng="pool")
        cw = [K.vec("convw%d%d" % (l, tp)) for tp in range(4)]
        cb = K.vec("convb%d" % l)
        skip = K.vec("mlskip%d" % l)
        hbs = [P.sb([128, 8, TB], BF16) for _ in range(2)]
        xms = [P.sb([128, 8, TB + 3]) for _ in range(2)]
        acc = P.sb([128, 8, TB])
        xc = P.sb([128, 8, TB])
        xcb = P.sb([128, 8, TB], BF16)
        qst = P.sb([128, 8, TB])
        kst = P.sb([128, 8, TB])
        ktk = [P.sb([128, D]) for _ in range(2)]
        vos = [P.sb([128, 2056]) for _ in range(2)]
        psA = [P.ps([128, TB]) for _ in range(2)]
        psB = [P.ps([128, 2, 512]) for _ in range(2)]
        psC = P.ps([128, 8])
        for tb in range(NTB):
            hb, xm = hbs[tb % 2], xms[tb % 2]
            xmp = xms[(tb + 1) % 2]
            P.dma(hb, xview(K.hT, tb))
            if tb % 4 == 0:
                P.memset(xm[:, :, 0:3], 0.0)
            else:
                P.copy(xm[:, :, 0:3], xmp[:, :, TB:TB + 3])
            for dc in range(8):
                ps = psA[dc % 2]
                P.mm_group(ps, [(wml[:, kc, dc * 128:(dc + 1) * 128], hb[:, kc, :]) for kc in range(8)])
                P.copy(xm[:, dc, 3:TB + 3], ps, eng="act")
            for dc in range(8):
                P.ts(acc[:, dc, :], xm[:, dc, 0:TB], cw[0][:, dc:dc + 1], ALU.mult, cb[:, dc:dc + 1], ALU.add)
                for tp in range(1, 4):
                    P.stt(acc[:, dc, :], xm[:, dc, tp:tp + TB], cw[tp][:, dc:dc + 1], acc[:, dc, :], ALU.mult, ALU.add)
            P.act(xc, acc, AF.Silu)
            P.act(xcb, acc, AF.Silu)
            P.tt(acc, xc, skip.un(2).bc([128, 8, TB]), ALU.mult, eng="pool")
            P.dma(xview(K.ml_skx, tb), acc)
            for (w_, st_, dst) in ((wq, qst, K.ml_qT), (wk, kst, K.ml_kT)):
                for h in range(4):
                    for ec in range(2):
                        ps = psA[ec % 2]
                        P.mm_group(ps, [(w_[:, h, dck, ec * 128:(ec + 1) * 128], xcb[:, 2 * h + dck, :]) for dck in range(2)])
                        if ec == 0:
                            P.copy(st_[:, 2 * h + ec, :], ps, eng="act")
                        else:
                            P.copy(st_[:, 2 * h + ec, :], ps, eng="dve")
                P.dma(xview(dst, tb), st_)
            for tt_ in range(4):
                t0 = tb * TB + tt_ * 128
                tsl = slice(tt_ * 128, (tt_ + 1) * 128)
                kt = ktk[tt_ % 2]
                pb = psB[0]
                for h in range(4):
                    P.mm_group(pb[:, h // 2, (h % 2) * 256:(h % 2) * 256 + 256],
                               [(xcb[:, 2 * h + dck, tsl], wk[:, h, dck, :]) for dck in range(2)])
                P.copy(kt, pb.re("p a b -> p (a b)"), eng="act")
                P.dma(tmview(K.ml_ktok, t0, 128), kt)
                vo = vos[tt_ % 2]
                for half in range(2):
                    pb2 = psB[1] if half == 0 else psB[0]
                    for q in range(2):
                        c0 = half * 1024 + q * 512
                        P.mm_group(pb2[:, q, :], [(hb[:, kc, tsl], wml[:, kc, c0:c0 + 512]) for kc in range(8)])
                    if half == 0:
                        P.copy(vo[:, 0:1024], pb2.re("p a b -> p (a b)"), eng="dve")
                    else:
                        P.copy(vo[:, 1024:2048], pb2.re("p a b -> p (a b)"), eng="act")
                P.mm_group(psC, [(hb[:, kc, tsl], wml[:, kc, 2048:2056]) for kc in range(8)])
                P.copy(vo[:, 2048:2056], psC, eng="dve")
                P.dma(tmview(K.ml_vo, t0, 128), vo)


def stage_ml_rec(K, l):
    P = K.P
    NCH = T // LML
    with P.scope():
        ib = K.vec("ib%d" % l)
        fb = K.vec("fb%d" % l)
        mlnw = K.vec("mlnw%d" % l)
        gin = P.sb([128, NCH, 8])
        sp = P.sb([128, NCH, 4])
        li = P.sb([128, NCH, 4])
        et = P.sb([128, NCH, 4])
        wv = P.sb([128, NCH, 4])
        dec = P.sb([128, NCH, 4])
        wend = P.sb([128, NCH, 4])
        tmpg = P.sb([128, NCH, 4])
        C = P.sb([128, 4, 2, 257])
        qTs = [P.sb([128, 8, LML]) for _ in range(2)]
        kTs = [P.sb([128, 8, LML]) for _ in range(2)]
        kts = [P.sb([128, D]) for _ in range(2)]
        vxs = [P.sb([128, 4, 257]) for _ in range(2)]
        for vx in vxs:
            P.memset(vx[:, :, 256:257], 1.0)
        osb = [P.sb([128, D]) for _ in range(2)]
        sko = [P.sb([128, 8, LML]) for _ in range(2)]
        sgo = P.sb([128, D])
        sts = [P.sb([128, 128]) for _ in range(2)]
        hh = P.sb([128, 4, 256])
        cen = P.sb([128, 4, 256])
        sq = P.sb([128, 4, 256])
        sm = P.sb([128, 16])
        kw = P.sb([128, 4, 256])
        yst = [P.sb([128, 8, LML], BF16) for _ in range(2)]
        stp = [P.ps([128, 128]) for _ in range(2)]
        nps = P.ps([128, 4, 512])
        npsv = [P.sub(nps[:, h, 0:257]) for h in range(4)]
        psX = [P.ps([128, 512]) for _ in range(2)]
        for b in range(NB):
            T0 = b * T
            P.dma(gin, K.ml_vo.v(T0, T0 + T)[:, 2048:2056].re("(c t) g -> t c g", t=LML))
            P.tt(tmpg, gin[:, :, 4:8], fb.un(1).bc([128, NCH, 4]), ALU.add)
            P.act(tmpg, tmpg, AF.Exp, scale=-1.0)
            P.act(sp, tmpg, AF.Ln, bias=K.epsb[:, 2:3])
            P.tt(li, gin[:, :, 0:4], ib.un(1).bc([128, NCH, 4]), ALU.add)
            bps = psX[0][:, 0:NCH * 4]
            tps = psX[1][:, 0:NCH * 4]
            P.mm_group(bps, [(K.tri, sp.re("p c h -> p (c h)"))])
            P.mm_group(tps, [(K.ones, sp.re("p c h -> p (c h)"))])
            P.act(et.re("p c h -> p (c h)"), bps, AF.Exp, scale=-1.0)
            P.tt(tmpg.re("p c h -> p (c h)"), li.re("p c h -> p (c h)"), bps, ALU.add)
            P.act(wv, tmpg, AF.Exp)
            P.ts(wv, wv, 1.0 / 16.0, ALU.mult)
            P.act(dec.re("p c h -> p (c h)"), tps, AF.Exp, scale=-1.0)
            P.tt(wend, wv, dec, ALU.mult)
            P.memset(C, 0.0)
            for c in range(NCH):
                t0 = T0 + c * LML
                qT, kT, kt, vx, ob, sk = qTs[c % 2], kTs[c % 2], kts[c % 2], vxs[c % 2], osb[c % 2], sko[c % 2]
                P.dma(qT, K.ml_qT.v(t0, t0 + LML).re("(c p) t -> p c t", p=128))
                P.dma(kT, K.ml_kT.v(t0, t0 + LML).re("(c p) t -> p c t", p=128))
                P.dma(kt, tmview(K.ml_ktok, t0, LML))
                vo = K.ml_vo.v(t0, t0 + LML)
                P.dma(vx[:, :, 0:256], vo[:, 0:1024].re("t (h e) -> t h e", h=4))
                P.dma(ob, vo[:, 1024:2048])
                P.dma(sk, K.ml_skx.v(t0, t0 + LML).re("(c p) t -> p c t", p=128))
                P.act(sgo, ob, AF.Sigmoid)
                for h in range(4):
                    sp_ = stp[h % 2]
                    st = sts[h % 2]
                    P.mm_group(sp_, [(kT[:, 2 * h + dck, :], qT[:, 2 * h + dck, :]) for dck in range(2)])
                    P.stt(st, sp_, wv[:, c, h:h + 1], K.tri, ALU.mult, ALU.mult)
                    P.mm_group(npsv[h], [(st, vx[:, h, :]),
                                         (qT[:, 2 * h, :], C[:, h, 0, :]),
                                         (qT[:, 2 * h + 1, :], C[:, h, 1, :])])
                for h in range(4):
                    P.tt(sm[:, h:h + 1], npsv[h][:, 256:257], et[:, c, h:h + 1], ALU.mult)
                P.ts(sm[:, 4:8], sm[:, 0:4], -1.0, ALU.mult)
                P.tt(sm[:, 4:8], sm[:, 4:8], sm[:, 0:4], ALU.max)
                P.ts(sm[:, 4:8], sm[:, 4:8], 1.0, ALU.max)
                P.recip(sm[:, 8:12], sm[:, 4:8])
                P.tt(sm[:, 12:16], sm[:, 8:12], et[:, c, :], ALU.mult)
                for h in range(4):
                    P.stt(hh[:, h, :], npsv[h][:, 0:256], sm[:, 12 + h:13 + h], sgo[:, h * 256:(h + 1) * 256],
                          ALU.mult, ALU.mult)
                P.reduce(sm[:, 0:4], hh, ALU.add)
                P.ts(sm[:, 0:4], sm[:, 0:4], 1.0 / 256.0, ALU.mult)
                P.tt(cen, hh, sm[:, 0:4].un(2).bc([128, 4, 256]), ALU.subtract)
                P.tt(sq, cen, cen, ALU.mult, eng="pool")
                P.reduce(sm[:, 4:8], sq, ALU.add)
                P.act(sm[:, 8:12], sm[:, 4:8], AF.Sqrt, bias=K.epsb[:, 0:1], scale=1.0 / 256.0)
                P.recip(sm[:, 8:12], sm[:, 8:12])
                P.tt(cen, cen, sm[:, 8:12].un(2).bc([128, 4, 256]), ALU.mult)
                cenf = cen.re("p h e -> p (h e)")
                ys = yst[c % 2]
                for half in range(2):
                    px = psX[half]
                    for jj in range(4):
                        j = half * 4 + jj
                        P.transpose(px[:, jj * 128:(jj + 1) * 128], cenf[:, j * 128:(j + 1) * 128], K.ident)
                    for jj in range(4):
                        j = half * 4 + jj
                        P.stt(ys[:, j, :], px[:, jj * 128:(jj + 1) * 128], mlnw[:, j:j + 1], sk[:, j, :],
                              ALU.mult, ALU.add)
                P.dma(K.yT[0].v(t0, t0 + LML).re("(c p) t -> p c t", p=128), ys)
                for h in range(4):
                    P.ts(kw[:, h, :], kt[:, h * 256:(h + 1) * 256], wend[:, c, h:h + 1], ALU.mult, eng="pool")
                for h in range(4):
                    for dck in range(2):
                        px = psX[dck]
                        P.mm_group(px[:, 0:257], [(kw[:, h, dck * 128:(dck + 1) * 128], vx[:, h, :])])
                        P.stt(C[:, h, dck, :], C[:, h, dck, :], dec[:, c, h:h + 1], px[:, 0:257], ALU.mult, ALU.add)


def stage_in_hg(K, l):
    P = K.P
    with P.scope():
        whg = P.sb([128, 8, 4096], BF16)
        load_w_cols(P, whg, K.w_in[l], C_HG, C_HG + 4096)
        lb = P.sb([128, 8])
        oml = P.sb([128, 8])
        noml = P.sb([128, 8])
        if l == 0:
            P.memset(lb, 0.0)
        else:
            P.tt(lb, K.vec("hglb1"), K.vec("hglb0"), ALU.subtract)
            P.act(lb, lb, AF.Sigmoid)
        P.ts(oml, lb, -1.0, ALU.mult, 1.0, ALU.add)
        P.ts(noml, oml, -1.0, ALU.mult)
        hbs = [P.sb([128, 8, TB], BF16) for _ in range(2)]
        q = P.sb([128, 8, TB])
        sg = P.sb([128, 8, TB])
        kk = P.sb([128, 8, TB])
        bc_ = P.sb([128, 8, TB])
        eb = P.sb([128, 8, TB])
        enb = P.sb([128, 8, TB])
        dec = P.sb([128, 8, 8])
        kst = [P.sb([128, D])] * 2
        vst = [P.sb([128, D])] * 2
        gst = [P.sb([128, D])] * 2
        psA = [P.ps([128, TB]) for _ in range(2)]
        psF = [P.ps([128, TB]) for _ in range(2)]
        psT = [P.ps([128, 2, 512]) for _ in range(2)]
        for tb in range(NTB):
            hb = hbs[tb % 2]
            P.dma(hb, xview(K.hT, tb))
            for dc in range(8):
                pq, pf = psA[dc % 2], psF[dc % 2]
                P.mm_group(pq, [(whg[:, kc, dc * 128:(dc + 1) * 128], hb[:, kc, :]) for kc in range(8)])
                P.mm_group(pf, [(whg[:, kc, 1024 + dc * 128:1024 + (dc + 1) * 128], hb[:, kc, :]) for kc in range(8)])
                P.act(q[:, dc, :], pq, AF.Silu)
                P.act(sg[:, dc, :], pf, AF.Sigmoid)
                P.ts(kk[:, dc, :], sg[:, dc, :], noml[:, dc:dc + 1], ALU.mult, oml[:, dc:dc + 1], ALU.add, eng="pool")
                P.ts(sg[:, dc, :], sg[:, dc, :], oml[:, dc:dc + 1], ALU.mult, lb[:, dc:dc + 1], ALU.add)
            P.ts(sg, sg, 1e-30, ALU.max)
            P.act(sg, sg, AF.Ln)
            for dc in range(8):
                P.scan(bc_[:, dc, :], K.rmask, sg[:, dc, :], 0.0, ALU.mult, ALU.add)
            P.act(eb, bc_, AF.Exp)
            P.act(enb, bc_, AF.Exp, scale=-1.0)
            P.tt(q, q, eb, ALU.mult)
            P.tt(kk, kk, enb, ALU.mult, eng="pool")
            P.copy(dec, eb.re("p c (n l) -> p c n l", l=LHG)[:, :, :, LHG - 1])
            P.tt(enb.re("p c (n l) -> p c n l", l=LHG), kk.re("p c (n l) -> p c n l", l=LHG),
                 dec.un(3).bc([128, 8, 8, LHG]), ALU.mult)
            P.dma(xview(K.hg_qT, tb), q)
            P.dma(xview(K.hg_kT, tb), kk)
            P.dma(K.hg_decT.v(tb * 8, tb * 8 + 8).re("(c p) n -> p c n", p=128), dec)
            for tt_ in range(4):
                t0 = tb * TB + tt_ * 128
                tsl = slice(tt_ * 128, (tt_ + 1) * 128)
                ks, vs, gs = kst[tt_ % 2], vst[tt_ % 2], gst[tt_ % 2]
                px = psT[0]
                for dc in range(8):
                    P.transpose(px[:, dc // 4, (dc % 4) * 128:(dc % 4) * 128 + 128], enb[:, dc, tsl], K.ident)
                P.copy(ks, px.re("p a b -> p (a b)"), eng="act")
                P.dma(tmview(K.hg_khat, t0, 128), ks)
                pv = psT[1]
                for qq in range(2):
                    c0 = 2048 + qq * 512
                    P.mm_group(pv[:, qq, :], [(hb[:, kc, tsl], whg[:, kc, c0:c0 + 512]) for kc in range(8)])
                P.copy(vs, pv.re("p a b -> p (a b)"), eng="dve")
                P.dma(tmview(K.hg_vtok, t0, 128), vs)
                pg = psT[0]
                for qq in range(2):
                    c0 = 3072 + qq * 512
                    P.mm_group(pg[:, qq, :], [(hb[:, kc, tsl], whg[:, kc, c0:c0 + 512]) for kc in range(8)])
                P.act(gs, pg.re("p a b -> p (a b)"), AF.Sigmoid)
                P.dma(tmview(K.hg_gtok, t0, 128), gs)


def stage_hg_rec(K, l):
    P = K.P
    NCH = T // LHG
    CG = 4
    with P.scope():
        hgnw = K.vec("hgnw%d" % l)
        S = P.sb([128, 8, 128])
        decall = P.sb([128, 8, NCH])
        qts = [P.sb([128, 8, CG * LHG]) for _ in range(2)]
        kts = [P.sb([128, 8, CG * LHG]) for _ in range(2)]
        vts = [P.sb([64, CG, D]) for _ in range(2)]
        khs = [P.sb([64, CG, D]) for _ in range(2)]
        gts = [P.sb([64, CG, D]) for _ in range(2)]
        at = P.sb([64, 8, 64])
        sq = P.sb([64, 8, 128])
        on = P.sb([64, 8, 128])
        sm = P.sb([64, 16])
        yst = [P.sb([128, 8, CG * LHG], BF16) for _ in range(2)]
        atp = P.ps([64, 8, 64])
        ops = P.ps([64, 2, 512])
        tp = P.ps([128, 8, 64])
        sps = P.ps([128, 2, 512])
        tri64 = K.tri[0:64, 0:64]
        id64 = K.ident[0:64, 0:64]
        for b in range(NB):
            T0 = b * T
            P.memset(S, 0.0)
            P.dma(decall, K.hg_decT.v(b * NCH, (b + 1) * NCH).re("(c p) n -> p c n", p=128))
            for cg in range(NCH // CG):
                t0 = T0 + cg * CG * LHG
                t1 = t0 + CG * LHG
                qt, kt, vt, kh, gt, ys = qts[cg % 2], kts[cg % 2], vts[cg % 2], khs[cg % 2], gts[cg % 2], yst[cg % 2]
                P.dma(qt, K.hg_qT.v(t0, t1).re("(c p) t -> p c t", p=128))
                P.dma(kt, K.hg_kT.v(t0, t1).re("(c p) t -> p c t", p=128))
                P.dma(vt, K.hg_vtok.v(t0, t1).re("(c t) e -> t c e", t=LHG))
                P.dma(kh, K.hg_khat.v(t0, t1).re("(c t) e -> t c e", t=LHG))
                P.dma(gt, K.hg_gtok.v(t0, t1).re("(c t) e -> t c e", t=LHG))
                for ci in range(CG):
                    c = cg * CG + ci
                    cs = slice(ci * LHG, (ci + 1) * LHG)
                    for h in range(8):
                        P.mm_group(atp[:, h, :], [(kt[:, h, cs], qt[:, h, cs])])
                    P.tt(at, atp, tri64.un(1).bc([64, 8, 64]), ALU.mult)
                    for h in range(8):
                        hs = slice(h * 128, (h + 1) * 128)
                        P.mm_group(ops[:, h // 4, (h % 4) * 128:(h % 4) * 128 + 128],
                                   [(at[:, h, :], vt[:, ci, hs]), (qt[:, h, cs], S[:, h, :])])
                    opf = ops.re("p a (h e) -> p (a h) e", e=128)
                    P.act(sq, opf, AF.Square)
                    P.reduce(sm[:, 0:8], sq, ALU.add)
                    P.act(sm[:, 8:16], sm[:, 0:8], AF.Sqrt, bias=K.epsb[0:64, 0:1], scale=1.0 / 128.0)
                    P.recip(sm[:, 8:16], sm[:, 8:16])
                    P.tt(on, opf, sm[:, 8:16].un(2).bc([64, 8, 128]), ALU.mult)
                    P.tt(on, on, gt[:, ci, :].re("p (h e) -> p h e", e=128), ALU.mult, eng="pool")
                    for h in range(8):
                        P.transpose(tp[:, h, :], on[:, h, :], id64)
                    P.tt(ys[:, :, cs], tp, hgnw.un(2).bc([128, 8, 64]), ALU.mult)
                    for h in range(8):
                        hs = slice(h * 128, (h + 1) * 128)
                        P.mm_group(sps[:, h // 4, (h % 4) * 128:(h % 4) * 128 + 128], [(kh[:, ci, hs], vt[:, ci, hs])])
                    P.tt(S, S, decall[:, :, c:c + 1].bc([128, 8, 128]), ALU.mult)
                    P.tt(S, S, sps.re("p a (h e) -> p (a h) e", e=128), ALU.add)
                P.dma(K.yT[2].v(t0, t1).re("(c p) t -> p c t", p=128), ys)


def stage_in_rw(K, l):
    P = K.P
    NZ = 3328
    with P.scope():
        wrw = P.sb([128, 8, NZ], BF16)
        load_w_cols(P, wrw, K.w_in[l], C_RW, C_RW + NZ)
        wa2 = P.sb([128, D])
        g2 = P.sb([128, D])
        P.dma(wa2, K.rw_wa2[l])
        P.dma(g2, K.rw_g2[l])
        if l == 1:
            wvr = P.sb([128, 8, 32], BF16)
            P.dma(wvr, K.w_vres, eng="pool")
            v2 = P.sb([32, D])
            P.dma(v2, K.rw_v2)
            v0 = K.vec("rw_v0")
            muv = K.vec("muvres")
        mu = K.vec("rwmu%d" % l)
        w0, a0 = K.vec("rw_w0%d" % l), K.vec("rw_a0%d" % l)
        k_k, k_a, r_k = K.vec("rw_k_k%d" % l), K.vec("rw_k_a%d" % l), K.vec("rw_r_k%d" % l)
        omka = P.sb([128, 8])
        P.ts(omka, k_a, -1.0, ALU.mult, 1.0, ALU.add)
        prev = P.sb([128, 28])
        hbs = [P.sb([128, 8, TB], BF16) for _ in range(2)]
        zl = P.sb([128, 3, TB + 1])
        dl = P.sb([128, 3, TB])
        twa = P.sb([128, TB])
        sgl = P.sb([128, TB])
        vl = P.sb([32, TB])
        z3 = P.sb([128, 3, TB + 1])
        d3 = P.sb([128, 3, TB])
        sh3 = P.sb([128, 3, TB])
        W_ = [P.sb([128, TB]) for _ in range(14)]
        gam = P.sb([128, 8])
        vfb = P.sb([128, TB])
        stg = [P.sb([128, 4, D]) for _ in range(3)]
        psA = [P.ps([128, TB]) for _ in range(2)]
        psL = [P.ps([128, TB]) for _ in range(3)]
        psT = [P.ps([128, TB]) for _ in range(3)]
        for tb in range(NTB):
            hb = hbs[tb % 2]
            first = (tb % 4 == 0)
            P.dma(hb, xview(K.hT, tb))
            if first:
                P.memset(prev, 0.0)
            nlo = 3 if l == 1 else 2
            for i in range(nlo):
                ps = psA[i % 2]
                if i < 2:
                    P.mm_group(ps, [(wrw[:, kc, (24 + i) * 128:(25 + i) * 128], hb[:, kc, :]) for kc in range(8)])
                    P.copy(zl[:, i, 1:TB + 1], ps, eng="act")
                else:
                    P.mm_group(ps[0:32, :], [(wvr[:, kc, :], hb[:, kc, :]) for kc in range(8)])
                    P.copy(zl[0:32, i, 1:TB + 1], ps[0:32, :], eng="act")
                P.copy(zl[:, i, 0:1], prev[:, 24 + i:25 + i])
                P.copy(prev[:, 24 + i:25 + i], zl[:, i, TB:TB + 1])
            P.tt(dl[:, 0:nlo, :], zl[:, 0:nlo, 0:TB], zl[:, 0:nlo, 1:TB + 1], ALU.subtract)
            P.stt(dl[:, 0, :], dl[:, 0, :], mu[:, 24:25], zl[:, 0, 1:TB + 1], ALU.mult, ALU.add)
            P.stt(dl[:, 1, :], dl[:, 1, :], mu[:, 25:26], zl[:, 1, 1:TB + 1], ALU.mult, ALU.add)
            P.act(twa[0:64, :], dl[0:64, 0, :], AF.Tanh)
            P.copy(twa[64:128, :], dl[64:128, 0, :], eng="act")
            P.act(sgl, dl[:, 1, :], AF.Sigmoid)
            if l == 1:
                P.stt(vl, dl[0:32, 2, :], muv[0:32, 0:1], zl[0:32, 2, 1:TB + 1], ALU.mult, ALU.add)
            for j in range(8):
                js = slice(j * 128, (j + 1) * 128)
                (kk, sq, t1, kmod, bb, rk, bon, sgw, a_, cum, G, Gi, Gp, gg) = W_
                for i in range(3):
                    ch = i * 8 + j
                    ps = psA[i % 2]
                    P.mm_group(ps, [(wrw[:, kc, ch * 128:(ch + 1) * 128], hb[:, kc, :]) for kc in range(8)])
                    P.copy(z3[:, i, 1:TB + 1], ps, eng="act")
                    P.copy(z3[:, i, 0:1], prev[:, ch:ch + 1])
                    P.copy(prev[:, ch:ch + 1], z3[:, i, TB:TB + 1])
                P.tt(d3, z3[:, :, 0:TB], z3[:, :, 1:TB + 1], ALU.subtract)
                for i in range(3):
                    ch = i * 8 + j
                    P.stt(sh3[:, i, :], d3[:, i, :], mu[:, ch:ch + 1], z3[:, i, 1:TB + 1], ALU.mult, ALU.add)
                r, k, v = sh3[:, 0, :], sh3[:, 1, :], sh3[:, 2, :]
                pw, pa, pg = psL
                P.mm_group(pw, [(wa2[0:64, js], twa[0:64, :])])
                P.mm_group(pa, [(wa2[64:128, js], twa[64:128, :])])
                P.mm_group(pg, [(g2[:, js], sgl)])
                P.act(sgw, pw, AF.Sigmoid, bias=w0[:, j:j + 1])
                P.act(a_, pa, AF.Sigmoid, bias=a0[:, j:j + 1])
                P.copy(gg, pg, eng="act")
                P.dma(K.rw_gT.v(tb * TB, (tb + 1) * TB)[js, :], gg)
                if l == 1:
                    pv = psT[0]
                    P.mm_group(pv, [(v2[0:32, js], vl[0:32, :])])
                    P.act(t1, pv, AF.Sigmoid, bias=v0[:, j:j + 1])
                    P.dma(vfb, K.rw_vfT.v(tb * TB, (tb + 1) * TB)[js, :])
                    P.tt(vfb, vfb, v, ALU.subtract)
                    P.tt(vfb, vfb, t1, ALU.mult)
                    P.tt(v, v, vfb, ALU.add)
                else:
                    P.dma(K.rw_vfT.v(tb * TB, (tb + 1) * TB)[js, :], v)
                P.ts(kk, k, k_k[:, j:j + 1], ALU.mult, eng="pool")
                P.act(sq, kk, AF.Square)
                pn = psT[1]
                P.mm_group(pn, [(K.blk64, sq)])
                P.act(sq, pn, AF.Sqrt)
                P.ts(sq, sq, 1e-12, ALU.max)
                P.recip(sq, sq)
                P.tt(kk, kk, sq, ALU.mult)
                P.ts(t1, a_, k_a[:, j:j + 1], ALU.mult, omka[:, j:j + 1], ALU.add)
                P.tt(kmod, k, t1, ALU.mult, eng="pool")
                P.tt(bb, kk, a_, ALU.mult)
                P.stt(rk, r, r_k[:, j:j + 1], kmod, ALU.mult, ALU.mult)
                pb_ = psT[2]
                P.mm_group(pb_, [(K.blk64, rk)])
                P.tt(bon, pb_, v, ALU.mult)
                P.dma(K.rw_bonT.v(tb * TB, (tb + 1) * TB)[js, :], bon)
                P.scan(cum, K.rmask, sgw, 0.0, ALU.mult, ALU.add)
                P.act(G, cum, AF.Exp, scale=-WSC)
                P.act(Gi, cum, AF.Exp, scale=WSC)
                P.tt(t1, cum, sgw, ALU.subtract, eng="pool")
                P.act(Gp, t1, AF.Exp, scale=-WSC)
                P.tt(kk, kk, Gp, ALU.mult)
                P.tt(rk, r, G, ALU.mult, eng="pool")
                P.tt(bb, bb, Gi, ALU.mult)
                P.tt(kmod, kmod, Gi, ALU.mult, eng="pool")
                P.copy(gam, G.re("p (n l) -> p n l", l=LRW)[:, :, LRW - 1])
                gb = gam.un(2).bc([128, 8, LRW])
                P.tt(t1.re("p (n l) -> p n l", l=LRW), bb.re("p (n l) -> p n l", l=LRW), gb, ALU.mult)
                P.tt(sq.re("p (n l) -> p n l", l=LRW), kmod.re("p (n l) -> p n l", l=LRW), gb, ALU.mult,
                     eng="pool")
                for (src, dst) in ((kk, K.rw_kapT), (rk, K.rw_rhoT), (bb, K.rw_betT), (kmod, K.rw_ktlT)):
                    P.dma(dst.v(tb * TB, (tb + 1) * TB)[js, :], src)
                P.dma(K.rw_gamT.v(tb * 8, tb * 8 + 8)[js, :], gam)
                for xi, X in enumerate((v, t1, sq)):
                    px = psT[xi]
                    for tt_ in range(4):
                        P.transpose(px[:, tt_ * 128:(tt_ + 1) * 128], X[:, tt_ * 128:(tt_ + 1) * 128], K.ident)
                    if xi == 1:
                        P.copy(stg[xi][:, :, js], px.re("p (a b) -> p a b", a=4), eng="act")
                    else:
                        P.copy(stg[xi][:, :, js], px.re("p (a b) -> p a b", a=4))
            for xi, dst in enumerate((K.rw_vtok, K.rw_bhat, K.rw_khat)):
                P.dma(dst.v(tb * TB, (tb + 1) * TB).re("(a t) e -> t a e", t=128), stg[xi])


def stage_rw_rec(K, l):
    P = K.P
    NCH = T // LRW
    CB = 8
    L = LRW
    with P.scope():
        mask12 = P.sb([64, 128])
        P.copy(mask12[:, 0:64], K.tris[0:64, 0:64])
        P.copy(mask12[:, 64:128], K.tri[0:64, 0:64])
        lows = K.lows[0:64, 0:64]
        id64 = K.ident[0:64, 0:64]
        KRs = [P.sb([64, 4, CB, 2, L]) for _ in range(2)]
        BTs = [P.sb([64, 4, CB * L]) for _ in range(2)]
        KTs = [P.sb([64, 4, CB * L]) for _ in range(2)]
        gms = [P.sb([64, 4, CB]) for _ in range(2)]
        vts = [P.sb([64, CB, 256]) for _ in range(2)]
        bhs = [P.sb([64, CB, 256]) for _ in range(2)]
        khs = [P.sb([64, CB, 256]) for _ in range(2)]
        ysts = [P.sb([64, CB, 256]) for _ in range(2)]
        S = P.sb([64, 4, L])
        M1 = P.sb([64, 4, 128])
        M2 = P.sb([64, 4, 128])
        Pm = [P.sb([64, 4, L]) for _ in range(2)]
        Qm = [P.sb([64, 4, L]) for _ in range(2)]
        TT = P.sb([64, 4, L])
        Rn = P.sb([64, 4, L])
        U = P.sb([64, 4, L])
        A1 = P.ps([64, 4, 128])
        A2 = P.ps([64, 4, 128])
        A3 = P.ps([64, 4, L])
        PP = P.ps([64, 4, L])
        QQ = P.ps([64, 4, L])
        TP = P.ps([64, 4, L])
        RU = P.ps([64, 2, 4, L])
        YS = P.ps([64, 2, 4, L])
        RP, UP = RU[:, 0], RU[:, 1]
        YP, SP = YS[:, 0], YS[:, 1]
        it = 0
        import os as _os
        lim = [int(x) for x in _os.environ.get("RW_LIM", "2,4,4").split(",")]
        for b in range(lim[0]):
            T0 = b * T
            for hg in range(lim[1]):
                P.memset(S, 0.0)
                for cb in range(lim[2]):
                    t0 = T0 + cb * CB * L
                    t1 = t0 + CB * L
                    KR, BT, KT, gm = KRs[it % 2], BTs[it % 2], KTs[it % 2], gms[it % 2]
                    vt, bh, kh, yst = vts[it % 2], bhs[it % 2], khs[it % 2], ysts[it % 2]
                    it += 1
                    for hh in range(4):
                        ch = slice(hg * 256 + hh * 64, hg * 256 + hh * 64 + 64)
                        P.dma(KR[:, hh, :, 0, :], K.rw_kapT.v(t0, t1)[ch, :].re("p (c l) -> p c l", l=L))
                        P.dma(KR[:, hh, :, 1, :], K.rw_rhoT.v(t0, t1)[ch, :].re("p (c l) -> p c l", l=L))
                        P.dma(BT[:, hh, :], K.rw_betT.v(t0, t1)[ch, :])
                        P.dma(KT[:, hh, :], K.rw_ktlT.v(t0, t1)[ch, :])
                        g0 = b * NCH + cb * CB
                        P.dma(gm[:, hh, :], K.rw_gamT.v(g0, g0 + CB)[ch, :])
                    es = slice(hg * 256, hg * 256 + 256)
                    P.dma(vt, K.rw_vtok.v(t0, t1)[:, es].re("(c t) e -> t c e", t=L))
                    P.dma(bh, K.rw_bhat.v(t0, t1)[:, es].re("(c t) e -> t c e", t=L))
                    P.dma(kh, K.rw_khat.v(t0, t1)[:, es].re("(c t) e -> t c e", t=L))
                    for ci in range(CB):
                        cs = slice(ci * L, (ci + 1) * L)
                        for hh in range(4):
                            krr = KR[:, hh, ci, :, :].re("p a l -> p (a l)")
                            P.mm_group(A1[:, hh, :], [(BT[:, hh, cs], krr)])
                            P.mm_group(A2[:, hh, :], [(KT[:, hh, cs], krr)])
                            P.mm_group(A3[:, hh, :], [(KR[:, hh, ci, 0, :], BT[:, hh, cs])])
                        P.tt(M1, A1, mask12.un(1).bc([64, 4, 128]), ALU.mult)
                        P.tt(M2, A2, mask12.un(1).bc([64, 4, 128]), ALU.mult)
                        P.tt(Pm[0], A3, lows.un(1).bc([64, 4, L]), ALU.mult)
                        Pc = Pm[0]
                        Qc = M1[:, :, 0:L]
                        ArbT = M1[:, :, L:2 * L]
                        AkkT = M2[:, :, 0:L]
                        ArkT = M2[:, :, L:2 * L]
                        P.tt(TT, id64.un(1).bc([64, 4, L]), Qc, ALU.subtract)
                        for lev in range(1, 6):
                            Pn = Pm[lev % 2]
                            Qn = Qm[lev % 2]
                            for hh in range(4):
                                P.mm_group(PP[:, hh, :], [(Qc[:, hh, :], Pc[:, hh, :])])
                            if lev < 5:
                                for hh in range(4):
                                    P.mm_group(QQ[:, hh, :], [(Pc[:, hh, :], Qc[:, hh, :])])
                            P.copy(Pn, PP)
                            if lev < 5:
                                P.copy(Qn, QQ, eng="act")
                            for hh in range(4):
                                P.mm_group(TP[:, hh, :], [(Pn[:, hh, :], TT[:, hh, :])])
                            P.tt(TT, TT, TP, ALU.add)
                            Pc, Qc = Pn, Qn
                        for hh in range(4):
                            vs = slice(hh * 64, hh * 64 + 64)
                            P.mm_group(RP[:, hh, :], [(KR[:, hh, ci, 0, :], S[:, hh, :]), (AkkT[:, hh, :], vt[:, ci, vs])])
                        P.ts(Rn, RP, -1.0, ALU.mult)
                        for hh in range(4):
                            P.mm_group(UP[:, hh, :], [(TT[:, hh, :], Rn[:, hh, :])])
                        P.copy(U, UP)
                        for hh in range(4):
                            vs = slice(hh * 64, hh * 64 + 64)
                            P.mm_group(YP[:, hh, :], [(KR[:, hh, ci, 1, :], S[:, hh, :]), (ArbT[:, hh, :], U[:, hh, :]),
                                                      (ArkT[:, hh, :], vt[:, ci, vs])])
                        P.copy(yst[:, ci, :].re("p (h e) -> p h e", h=4), YP, eng="act")
                        for hh in range(4):
                            vs = slice(hh * 64, hh * 64 + 64)
                            P.mm_group(SP[:, hh, :], [(bh[:, ci, vs], U[:, hh, :]), (kh[:, ci, vs], vt[:, ci, vs])])
                        P.tt(S, S, gm[:, :, ci:ci + 1].bc([64, 4, L]), ALU.mult)
                        P.tt(S, S, SP, ALU.add)
                    P.dma(K.rw_ytok.v(t0, t1)[:, es].re("(c t) e -> t c e", t=L), yst)
    with P.scope():
        lnw, lnb = K.vec("rw_ln_w%d" % l), K.vec("rw_ln_b%d" % l)
        ys = [P.sb([128, 16, 64]) for _ in range(2)]
        cen = P.sb([128, 16, 64])
        sq = P.sb([128, 16, 64])
        sm = P.sb([128, 48])
        bons = [P.sb([128, 8, 128]) for _ in range(2)]
        ggs = [P.sb([128, 8, 128]) for _ in range(2)]
        tmp = P.sb([128, 8, 128])
        yo = [P.sb([128, 8, 128], BF16) for _ in range(2)]
        px = [P.ps([128, 4, 128]) for _ in range(2)]
        for tt_ in range(TOK // 128):
            t0 = tt_ * 128
            y, bon, gg, o = ys[tt_ % 2], bons[tt_ % 2], ggs[tt_ % 2], yo[tt_ % 2]
            P.dma(y, K.rw_ytok.v(t0, t0 + 128).re("t (h e) -> t h e", e=64))
            P.dma(bon, K.rw_bonT.v(t0, t0 + 128).re("(c p) t -> p c t", p=128))
            P.dma(gg, K.rw_gT.v(t0, t0 + 128).re("(c p) t -> p c t", p=128))
            P.reduce(sm[:, 0:16], y, ALU.add)
            P.ts(sm[:, 0:16], sm[:, 0:16], 1.0 / 64.0, ALU.mult)
            P.tt(cen, y, sm[:, 0:16].un(2).bc([128, 16, 64]), ALU.subtract)
            P.tt(sq, cen, cen, ALU.mult, eng="pool")
            P.reduce(sm[:, 16:32], sq, ALU.add)
            P.act(sm[:, 32:48], sm[:, 16:32], AF.Sqrt, bias=K.epsb[:, 1:2], scale=1.0 / 64.0)
            P.recip(sm[:, 32:48], sm[:, 32:48])
            P.tt(cen, cen, sm[:, 32:48].un(2).bc([128, 16, 64]), ALU.mult)
            cf = cen.re("p h e -> p (h e)")
            for j in range(8):
                P.transpose(px[j // 4][:, j % 4, :], cf[:, j * 128:(j + 1) * 128], K.ident)
            for j in range(8):
                P.ts(tmp[:, j, :], px[j // 4][:, j % 4, :], lnw[:, j:j + 1], ALU.mult, lnb[:, j:j + 1], ALU.add)
            P.tt(tmp, tmp, bon, ALU.add, eng="pool")
            P.tt(o, tmp, gg, ALU.mult)
            P.dma(K.yT[1].v(t0, t0 + 128).re("(c p) t -> p c t", p=128), o)


def stage_mix(K, l):
    P = K.P
    with P.scope():
        wgt = P.sb([128, 8, 3072], BF16)
        load_w_cols(P, wgt, K.w_in[l], C_GATE, C_GATE + 3072)
        wbr = P.sb([128, 3, 8, D], BF16)
        for br in range(3):
            for kc in range(8):
                P.dma(wbr[:, br, kc, :], K.w_branch[l][:, br, kc, :], eng="pool")
        wo = P.sb([128, 8, D], BF16)
        for kc in range(8):
            P.dma(wo[:, kc, :], K.w_out[l][:, kc, :], eng="pool")
        hbs = [P.sb([128, 8, TB], BF16)] * 2
        yb = [P.sb([128, 8, TB], BF16) for _ in range(3)]
        xb = P.sb([128, 8, TB])
        xo = P.sb([128, 8, TB])
        mixb = P.sb([128, 8, TB], BF16)
        gts = [P.sb([128, TB]) for _ in range(2)]
        tmp = P.sb([128, TB])
        acc = P.sb([128, TB])
        psG = [P.ps([128, TB]) for _ in range(2)]
        psY = [P.ps([128, TB]) for _ in range(2)]
        psO = [P.ps([128, TB]) for _ in range(2)]
        for tb in range(NTB):
            b = tb // 4
            hb = hbs[tb % 2]
            P.dma(hb, xview(K.hT, tb))
            for br in range(3):
                P.dma(yb[br], xview(K.yT[br], tb))
            P.dma(xb, xview(K.xs, tb))
            i = 0
            for n in range(8):
                ns = slice(n * 128, (n + 1) * 128)
                for br in range(3):
                    pg, py, g = psG[i % 2], psY[i % 2], gts[i % 2]
                    i += 1
                    P.mm_group(pg, [(wgt[:, kc, br * 1024 + n * 128:br * 1024 + (n + 1) * 128], hb[:, kc, :]) for kc in range(8)])
                    P.mm_group(py, [(wbr[:, br, kc, ns], yb[br][:, kc, :]) for kc in range(8)])
                    P.act(g, pg, AF.Sigmoid)
                    if br == 0:
                        P.tt(acc, g, py, ALU.mult)
                    elif br == 1:
                        P.tt(tmp, g, py, ALU.mult)
                        P.tt(acc, acc, tmp, ALU.add, eng="pool")
                    else:
                        P.tt(tmp, g, py, ALU.mult)
                        P.tt(mixb[:, n, :], acc, tmp, ALU.add, eng="pool")
            for m in range(8):
                po = psO[m % 2]
                P.mm_group(po, [(wo[:, kc, m * 128:(m + 1) * 128], mixb[:, kc, :]) for kc in range(8)])
                P.stt(xo[:, m, :], po, K.gt[l][1][:, m, b:b + 1], xb[:, m, :], ALU.mult, ALU.add)
            P.dma(xview(K.xs, tb), xo)
def prep_core_inputs(inp, vecs, W, core):
    b0 = core * NB
    x = np.asarray(inp["x"], np.float32)[b0:b0 + NB].reshape(TOK, D)
    c = np.asarray(inp["c"], np.float32)[b0:b0 + NB]
    m = dict(W)
    m["xT"] = np.ascontiguousarray(x.T)
    m["cT"] = np.ascontiguousarray(c.reshape(NB, KC, 128).transpose(2, 1, 0))
    m["vecs"] = vecs
    return m


_CACHE = {}


def kernel(**inputs):
    from concourse.bass_utils import run_bass_kernel_spmd
    vp = pack_vecs(inputs)
    vecs = vp.build()
    W = host_weights(inputs)
    key = "main"
    if key not in _CACHE:
        _CACHE[key] = build_program(vp.off, vp.n)
    nc = _CACHE[key]
    in_maps = [prep_core_inputs(inputs, vecs, W, c) for c in range(8)]
    res = run_bass_kernel_spmd(nc, in_maps, core_ids=list(range(8)))
    out = np.empty((16, T, D), np.float32)
    for c in range(8):
        o = np.asarray(res.results[c]["outT"])
        out[c * NB:(c + 1) * NB] = o.T.reshape(NB, T, D)
    return out
```

```python
import contextlib
import numpy as np
import concourse.bass as bass
import concourse.mybir as mybir

F32 = mybir.dt.float32
BF16 = mybir.dt.bfloat16
ALU = mybir.AluOpType
AF = mybir.ActivationFunctionType
AX = mybir.AxisListType

ENGS = ("pe", "act", "dve", "pool", "sp")


class Tok:
    __slots__ = ("w", "r", "name")

    def __init__(self, name=""):
        self.w = None
        self.r = {}
        self.name = name


class V:
    __slots__ = ("ap", "toks")

    def __init__(self, ap, toks):
        self.ap = ap
        self.toks = toks

    def __getitem__(self, idx):
        return V(self.ap[idx], self.toks)

    def re(self, s, **kw):
        return V(self.ap.rearrange(s, **kw), self.toks)

    def bc(self, shape):
        return V(self.ap.broadcast_to(shape), self.toks)

    def un(self, axis):
        return V(self.ap.unsqueeze(axis), self.toks)

    def bitcast(self, dt):
        return V(self.ap.bitcast(dt), self.toks)

    @property
    def shape(self):
        return self.ap.shape


class Prog:
    def __init__(self, nc, es, n_dma_sems=40):
        self.nc = nc
        self.es = es
        self.q = {e: [] for e in ENGS}
        self.cnt = {e: 0 for e in ENGS}
        self.sem = {e: es.enter_context(nc.semaphore("s_" + e)) for e in ENGS}
        self.semobj = dict(self.sem)
        self.waited = {e: {} for e in ENGS}
        self.dsems = []
        self.n_ring = n_dma_sems
        for i in range(n_dma_sems):
            k = "d%d" % i
            self.semobj[k] = es.enter_context(nc.semaphore(k))
            self.dsems.append([k, 0, None])
        self.dnext = 0
        self.uid = 0
        self.psems = []
        for i in range(48):
            k = "q%d" % i
            self.semobj[k] = es.enter_context(nc.semaphore(k))
            self.psems.append(k)
        self.pnext = 0

    def sb(self, shape, dt=F32, name=None):
        self.uid += 1
        name = name or ("t%d" % self.uid)
        t = self.es.enter_context(self.nc.sbuf_tensor(name, list(shape), dt))
        return V(t[:], (Tok(name),))

    def ps(self, shape, dt=F32, name=None):
        self.uid += 1
        name = name or ("p%d" % self.uid)
        shape = list(shape)
        n = 1
        for d_ in shape[1:]:
            n *= d_
        nb = (n + 511) // 512
        t = self.es.enter_context(self.nc.psum_tensor(name, [shape[0], nb * 512], dt))
        ap = t[:][:, 0:n]
        if len(shape) == 3:
            ap = ap.rearrange("p (a b) -> p a b", a=shape[1])
        elif len(shape) == 4:
            ap = ap.rearrange("p (a b c) -> p a b c", a=shape[1], b=shape[2])
        return V(ap, (Tok(name),))

    def dram(self, name, shape, dt=F32, kind="Internal"):
        t = self.nc.dram_tensor(name, list(shape), dt, kind=kind)
        return V(t.ap(), (Tok(name),))

    def sub(self, v, name=""):
        return V(v.ap, (Tok(name),))

    def _needs(self, reads, writes):
        needs = {}

        def need(k, val):
            if val > needs.get(k, 0):
                needs[k] = val
        for v in reads:
            for t in v.toks:
                if t.w is not None:
                    need(*t.w)
        for v in writes:
            for t in v.toks:
                if t.w is not None:
                    need(*t.w)
                for k, val in t.r.items():
                    need(k, val)
        return needs

    def issue(self, eng, fn, reads, writes, inc=True, pe_group=False):
        needs = self._needs(reads, writes)
        waits = []
        wd = self.waited[eng]
        for k, val in needs.items():
            if k == eng and eng == "pe":
                continue
            if wd.get(k, 0) < val:
                wd[k] = val
                waits.append((k, val))
        n = self.cnt[eng] + 1
        if inc:
            self.cnt[eng] = n
        self.q[eng].append((waits, fn, inc))
        for v in reads:
            for t in v.toks:
                if t.r.get(eng, 0) < n:
                    t.r[eng] = n
        for v in writes:
            for t in v.toks:
                t.w = (eng, n)
                t.r = {}
        return n

    def dma(self, out, in_, eng="sp", **kw):
        if eng == "pool":
            slot = [self.psems[self.pnext], 0, None]
            self.pnext += 1
            self.dsems.append(slot)
        else:
            slot = self.dsems[self.dnext]
            self.dnext = (self.dnext + 1) % self.n_ring
        key, cur, _ = slot
        needs = self._needs([in_], [out])
        wd = self.waited[eng]
        waits = []
        if cur > 0:
            needs[key] = max(needs.get(key, 0), cur)
        for k, val in needs.items():
            if wd.get(k, 0) < val:
                wd[k] = val
                waits.append((k, val))
        newv = cur + 16
        slot[1] = newv
        oap, iap = out.ap, in_.ap

        def fn(e, oap=oap, iap=iap, kw=kw):
            return e.dma_start(out=oap, in_=iap, **kw)
        self.q[eng].append((waits, fn, ("dma", key)))
        for t in in_.toks:
            if t.r.get(key, 0) < newv:
                t.r[key] = newv
        for t in out.toks:
            t.w = (key, newv)
            t.r = {}

    def emit(self, final_waits=()):
        nc = self.nc
        engmap = {"pe": "tensor", "act": "scalar", "dve": "vector", "pool": "gpsimd", "sp": "sync"}
        with nc.Block() as block:
            for e in ENGS:
                lst = self.q[e]

                def body(eh, lst=lst, e=e):
                    for waits, fn, inc in lst:
                        for k, val in waits:
                            eh.wait_ge(self.semobj[k], val)
                        if fn is None:
                            continue
                        ins = fn(eh)
                        if inc is True:
                            ins.then_inc(self.semobj[e], 1)
                        elif inc:
                            ins.then_inc(self.semobj[inc[1]], 16)
                    if e == "sp":
                        for k, val in final_waits:
                            eh.wait_ge(self.semobj[k], val)
                getattr(block, engmap[e])(body)

    def mm(self, out, lhsT, rhs, start=True, stop=True):
        o, l, r = out.ap, lhsT.ap, rhs.ap

        def fn(e):
            return e.matmul(o, l, r, start=start, stop=stop)
        if stop:
            self.issue("pe", fn, [lhsT, rhs], [out], inc=True)
        else:
            self.issue("pe", fn, [lhsT, rhs], [], inc=False)
        return

    def mm_group(self, out, pairs):
        n = len(pairs)
        for i, (l, r) in enumerate(pairs):
            o, la, ra = out.ap, l.ap, r.ap
            st, sp_ = (i == 0), (i == n - 1)

            def fn(e, o=o, la=la, ra=ra, st=st, sp_=sp_):
                return e.matmul(o, la, ra, start=st, stop=sp_)
            if sp_ and n == 1:
                self.issue("pe", fn, [l, r], [out], inc=True)
            elif st:
                needs_v = V(out.ap, out.toks)
                self.issue_pe_first(fn, [l, r], needs_v)
            elif sp_:
                self.issue("pe", fn, [l, r], [out], inc=True)
            else:
                self.issue("pe", fn, [l, r], [], inc=False)

    def issue_pe_first(self, fn, reads, outv):
        eng = "pe"
        needs = self._needs(reads, [outv])
        waits = []
        wd = self.waited[eng]
        for k, val in needs.items():
            if k == eng:
                continue
            if wd.get(k, 0) < val:
                wd[k] = val
                waits.append((k, val))
        n = self.cnt[eng] + 1
        self.q[eng].append((waits, fn, False))
        for v in reads:
            for t in v.toks:
                if t.r.get(eng, 0) < n:
                    t.r[eng] = n

    def transpose(self, out, in_, ident):
        o, i, d = out.ap, in_.ap, ident.ap

        def fn(e):
            return e.transpose(o, i, d)
        self.issue("pe", fn, [in_, ident], [out])

    def act(self, out, in_, func, bias=None, scale=None, accum=None, eng="act"):
        o, i = out.ap, in_.ap
        reads = [in_]
        kw = {}
        if bias is not None:
            if isinstance(bias, V):
                reads.append(bias)
                kw["bias"] = bias.ap
            else:
                kw["bias"] = bias
        if scale is not None:
            if isinstance(scale, V):
                reads.append(scale)
                kw["scale"] = scale.ap
            else:
                kw["scale"] = scale
        writes = [out]
        if accum is not None:
            kw["accum_out"] = accum.ap
            writes.append(accum)

        def fn(e):
            return e.activation(o, i, func, **kw)
        self.issue("act", fn, reads, writes)

    def tt(self, out, a, b, op, eng="dve"):
        o, x, y = out.ap, a.ap, b.ap

        def fn(e):
            return e.tensor_tensor(o, x, y, op)
        self.issue(eng, fn, [a, b], [out])

    def ts(self, out, a, s1, op0, s2=None, op1=None, eng="dve", accum=None):
        o, x = out.ap, a.ap
        reads = [a]

        def cv(s):
            if isinstance(s, V):
                reads.append(s)
                return s.ap
            return s
        s1a = cv(s1)
        s2a = cv(s2)
        writes = [out]
        kw = {}
        if accum is not None:
            kw["accum_out"] = accum.ap
            writes.append(accum)

        def fn(e):
            if op1 is None:
                return e.tensor_scalar(o, x, s1a, None, op0, **kw)
            return e.tensor_scalar(o, x, s1a, s2a, op0, op1, **kw)
        self.issue(eng, fn, reads, writes)

    def stt(self, out, a, s, b, op0, op1, eng="dve"):
        o, x, y = out.ap, a.ap, b.ap
        reads = [a, b]
        if isinstance(s, V):
            reads.append(s)
            sa = s.ap
        else:
            sa = s

        def fn(e):
            return e.scalar_tensor_tensor(o, x, sa, y, op0, op1)
        self.issue(eng, fn, reads, [out])

    def copy(self, out, in_, eng="dve"):
        o, i = out.ap, in_.ap
        if eng == "act":
            def fn(e):
                return e.copy(o, i)
        else:
            def fn(e):
                return e.tensor_copy(o, i)
        self.issue(eng, fn, [in_], [out])

    def memset(self, out, val, eng="dve"):
        o = out.ap

        def fn(e):
            return e.memset(o, val)
        self.issue(eng, fn, [], [out])

    def recip(self, out, in_, eng="dve"):
        o, i = out.ap, in_.ap

        def fn(e):
            return e.reciprocal(o, i)
        self.issue(eng, fn, [in_], [out])

    def scan(self, out, d0, d1, init, op0, op1):
        o, a, b = out.ap, d0.ap, d1.ap
        reads = [d0, d1]
        if isinstance(init, V):
            reads.append(init)
            ia = init.ap
        else:
            ia = init

        def fn(e):
            return e.tensor_tensor_scan(o, a, b, ia, op0, op1)
        self.issue("dve", fn, reads, [out])

    def reduce(self, out, in_, op, axis=AX.X, eng="dve"):
        o, i = out.ap, in_.ap

        def fn(e):
            return e.tensor_reduce(o, i, axis, op)
        self.issue(eng, fn, [in_], [out])


def _prog_barrier(self):
    targets = {e: self.cnt[e] for e in ENGS if self.cnt[e] > 0}
    for s in self.dsems:
        if s[1] > 0:
            targets[s[0]] = s[1]
    for e in ENGS:
        wd = self.waited[e]
        waits = []
        for k, val in targets.items():
            if wd.get(k, 0) < val:
                wd[k] = val
                waits.append((k, val))
        if waits:
            self.q[e].append((waits, None, False))


@contextlib.contextmanager
def _prog_scope(self):
    old = self.es
    with contextlib.ExitStack() as es2:
        self.es = es2
        try:
            yield
        finally:
            self.barrier()
            self.es = old


Prog.barrier = _prog_barrier
Prog.scope = _prog_scope
D = 1024; T = 2048; NB = 2; TOK = NB * T; TB = 512; NTB = TOK // TB; KC = 8
DFF = 2816; NJ = 22; NIN = 12552
C_MLX, C_MLO, C_MLG = 0, 1024, 2048
C_RW = 2056; C_HG = 5384; C_GATE = 9480
EPS = 1e-6; RW_LN_EPS = 64e-5
LRW = 64; LHG = 64; LML = 128
WSC = 0.6065306597126334


def fm(v):
    v = np.asarray(v, np.float32).reshape(-1, 128)
    return np.ascontiguousarray(v.T)


def kxn(W):
    K, N = W.shape
    return np.ascontiguousarray(W.reshape(K // 128, 128, N).transpose(1, 0, 2))


class VecPack:
    def __init__(self):
        self.cols = []
        self.off = {}
        self.n = 0

    def add(self, name, arr):
        arr = np.asarray(arr, np.float32)
        assert arr.shape[0] == 128
        self.off[name] = (self.n, arr.shape[1])
        self.cols.append(arr)
        self.n += arr.shape[1]

    def build(self):
        return np.ascontiguousarray(np.concatenate(self.cols, axis=1))


def pack_vecs(inp):
    vp = VecPack()
    rep = lambda v: np.tile(np.asarray(v, np.float32)[None, :], (128, 1))
    for l in range(2):
        for s in range(3):
            vp.add("normw%d%d" % (l, s), fm(inp["norm_w"][l, s]))
        vp.add("adab%d" % l, fm(inp["ada_b"][l]))
        for tp in range(4):
            vp.add("convw%d%d" % (l, tp), fm(inp["ml_conv_w"][l, tp]))
        vp.add("convb%d" % l, fm(inp["ml_conv_b"][l]))
        vp.add("mlnw%d" % l, fm(inp["ml_norm_w"][l]))
        vp.add("mlskip%d" % l, fm(inp["ml_skip"][l]))
        vp.add("ib%d" % l, rep(inp["ml_i_b"][l]))
        vp.add("fb%d" % l, rep(inp["ml_f_b"][l]))
        vp.add("rwmu%d" % l, fm(inp["rw_mu"][l]))
        for nm in ("rw_w0", "rw_a0", "rw_k_k", "rw_k_a", "rw_ln_w", "rw_ln_b"):
            vp.add(nm + str(l), fm(inp[nm][l]))
        vp.add("rw_r_k%d" % l, fm(inp["rw_r_k"][l].reshape(-1)))
        vp.add("hglb%d" % l, fm(inp["hg_lb_logits"][l]))
        vp.add("hgnw%d" % l, fm(inp["hg_norm_w"][l]))
    vp.add("finw", fm(inp["final_norm_w"]))
    vp.add("rw_v0", fm(inp["rw_v0"][0]))
    mv = np.zeros((128, 1), np.float32)
    mv[:32, 0] = inp["rw_mu_vres"][0]
    vp.add("muvres", mv)
    return vp


def host_weights(inp):
    w = {}
    w["ada_w"] = np.ascontiguousarray(
        np.asarray(inp["ada_w"], np.float32).reshape(2, 8, 128, 18, 512).transpose(0, 3, 2, 1, 4))
    w["ffn_up"] = np.ascontiguousarray(
        np.asarray(inp["ffn_up"], np.float32).reshape(2, 2, 8, 128, 2 * DFF).transpose(0, 1, 3, 2, 4))
    w["ffn_down"] = np.ascontiguousarray(
        np.asarray(inp["ffn_down"], np.float32).reshape(2, 2, NJ, 128, D).transpose(0, 1, 3, 2, 4))
    w["w_in"] = np.ascontiguousarray(
        np.asarray(inp["w_in"], np.float32).reshape(2, 8, 128, NIN).transpose(0, 2, 1, 3))
    w["w_vres"] = np.ascontiguousarray(
        np.asarray(inp["w_in_vres"], np.float32).reshape(8, 128, 32).transpose(1, 0, 2))
    w["wq"] = np.ascontiguousarray(
        np.asarray(inp["ml_wq"], np.float32).reshape(2, 4, 2, 128, 256).transpose(0, 3, 1, 2, 4))
    w["wk"] = np.ascontiguousarray(
        np.asarray(inp["ml_wk"], np.float32).reshape(2, 4, 2, 128, 256).transpose(0, 3, 1, 2, 4))
    w["rw_wa2"] = np.ascontiguousarray(np.concatenate(
        [np.asarray(inp["rw_w2"], np.float32), np.asarray(inp["rw_a2"], np.float32)], axis=1))
    w["rw_g2"] = np.ascontiguousarray(np.asarray(inp["rw_g2"], np.float32))
    w["rw_v2"] = np.ascontiguousarray(np.asarray(inp["rw_v2"], np.float32)[0])
    w["w_branch"] = np.ascontiguousarray(
        np.asarray(inp["w_branch"], np.float32).reshape(2, 3, 8, 128, D).transpose(0, 3, 1, 2, 4))
    w["w_out"] = np.ascontiguousarray(
        np.asarray(inp["w_out"], np.float32).reshape(2, 8, 128, D).transpose(0, 2, 1, 3))
    return w


class DT:
    def __init__(self, P, name, shape, dt, tok_axis, kind="Internal", blk=TB):
        self.t = P.nc.dram_tensor(name, list(shape), dt, kind=kind)
        self.ap = self.t.ap()
        self.tok_axis = tok_axis
        self.blk = blk
        n = shape[tok_axis] // blk
        self.toks = [Tok("%s_%d" % (name, i)) for i in range(max(n, 1))]

    def v(self, t0, t1):
        i0, i1 = t0 // self.blk, (t1 - 1) // self.blk
        toks = tuple(self.toks[i0:i1 + 1])
        if self.tok_axis == 0:
            return V(self.ap[t0:t1], toks)
        return V(self.ap[:, t0:t1], toks)

    def all(self):
        return V(self.ap, tuple(self.toks))
class Ctx:
    pass


STAGE_LOG = []


def build_program(vec_off, nvec, stop_after=None, debug_outs=(), only=None):
    nc = bass.Bass("TRN2", target_bir_lowering=False)
    es = contextlib.ExitStack()
    with es:
        P = Prog(nc, es)
        K = Ctx()
        K.P = P
        K.debug = {}

        def din(name, shape, dt=F32):
            return P.dram(name, shape, dt, kind="ExternalInput")
        K.xT_in = din("xT", [D, TOK])
        K.cT = din("cT", [128, KC, NB])
        K.vecs_d = din("vecs", [128, nvec])
        K.ada_w = din("ada_w", [2, 18, 128, 8, 512])
        K.ffn_up = din("ffn_up", [2, 2, 128, 8, 2 * DFF])
        K.ffn_down = din("ffn_down", [2, 2, 128, NJ, D])
        K.w_in = din("w_in", [2, 128, 8, NIN])
        K.w_vres = din("w_vres", [128, 8, 32])
        K.wq = din("wq", [2, 128, 4, 2, 256])
        K.wk = din("wk", [2, 128, 4, 2, 256])
        K.rw_wa2 = din("rw_wa2", [2, 128, D])
        K.rw_g2 = din("rw_g2", [2, 128, D])
        K.rw_v2 = din("rw_v2", [32, D])
        K.w_branch = din("w_branch", [2, 128, 3, 8, D])
        K.w_out = din("w_out", [2, 128, 8, D])
        K.outT = DT(P, "outT", [D, TOK], F32, 1, kind="ExternalOutput")

        def scr(name, shape, dt, tok_axis):
            kind = "ExternalOutput" if name in debug_outs else "Internal"
            return DT(P, name, shape, dt, tok_axis, kind=kind)
        K.xs = scr("xs", [D, TOK], F32, 1)
        K.act = scr("actT", [DFF, TOK], BF16, 1)
        K.hT = scr("hT", [D, TOK], BF16, 1)
        K.yT = [scr("yT%d" % i, [D, TOK], BF16, 1) for i in range(3)]
        K.ml_qT = scr("ml_qT", [D, TOK], F32, 1)
        K.ml_kT = scr("ml_kT", [D, TOK], F32, 1)
        K.ml_skx = scr("ml_skx", [D, TOK], F32, 1)
        K.ml_ktok = scr("ml_ktok", [TOK, D], F32, 0)
        K.ml_vo = scr("ml_vo", [TOK, 2056], F32, 0)
        K.hg_qT = scr("hg_qT", [D, TOK], F32, 1)
        K.hg_kT = scr("hg_kT", [D, TOK], F32, 1)
        K.hg_dec = scr("hg_dec", [D, TOK // LHG], F32, 1) if False else None
        K.hg_decT = DT(P, "hg_decT", [D, TOK // LHG], F32, 1, blk=TB // LHG)
        K.hg_vtok = scr("hg_vtok", [TOK, D], F32, 0)
        K.hg_khat = scr("hg_khat", [TOK, D], F32, 0)
        K.hg_gtok = scr("hg_gtok", [TOK, D], F32, 0)
        for nm in ("rw_kapT", "rw_rhoT", "rw_betT", "rw_ktlT", "rw_bonT", "rw_gT", "rw_vfT"):
            setattr(K, nm, scr(nm, [D, TOK], F32, 1))
        K.rw_gamT = DT(P, "rw_gamT", [D, TOK // LRW], F32, 1, blk=TB // LRW)
        for nm in ("rw_vtok", "rw_bhat", "rw_khat", "rw_ytok"):
            setattr(K, nm, scr(nm, [TOK, D], F32, 0))

        K.vecs = P.sb([128, nvec], F32, "vecs_sb")
        P.dma(K.vecs, K.vecs_d)

        def vec(name):
            o, n = vec_off[name]
            return K.vecs[:, o:o + n]
        K.vec = vec
        K.epsb = P.sb([128, 4], F32, "epsb")
        P.memset(K.epsb[:, 0:1], EPS)
        P.memset(K.epsb[:, 1:2], RW_LN_EPS)
        P.memset(K.epsb[:, 2:3], 1.0)
        P.memset(K.epsb[:, 3:4], 0.0)
        K.ones = P.sb([128, 128], F32, "ones")
        P.memset(K.ones, 1.0)
        K.ident = P.sb([128, 128], F32, "ident")
        P.memset(K.ident, 0.0)
        io = P.sb([128, 128], F32, "iota_f")
        ip = P.sb([128, 1], F32, "iota_p")
        io_i = P.sb([128, 128], mybir.dt.int32, "iota_fi")
        ip_i = P.sb([128, 1], mybir.dt.int32, "iota_pi")

        def iota(out, pattern, cm):
            o = out.ap

            def fn(e):
                return e.iota(o, pattern, base=0, channel_multiplier=cm)
            P.issue("pool", fn, [], [out])
        iota(io_i, [[1, 128]], 0)
        iota(ip_i, [[0, 1]], 1)
        P.copy(io, io_i)
        P.copy(ip, ip_i)
        K.iota_f = io
        K.iota_p = ip
        P.ts(K.ident, io, ip[:, 0:1], ALU.is_equal)
        K.tri = P.sb([128, 128], F32, "tri")
        P.ts(K.tri, io, ip[:, 0:1], ALU.is_ge)
        K.tris = P.sb([128, 128], F32, "tris")
        P.ts(K.tris, io, ip[:, 0:1], ALU.is_gt)
        K.lows = P.sb([128, 128], F32, "lows")
        P.ts(K.lows, io, ip[:, 0:1], ALU.is_lt)
        K.blk64 = P.sb([128, 128], F32, "blk64")
        P.memset(K.blk64, 0.0)
        P.memset(K.blk64[0:64, 0:64], 1.0)
        P.memset(K.blk64[64:128, 64:128], 1.0)
        K.rmask = P.sb([128, TB], F32, "rmask")
        P.memset(K.rmask, 1.0)
        P.memset(K.rmask.re("p (c l) -> p c l", l=64)[:, :, 0:1], 0.0)

        K.modT = [P.sb([128, 72, NB], F32, "modT%d" % l) for l in range(2)]
        K.sc1 = [[P.sb([128, 8, NB], F32) for s in range(3)] for l in range(2)]
        K.gt = [[P.sb([128, 8, NB], F32) for s in range(3)] for l in range(2)]
        stage_mod(K)
        stages = [
            ("copyx", lambda: stage_copyx(K)),
        ]
        for l in range(2):
            stages += [
                ("ffn%d0" % l, lambda l=l: stage_ffn(K, l, 0)),
                ("normmix%d" % l, lambda l=l: stage_normmix(K, l)),
                ("inml%d" % l, lambda l=l: stage_in_ml(K, l)),
                ("mlrec%d" % l, lambda l=l: stage_ml_rec(K, l)),
                ("inhg%d" % l, lambda l=l: stage_in_hg(K, l)),
                ("hgrec%d" % l, lambda l=l: stage_hg_rec(K, l)),
                ("inrw%d" % l, lambda l=l: stage_in_rw(K, l)),
                ("rwrec%d" % l, lambda l=l: stage_rw_rec(K, l)),
                ("mix%d" % l, lambda l=l: stage_mix(K, l)),
                ("ffn%d1" % l, lambda l=l: stage_ffn(K, l, 1)),
            ]
        stages.append(("final", lambda: stage_final(K)))
        for name, fn in stages:
            if only is not None and name not in only:
                continue
            fn()
            STAGE_LOG.append((name, dict(P.cnt)))
            if stop_after == name:
                break
        P.barrier()
        fin = [(k, v) for k, v, _ in P.dsems if v > 0]
        P.emit(final_waits=fin)
    return nc


def stage_mod(K):
    P = K.P
    with P.scope():
        cT = P.sb([128, KC, NB])
        P.dma(cT, K.cT)
        cond = P.sb([128, KC, NB])
        P.act(cond, cT, AF.Silu)
        wb = [P.sb([128, 8, 512]) for _ in range(2)]
        for l in range(2):
            ps = P.ps([128, 72, NB])
            for pc in range(18):
                wt = wb[pc % 2]
                P.dma(wt, K.ada_w[l, pc])
                for jj in range(4):
                    j = pc * 4 + jj
                    P.mm_group(ps[:, j, :], [(wt[:, kc, jj * 128:(jj + 1) * 128], cond[:, kc, :]) for kc in range(KC)])
            P.tt(K.modT[l], ps, K.vec("adab%d" % l).un(2).bc([128, 72, NB]), ALU.add)
            for s in range(3):
                m = K.modT[l]
                nw = K.vec("normw%d%d" % (l, s))
                P.ts(K.sc1[l][s], m[:, s * 24 + 8:s * 24 + 16, :], 1.0, ALU.add)
                P.tt(K.sc1[l][s], K.sc1[l][s], nw.un(2).bc([128, 8, NB]), ALU.mult)
                f = 1.0 if s == 1 else 0.5
                P.ts(K.gt[l][s], m[:, s * 24 + 16:s * 24 + 24, :], 1.0, ALU.add, f, ALU.mult)


def stage_copyx(K):
    P = K.P
    for tb in range(NTB):
        P.dma(K.xs.v(tb * TB, (tb + 1) * TB), K.xT_in[:, tb * TB:(tb + 1) * TB])


def norm_block(K, xb, hb, l, s, b, sq, ssp, sd, rstd, hn, plain_w=None, out_f32=None):
    P = K.P
    P.act(sq, xb, AF.Square)
    P.mm_group(ssp, [(K.ones, sq[:, dc, :]) for dc in range(8)])
    P.act(sd, ssp, AF.Sqrt, bias=K.epsb[:, 0:1], scale=1.0 / D)
    P.recip(rstd, sd)
    P.tt(hn, xb, rstd.un(1).bc([128, 8, TB]), ALU.mult)
    if plain_w is not None:
        P.tt(out_f32, hn, plain_w.un(2).bc([128, 8, TB]), ALU.mult)
        return
    sh = K.modT[l][:, s * 24:s * 24 + 8, :]
    for dc in range(8):
        P.ts(hb[:, dc, :], hn[:, dc, :], K.sc1[l][s][:, dc, b:b + 1], ALU.mult, sh[:, dc, b:b + 1], ALU.add)


def alloc_norm_tmps(K):
    P = K.P
    if not hasattr(K, "epsb"):
        pass
    sq = P.sb([128, 8, TB])
    sd = P.sb([128, TB])
    return dict(sq=sq, ssp=P.ps([128, TB]), sd=sd, rstd=sd, hn=sq)


def xview(dt_, tb):
    return dt_.v(tb * TB, (tb + 1) * TB).re("(c p) t -> p c t", p=128)


def stage_ffn(K, l, f):
    P = K.P
    s = 0 if f == 0 else 2
    with P.scope():
        wup = P.sb([128, 8, 2 * DFF], BF16)
        P.dma(wup.re("p k (a n) -> p k a n", a=4), K.ffn_up[l, f].re("p k (a n) -> p k a n", a=4), eng="pool")
        tm = alloc_norm_tmps(K)
        xbs = [P.sb([128, 8, TB]) for _ in range(2)]
        hbs = [P.sb([128, 8, TB], BF16)] * 2
        acts = [P.sb([128, NJ, TB], BF16) for _ in range(2)]
        sgs = [P.sb([128, TB]) for _ in range(2)]
        pas = [P.ps([128, TB]) for _ in range(2)]
        pbs = [P.ps([128, TB]) for _ in range(2)]
        for tb in range(NTB):
            b = tb // 4
            xb, hb, ab = xbs[tb % 2], hbs[tb % 2], acts[tb % 2]
            P.dma(xb, xview(K.xs, tb))
            norm_block(K, xb, hb, l, s, b, **tm)
            for j in range(NJ):
                pa, pb, sg = pas[j % 2], pbs[j % 2], sgs[j % 2]
                P.mm_group(pa, [(wup[:, kc, j * 128:(j + 1) * 128], hb[:, kc, :]) for kc in range(8)])
                P.mm_group(pb, [(wup[:, kc, DFF + j * 128:DFF + (j + 1) * 128], hb[:, kc, :]) for kc in range(8)])
                P.act(sg, pa, AF.Silu)
                P.tt(ab[:, j, :], sg, pb, ALU.mult)
            P.dma(xview(K.act, tb), ab)
    with P.scope():
        wdn = P.sb([128, NJ, D], BF16)
        P.dma(wdn, K.ffn_down[l, f], eng="pool")
        xbs = [P.sb([128, 8, TB]) for _ in range(2)]
        xos = [P.sb([128, 8, TB]) for _ in range(2)]
        abs_ = [P.sb([128, NJ, TB], BF16) for _ in range(2)]
        pos = [P.ps([128, TB]) for _ in range(2)]
        for tb in range(NTB):
            b = tb // 4
            xb, xo, ab = xbs[tb % 2], xos[tb % 2], abs_[tb % 2]
            P.dma(ab, xview(K.act, tb))
            P.dma(xb, xview(K.xs, tb))
            for m in range(8):
                po = pos[m % 2]
                P.mm_group(po, [(wdn[:, kc, m * 128:(m + 1) * 128], ab[:, kc, :]) for kc in range(NJ)])
                P.stt(xo[:, m, :], po, K.gt[l][s][:, m, b:b + 1], xb[:, m, :], ALU.mult, ALU.add)
            P.dma(xview(K.xs, tb), xo)


def stage_final(K):
    P = K.P
    with P.scope():
        tm = alloc_norm_tmps(K)
        xbs = [P.sb([128, 8, TB]) for _ in range(2)]
        obs = [P.sb([128, 8, TB]) for _ in range(2)]
        for tb in range(NTB):
            xb, ob = xbs[tb % 2], obs[tb % 2]
            P.dma(xb, xview(K.xs, tb))
            norm_block(K, xb, None, 0, 0, 0, plain_w=K.vec("finw"), out_f32=ob, **tm)
            P.dma(xview(K.outT, tb), ob)


def stage_normmix(K, l):
    P = K.P
    with P.scope():
        tm = alloc_norm_tmps(K)
        xbs = [P.sb([128, 8, TB]) for _ in range(2)]
        hbs = [P.sb([128, 8, TB], BF16) for _ in range(2)]
        for tb in range(NTB):
            xb, hb = xbs[tb % 2], hbs[tb % 2]
            P.dma(xb, xview(K.xs, tb))
            norm_block(K, xb, hb, l, 1, tb // 4, **tm)
            P.dma(xview(K.hT, tb), hb)
def load_w_cols(P, dst, src, c0, c1, eng="pool"):
    n = c1 - c0
    h = n // 2
    P.dma(dst[:, :, 0:h], src[:, :, c0:c0 + h], eng=eng)
    P.dma(dst[:, :, h:n], src[:, :, c0 + h:c1], eng=eng)


def tmview(dt_, t0, n):
    return dt_.v(t0, t0 + n)


def stage_in_ml(K, l):
    P = K.P
    with P.scope():
        wml = P.sb([128, 8, 2056], BF16)
        load_w_cols(P, wml, K.w_in[l], 0, 2056)
        wq = P.sb([128, 4, 2, 256], BF16)
        wk = P.sb([128, 4, 2, 256], BF16)
        P.dma(wq, K.wq[l], eng="pool")
        P.dma(wk, K.wk[l], eng="pool")
        cw = [K.vec("convw%d%d" % (l, tp)) for tp in range(4)]
        cb = K.vec("convb%d" % l)
        skip = K.vec("mlskip%d" % l)
        hbs = [P.sb([128, 8, TB], BF16) for _ in range(2)]
        xms = [P.sb([128, 8, TB + 3]) for _ in range(2)]
        acc = P.sb([128, 8, TB])
        xc = P.sb([128, 8, TB])
        xcb = P.sb([128, 8, TB], BF16)
        qst = P.sb([128, 8, TB])
        kst = P.sb([128, 8, TB])
        ktk = [P.sb([128, D]) for _ in range(2)]
        vos = [P.sb([128, 2056]) for _ in range(2)]
        psA = [P.ps([128, TB]) for _ in range(2)]
        psB = [P.ps([128, 2, 512]) for _ in range(2)]
        psC = P.ps([128, 8])
        for tb in range(NTB):
            hb, xm = hbs[tb % 2], xms[tb % 2]
            xmp = xms[(tb + 1) % 2]
            P.dma(hb, xview(K.hT, tb))
            if tb % 4 == 0:
                P.memset(xm[:, :, 0:3], 0.0)
            else:
                P.copy(xm[:, :, 0:3], xmp[:, :, TB:TB + 3])
            for dc in range(8):
                ps = psA[dc % 2]
                P.mm_group(ps, [(wml[:, kc, dc * 128:(dc + 1) * 128], hb[:, kc, :]) for kc in range(8)])
                P.copy(xm[:, dc, 3:TB + 3], ps, eng="act")
            for dc in range(8):
                P.ts(acc[:, dc, :], xm[:, dc, 0:TB], cw[0][:, dc:dc + 1], ALU.mult, cb[:, dc:dc + 1], ALU.add)
                for tp in range(1, 4):
                    P.stt(acc[:, dc, :], xm[:, dc, tp:tp + TB], cw[tp][:, dc:dc + 1], acc[:, dc, :], ALU.mult, ALU.add)
            P.act(xc, acc, AF.Silu)
            P.act(xcb, acc, AF.Silu)
            P.tt(acc, xc, skip.un(2).bc([128, 8, TB]), ALU.mult, eng="pool")
            P.dma(xview(K.ml_skx, tb), acc)
            for (w_, st_, dst) in ((wq, qst, K.ml_qT), (wk, kst, K.ml_kT)):
                for h in range(4):
                    for ec in range(2):
                        ps = psA[ec % 2]
                        P.mm_group(ps, [(w_[:, h, dck, ec * 128:(ec + 1) * 128], xcb[:, 2 * h + dck, :]) for dck in range(2)])
                        if ec == 0:
                            P.copy(st_[:, 2 * h + ec, :], ps, eng="act")
                        else:
                            P.copy(st_[:, 2 * h + ec, :], ps, eng="dve")
                P.dma(xview(dst, tb), st_)
            for tt_ in range(4):
                t0 = tb * TB + tt_ * 128
                tsl = slice(tt_ * 128, (tt_ + 1) * 128)
                kt = ktk[tt_ % 2]
                pb = psB[0]
                for h in range(4):
                    P.mm_group(pb[:, h // 2, (h % 2) * 256:(h % 2) * 256 + 256],
                               [(xcb[:, 2 * h + dck, tsl], wk[:, h, dck, :]) for dck in range(2)])
                P.copy(kt, pb.re("p a b -> p (a b)"), eng="act")
                P.dma(tmview(K.ml_ktok, t0, 128), kt)
                vo = vos[tt_ % 2]
                for half in range(2):
                    pb2 = psB[1] if half == 0 else psB[0]
                    for q in range(2):
                        c0 = half * 1024 + q * 512
                        P.mm_group(pb2[:, q, :], [(hb[:, kc, tsl], wml[:, kc, c0:c0 + 512]) for kc in range(8)])
                    if half == 0:
                        P.copy(vo[:, 0:1024], pb2.re("p a b -> p (a b)"), eng="dve")
                    else:
                        P.copy(vo[:, 1024:2048], pb2.re("p a b -> p (a b)"), eng="act")
                P.mm_group(psC, [(hb[:, kc, tsl], wml[:, kc, 2048:2056]) for kc in range(8)])
                P.copy(vo[:, 2048:2056], psC, eng="dve")
                P.dma(tmview(K.ml_vo, t0, 128), vo)


def stage_ml_rec(K, l):
    P = K.P
    NCH = T // LML
    with P.scope():
        ib = K.vec("ib%d" % l)
        fb = K.vec("fb%d" % l)
        mlnw = K.vec("mlnw%d" % l)
        gin = P.sb([128, NCH, 8])
        sp = P.sb([128, NCH, 4])
        li = P.sb([128, NCH, 4])
        et = P.sb([128, NCH, 4])
        wv = P.sb([128, NCH, 4])
        dec = P.sb([128, NCH, 4])
        wend = P.sb([128, NCH, 4])
        tmpg = P.sb([128, NCH, 4])
        C = P.sb([128, 4, 2, 257])
        qTs = [P.sb([128, 8, LML]) for _ in range(2)]
        kTs = [P.sb([128, 8, LML]) for _ in range(2)]
        kts = [P.sb([128, D]) for _ in range(2)]
        vxs = [P.sb([128, 4, 257]) for _ in range(2)]
        for vx in vxs:
            P.memset(vx[:, :, 256:257], 1.0)
        osb = [P.sb([128, D]) for _ in range(2)]
        sko = [P.sb([128, 8, LML]) for _ in range(2)]
        sgo = P.sb([128, D])
        sts = [P.sb([128, 128]) for _ in range(2)]
        hh = P.sb([128, 4, 256])
        cen = P.sb([128, 4, 256])
        sq = P.sb([128, 4, 256])
        sm = P.sb([128, 16])
        kw = P.sb([128, 4, 256])
        yst = [P.sb([128, 8, LML], BF16) for _ in range(2)]
        stp = [P.ps([128, 128]) for _ in range(2)]
        nps = P.ps([128, 4, 512])
        npsv = [P.sub(nps[:, h, 0:257]) for h in range(4)]
        psX = [P.ps([128, 512]) for _ in range(2)]
        for b in range(NB):
            T0 = b * T
            P.dma(gin, K.ml_vo.v(T0, T0 + T)[:, 2048:2056].re("(c t) g -> t c g", t=LML))
            P.tt(tmpg, gin[:, :, 4:8], fb.un(1).bc([128, NCH, 4]), ALU.add)
            P.act(tmpg, tmpg, AF.Exp, scale=-1.0)
            P.act(sp, tmpg, AF.Ln, bias=K.epsb[:, 2:3])
            P.tt(li, gin[:, :, 0:4], ib.un(1).bc([128, NCH, 4]), ALU.add)
            bps = psX[0][:, 0:NCH * 4]
            tps = psX[1][:, 0:NCH * 4]
            P.mm_group(bps, [(K.tri, sp.re("p c h -> p (c h)"))])
            P.mm_group(tps, [(K.ones, sp.re("p c h -> p (c h)"))])
            P.act(et.re("p c h -> p (c h)"), bps, AF.Exp, scale=-1.0)
            P.tt(tmpg.re("p c h -> p (c h)"), li.re("p c h -> p (c h)"), bps, ALU.add)
            P.act(wv, tmpg, AF.Exp)
            P.ts(wv, wv, 1.0 / 16.0, ALU.mult)
            P.act(dec.re("p c h -> p (c h)"), tps, AF.Exp, scale=-1.0)
            P.tt(wend, wv, dec, ALU.mult)
            P.memset(C, 0.0)
            for c in range(NCH):
                t0 = T0 + c * LML
                qT, kT, kt, vx, ob, sk = qTs[c % 2], kTs[c % 2], kts[c % 2], vxs[c % 2], osb[c % 2], sko[c % 2]
                P.dma(qT, K.ml_qT.v(t0, t0 + LML).re("(c p) t -> p c t", p=128))
                P.dma(kT, K.ml_kT.v(t0, t0 + LML).re("(c p) t -> p c t", p=128))
                P.dma(kt, tmview(K.ml_ktok, t0, LML))
                vo = K.ml_vo.v(t0, t0 + LML)
                P.dma(vx[:, :, 0:256], vo[:, 0:1024].re("t (h e) -> t h e", h=4))
                P.dma(ob, vo[:, 1024:2048])
                P.dma(sk, K.ml_skx.v(t0, t0 + LML).re("(c p) t -> p c t", p=128))
                P.act(sgo, ob, AF.Sigmoid)
                for h in range(4):
                    sp_ = stp[h % 2]
                    st = sts[h % 2]
                    P.mm_group(sp_, [(kT[:, 2 * h + dck, :], qT[:, 2 * h + dck, :]) for dck in range(2)])
                    P.stt(st, sp_, wv[:, c, h:h + 1], K.tri, ALU.mult, ALU.mult)
                    P.mm_group(npsv[h], [(st, vx[:, h, :]),
                                         (qT[:, 2 * h, :], C[:, h, 0, :]),
                                         (qT[:, 2 * h + 1, :], C[:, h, 1, :])])
                for h in range(4):
                    P.tt(sm[:, h:h + 1], npsv[h][:, 256:257], et[:, c, h:h + 1], ALU.mult)
                P.ts(sm[:, 4:8], sm[:, 0:4], -1.0, ALU.mult)
                P.tt(sm[:, 4:8], sm[:, 4:8], sm[:, 0:4], ALU.max)
                P.ts(sm[:, 4:8], sm[:, 4:8], 1.0, ALU.max)
                P.recip(sm[:, 8:12], sm[:, 4:8])
                P.tt(sm[:, 12:16], sm[:, 8:12], et[:, c, :], ALU.mult)
                for h in range(4):
                    P.stt(hh[:, h, :], npsv[h][:, 0:256], sm[:, 12 + h:13 + h], sgo[:, h * 256:(h + 1) * 256],
                          ALU.mult, ALU.mult)
                P.reduce(sm[:, 0:4], hh, ALU.add)
                P.ts(sm[:, 0:4], sm[:, 0:4], 1.0 / 256.0, ALU.mult)
                P.tt(cen, hh, sm[:, 0:4].un(2).bc([128, 4, 256]), ALU.subtract)
                P.tt(sq, cen, cen, ALU.mult, eng="pool")
                P.reduce(sm[:, 4:8], sq, ALU.add)
                P.act(sm[:, 8:12], sm[:, 4:8], AF.Sqrt, bias=K.epsb[:, 0:1], scale=1.0 / 256.0)
                P.recip(sm[:, 8:12], sm[:, 8:12])
                P.tt(cen, cen, sm[:, 8:12].un(2).bc([128, 4, 256]), ALU.mult)
                cenf = cen.re("p h e -> p (h e)")
                ys = yst[c % 2]
                for half in range(2):
                    px = psX[half]
                    for jj in range(4):
                        j = half * 4 + jj
                        P.transpose(px[:, jj * 128:(jj + 1) * 128], cenf[:, j * 128:(j + 1) * 128], K.ident)
                    for jj in range(4):
                        j = half * 4 + jj
                        P.stt(ys[:, j, :], px[:, jj * 128:(jj + 1) * 128], mlnw[:, j:j + 1], sk[:, j, :],
                              ALU.mult, ALU.add)
                P.dma(K.yT[0].v(t0, t0 + LML).re("(c p) t -> p c t", p=128), ys)
                for h in range(4):
                    P.ts(kw[:, h, :], kt[:, h * 256:(h + 1) * 256], wend[:, c, h:h + 1], ALU.mult, eng="pool")
                for h in range(4):
                    for dck in range(2):
                        px = psX[dck]
                        P.mm_group(px[:, 0:257], [(kw[:, h, dck * 128:(dck + 1) * 128], vx[:, h, :])])
                        P.stt(C[:, h, dck, :], C[:, h, dck, :], dec[:, c, h:h + 1], px[:, 0:257], ALU.mult, ALU.add)


def stage_in_hg(K, l):
    P = K.P
    with P.scope():
        whg = P.sb([128, 8, 4096], BF16)
        load_w_cols(P, whg, K.w_in[l], C_HG, C_HG + 4096)
        lb = P.sb([128, 8])
        oml = P.sb([128, 8])
        noml = P.sb([128, 8])
        if l == 0:
            P.memset(lb, 0.0)
        else:
            P.tt(lb, K.vec("hglb1"), K.vec("hglb0"), ALU.subtract)
            P.act(lb, lb, AF.Sigmoid)
        P.ts(oml, lb, -1.0, ALU.mult, 1.0, ALU.add)
        P.ts(noml, oml, -1.0, ALU.mult)
        hbs = [P.sb([128, 8, TB], BF16) for _ in range(2)]
        q = P.sb([128, 8, TB])
        sg = P.sb([128, 8, TB])
        kk = P.sb([128, 8, TB])
        bc_ = P.sb([128, 8, TB])
        eb = P.sb([128, 8, TB])
        enb = P.sb([128, 8, TB])
        dec = P.sb([128, 8, 8])
        kst = [P.sb([128, D])] * 2
        vst = [P.sb([128, D])] * 2
        gst = [P.sb([128, D])] * 2
        psA = [P.ps([128, TB]) for _ in range(2)]
        psF = [P.ps([128, TB]) for _ in range(2)]
        psT = [P.ps([128, 2, 512]) for _ in range(2)]
        for tb in range(NTB):
            hb = hbs[tb % 2]
            P.dma(hb, xview(K.hT, tb))
            for dc in range(8):
                pq, pf = psA[dc % 2], psF[dc % 2]
                P.mm_group(pq, [(whg[:, kc, dc * 128:(dc + 1) * 128], hb[:, kc, :]) for kc in range(8)])
                P.mm_group(pf, [(whg[:, kc, 1024 + dc * 128:1024 + (dc + 1) * 128], hb[:, kc, :]) for kc in range(8)])
                P.act(q[:, dc, :], pq, AF.Silu)
                P.act(sg[:, dc, :], pf, AF.Sigmoid)
                P.ts(kk[:, dc, :], sg[:, dc, :], noml[:, dc:dc + 1], ALU.mult, oml[:, dc:dc + 1], ALU.add, eng="pool")
                P.ts(sg[:, dc, :], sg[:, dc, :], oml[:, dc:dc + 1], ALU.mult, lb[:, dc:dc + 1], ALU.add)
            P.ts(sg, sg, 1e-30, ALU.max)
            P.act(sg, sg, AF.Ln)
            for dc in range(8):
                P.scan(bc_[:, dc, :], K.rmask, sg[:, dc, :], 0.0, ALU.mult, ALU.add)
            P.act(eb, bc_, AF.Exp)
            P.act(enb, bc_, AF.Exp, scale=-1.0)
            P.tt(q, q, eb, ALU.mult)
            P.tt(kk, kk, enb, ALU.mult, eng="pool")
            P.copy(dec, eb.re("p c (n l) -> p c n l", l=LHG)[:, :, :, LHG - 1])
            P.tt(enb.re("p c (n l) -> p c n l", l=LHG), kk.re("p c (n l) -> p c n l", l=LHG),
                 dec.un(3).bc([128, 8, 8, LHG]), ALU.mult)
            P.dma(xview(K.hg_qT, tb), q)
            P.dma(xview(K.hg_kT, tb), kk)
            P.dma(K.hg_decT.v(tb * 8, tb * 8 + 8).re("(c p) n -> p c n", p=128), dec)
            for tt_ in range(4):
                t0 = tb * TB + tt_ * 128
                tsl = slice(tt_ * 128, (tt_ + 1) * 128)
                ks, vs, gs = kst[tt_ % 2], vst[tt_ % 2], gst[tt_ % 2]
                px = psT[0]
                for dc in range(8):
                    P.transpose(px[:, dc // 4, (dc % 4) * 128:(dc % 4) * 128 + 128], enb[:, dc, tsl], K.ident)
                P.copy(ks, px.re("p a b -> p (a b)"), eng="act")
                P.dma(tmview(K.hg_khat, t0, 128), ks)
                pv = psT[1]
                for qq in range(2):
                    c0 = 2048 + qq * 512
                    P.mm_group(pv[:, qq, :], [(hb[:, kc, tsl], whg[:, kc, c0:c0 + 512]) for kc in range(8)])
                P.copy(vs, pv.re("p a b -> p (a b)"), eng="dve")
                P.dma(tmview(K.hg_vtok, t0, 128), vs)
                pg = psT[0]
                for qq in range(2):
                    c0 = 3072 + qq * 512
                    P.mm_group(pg[:, qq, :], [(hb[:, kc, tsl], whg[:, kc, c0:c0 + 512]) for kc in range(8)])
                P.act(gs, pg.re("p a b -> p (a b)"), AF.Sigmoid)
                P.dma(tmview(K.hg_gtok, t0, 128), gs)


def stage_hg_rec(K, l):
    P = K.P
    NCH = T // LHG
    CG = 4
    with P.scope():
        hgnw = K.vec("hgnw%d" % l)
        S = P.sb([128, 8, 128])
        decall = P.sb([128, 8, NCH])
        qts = [P.sb([128, 8, CG * LHG]) for _ in range(2)]
        kts = [P.sb([128, 8, CG * LHG]) for _ in range(2)]
        vts = [P.sb([64, CG, D]) for _ in range(2)]
        khs = [P.sb([64, CG, D]) for _ in range(2)]
        gts = [P.sb([64, CG, D]) for _ in range(2)]
        at = P.sb([64, 8, 64])
        sq = P.sb([64, 8, 128])
        on = P.sb([64, 8, 128])
        sm = P.sb([64, 16])
        yst = [P.sb([128, 8, CG * LHG], BF16) for _ in range(2)]
        atp = P.ps([64, 8, 64])
        ops = P.ps([64, 2, 512])
        tp = P.ps([128, 8, 64])
        sps = P.ps([128, 2, 512])
        tri64 = K.tri[0:64, 0:64]
        id64 = K.ident[0:64, 0:64]
        for b in range(NB):
            T0 = b * T
            P.memset(S, 0.0)
            P.dma(decall, K.hg_decT.v(b * NCH, (b + 1) * NCH).re("(c p) n -> p c n", p=128))
            for cg in range(NCH // CG):
                t0 = T0 + cg * CG * LHG
                t1 = t0 + CG * LHG
                qt, kt, vt, kh, gt, ys = qts[cg % 2], kts[cg % 2], vts[cg % 2], khs[cg % 2], gts[cg % 2], yst[cg % 2]
                P.dma(qt, K.hg_qT.v(t0, t1).re("(c p) t -> p c t", p=128))
                P.dma(kt, K.hg_kT.v(t0, t1).re("(c p) t -> p c t", p=128))
                P.dma(vt, K.hg_vtok.v(t0, t1).re("(c t) e -> t c e", t=LHG))
                P.dma(kh, K.hg_khat.v(t0, t1).re("(c t) e -> t c e", t=LHG))
                P.dma(gt, K.hg_gtok.v(t0, t1).re("(c t) e -> t c e", t=LHG))
                for ci in range(CG):
                    c = cg * CG + ci
                    cs = slice(ci * LHG, (ci + 1) * LHG)
                    for h in range(8):
                        P.mm_group(atp[:, h, :], [(kt[:, h, cs], qt[:, h, cs])])
                    P.tt(at, atp, tri64.un(1).bc([64, 8, 64]), ALU.mult)
                    for h in range(8):
                        hs = slice(h * 128, (h + 1) * 128)
                        P.mm_group(ops[:, h // 4, (h % 4) * 128:(h % 4) * 128 + 128],
                                   [(at[:, h, :], vt[:, ci, hs]), (qt[:, h, cs], S[:, h, :])])
                    opf = ops.re("p a (h e) -> p (a h) e", e=128)
                    P.act(sq, opf, AF.Square)
                    P.reduce(sm[:, 0:8], sq, ALU.add)
                    P.act(sm[:, 8:16], sm[:, 0:8], AF.Sqrt, bias=K.epsb[0:64, 0:1], scale=1.0 / 128.0)
                    P.recip(sm[:, 8:16], sm[:, 8:16])
                    P.tt(on, opf, sm[:, 8:16].un(2).bc([64, 8, 128]), ALU.mult)
                    P.tt(on, on, gt[:, ci, :].re("p (h e) -> p h e", e=128), ALU.mult, eng="pool")
                    for h in range(8):
                        P.transpose(tp[:, h, :], on[:, h, :], id64)
                    P.tt(ys[:, :, cs], tp, hgnw.un(2).bc([128, 8, 64]), ALU.mult)
                    for h in range(8):
                        hs = slice(h * 128, (h + 1) * 128)
                        P.mm_group(sps[:, h // 4, (h % 4) * 128:(h % 4) * 128 + 128], [(kh[:, ci, hs], vt[:, ci, hs])])
                    P.tt(S, S, decall[:, :, c:c + 1].bc([128, 8, 128]), ALU.mult)
                    P.tt(S, S, sps.re("p a (h e) -> p (a h) e", e=128), ALU.add)
                P.dma(K.yT[2].v(t0, t1).re("(c p) t -> p c t", p=128), ys)


def stage_in_rw(K, l):
    P = K.P
    NZ = 3328
    with P.scope():
        wrw = P.sb([128, 8, NZ], BF16)
        load_w_cols(P, wrw, K.w_in[l], C_RW, C_RW + NZ)
        wa2 = P.sb([128, D])
        g2 = P.sb([128, D])
        P.dma(wa2, K.rw_wa2[l])
        P.dma(g2, K.rw_g2[l])
        if l == 1:
            wvr = P.sb([128, 8, 32], BF16)
            P.dma(wvr, K.w_vres, eng="pool")
            v2 = P.sb([32, D])
            P.dma(v2, K.rw_v2)
            v0 = K.vec("rw_v0")
            muv = K.vec("muvres")
        mu = K.vec("rwmu%d" % l)
        w0, a0 = K.vec("rw_w0%d" % l), K.vec("rw_a0%d" % l)
        k_k, k_a, r_k = K.vec("rw_k_k%d" % l), K.vec("rw_k_a%d" % l), K.vec("rw_r_k%d" % l)
        omka = P.sb([128, 8])
        P.ts(omka, k_a, -1.0, ALU.mult, 1.0, ALU.add)
        prev = P.sb([128, 28])
        hbs = [P.sb([128, 8, TB], BF16) for _ in range(2)]
        zl = P.sb([128, 3, TB + 1])
        dl = P.sb([128, 3, TB])
        twa = P.sb([128, TB])
        sgl = P.sb([128, TB])
        vl = P.sb([32, TB])
        z3 = P.sb([128, 3, TB + 1])
        d3 = P.sb([128, 3, TB])
        sh3 = P.sb([128, 3, TB])
        W_ = [P.sb([128, TB]) for _ in range(14)]
        gam = P.sb([128, 8])
        vfb = P.sb([128, TB])
        stg = [P.sb([128, 4, D]) for _ in range(3)]
        psA = [P.ps([128, TB]) for _ in range(2)]
        psL = [P.ps([128, TB]) for _ in range(3)]
        psT = [P.ps([128, TB]) for _ in range(3)]
        for tb in range(NTB):
            hb = hbs[tb % 2]
            first = (tb % 4 == 0)
            P.dma(hb, xview(K.hT, tb))
            if first:
                P.memset(prev, 0.0)
            nlo = 3 if l == 1 else 2
            for i in range(nlo):
                ps = psA[i % 2]
                if i < 2:
                    P.mm_group(ps, [(wrw[:, kc, (24 + i) * 128:(25 + i) * 128], hb[:, kc, :]) for kc in range(8)])
                    P.copy(zl[:, i, 1:TB + 1], ps, eng="act")
                else:
                    P.mm_group(ps[0:32, :], [(wvr[:, kc, :], hb[:, kc, :]) for kc in range(8)])
                    P.copy(zl[0:32, i, 1:TB + 1], ps[0:32, :], eng="act")
                P.copy(zl[:, i, 0:1], prev[:, 24 + i:25 + i])
                P.copy(prev[:, 24 + i:25 + i], zl[:, i, TB:TB + 1])
            P.tt(dl[:, 0:nlo, :], zl[:, 0:nlo, 0:TB], zl[:, 0:nlo, 1:TB + 1], ALU.subtract)
            P.stt(dl[:, 0, :], dl[:, 0, :], mu[:, 24:25], zl[:, 0, 1:TB + 1], ALU.mult, ALU.add)
            P.stt(dl[:, 1, :], dl[:, 1, :], mu[:, 25:26], zl[:, 1, 1:TB + 1], ALU.mult, ALU.add)
            P.act(twa[0:64, :], dl[0:64, 0, :], AF.Tanh)
            P.copy(twa[64:128, :], dl[64:128, 0, :], eng="act")
            P.act(sgl, dl[:, 1, :], AF.Sigmoid)
            if l == 1:
                P.stt(vl, dl[0:32, 2, :], muv[0:32, 0:1], zl[0:32, 2, 1:TB + 1], ALU.mult, ALU.add)
            for j in range(8):
                js = slice(j * 128, (j + 1) * 128)
                (kk, sq, t1, kmod, bb, rk, bon, sgw, a_, cum, G, Gi, Gp, gg) = W_
                for i in range(3):
                    ch = i * 8 + j
                    ps = psA[i % 2]
                    P.mm_group(ps, [(wrw[:, kc, ch * 128:(ch + 1) * 128], hb[:, kc, :]) for kc in range(8)])
                    P.copy(z3[:, i, 1:TB + 1], ps, eng="act")
                    P.copy(z3[:, i, 0:1], prev[:, ch:ch + 1])
                    P.copy(prev[:, ch:ch + 1], z3[:, i, TB:TB + 1])
                P.tt(d3, z3[:, :, 0:TB], z3[:, :, 1:TB + 1], ALU.subtract)
                for i in range(3):
                    ch = i * 8 + j
                    P.stt(sh3[:, i, :], d3[:, i, :], mu[:, ch:ch + 1], z3[:, i, 1:TB + 1], ALU.mult, ALU.add)
                r, k, v = sh3[:, 0, :], sh3[:, 1, :], sh3[:, 2, :]
                pw, pa, pg = psL
                P.mm_group(pw, [(wa2[0:64, js], twa[0:64, :])])
                P.mm_group(pa, [(wa2[64:128, js], twa[64:128, :])])
                P.mm_group(pg, [(g2[:, js], sgl)])
                P.act(sgw, pw, AF.Sigmoid, bias=w0[:, j:j + 1])
                P.act(a_, pa, AF.Sigmoid, bias=a0[:, j:j + 1])
                P.copy(gg, pg, eng="act")
                P.dma(K.rw_gT.v(tb * TB, (tb + 1) * TB)[js, :], gg)
                if l == 1:
                    pv = psT[0]
                    P.mm_group(pv, [(v2[0:32, js], vl[0:32, :])])
                    P.act(t1, pv, AF.Sigmoid, bias=v0[:, j:j + 1])
                    P.dma(vfb, K.rw_vfT.v(tb * TB, (tb + 1) * TB)[js, :])
                    P.tt(vfb, vfb, v, ALU.subtract)
                    P.tt(vfb, vfb, t1, ALU.mult)
                    P.tt(v, v, vfb, ALU.add)
                else:
                    P.dma(K.rw_vfT.v(tb * TB, (tb + 1) * TB)[js, :], v)
                P.ts(kk, k, k_k[:, j:j + 1], ALU.mult, eng="pool")
                P.act(sq, kk, AF.Square)
                pn = psT[1]
                P.mm_group(pn, [(K.blk64, sq)])
                P.act(sq, pn, AF.Sqrt)
                P.ts(sq, sq, 1e-12, ALU.max)
                P.recip(sq, sq)
                P.tt(kk, kk, sq, ALU.mult)
                P.ts(t1, a_, k_a[:, j:j + 1], ALU.mult, omka[:, j:j + 1], ALU.add)
                P.tt(kmod, k, t1, ALU.mult, eng="pool")
                P.tt(bb, kk, a_, ALU.mult)
                P.stt(rk, r, r_k[:, j:j + 1], kmod, ALU.mult, ALU.mult)
                pb_ = psT[2]
                P.mm_group(pb_, [(K.blk64, rk)])
                P.tt(bon, pb_, v, ALU.mult)
                P.dma(K.rw_bonT.v(tb * TB, (tb + 1) * TB)[js, :], bon)
                P.scan(cum, K.rmask, sgw, 0.0, ALU.mult, ALU.add)
                P.act(G, cum, AF.Exp, scale=-WSC)
                P.act(Gi, cum, AF.Exp, scale=WSC)
                P.tt(t1, cum, sgw, ALU.subtract, eng="pool")
                P.act(Gp, t1, AF.Exp, scale=-WSC)
                P.tt(kk, kk, Gp, ALU.mult)
                P.tt(rk, r, G, ALU.mult, eng="pool")
                P.tt(bb, bb, Gi, ALU.mult)
                P.tt(kmod, kmod, Gi, ALU.mult, eng="pool")
                P.copy(gam, G.re("p (n l) -> p n l", l=LRW)[:, :, LRW - 1])
                gb = gam.un(2).bc([128, 8, LRW])
                P.tt(t1.re("p (n l) -> p n l", l=LRW), bb.re("p (n l) -> p n l", l=LRW), gb, ALU.mult)
                P.tt(sq.re("p (n l) -> p n l", l=LRW), kmod.re("p (n l) -> p n l", l=LRW), gb, ALU.mult,
                     eng="pool")
                for (src, dst) in ((kk, K.rw_kapT), (rk, K.rw_rhoT), (bb, K.rw_betT), (kmod, K.rw_ktlT)):
                    P.dma(dst.v(tb * TB, (tb + 1) * TB)[js, :], src)
                P.dma(K.rw_gamT.v(tb * 8, tb * 8 + 8)[js, :], gam)
                for xi, X in enumerate((v, t1, sq)):
                    px = psT[xi]
                    for tt_ in range(4):
                        P.transpose(px[:, tt_ * 128:(tt_ + 1) * 128], X[:, tt_ * 128:(tt_ + 1) * 128], K.ident)
                    if xi == 1:
                        P.copy(stg[xi][:, :, js], px.re("p (a b) -> p a b", a=4), eng="act")
                    else:
                        P.copy(stg[xi][:, :, js], px.re("p (a b) -> p a b", a=4))
            for xi, dst in enumerate((K.rw_vtok, K.rw_bhat, K.rw_khat)):
                P.dma(dst.v(tb * TB, (tb + 1) * TB).re("(a t) e -> t a e", t=128), stg[xi])


def stage_rw_rec_old(K, l, post):
    P = K.P
    NCH = T // LRW
    CB = 8
    L = LRW
    with P.scope():
        mask12 = P.sb([64, 128])
        P.copy(mask12[:, 0:64], K.tris[0:64, 0:64])
        P.copy(mask12[:, 64:128], K.tri[0:64, 0:64])
        lows = K.lows[0:64, 0:64]
        id64 = K.ident[0:64, 0:64]
        KRs = [P.sb([64, 4, CB, 2, L]) for _ in range(2)]
        BTs = [P.sb([64, 4, CB * L]) for _ in range(2)]
        KTs = [P.sb([64, 4, CB * L]) for _ in range(2)]
        gms = [P.sb([64, 4, CB]) for _ in range(2)]
        vts = [P.sb([64, CB, 256]) for _ in range(2)]
        bhs = [P.sb([64, CB, 256]) for _ in range(2)]
        khs = [P.sb([64, CB, 256]) for _ in range(2)]
        ysts = [P.sb([64, CB, 256]) for _ in range(2)]
        S = P.sb([64, 4, L])
        M1 = P.sb([64, 4, 128])
        M2 = P.sb([64, 4, 128])
        Pm = [P.sb([64, 4, L]) for _ in range(2)]
        Qm = [P.sb([64, 4, L]) for _ in range(2)]
        TT = P.sb([64, 4, L])
        Rn = P.sb([64, 4, L])
        U = P.sb([64, 4, L])
        A1 = P.ps([64, 4, 128])
        A2 = P.ps([64, 4, 128])
        A3 = P.ps([64, 4, L])
        PP = P.ps([64, 4, L])
        QQ = P.ps([64, 4, L])
        TP = P.ps([64, 4, L])
        RU = P.ps([64, 2, 4, L])
        YS = P.ps([64, 2, 4, L])
        RP, UP = RU[:, 0], RU[:, 1]
        YP, SP = YS[:, 0], YS[:, 1]
        it = 0
        import os as _os
        lim = [2, 4, 4]
        VAR = _os.environ.get("RW_VAR", "")
        gmall = P.sb([64, 4, NCH])
        for b in range(lim[0]):
            T0 = b * T
            for hg in range(lim[1]):
                P.memset(S, 0.0)
                for cb in range(lim[2]):
                    t0 = T0 + cb * CB * L
                    t1 = t0 + CB * L
                    KR, BT, KT, gm = KRs[it % 2], BTs[it % 2], KTs[it % 2], gms[it % 2]
                    vt, bh, kh, yst = vts[it % 2], bhs[it % 2], khs[it % 2], ysts[it % 2]
                    it += 1
                    for hh in range(4):
                        ch = slice(hg * 256 + hh * 64, hg * 256 + hh * 64 + 64)
                        P.dma(KR[:, hh, :, 0, :], K.rw_kapT.v(t0, t1)[ch, :].re("p (c l) -> p c l", l=L))
                        P.dma(KR[:, hh, :, 1, :], K.rw_rhoT.v(t0, t1)[ch, :].re("p (c l) -> p c l", l=L))
                        P.dma(BT[:, hh, :], K.rw_betT.v(t0, t1)[ch, :])
                        P.dma(KT[:, hh, :], K.rw_ktlT.v(t0, t1)[ch, :])
                        g0 = b * NCH + cb * CB
                        P.dma(gm[:, hh, :], K.rw_gamT.v(g0, g0 + CB)[ch, :])
                        if "gmall" in VAR and cb == 0:
                            P.dma(gmall[:, hh, :], K.rw_gamT.v(b * NCH, (b + 1) * NCH)[ch, :])
                    es = slice(hg * 256, hg * 256 + 256)
                    P.dma(vt, K.rw_vtok.v(t0, t1)[:, es].re("(c t) e -> t c e", t=L))
                    P.dma(bh, K.rw_bhat.v(t0, t1)[:, es].re("(c t) e -> t c e", t=L))
                    P.dma(kh, K.rw_khat.v(t0, t1)[:, es].re("(c t) e -> t c e", t=L))
                    for ci in range(CB):
                        cs = slice(ci * L, (ci + 1) * L)
                        for hh in range(4):
                            krr = KR[:, hh, ci, :, :].re("p a l -> p (a l)")
                            P.mm_group(A1[:, hh, :], [(BT[:, hh, cs], krr)])
                            P.mm_group(A2[:, hh, :], [(KT[:, hh, cs], krr)])
                            P.mm_group(A3[:, hh, :], [(KR[:, hh, ci, 0, :], BT[:, hh, cs])])
                        P.tt(M1, A1, mask12.un(1).bc([64, 4, 128]), ALU.mult)
                        P.tt(M2, A2, mask12.un(1).bc([64, 4, 128]), ALU.mult)
                        P.tt(Pm[0], A3, lows.un(1).bc([64, 4, L]), ALU.mult)
                        Pc = Pm[0]
                        Qc = M1[:, :, 0:L]
                        ArbT = M1[:, :, L:2 * L]
                        AkkT = M2[:, :, 0:L]
                        ArkT = M2[:, :, L:2 * L]
                        P.tt(TT, id64.un(1).bc([64, 4, L]), Qc, ALU.subtract)
                        for lev in range(1, 6):
                            Pn = Pm[lev % 2]
                            Qn = Qm[lev % 2]
                            for hh in range(4):
                                P.mm_group(PP[:, hh, :], [(Qc[:, hh, :], Pc[:, hh, :])])
                            if lev < 5:
                                for hh in range(4):
                                    P.mm_group(QQ[:, hh, :], [(Pc[:, hh, :], Qc[:, hh, :])])
                            P.copy(Pn, PP)
                            if lev < 5:
                                P.copy(Qn, QQ, eng="act")
                            for hh in range(4):
                                P.mm_group(TP[:, hh, :], [(Pn[:, hh, :], TT[:, hh, :])])
                            P.tt(TT, TT, TP, ALU.add)
                            Pc, Qc = Pn, Qn
                        for hh in range(4):
                            vs = slice(hh * 64, hh * 64 + 64)
                            P.mm_group(RP[:, hh, :], [(KR[:, hh, ci, 0, :], S[:, hh, :]), (AkkT[:, hh, :], vt[:, ci, vs])])
                        P.ts(Rn, RP, -1.0, ALU.mult)
                        for hh in range(4):
                            P.mm_group(UP[:, hh, :], [(TT[:, hh, :], Rn[:, hh, :])])
                        P.copy(U, UP)
                        for hh in range(4):
                            vs = slice(hh * 64, hh * 64 + 64)
                            P.mm_group(YP[:, hh, :], [(KR[:, hh, ci, 1, :], S[:, hh, :]), (ArbT[:, hh, :], U[:, hh, :]),
                                                      (ArkT[:, hh, :], vt[:, ci, vs])])
                        if "reorder" not in VAR:
                            P.copy(yst[:, ci, :].re("p (h e) -> p h e", h=4), YP, eng="act")
                        for hh in range(4):
                            vs = slice(hh * 64, hh * 64 + 64)
                            P.mm_group(SP[:, hh, :], [(bh[:, ci, vs], U[:, hh, :]), (kh[:, ci, vs], vt[:, ci, vs])])
                        if "reorder" in VAR:
                            P.copy(yst[:, ci, :].re("p (h e) -> p h e", h=4), YP, eng="act")
                        if "gmall" in VAR:
                            cc = cb * CB + ci
                            P.tt(S, S, gmall[:, :, cc:cc + 1].bc([64, 4, L]), ALU.mult)
                        else:
                            P.tt(S, S, gm[:, :, ci:ci + 1].bc([64, 4, L]), ALU.mult)
                        P.tt(S, S, SP, ALU.add)
                    P.dma(K.rw_ytok.v(t0, t1)[:, es].re("(c t) e -> t c e", t=L), yst)


def lockstep(gens):
    gens = list(gens)
    while gens:
        for g in list(gens):
            try:
                next(g)
            except StopIteration:
                gens.remove(g)


def stage_rw_rec(K, l):
    P = K.P
    import os as _os
    if _os.environ.get("RW_OLD"):
        stage_rw_rec_old(K, l, None)
        return
    NCH = T // LRW
    CB = int(_os.environ.get("RW_CB", "4"))
    NBUF = int(_os.environ.get("RW_NBUF", "2"))
    L = LRW
    with P.scope():
        mask12 = P.sb([64, 128])
        P.copy(mask12[:, 0:64], K.tris[0:64, 0:64])
        P.copy(mask12[:, 64:128], K.tri[0:64, 0:64])
        lows = K.lows[0:64, 0:64]
        id64 = K.ident[0:64, 0:64]

        def stream(b):
            KRs = [P.sb([64, 4, CB, 2, L]) for _ in range(NBUF)]
            BTs = [P.sb([64, 4, CB * L]) for _ in range(NBUF)]
            KTs = [P.sb([64, 4, CB * L]) for _ in range(NBUF)]
            gms = [None, None]
            gmall = P.sb([64, 4, NCH])
            vts = [P.sb([64, CB, 256]) for _ in range(NBUF)]
            bhs = [P.sb([64, CB, 256]) for _ in range(NBUF)]
            khs = [P.sb([64, CB, 256]) for _ in range(NBUF)]
            ysts = [P.sb([64, CB, 256]) for _ in range(NBUF)]
            S = P.sb([64, 4, L])
            M1 = P.sb([64, 4, 128])
            M2 = P.sb([64, 4, 128])
            Pm = [P.sb([64, 4, L]) for _ in range(2)]
            Qm = [P.sb([64, 4, L]) for _ in range(2)]
            TT = P.sb([64, 4, L])
            Rn = P.sb([64, 4, L])
            U = P.sb([64, 4, L])
            import os as _os
            if _os.environ.get("RW_PS") == "old":
                if not hasattr(K, "_rwps"):
                    K._rwps = dict(A1=P.ps([64, 4, 128]), A2=P.ps([64, 4, 128]), A3=P.ps([64, 4, L]), PP=P.ps([64, 4, L]),
                                   QQ=P.ps([64, 4, L]), TP=P.ps([64, 4, L]), RU=P.ps([64, 2, 4, L]), YS=P.ps([64, 2, 4, L]))
                d_ = K._rwps
                A1, A2, A3, PP, QQ, TP = d_["A1"], d_["A2"], d_["A3"], d_["PP"], d_["QQ"], d_["TP"]
                RP, UP = d_["RU"][:, 0], d_["RU"][:, 1]
                YP, SP = d_["YS"][:, 0], d_["YS"][:, 1]
            else:
                B0 = P.ps([64, 4, 128])
                B1 = P.ps([64, 4, 128])
                B2 = P.ps([64, 2, 4, L])
                B3 = P.ps([64, 2, 4, L])
                A1, A2 = B0, B1
                A3, PP = B2[:, 0], B2[:, 1]
                QQ, TP = B3[:, 0], B3[:, 1]
                RP, UP = B0[:, :, 0:L], B0[:, :, L:2 * L]
                YP, SP = B1[:, :, 0:L], B1[:, :, L:2 * L]
            it = 0
            yield
            T0 = b * T
            for hg in range(4):
                P.memset(S, 0.0)
                for cb in range(NCH // CB):
                    t0 = T0 + cb * CB * L
                    t1 = t0 + CB * L
                    KR, BT, KT = KRs[it % NBUF], BTs[it % NBUF], KTs[it % NBUF]
                    vt, bh, kh, yst = vts[it % NBUF], bhs[it % NBUF], khs[it % NBUF], ysts[it % NBUF]
                    it += 1
                    for hh in range(4):
                        ch = slice(hg * 256 + hh * 64, hg * 256 + hh * 64 + 64)
                        P.dma(KR[:, hh, :, 0, :], K.rw_kapT.v(t0, t1)[ch, :].re("p (c l) -> p c l", l=L))
                        P.dma(KR[:, hh, :, 1, :], K.rw_rhoT.v(t0, t1)[ch, :].re("p (c l) -> p c l", l=L))
                        P.dma(BT[:, hh, :], K.rw_betT.v(t0, t1)[ch, :])
                        P.dma(KT[:, hh, :], K.rw_ktlT.v(t0, t1)[ch, :])
                        if cb == 0:
                            P.dma(gmall[:, hh, :], K.rw_gamT.v(b * NCH, (b + 1) * NCH)[ch, :])
                    es = slice(hg * 256, hg * 256 + 256)
                    P.dma(vt, K.rw_vtok.v(t0, t1)[:, es].re("(c t) e -> t c e", t=L))
                    P.dma(bh, K.rw_bhat.v(t0, t1)[:, es].re("(c t) e -> t c e", t=L))
                    P.dma(kh, K.rw_khat.v(t0, t1)[:, es].re("(c t) e -> t c e", t=L))
                    yield
                    for ci in range(CB):
                        cs = slice(ci * L, (ci + 1) * L)
                        for hh in range(4):
                            krr = KR[:, hh, ci, :, :].re("p a l -> p (a l)")
                            P.mm_group(A1[:, hh, :], [(BT[:, hh, cs], krr)])
                            P.mm_group(A2[:, hh, :], [(KT[:, hh, cs], krr)])
                            P.mm_group(A3[:, hh, :], [(KR[:, hh, ci, 0, :], BT[:, hh, cs])])
                        yield
                        P.tt(M1, A1, mask12.un(1).bc([64, 4, 128]), ALU.mult)
                        P.tt(Pm[0], A3, lows.un(1).bc([64, 4, L]), ALU.mult)
                        P.tt(M2, A2, mask12.un(1).bc([64, 4, 128]), ALU.mult, eng="pool") if False else P.tt(M2, A2, mask12.un(1).bc([64, 4, 128]), ALU.mult)
                        Pc = Pm[0]
                        Qc = M1[:, :, 0:L]
                        ArbT = M1[:, :, L:2 * L]
                        AkkT = M2[:, :, 0:L]
                        ArkT = M2[:, :, L:2 * L]
                        P.tt(TT, id64.un(1).bc([64, 4, L]), Qc, ALU.subtract)
                        yield
                        for lev in range(1, 6):
                            Pn = Pm[lev % 2]
                            Qn = Qm[lev % 2]
                            for hh in range(4):
                                P.mm_group(PP[:, hh, :], [(Qc[:, hh, :], Pc[:, hh, :])])
                            if lev < 5:
                                for hh in range(4):
                                    P.mm_group(QQ[:, hh, :], [(Pc[:, hh, :], Qc[:, hh, :])])
                            yield
                            P.copy(Pn, PP)
                            if lev < 5:
                                P.copy(Qn, QQ, eng="act")
                            yield
                            for hh in range(4):
                                P.mm_group(TP[:, hh, :], [(Pn[:, hh, :], TT[:, hh, :])])
                            yield
                            P.tt(TT, TT, TP, ALU.add)
                            yield
                            Pc, Qc = Pn, Qn
                        for hh in range(4):
                            vs = slice(hh * 64, hh * 64 + 64)
                            P.mm_group(RP[:, hh, :], [(KR[:, hh, ci, 0, :], S[:, hh, :]), (AkkT[:, hh, :], vt[:, ci, vs])])
                        yield
                        P.ts(Rn, RP, -1.0, ALU.mult)
                        yield
                        for hh in range(4):
                            P.mm_group(UP[:, hh, :], [(TT[:, hh, :], Rn[:, hh, :])])
                        yield
                        P.copy(U, UP)
                        yield
                        for hh in range(4):
                            vs = slice(hh * 64, hh * 64 + 64)
                            P.mm_group(YP[:, hh, :], [(KR[:, hh, ci, 1, :], S[:, hh, :]), (ArbT[:, hh, :], U[:, hh, :]),
                                                      (ArkT[:, hh, :], vt[:, ci, vs])])
                        yield
                        P.copy(yst[:, ci, :].re("p (h e) -> p h e", h=4), YP, eng="act")
                        yield
                        for hh in range(4):
                            vs = slice(hh * 64, hh * 64 + 64)
                            P.mm_group(SP[:, hh, :], [(bh[:, ci, vs], U[:, hh, :]), (kh[:, ci, vs], vt[:, ci, vs])])
                        yield
                        cc = cb * CB + ci
                        P.tt(S, S, gmall[:, :, cc:cc + 1].bc([64, 4, L]), ALU.mult)
                        P.tt(S, S, SP, ALU.add)
                        yield
                    P.dma(K.rw_ytok.v(t0, t1)[:, es].re("(c t) e -> t c e", t=L), yst)
        import os as _os
        if _os.environ.get("RW_SEQ"):
            for b in range(NB):
                lockstep([stream(b)])
        else:
            lockstep([stream(b) for b in range(NB)])
    with P.scope():
        lnw, lnb = K.vec("rw_ln_w%d" % l), K.vec("rw_ln_b%d" % l)
        ys = [P.sb([128, 16, 64]) for _ in range(2)]
        cen = P.sb([128, 16, 64])
        sq = P.sb([128, 16, 64])
        sm = P.sb([128, 48])
        bons = [P.sb([128, 8, 128]) for _ in range(2)]
        ggs = [P.sb([128, 8, 128]) for _ in range(2)]
        tmp = P.sb([128, 8, 128])
        yo = [P.sb([128, 8, 128], BF16) for _ in range(2)]
        px = [P.ps([128, 4, 128]) for _ in range(2)]
        for tt_ in range(TOK // 128):
            t0 = tt_ * 128
            y, bon, gg, o = ys[tt_ % 2], bons[tt_ % 2], ggs[tt_ % 2], yo[tt_ % 2]
            P.dma(y, K.rw_ytok.v(t0, t0 + 128).re("t (h e) -> t h e", e=64))
            P.dma(bon, K.rw_bonT.v(t0, t0 + 128).re("(c p) t -> p c t", p=128))
            P.dma(gg, K.rw_gT.v(t0, t0 + 128).re("(c p) t -> p c t", p=128))
            P.reduce(sm[:, 0:16], y, ALU.add)
            P.ts(sm[:, 0:16], sm[:, 0:16], 1.0 / 64.0, ALU.mult)
            P.tt(cen, y, sm[:, 0:16].un(2).bc([128, 16, 64]), ALU.subtract)
            P.tt(sq, cen, cen, ALU.mult, eng="pool")
            P.reduce(sm[:, 16:32], sq, ALU.add)
            P.act(sm[:, 32:48], sm[:, 16:32], AF.Sqrt, bias=K.epsb[:, 1:2], scale=1.0 / 64.0)
            P.recip(sm[:, 32:48], sm[:, 32:48])
            P.tt(cen, cen, sm[:, 32:48].un(2).bc([128, 16, 64]), ALU.mult)
            cf = cen.re("p h e -> p (h e)")
            for j in range(8):
                P.transpose(px[j // 4][:, j % 4, :], cf[:, j * 128:(j + 1) * 128], K.ident)
            for j in range(8):
                P.ts(tmp[:, j, :], px[j // 4][:, j % 4, :], lnw[:, j:j + 1], ALU.mult, lnb[:, j:j + 1], ALU.add)
            P.tt(tmp, tmp, bon, ALU.add, eng="pool")
            P.tt(o, tmp, gg, ALU.mult)
            P.dma(K.yT[1].v(t0, t0 + 128).re("(c p) t -> p c t", p=128), o)


def stage_mix(K, l):
    P = K.P
    with P.scope():
        wgt = P.sb([128, 8, 3072], BF16)
        load_w_cols(P, wgt, K.w_in[l], C_GATE, C_GATE + 3072)
        wbr = P.sb([128, 3, 8, D], BF16)
        P.dma(wbr, K.w_branch[l], eng="pool")
        wo = P.sb([128, 8, D], BF16)
        P.dma(wo, K.w_out[l], eng="pool")
        hbs = [P.sb([128, 8, TB], BF16)] * 2
        yb = [P.sb([128, 8, TB], BF16) for _ in range(3)]
        xb = P.sb([128, 8, TB])
        xo = P.sb([128, 8, TB])
        mixb = P.sb([128, 8, TB], BF16)
        gts = [P.sb([128, TB]) for _ in range(2)]
        tmp = P.sb([128, TB])
        acc = P.sb([128, TB])
        psG = [P.ps([128, TB]) for _ in range(2)]
        psY = [P.ps([128, TB]) for _ in range(2)]
        psO = [P.ps([128, TB]) for _ in range(2)]
        for tb in range(NTB):
            b = tb // 4
            hb = hbs[tb % 2]
            P.dma(hb, xview(K.hT, tb))
            for br in range(3):
                P.dma(yb[br], xview(K.yT[br], tb))
            P.dma(xb, xview(K.xs, tb))
            i = 0
            for n in range(8):
                ns = slice(n * 128, (n + 1) * 128)
                for br in range(3):
                    pg, py, g = psG[i % 2], psY[i % 2], gts[i % 2]
                    i += 1
                    P.mm_group(pg, [(wgt[:, kc, br * 1024 + n * 128:br * 1024 + (n + 1) * 128], hb[:, kc, :]) for kc in range(8)])
                    P.mm_group(py, [(wbr[:, br, kc, ns], yb[br][:, kc, :]) for kc in range(8)])
                    P.act(g, pg, AF.Sigmoid)
                    if br == 0:
                        P.tt(acc, g, py, ALU.mult)
                    elif br == 1:
                        P.tt(tmp, g, py, ALU.mult)
                        P.tt(acc, acc, tmp, ALU.add, eng="pool")
                    else:
                        P.tt(tmp, g, py, ALU.mult)
                        P.tt(mixb[:, n, :], acc, tmp, ALU.add, eng="pool")
            for m in range(8):
                po = psO[m % 2]
                P.mm_group(po, [(wo[:, kc, m * 128:(m + 1) * 128], mixb[:, kc, :]) for kc in range(8)])
                P.stt(xo[:, m, :], po, K.gt[l][1][:, m, b:b + 1], xb[:, m, :], ALU.mult, ALU.add)
            P.dma(xview(K.xs, tb), xo)
def prep_core_inputs(inp, vecs, W, core):
    b0 = core * NB
    x = np.asarray(inp["x"], np.float32)[b0:b0 + NB].reshape(TOK, D)
    c = np.asarray(inp["c"], np.float32)[b0:b0 + NB]
    m = dict(W)
    m["xT"] = np.ascontiguousarray(x.T)
    m["cT"] = np.ascontiguousarray(c.reshape(NB, KC, 128).transpose(2, 1, 0))
    m["vecs"] = vecs
    return m


_CACHE = {}


def kernel(**inputs):
    from concourse.bass_utils import run_bass_kernel_spmd
    vp = pack_vecs(inputs)
    vecs = vp.build()
    W = host_weights(inputs)
    key = "main"
    if key not in _CACHE:
        _CACHE[key] = build_program(vp.off, vp.n)
    nc = _CACHE[key]
    in_maps = [prep_core_inputs(inputs, vecs, W, c) for c in range(8)]
    res = run_bass_kernel_spmd(nc, in_maps, core_ids=list(range(8)))
    out = np.empty((16, T, D), np.float32)
    for c in range(8):
        o = np.asarray(res.results[c]["outT"])
        out[c * NB:(c + 1) * NB] = o.T.reshape(NB, T, D)
    return out
```

```python
import contextlib
import numpy as np
import concourse.bass as bass
import concourse.mybir as mybir

F32 = mybir.dt.float32
BF16 = mybir.dt.bfloat16
ALU = mybir.AluOpType
AF = mybir.ActivationFunctionType
AX = mybir.AxisListType

ENGS = ("pe", "act", "dve", "pool", "sp")


class Tok:
    __slots__ = ("w", "r", "name")

    def __init__(self, name=""):
        self.w = None
        self.r = {}
        self.name = name


class V:
    __slots__ = ("ap", "toks")

    def __init__(self, ap, toks):
        self.ap = ap
        self.toks = toks

    def __getitem__(self, idx):
        return V(self.ap[idx], self.toks)

    def re(self, s, **kw):
        return V(self.ap.rearrange(s, **kw), self.toks)

    def bc(self, shape):
        return V(self.ap.broadcast_to(shape), self.toks)

    def un(self, axis):
        return V(self.ap.unsqueeze(axis), self.toks)

    def bitcast(self, dt):
        return V(self.ap.bitcast(dt), self.toks)

    @property
    def shape(self):
        return self.ap.shape


class Prog:
    def __init__(self, nc, es, n_dma_sems=40):
        self.nc = nc
        self.es = es
        self.q = {e: [] for e in ENGS}
        self.cnt = {e: 0 for e in ENGS}
        self.sem = {e: es.enter_context(nc.semaphore("s_" + e)) for e in ENGS}
        self.semobj = dict(self.sem)
        self.waited = {e: {} for e in ENGS}
        self.dsems = []
        self.n_ring = n_dma_sems
        for i in range(n_dma_sems):
            k = "d%d" % i
            self.semobj[k] = es.enter_context(nc.semaphore(k))
            self.dsems.append([k, 0, None])
        self.dnext = 0
        self.uid = 0
        self.psems = []
        for i in range(48):
            k = "q%d" % i
            self.semobj[k] = es.enter_context(nc.semaphore(k))
            self.psems.append(k)
        self.pnext = 0

    def sb(self, shape, dt=F32, name=None):
        self.uid += 1
        name = name or ("t%d" % self.uid)
        t = self.es.enter_context(self.nc.sbuf_tensor(name, list(shape), dt))
        return V(t[:], (Tok(name),))

    def ps(self, shape, dt=F32, name=None):
        self.uid += 1
        name = name or ("p%d" % self.uid)
        shape = list(shape)
        n = 1
        for d_ in shape[1:]:
            n *= d_
        nb = (n + 511) // 512
        t = self.es.enter_context(self.nc.psum_tensor(name, [shape[0], nb * 512], dt))
        ap = t[:][:, 0:n]
        if len(shape) == 3:
            ap = ap.rearrange("p (a b) -> p a b", a=shape[1])
        elif len(shape) == 4:
            ap = ap.rearrange("p (a b c) -> p a b c", a=shape[1], b=shape[2])
        return V(ap, (Tok(name),))

    def dram(self, name, shape, dt=F32, kind="Internal"):
        t = self.nc.dram_tensor(name, list(shape), dt, kind=kind)
        return V(t.ap(), (Tok(name),))

    def sub(self, v, name=""):
        return V(v.ap, (Tok(name),))

    def _needs(self, reads, writes):
        needs = {}

        def need(k, val):
            if val > needs.get(k, 0):
                needs[k] = val
        for v in reads:
            for t in v.toks:
                if t.w is not None:
                    need(*t.w)
        for v in writes:
            for t in v.toks:
                if t.w is not None:
                    need(*t.w)
                for k, val in t.r.items():
                    need(k, val)
        return needs

    def issue(self, eng, fn, reads, writes, inc=True, pe_group=False):
        needs = self._needs(reads, writes)
        waits = []
        wd = self.waited[eng]
        for k, val in needs.items():
            if k == eng and eng == "pe":
                continue
            if wd.get(k, 0) < val:
                wd[k] = val
                waits.append((k, val))
        n = self.cnt[eng] + 1
        if inc:
            self.cnt[eng] = n
        self.q[eng].append((waits, fn, inc))
        for v in reads:
            for t in v.toks:
                if t.r.get(eng, 0) < n:
                    t.r[eng] = n
        for v in writes:
            for t in v.toks:
                t.w = (eng, n)
                t.r = {}
        return n

    def dma(self, out, in_, eng="sp", **kw):
        if eng == "pool":
            slot = [self.psems[self.pnext], 0, None]
            self.pnext += 1
            self.dsems.append(slot)
        else:
            slot = self.dsems[self.dnext]
            self.dnext = (self.dnext + 1) % self.n_ring
        key, cur, _ = slot
        needs = self._needs([in_], [out])
        wd = self.waited[eng]
        waits = []
        if cur > 0:
            needs[key] = max(needs.get(key, 0), cur)
        for k, val in needs.items():
            if wd.get(k, 0) < val:
                wd[k] = val
                waits.append((k, val))
        newv = cur + 16
        slot[1] = newv
        oap, iap = out.ap, in_.ap

        def fn(e, oap=oap, iap=iap, kw=kw):
            return e.dma_start(out=oap, in_=iap, **kw)
        self.q[eng].append((waits, fn, ("dma", key)))
        for t in in_.toks:
            if t.r.get(key, 0) < newv:
                t.r[key] = newv
        for t in out.toks:
            t.w = (key, newv)
            t.r = {}

    def emit(self, final_waits=()):
        nc = self.nc
        engmap = {"pe": "tensor", "act": "scalar", "dve": "vector", "pool": "gpsimd", "sp": "sync"}
        with nc.Block() as block:
            for e in ENGS:
                lst = self.q[e]

                def body(eh, lst=lst, e=e):
                    for waits, fn, inc in lst:
                        for k, val in waits:
                            eh.wait_ge(self.semobj[k], val)
                        if fn is None:
                            continue
                        ins = fn(eh)
                        if inc is True:
                            ins.then_inc(self.semobj[e], 1)
                        elif inc:
                            ins.then_inc(self.semobj[inc[1]], 16)
                    if e == "sp":
                        for k, val in final_waits:
                            eh.wait_ge(self.semobj[k], val)
                getattr(block, engmap[e])(body)

    def mm(self, out, lhsT, rhs, start=True, stop=True):
        o, l, r = out.ap, lhsT.ap, rhs.ap

        def fn(e):
            return e.matmul(o, l, r, start=start, stop=stop)
        if stop:
            self.issue("pe", fn, [lhsT, rhs], [out], inc=True)
        else:
            self.issue("pe", fn, [lhsT, rhs], [], inc=False)
        return

    def mm_group(self, out, pairs):
        n = len(pairs)
        for i, (l, r) in enumerate(pairs):
            o, la, ra = out.ap, l.ap, r.ap
            st, sp_ = (i == 0), (i == n - 1)

            def fn(e, o=o, la=la, ra=ra, st=st, sp_=sp_):
                return e.matmul(o, la, ra, start=st, stop=sp_)
            if sp_ and n == 1:
                self.issue("pe", fn, [l, r], [out], inc=True)
            elif st:
                needs_v = V(out.ap, out.toks)
                self.issue_pe_first(fn, [l, r], needs_v)
            elif sp_:
                self.issue("pe", fn, [l, r], [out], inc=True)
            else:
                self.issue("pe", fn, [l, r], [], inc=False)

    def issue_pe_first(self, fn, reads, outv):
        eng = "pe"
        needs = self._needs(reads, [outv])
        waits = []
        wd = self.waited[eng]
        for k, val in needs.items():
            if k == eng:
                continue
            if wd.get(k, 0) < val:
                wd[k] = val
                waits.append((k, val))
        n = self.cnt[eng] + 1
        self.q[eng].append((waits, fn, False))
        for v in reads:
            for t in v.toks:
                if t.r.get(eng, 0) < n:
                    t.r[eng] = n

    def transpose(self, out, in_, ident):
        o, i, d = out.ap, in_.ap, ident.ap

        def fn(e):
            return e.transpose(o, i, d)
        self.issue("pe", fn, [in_, ident], [out])

    def act(self, out, in_, func, bias=None, scale=None, accum=None, eng="act"):
        o, i = out.ap, in_.ap
        reads = [in_]
        kw = {}
        if bias is not None:
            if isinstance(bias, V):
                reads.append(bias)
                kw["bias"] = bias.ap
            else:
                kw["bias"] = bias
        if scale is not None:
            if isinstance(scale, V):
                reads.append(scale)
                kw["scale"] = scale.ap
            else:
                kw["scale"] = scale
        writes = [out]
        if accum is not None:
            kw["accum_out"] = accum.ap
            writes.append(accum)

        def fn(e):
            return e.activation(o, i, func, **kw)
        self.issue("act", fn, reads, writes)

    def tt(self, out, a, b, op, eng="dve"):
        o, x, y = out.ap, a.ap, b.ap

        def fn(e):
            return e.tensor_tensor(o, x, y, op)
        self.issue(eng, fn, [a, b], [out])

    def ts(self, out, a, s1, op0, s2=None, op1=None, eng="dve", accum=None):
        o, x = out.ap, a.ap
        reads = [a]

        def cv(s):
            if isinstance(s, V):
                reads.append(s)
                return s.ap
            return s
        s1a = cv(s1)
        s2a = cv(s2)
        writes = [out]
        kw = {}
        if accum is not None:
            kw["accum_out"] = accum.ap
            writes.append(accum)

        def fn(e):
            if op1 is None:
                return e.tensor_scalar(o, x, s1a, None, op0, **kw)
            return e.tensor_scalar(o, x, s1a, s2a, op0, op1, **kw)
        self.issue(eng, fn, reads, writes)

    def stt(self, out, a, s, b, op0, op1, eng="dve"):
        o, x, y = out.ap, a.ap, b.ap
        reads = [a, b]
        if isinstance(s, V):
            reads.append(s)
            sa = s.ap
        else:
            sa = s

        def fn(e):
            return e.scalar_tensor_tensor(o, x, sa, y, op0, op1)
        self.issue(eng, fn, reads, [out])

    def copy(self, out, in_, eng="dve"):
        o, i = out.ap, in_.ap
        if eng == "act":
            def fn(e):
                return e.copy(o, i)
        else:
            def fn(e):
                return e.tensor_copy(o, i)
        self.issue(eng, fn, [in_], [out])

    def memset(self, out, val, eng="dve"):
        o = out.ap

        def fn(e):
            return e.memset(o, val)
        self.issue(eng, fn, [], [out])

    def recip(self, out, in_, eng="dve"):
        o, i = out.ap, in_.ap

        def fn(e):
            return e.reciprocal(o, i)
        self.issue(eng, fn, [in_], [out])

    def scan(self, out, d0, d1, init, op0, op1):
        o, a, b = out.ap, d0.ap, d1.ap
        reads = [d0, d1]
        if isinstance(init, V):
            reads.append(init)
            ia = init.ap
        else:
            ia = init

        def fn(e):
            return e.tensor_tensor_scan(o, a, b, ia, op0, op1)
        self.issue("dve", fn, reads, [out])

    def reduce(self, out, in_, op, axis=AX.X, eng="dve"):
        o, i = out.ap, in_.ap

        def fn(e):
            return e.tensor_reduce(o, i, axis, op)
        self.issue(eng, fn, [in_], [out])


def _prog_barrier(self):
    targets = {e: self.cnt[e] for e in ENGS if self.cnt[e] > 0}
    for s in self.dsems:
        if s[1] > 0:
            targets[s[0]] = s[1]
    for e in ENGS:
        wd = self.waited[e]
        waits = []
        for k, val in targets.items():
            if wd.get(k, 0) < val:
                wd[k] = val
                waits.append((k, val))
        if waits:
            self.q[e].append((waits, None, False))


@contextlib.contextmanager
def _prog_scope(self):
    old = self.es
    with contextlib.ExitStack() as es2:
        self.es = es2
        try:
            yield
        finally:
            self.barrier()
            self.es = old


Prog.barrier = _prog_barrier
Prog.scope = _prog_scope
D = 1024; T = 2048; NB = 2; TOK = NB * T; TB = 512; NTB = TOK // TB; KC = 8
DFF = 2816; NJ = 22; NIN = 12552
C_MLX, C_MLO, C_MLG = 0, 1024, 2048
C_RW = 2056; C_HG = 5384; C_GATE = 9480
EPS = 1e-6; RW_LN_EPS = 64e-5
LRW = 64; LHG = 64; LML = 128
WSC = 0.6065306597126334


def fm(v):
    v = np.asarray(v, np.float32).reshape(-1, 128)
    return np.ascontiguousarray(v.T)


def kxn(W):
    K, N = W.shape
    return np.ascontiguousarray(W.reshape(K // 128, 128, N).transpose(1, 0, 2))


class VecPack:
    def __init__(self):
        self.cols = []
        self.off = {}
        self.n = 0

    def add(self, name, arr):
        arr = np.asarray(arr, np.float32)
        assert arr.shape[0] == 128
        self.off[name] = (self.n, arr.shape[1])
        self.cols.append(arr)
        self.n += arr.shape[1]

    def build(self):
        return np.ascontiguousarray(np.concatenate(self.cols, axis=1))


def pack_vecs(inp):
    vp = VecPack()
    rep = lambda v: np.tile(np.asarray(v, np.float32)[None, :], (128, 1))
    for l in range(2):
        for s in range(3):
            vp.add("normw%d%d" % (l, s), fm(inp["norm_w"][l, s]))
        vp.add("adab%d" % l, fm(inp["ada_b"][l]))
        for tp in range(4):
            vp.add("convw%d%d" % (l, tp), fm(inp["ml_conv_w"][l, tp]))
        vp.add("convb%d" % l, fm(inp["ml_conv_b"][l]))
        vp.add("mlnw%d" % l, fm(inp["ml_norm_w"][l]))
        vp.add("mlskip%d" % l, fm(inp["ml_skip"][l]))
        vp.add("ib%d" % l, rep(inp["ml_i_b"][l]))
        vp.add("fb%d" % l, rep(inp["ml_f_b"][l]))
        vp.add("rwmu%d" % l, fm(inp["rw_mu"][l]))
        for nm in ("rw_w0", "rw_a0", "rw_k_k", "rw_k_a", "rw_ln_w", "rw_ln_b"):
            vp.add(nm + str(l), fm(inp[nm][l]))
        vp.add("rw_r_k%d" % l, fm(inp["rw_r_k"][l].reshape(-1)))
        vp.add("hglb%d" % l, fm(inp["hg_lb_logits"][l]))
        vp.add("hgnw%d" % l, fm(inp["hg_norm_w"][l]))
    vp.add("finw", fm(inp["final_norm_w"]))
    vp.add("rw_v0", fm(inp["rw_v0"][0]))
    mv = np.zeros((128, 1), np.float32)
    mv[:32, 0] = inp["rw_mu_vres"][0]
    vp.add("muvres", mv)
    return vp


def host_weights(inp):
    w = {}
    w["ada_w"] = np.ascontiguousarray(
        np.asarray(inp["ada_w"], np.float32).reshape(2, 8, 128, 18, 512).transpose(0, 3, 2, 1, 4))
    w["ffn_up"] = np.ascontiguousarray(
        np.asarray(inp["ffn_up"], np.float32).reshape(2, 2, 8, 128, 2 * DFF).transpose(0, 1, 3, 2, 4))
    w["ffn_down"] = np.ascontiguousarray(
        np.asarray(inp["ffn_down"], np.float32).reshape(2, 2, NJ, 128, D).transpose(0, 1, 3, 2, 4))
    w["w_in"] = np.ascontiguousarray(
        np.asarray(inp["w_in"], np.float32).reshape(2, 8, 128, NIN).transpose(0, 2, 1, 3))
    w["w_vres"] = np.ascontiguousarray(
        np.asarray(inp["w_in_vres"], np.float32).reshape(8, 128, 32).transpose(1, 0, 2))
    w["wq"] = np.ascontiguousarray(
        np.asarray(inp["ml_wq"], np.float32).reshape(2, 4, 2, 128, 256).transpose(0, 3, 1, 2, 4))
    w["wk"] = np.ascontiguousarray(
        np.asarray(inp["ml_wk"], np.float32).reshape(2, 4, 2, 128, 256).transpose(0, 3, 1, 2, 4))
    w["rw_wa2"] = np.ascontiguousarray(np.concatenate(
        [np.asarray(inp["rw_w2"], np.float32), np.asarray(inp["rw_a2"], np.float32)], axis=1))
    w["rw_g2"] = np.ascontiguousarray(np.asarray(inp["rw_g2"], np.float32))
    w["rw_v2"] = np.ascontiguousarray(np.asarray(inp["rw_v2"], np.float32)[0])
    w["w_branch"] = np.ascontiguousarray(
        np.asarray(inp["w_branch"], np.float32).reshape(2, 3, 8, 128, D).transpose(0, 3, 1, 2, 4))
    w["w_out"] = np.ascontiguousarray(
        np.asarray(inp["w_out"], np.float32).reshape(2, 8, 128, D).transpose(0, 2, 1, 3))
    return w


class DT:
    def __init__(self, P, name, shape, dt, tok_axis, kind="Internal", blk=TB):
        self.t = P.nc.dram_tensor(name, list(shape), dt, kind=kind)
        self.ap = self.t.ap()
        self.tok_axis = tok_axis
        self.blk = blk
        n = shape[tok_axis] // blk
        self.toks = [Tok("%s_%d" % (name, i)) for i in range(max(n, 1))]

    def v(self, t0, t1):
        i0, i1 = t0 // self.blk, (t1 - 1) // self.blk
        toks = tuple(self.toks[i0:i1 + 1])
        if self.tok_axis == 0:
            return V(self.ap[t0:t1], toks)
        return V(self.ap[:, t0:t1], toks)

    def all(self):
        return V(self.ap, tuple(self.toks))
class Ctx:
    pass


STAGE_LOG = []


def build_program(vec_off, nvec, stop_after=None, debug_outs=(), only=None):
    nc = bass.Bass("TRN2", target_bir_lowering=False)
    es = contextlib.ExitStack()
    with es:
        P = Prog(nc, es)
        K = Ctx()
        K.P = P
        K.debug = {}

        def din(name, shape, dt=F32):
            return P.dram(name, shape, dt, kind="ExternalInput")
        K.xT_in = din("xT", [D, TOK])
        K.cT = din("cT", [128, KC, NB])
        K.vecs_d = din("vecs", [128, nvec])
        K.ada_w = din("ada_w", [2, 18, 128, 8, 512])
        K.ffn_up = din("ffn_up", [2, 2, 128, 8, 2 * DFF])
        K.ffn_down = din("ffn_down", [2, 2, 128, NJ, D])
        K.w_in = din("w_in", [2, 128, 8, NIN])
        K.w_vres = din("w_vres", [128, 8, 32])
        K.wq = din("wq", [2, 128, 4, 2, 256])
        K.wk = din("wk", [2, 128, 4, 2, 256])
        K.rw_wa2 = din("rw_wa2", [2, 128, D])
        K.rw_g2 = din("rw_g2", [2, 128, D])
        K.rw_v2 = din("rw_v2", [32, D])
        K.w_branch = din("w_branch", [2, 128, 3, 8, D])
        K.w_out = din("w_out", [2, 128, 8, D])
        K.outT = DT(P, "outT", [D, TOK], F32, 1, kind="ExternalOutput")

        def scr(name, shape, dt, tok_axis):
            kind = "ExternalOutput" if name in debug_outs else "Internal"
            return DT(P, name, shape, dt, tok_axis, kind=kind)
        K.xs = scr("xs", [D, TOK], F32, 1)
        K.act = scr("actT", [DFF, TOK], BF16, 1)
        K.hT = scr("hT", [D, TOK], BF16, 1)
        K.yT = [scr("yT%d" % i, [D, TOK], BF16, 1) for i in range(3)]
        K.ml_qT = scr("ml_qT", [D, TOK], F32, 1)
        K.ml_kT = scr("ml_kT", [D, TOK], F32, 1)
        K.ml_skx = scr("ml_skx", [D, TOK], F32, 1)
        K.ml_ktok = scr("ml_ktok", [TOK, D], F32, 0)
        K.ml_vo = scr("ml_vo", [TOK, 2056], F32, 0)
        K.hg_qT = scr("hg_qT", [D, TOK], F32, 1)
        K.hg_kT = scr("hg_kT", [D, TOK], F32, 1)
        K.hg_dec = scr("hg_dec", [D, TOK // LHG], F32, 1) if False else None
        K.hg_decT = DT(P, "hg_decT", [D, TOK // LHG], F32, 1, blk=TB // LHG)
        K.hg_vtok = scr("hg_vtok", [TOK, D], F32, 0)
        K.hg_khat = scr("hg_khat", [TOK, D], F32, 0)
        K.hg_gtok = scr("hg_gtok", [TOK, D], F32, 0)
        for nm in ("rw_kapT", "rw_rhoT", "rw_betT", "rw_ktlT"):
            setattr(K, nm, scr(nm, [D, TOK], BF16, 1))
        for nm in ("rw_bonT", "rw_gT", "rw_vfT"):
            setattr(K, nm, scr(nm, [D, TOK], F32, 1))
        K.rw_gamT = DT(P, "rw_gamT", [D, TOK // LRW], F32, 1, blk=TB // LRW)
        for nm in ("rw_vtok", "rw_bhat", "rw_khat"):
            setattr(K, nm, scr(nm, [TOK, D], BF16, 0))
        K.rw_ytok = scr("rw_ytok", [TOK, D], F32, 0)

        K.vecs = P.sb([128, nvec], F32, "vecs_sb")
        P.dma(K.vecs, K.vecs_d)

        def vec(name):
            o, n = vec_off[name]
            return K.vecs[:, o:o + n]
        K.vec = vec
        K.epsb = P.sb([128, 4], F32, "epsb")
        P.memset(K.epsb[:, 0:1], EPS)
        P.memset(K.epsb[:, 1:2], RW_LN_EPS)
        P.memset(K.epsb[:, 2:3], 1.0)
        P.memset(K.epsb[:, 3:4], 0.0)
        K.ones = P.sb([128, 128], F32, "ones")
        P.memset(K.ones, 1.0)
        K.ident = P.sb([128, 128], F32, "ident")
        P.memset(K.ident, 0.0)
        io = P.sb([128, 128], F32, "iota_f")
        ip = P.sb([128, 1], F32, "iota_p")
        io_i = P.sb([128, 128], mybir.dt.int32, "iota_fi")
        ip_i = P.sb([128, 1], mybir.dt.int32, "iota_pi")

        def iota(out, pattern, cm):
            o = out.ap

            def fn(e):
                return e.iota(o, pattern, base=0, channel_multiplier=cm)
            P.issue("pool", fn, [], [out])
        iota(io_i, [[1, 128]], 0)
        iota(ip_i, [[0, 1]], 1)
        P.copy(io, io_i)
        P.copy(ip, ip_i)
        K.iota_f = io
        K.iota_p = ip
        P.ts(K.ident, io, ip[:, 0:1], ALU.is_equal)
        K.identb = P.sb([128, 128], BF16, "identb")
        P.copy(K.identb, K.ident)
        K.tri = P.sb([128, 128], F32, "tri")
        P.ts(K.tri, io, ip[:, 0:1], ALU.is_ge)
        K.tris = P.sb([128, 128], F32, "tris")
        P.ts(K.tris, io, ip[:, 0:1], ALU.is_gt)
        K.lows = P.sb([128, 128], F32, "lows")
        P.ts(K.lows, io, ip[:, 0:1], ALU.is_lt)
        K.blk64 = P.sb([128, 128], F32, "blk64")
        P.memset(K.blk64, 0.0)
        P.memset(K.blk64[0:64, 0:64], 1.0)
        P.memset(K.blk64[64:128, 64:128], 1.0)
        K.rmask = P.sb([128, TB], F32, "rmask")
        P.memset(K.rmask, 1.0)
        P.memset(K.rmask.re("p (c l) -> p c l", l=64)[:, :, 0:1], 0.0)

        K.modT = [P.sb([128, 72, NB], F32, "modT%d" % l) for l in range(2)]
        K.sc1 = [[P.sb([128, 8, NB], F32) for s in range(3)] for l in range(2)]
        K.gt = [[P.sb([128, 8, NB], F32) for s in range(3)] for l in range(2)]
        stage_mod(K)
        stages = [
            ("copyx", lambda: stage_copyx(K)),
        ]
        for l in range(2):
            stages += [
                ("ffn%d0" % l, lambda l=l: stage_ffn(K, l, 0)),
                ("normmix%d" % l, lambda l=l: stage_normmix(K, l)),
                ("inml%d" % l, lambda l=l: stage_in_ml(K, l)),
                ("mlrec%d" % l, lambda l=l: stage_ml_rec(K, l)),
                ("inhg%d" % l, lambda l=l: stage_in_hg(K, l)),
                ("hgrec%d" % l, lambda l=l: stage_hg_rec(K, l)),
                ("inrw%d" % l, lambda l=l: stage_in_rw(K, l)),
                ("rwrec%d" % l, lambda l=l: stage_rw_rec(K, l)),
                ("mix%d" % l, lambda l=l: stage_mix(K, l)),
                ("ffn%d1" % l, lambda l=l: stage_ffn(K, l, 1)),
            ]
        stages.append(("final", lambda: stage_final(K)))
        for name, fn in stages:
            if only is not None and name not in only:
                continue
            fn()
            STAGE_LOG.append((name, dict(P.cnt)))
            if stop_after == name:
                break
        P.barrier()
        fin = [(k, v) for k, v, _ in P.dsems if v > 0]
        P.emit(final_waits=fin)
    return nc


def stage_mod(K):
    P = K.P
    with P.scope():
        cT = P.sb([128, KC, NB])
        P.dma(cT, K.cT)
        cond = P.sb([128, KC, NB])
        P.act(cond, cT, AF.Silu)
        wb = [P.sb([128, 8, 512]) for _ in range(2)]
        for l in range(2):
            ps = P.ps([128, 72, NB])
            for pc in range(18):
                wt = wb[pc % 2]
                P.dma(wt, K.ada_w[l, pc])
                for jj in range(4):
                    j = pc * 4 + jj
                    P.mm_group(ps[:, j, :], [(wt[:, kc, jj * 128:(jj + 1) * 128], cond[:, kc, :]) for kc in range(KC)])
            P.tt(K.modT[l], ps, K.vec("adab%d" % l).un(2).bc([128, 72, NB]), ALU.add)
            for s in range(3):
                m = K.modT[l]
                nw = K.vec("normw%d%d" % (l, s))
                P.ts(K.sc1[l][s], m[:, s * 24 + 8:s * 24 + 16, :], 1.0, ALU.add)
                P.tt(K.sc1[l][s], K.sc1[l][s], nw.un(2).bc([128, 8, NB]), ALU.mult)
                f = 1.0 if s == 1 else 0.5
                P.ts(K.gt[l][s], m[:, s * 24 + 16:s * 24 + 24, :], 1.0, ALU.add, f, ALU.mult)


def stage_copyx(K):
    P = K.P
    for tb in range(NTB):
        P.dma(K.xs.v(tb * TB, (tb + 1) * TB), K.xT_in[:, tb * TB:(tb + 1) * TB])


def norm_block(K, xb, hb, l, s, b, sq, ssp, sd, rstd, hn, plain_w=None, out_f32=None):
    P = K.P
    P.act(sq, xb, AF.Square)
    P.mm_group(ssp, [(K.ones, sq[:, dc, :]) for dc in range(8)])
    P.act(sd, ssp, AF.Sqrt, bias=K.epsb[:, 0:1], scale=1.0 / D)
    P.recip(rstd, sd)
    P.tt(hn, xb, rstd.un(1).bc([128, 8, TB]), ALU.mult)
    if plain_w is not None:
        P.tt(out_f32, hn, plain_w.un(2).bc([128, 8, TB]), ALU.mult)
        return
    sh = K.modT[l][:, s * 24:s * 24 + 8, :]
    for dc in range(8):
        P.ts(hb[:, dc, :], hn[:, dc, :], K.sc1[l][s][:, dc, b:b + 1], ALU.mult, sh[:, dc, b:b + 1], ALU.add)


def alloc_norm_tmps(K):
    P = K.P
    if not hasattr(K, "epsb"):
        pass
    sq = P.sb([128, 8, TB])
    sd = P.sb([128, TB])
    return dict(sq=sq, ssp=P.ps([128, TB]), sd=sd, rstd=sd, hn=sq)


def xview(dt_, tb):
    return dt_.v(tb * TB, (tb + 1) * TB).re("(c p) t -> p c t", p=128)


def stage_ffn(K, l, f):
    P = K.P
    s = 0 if f == 0 else 2
    with P.scope():
        wup = P.sb([128, 8, 2 * DFF], BF16)
        P.dma(wup.re("p k (a n) -> p k a n", a=4), K.ffn_up[l, f].re("p k (a n) -> p k a n", a=4), eng="pool")
        tm = alloc_norm_tmps(K)
        xbs = [P.sb([128, 8, TB]) for _ in range(2)]
        hbs = [P.sb([128, 8, TB], BF16)] * 2
        acts = [P.sb([128, NJ, TB], BF16) for _ in range(2)]
        sgs = [P.sb([128, TB]) for _ in range(2)]
        pas = [P.ps([128, TB]) for _ in range(2)]
        pbs = [P.ps([128, TB]) for _ in range(2)]
        for tb in range(NTB):
            b = tb // 4
            xb, hb, ab = xbs[tb % 2], hbs[tb % 2], acts[tb % 2]
            P.dma(xb, xview(K.xs, tb))
            norm_block(K, xb, hb, l, s, b, **tm)
            for j in range(NJ):
                pa, pb, sg = pas[j % 2], pbs[j % 2], sgs[j % 2]
                P.mm_group(pa, [(wup[:, kc, j * 128:(j + 1) * 128], hb[:, kc, :]) for kc in range(8)])
                P.mm_group(pb, [(wup[:, kc, DFF + j * 128:DFF + (j + 1) * 128], hb[:, kc, :]) for kc in range(8)])
                P.act(sg, pa, AF.Silu)
                P.tt(ab[:, j, :], sg, pb, ALU.mult)
            P.dma(xview(K.act, tb), ab)
    with P.scope():
        wdn = P.sb([128, NJ, D], BF16)
        P.dma(wdn, K.ffn_down[l, f], eng="pool")
        xbs = [P.sb([128, 8, TB]) for _ in range(2)]
        xos = [P.sb([128, 8, TB]) for _ in range(2)]
        abs_ = [P.sb([128, NJ, TB], BF16) for _ in range(2)]
        pos = [P.ps([128, TB]) for _ in range(2)]
        for tb in range(NTB):
            b = tb // 4
            xb, xo, ab = xbs[tb % 2], xos[tb % 2], abs_[tb % 2]
            P.dma(ab, xview(K.act, tb))
            P.dma(xb, xview(K.xs, tb))
            for m in range(8):
                po = pos[m % 2]
                P.mm_group(po, [(wdn[:, kc, m * 128:(m + 1) * 128], ab[:, kc, :]) for kc in range(NJ)])
                P.stt(xo[:, m, :], po, K.gt[l][s][:, m, b:b + 1], xb[:, m, :], ALU.mult, ALU.add)
            P.dma(xview(K.xs, tb), xo)


def stage_final(K):
    P = K.P
    with P.scope():
        tm = alloc_norm_tmps(K)
        xbs = [P.sb([128, 8, TB]) for _ in range(2)]
        obs = [P.sb([128, 8, TB]) for _ in range(2)]
        for tb in range(NTB):
            xb, ob = xbs[tb % 2], obs[tb % 2]
            P.dma(xb, xview(K.xs, tb))
            norm_block(K, xb, None, 0, 0, 0, plain_w=K.vec("finw"), out_f32=ob, **tm)
            P.dma(xview(K.outT, tb), ob)


def stage_normmix(K, l):
    P = K.P
    with P.scope():
        tm = alloc_norm_tmps(K)
        xbs = [P.sb([128, 8, TB]) for _ in range(2)]
        hbs = [P.sb([128, 8, TB], BF16) for _ in range(2)]
        for tb in range(NTB):
            xb, hb = xbs[tb % 2], hbs[tb % 2]
            P.dma(xb, xview(K.xs, tb))
            norm_block(K, xb, hb, l, 1, tb // 4, **tm)
            P.dma(xview(K.hT, tb), hb)
def load_w_cols(P, dst, src, c0, c1, eng="pool"):
    n = c1 - c0
    h = n // 2
    P.dma(dst[:, :, 0:h], src[:, :, c0:c0 + h], eng=eng)
    P.dma(dst[:, :, h:n], src[:, :, c0 + h:c1], eng=eng)


def tmview(dt_, t0, n):
    return dt_.v(t0, t0 + n)


def stage_in_ml(K, l):
    P = K.P
    with P.scope():
        wml = P.sb([128, 8, 2056], BF16)
        load_w_cols(P, wml, K.w_in[l], 0, 2056)
        wq = P.sb([128, 4, 2, 256], BF16)
        wk = P.sb([128, 4, 2, 256], BF16)
        P.dma(wq, K.wq[l], eng="pool")
        P.dma(wk, K.wk[l], eng="pool")
        cw = [K.vec("convw%d%d" % (l, tp)) for tp in range(4)]
        cb = K.vec("convb%d" % l)
        skip = K.vec("mlskip%d" % l)
        hbs = [P.sb([128, 8, TB], BF16) for _ in range(2)]
        xms = [P.sb([128, 8, TB + 3]) for _ in range(2)]
        acc = P.sb([128, 8, TB])
        xc = P.sb([128, 8, TB])
        xcb = P.sb([128, 8, TB], BF16)
        qst = P.sb([128, 8, TB])
        kst = P.sb([128, 8, TB])
        ktk = [P.sb([128, D]) for _ in range(2)]
        vos = [P.sb([128, 2056]) for _ in range(2)]
        psA = [P.ps([128, TB]) for _ in range(2)]
        psB = [P.ps([128, 2, 512]) for _ in range(2)]
        psC = P.ps([128, 8])
        for tb in range(NTB):
            hb, xm = hbs[tb % 2], xms[tb % 2]
            xmp = xms[(tb + 1) % 2]
            P.dma(hb, xview(K.hT, tb))
            if tb % 4 == 0:
                P.memset(xm[:, :, 0:3], 0.0)
            else:
                P.copy(xm[:, :, 0:3], xmp[:, :, TB:TB + 3])
            for dc in range(8):
                ps = psA[dc % 2]
                P.mm_group(ps, [(wml[:, kc, dc * 128:(dc + 1) * 128], hb[:, kc, :]) for kc in range(8)])
                P.copy(xm[:, dc, 3:TB + 3], ps, eng="act")
            for dc in range(8):
                P.ts(acc[:, dc, :], xm[:, dc, 0:TB], cw[0][:, dc:dc + 1], ALU.mult, cb[:, dc:dc + 1], ALU.add)
                for tp in range(1, 4):
                    P.stt(acc[:, dc, :], xm[:, dc, tp:tp + TB], cw[tp][:, dc:dc + 1], acc[:, dc, :], ALU.mult, ALU.add)
            P.act(xc, acc, AF.Silu)
            P.act(xcb, acc, AF.Silu)
            P.tt(acc, xc, skip.un(2).bc([128, 8, TB]), ALU.mult, eng="pool")
            P.dma(xview(K.ml_skx, tb), acc)
            for (w_, st_, dst) in ((wq, qst, K.ml_qT), (wk, kst, K.ml_kT)):
                for h in range(4):
                    for ec in range(2):
                        ps = psA[ec % 2]
                        P.mm_group(ps, [(w_[:, h, dck, ec * 128:(ec + 1) * 128], xcb[:, 2 * h + dck, :]) for dck in range(2)])
                        if ec == 0:
                            P.copy(st_[:, 2 * h + ec, :], ps, eng="act")
                        else:
                            P.copy(st_[:, 2 * h + ec, :], ps, eng="dve")
                P.dma(xview(dst, tb), st_)
            for tt_ in range(4):
                t0 = tb * TB + tt_ * 128
                tsl = slice(tt_ * 128, (tt_ + 1) * 128)
                kt = ktk[tt_ % 2]
                pb = psB[0]
                for h in range(4):
                    P.mm_group(pb[:, h // 2, (h % 2) * 256:(h % 2) * 256 + 256],
                               [(xcb[:, 2 * h + dck, tsl], wk[:, h, dck, :]) for dck in range(2)])
                P.copy(kt, pb.re("p a b -> p (a b)"), eng="act")
                P.dma(tmview(K.ml_ktok, t0, 128), kt)
                vo = vos[tt_ % 2]
                for half in range(2):
                    pb2 = psB[1] if half == 0 else psB[0]
                    for q in range(2):
                        c0 = half * 1024 + q * 512
                        P.mm_group(pb2[:, q, :], [(hb[:, kc, tsl], wml[:, kc, c0:c0 + 512]) for kc in range(8)])
                    if half == 0:
                        P.copy(vo[:, 0:1024], pb2.re("p a b -> p (a b)"), eng="dve")
                    else:
                        P.copy(vo[:, 1024:2048], pb2.re("p a b -> p (a b)"), eng="act")
                P.mm_group(psC, [(hb[:, kc, tsl], wml[:, kc, 2048:2056]) for kc in range(8)])
                P.copy(vo[:, 2048:2056], psC, eng="dve")
                P.dma(tmview(K.ml_vo, t0, 128), vo)


def stage_ml_rec(K, l):
    P = K.P
    NCH = T // LML
    with P.scope():
        ib = K.vec("ib%d" % l)
        fb = K.vec("fb%d" % l)
        mlnw = K.vec("mlnw%d" % l)
        gin = P.sb([128, NCH, 8])
        sp = P.sb([128, NCH, 4])
        li = P.sb([128, NCH, 4])
        et = P.sb([128, NCH, 4])
        wv = P.sb([128, NCH, 4])
        dec = P.sb([128, NCH, 4])
        wend = P.sb([128, NCH, 4])
        tmpg = P.sb([128, NCH, 4])
        C = P.sb([128, 4, 2, 257])
        qTs = [P.sb([128, 8, LML]) for _ in range(2)]
        kTs = [P.sb([128, 8, LML]) for _ in range(2)]
        kts = [P.sb([128, D]) for _ in range(2)]
        vxs = [P.sb([128, 4, 257]) for _ in range(2)]
        for vx in vxs:
            P.memset(vx[:, :, 256:257], 1.0)
        osb = [P.sb([128, D]) for _ in range(2)]
        sko = [P.sb([128, 8, LML]) for _ in range(2)]
        sgo = P.sb([128, D])
        sts = [P.sb([128, 128]) for _ in range(2)]
        hh = P.sb([128, 4, 256])
        cen = P.sb([128, 4, 256])
        sq = P.sb([128, 4, 256])
        sm = P.sb([128, 16])
        kw = P.sb([128, 4, 256])
        yst = [P.sb([128, 8, LML], BF16) for _ in range(2)]
        stp = [P.ps([128, 128]) for _ in range(2)]
        nps = P.ps([128, 4, 512])
        npsv = [P.sub(nps[:, h, 0:257]) for h in range(4)]
        psX = [P.ps([128, 512]) for _ in range(2)]
        for b in range(NB):
            T0 = b * T
            P.dma(gin, K.ml_vo.v(T0, T0 + T)[:, 2048:2056].re("(c t) g -> t c g", t=LML))
            P.tt(tmpg, gin[:, :, 4:8], fb.un(1).bc([128, NCH, 4]), ALU.add)
            P.act(tmpg, tmpg, AF.Exp, scale=-1.0)
            P.act(sp, tmpg, AF.Ln, bias=K.epsb[:, 2:3])
            P.tt(li, gin[:, :, 0:4], ib.un(1).bc([128, NCH, 4]), ALU.add)
            bps = psX[0][:, 0:NCH * 4]
            tps = psX[1][:, 0:NCH * 4]
            P.mm_group(bps, [(K.tri, sp.re("p c h -> p (c h)"))])
            P.mm_group(tps, [(K.ones, sp.re("p c h -> p (c h)"))])
            P.act(et.re("p c h -> p (c h)"), bps, AF.Exp, scale=-1.0)
            P.tt(tmpg.re("p c h -> p (c h)"), li.re("p c h -> p (c h)"), bps, ALU.add)
            P.act(wv, tmpg, AF.Exp)
            P.ts(wv, wv, 1.0 / 16.0, ALU.mult)
            P.act(dec.re("p c h -> p (c h)"), tps, AF.Exp, scale=-1.0)
            P.tt(wend, wv, dec, ALU.mult)
            P.memset(C, 0.0)
            for c in range(NCH):
                t0 = T0 + c * LML
                qT, kT, kt, vx, ob, sk = qTs[c % 2], kTs[c % 2], kts[c % 2], vxs[c % 2], osb[c % 2], sko[c % 2]
                P.dma(qT, K.ml_qT.v(t0, t0 + LML).re("(c p) t -> p c t", p=128))
                P.dma(kT, K.ml_kT.v(t0, t0 + LML).re("(c p) t -> p c t", p=128))
                P.dma(kt, tmview(K.ml_ktok, t0, LML))
                vo = K.ml_vo.v(t0, t0 + LML)
                P.dma(vx[:, :, 0:256], vo[:, 0:1024].re("t (h e) -> t h e", h=4))
                P.dma(ob, vo[:, 1024:2048])
                P.dma(sk, K.ml_skx.v(t0, t0 + LML).re("(c p) t -> p c t", p=128))
                P.act(sgo, ob, AF.Sigmoid)
                for h in range(4):
                    sp_ = stp[h % 2]
                    st = sts[h % 2]
                    P.mm_group(sp_, [(kT[:, 2 * h + dck, :], qT[:, 2 * h + dck, :]) for dck in range(2)])
                    P.stt(st, sp_, wv[:, c, h:h + 1], K.tri, ALU.mult, ALU.mult)
                    P.mm_group(npsv[h], [(st, vx[:, h, :]),
                                         (qT[:, 2 * h, :], C[:, h, 0, :]),
                                         (qT[:, 2 * h + 1, :], C[:, h, 1, :])])
                for h in range(4):
                    P.tt(sm[:, h:h + 1], npsv[h][:, 256:257], et[:, c, h:h + 1], ALU.mult)
                P.ts(sm[:, 4:8], sm[:, 0:4], -1.0, ALU.mult)
                P.tt(sm[:, 4:8], sm[:, 4:8], sm[:, 0:4], ALU.max)
                P.ts(sm[:, 4:8], sm[:, 4:8], 1.0, ALU.max)
                P.recip(sm[:, 8:12], sm[:, 4:8])
                P.tt(sm[:, 12:16], sm[:, 8:12], et[:, c, :], ALU.mult)
                for h in range(4):
                    P.stt(hh[:, h, :], npsv[h][:, 0:256], sm[:, 12 + h:13 + h], sgo[:, h * 256:(h + 1) * 256],
                          ALU.mult, ALU.mult)
                P.reduce(sm[:, 0:4], hh, ALU.add)
                P.ts(sm[:, 0:4], sm[:, 0:4], 1.0 / 256.0, ALU.mult)
                P.tt(cen, hh, sm[:, 0:4].un(2).bc([128, 4, 256]), ALU.subtract)
                P.tt(sq, cen, cen, ALU.mult, eng="pool")
                P.reduce(sm[:, 4:8], sq, ALU.add)
                P.act(sm[:, 8:12], sm[:, 4:8], AF.Sqrt, bias=K.epsb[:, 0:1], scale=1.0 / 256.0)
                P.recip(sm[:, 8:12], sm[:, 8:12])
                P.tt(cen, cen, sm[:, 8:12].un(2).bc([128, 4, 256]), ALU.mult)
                cenf = cen.re("p h e -> p (h e)")
                ys = yst[c % 2]
                for half in range(2):
                    px = psX[half]
                    for jj in range(4):
                        j = half * 4 + jj
                        P.transpose(px[:, jj * 128:(jj + 1) * 128], cenf[:, j * 128:(j + 1) * 128], K.ident)
                    for jj in range(4):
                        j = half * 4 + jj
                        P.stt(ys[:, j, :], px[:, jj * 128:(jj + 1) * 128], mlnw[:, j:j + 1], sk[:, j, :],
                              ALU.mult, ALU.add)
                P.dma(K.yT[0].v(t0, t0 + LML).re("(c p) t -> p c t", p=128), ys)
                for h in range(4):
                    P.ts(kw[:, h, :], kt[:, h * 256:(h + 1) * 256], wend[:, c, h:h + 1], ALU.mult, eng="pool")
                for h in range(4):
                    for dck in range(2):
                        px = psX[dck]
                        P.mm_group(px[:, 0:257], [(kw[:, h, dck * 128:(dck + 1) * 128], vx[:, h, :])])
                        P.stt(C[:, h, dck, :], C[:, h, dck, :], dec[:, c, h:h + 1], px[:, 0:257], ALU.mult, ALU.add)


def stage_in_hg(K, l):
    P = K.P
    with P.scope():
        whg = P.sb([128, 8, 4096], BF16)
        load_w_cols(P, whg, K.w_in[l], C_HG, C_HG + 4096)
        lb = P.sb([128, 8])
        oml = P.sb([128, 8])
        noml = P.sb([128, 8])
        if l == 0:
            P.memset(lb, 0.0)
        else:
            P.tt(lb, K.vec("hglb1"), K.vec("hglb0"), ALU.subtract)
            P.act(lb, lb, AF.Sigmoid)
        P.ts(oml, lb, -1.0, ALU.mult, 1.0, ALU.add)
        P.ts(noml, oml, -1.0, ALU.mult)
        hbs = [P.sb([128, 8, TB], BF16) for _ in range(2)]
        q = P.sb([128, 8, TB])
        sg = P.sb([128, 8, TB])
        kk = P.sb([128, 8, TB])
        bc_ = P.sb([128, 8, TB])
        eb = P.sb([128, 8, TB])
        enb = P.sb([128, 8, TB])
        dec = P.sb([128, 8, 8])
        kst = [P.sb([128, D])] * 2
        vst = [P.sb([128, D])] * 2
        gst = [P.sb([128, D])] * 2
        psA = [P.ps([128, TB]) for _ in range(2)]
        psF = [P.ps([128, TB]) for _ in range(2)]
        psT = [P.ps([128, 2, 512]) for _ in range(2)]
        for tb in range(NTB):
            hb = hbs[tb % 2]
            P.dma(hb, xview(K.hT, tb))
            for dc in range(8):
                pq, pf = psA[dc % 2], psF[dc % 2]
                P.mm_group(pq, [(whg[:, kc, dc * 128:(dc + 1) * 128], hb[:, kc, :]) for kc in range(8)])
                P.mm_group(pf, [(whg[:, kc, 1024 + dc * 128:1024 + (dc + 1) * 128], hb[:, kc, :]) for kc in range(8)])
                P.act(q[:, dc, :], pq, AF.Silu)
                P.act(sg[:, dc, :], pf, AF.Sigmoid)
                P.ts(kk[:, dc, :], sg[:, dc, :], noml[:, dc:dc + 1], ALU.mult, oml[:, dc:dc + 1], ALU.add, eng="pool")
                P.ts(sg[:, dc, :], sg[:, dc, :], oml[:, dc:dc + 1], ALU.mult, lb[:, dc:dc + 1], ALU.add)
            P.ts(sg, sg, 1e-30, ALU.max)
            P.act(sg, sg, AF.Ln)
            for dc in range(8):
                P.scan(bc_[:, dc, :], K.rmask, sg[:, dc, :], 0.0, ALU.mult, ALU.add)
            P.act(eb, bc_, AF.Exp)
            P.act(enb, bc_, AF.Exp, scale=-1.0)
            P.tt(q, q, eb, ALU.mult)
            P.tt(kk, kk, enb, ALU.mult, eng="pool")
            P.copy(dec, eb.re("p c (n l) -> p c n l", l=LHG)[:, :, :, LHG - 1])
            P.tt(enb.re("p c (n l) -> p c n l", l=LHG), kk.re("p c (n l) -> p c n l", l=LHG),
                 dec.un(3).bc([128, 8, 8, LHG]), ALU.mult)
            P.dma(xview(K.hg_qT, tb), q)
            P.dma(xview(K.hg_kT, tb), kk)
            P.dma(K.hg_decT.v(tb * 8, tb * 8 + 8).re("(c p) n -> p c n", p=128), dec)
            for tt_ in range(4):
                t0 = tb * TB + tt_ * 128
                tsl = slice(tt_ * 128, (tt_ + 1) * 128)
                ks, vs, gs = kst[tt_ % 2], vst[tt_ % 2], gst[tt_ % 2]
                px = psT[0]
                for dc in range(8):
                    P.transpose(px[:, dc // 4, (dc % 4) * 128:(dc % 4) * 128 + 128], enb[:, dc, tsl], K.ident)
                P.copy(ks, px.re("p a b -> p (a b)"), eng="act")
                P.dma(tmview(K.hg_khat, t0, 128), ks)
                pv = psT[1]
                for qq in range(2):
                    c0 = 2048 + qq * 512
                    P.mm_group(pv[:, qq, :], [(hb[:, kc, tsl], whg[:, kc, c0:c0 + 512]) for kc in range(8)])
                P.copy(vs, pv.re("p a b -> p (a b)"), eng="dve")
                P.dma(tmview(K.hg_vtok, t0, 128), vs)
                pg = psT[0]
                for qq in range(2):
                    c0 = 3072 + qq * 512
                    P.mm_group(pg[:, qq, :], [(hb[:, kc, tsl], whg[:, kc, c0:c0 + 512]) for kc in range(8)])
                P.act(gs, pg.re("p a b -> p (a b)"), AF.Sigmoid)
                P.dma(tmview(K.hg_gtok, t0, 128), gs)


def stage_hg_rec(K, l):
    P = K.P
    NCH = T // LHG
    CG = 4
    with P.scope():
        hgnw = K.vec("hgnw%d" % l)
        S = P.sb([128, 8, 128])
        decall = P.sb([128, 8, NCH])
        qts = [P.sb([128, 8, CG * LHG]) for _ in range(2)]
        kts = [P.sb([128, 8, CG * LHG]) for _ in range(2)]
        vts = [P.sb([64, CG, D]) for _ in range(2)]
        khs = [P.sb([64, CG, D]) for _ in range(2)]
        gts = [P.sb([64, CG, D]) for _ in range(2)]
        at = P.sb([64, 8, 64])
        sq = P.sb([64, 8, 128])
        on = P.sb([64, 8, 128])
        sm = P.sb([64, 16])
        yst = [P.sb([128, 8, CG * LHG], BF16) for _ in range(2)]
        atp = P.ps([64, 8, 64])
        ops = P.ps([64, 2, 512])
        tp = P.ps([128, 8, 64])
        sps = P.ps([128, 2, 512])
        tri64 = K.tri[0:64, 0:64]
        id64 = K.ident[0:64, 0:64]
        for b in range(NB):
            T0 = b * T
            P.memset(S, 0.0)
            P.dma(decall, K.hg_decT.v(b * NCH, (b + 1) * NCH).re("(c p) n -> p c n", p=128))
            for cg in range(NCH // CG):
                t0 = T0 + cg * CG * LHG
                t1 = t0 + CG * LHG
                qt, kt, vt, kh, gt, ys = qts[cg % 2], kts[cg % 2], vts[cg % 2], khs[cg % 2], gts[cg % 2], yst[cg % 2]
                P.dma(qt, K.hg_qT.v(t0, t1).re("(c p) t -> p c t", p=128))
                P.dma(kt, K.hg_kT.v(t0, t1).re("(c p) t -> p c t", p=128))
                P.dma(vt, K.hg_vtok.v(t0, t1).re("(c t) e -> t c e", t=LHG))
                P.dma(kh, K.hg_khat.v(t0, t1).re("(c t) e -> t c e", t=LHG))
                P.dma(gt, K.hg_gtok.v(t0, t1).re("(c t) e -> t c e", t=LHG))
                for ci in range(CG):
                    c = cg * CG + ci
                    cs = slice(ci * LHG, (ci + 1) * LHG)
                    for h in range(8):
                        P.mm_group(atp[:, h, :], [(kt[:, h, cs], qt[:, h, cs])])
                    P.tt(at, atp, tri64.un(1).bc([64, 8, 64]), ALU.mult)
                    for h in range(8):
                        hs = slice(h * 128, (h + 1) * 128)
                        P.mm_group(ops[:, h // 4, (h % 4) * 128:(h % 4) * 128 + 128],
                                   [(at[:, h, :], vt[:, ci, hs]), (qt[:, h, cs], S[:, h, :])])
                    opf = ops.re("p a (h e) -> p (a h) e", e=128)
                    P.act(sq, opf, AF.Square)
                    P.reduce(sm[:, 0:8], sq, ALU.add)
                    P.act(sm[:, 8:16], sm[:, 0:8], AF.Sqrt, bias=K.epsb[0:64, 0:1], scale=1.0 / 128.0)
                    P.recip(sm[:, 8:16], sm[:, 8:16])
                    P.tt(on, opf, sm[:, 8:16].un(2).bc([64, 8, 128]), ALU.mult)
                    P.tt(on, on, gt[:, ci, :].re("p (h e) -> p h e", e=128), ALU.mult, eng="pool")
                    for h in range(8):
                        P.transpose(tp[:, h, :], on[:, h, :], id64)
                    P.tt(ys[:, :, cs], tp, hgnw.un(2).bc([128, 8, 64]), ALU.mult)
                    for h in range(8):
                        hs = slice(h * 128, (h + 1) * 128)
                        P.mm_group(sps[:, h // 4, (h % 4) * 128:(h % 4) * 128 + 128], [(kh[:, ci, hs], vt[:, ci, hs])])
                    P.tt(S, S, decall[:, :, c:c + 1].bc([128, 8, 128]), ALU.mult)
                    P.tt(S, S, sps.re("p a (h e) -> p (a h) e", e=128), ALU.add)
                P.dma(K.yT[2].v(t0, t1).re("(c p) t -> p c t", p=128), ys)


def stage_in_rw(K, l):
    P = K.P
    NZ = 3328
    with P.scope():
        wrw = P.sb([128, 8, NZ], BF16)
        load_w_cols(P, wrw, K.w_in[l], C_RW, C_RW + NZ)
        wa2 = P.sb([128, D])
        g2 = P.sb([128, D])
        P.dma(wa2, K.rw_wa2[l])
        P.dma(g2, K.rw_g2[l])
        if l == 1:
            wvr = P.sb([128, 8, 32], BF16)
            P.dma(wvr, K.w_vres, eng="pool")
            v2 = P.sb([32, D])
            P.dma(v2, K.rw_v2)
            v0 = K.vec("rw_v0")
            muv = K.vec("muvres")
        mu = K.vec("rwmu%d" % l)
        w0, a0 = K.vec("rw_w0%d" % l), K.vec("rw_a0%d" % l)
        k_k, k_a, r_k = K.vec("rw_k_k%d" % l), K.vec("rw_k_a%d" % l), K.vec("rw_r_k%d" % l)
        omka = P.sb([128, 8])
        P.ts(omka, k_a, -1.0, ALU.mult, 1.0, ALU.add)
        prev = P.sb([128, 28])
        hbs = [P.sb([128, 8, TB], BF16) for _ in range(2)]
        zl = P.sb([128, 3, TB + 1])
        dl = P.sb([128, 3, TB])
        twa = P.sb([128, TB])
        sgl = P.sb([128, TB])
        vl = P.sb([32, TB])
        z3 = P.sb([128, 3, TB + 1])
        d3 = P.sb([128, 3, TB])
        sh3 = P.sb([128, 3, TB])
        W_ = [P.sb([128, TB]) for _ in range(14)]
        gam = P.sb([128, 8])
        vfb = P.sb([128, TB])
        stg = [P.sb([128, 4, D], BF16) for _ in range(3)]
        ob = [P.sb([128, TB], BF16) for _ in range(7)]
        psA = [P.ps([128, TB]) for _ in range(2)]
        psL = [P.ps([128, TB]) for _ in range(3)]
        psT = [P.ps([128, TB]) for _ in range(2)]
        psX = [P.ps([128, 2 * TB], BF16) for _ in range(1)]
        for tb in range(NTB):
            hb = hbs[tb % 2]
            first = (tb % 4 == 0)
            P.dma(hb, xview(K.hT, tb))
            if first:
                P.memset(prev, 0.0)
            nlo = 3 if l == 1 else 2
            for i in range(nlo):
                ps = psA[i % 2]
                if i < 2:
                    P.mm_group(ps, [(wrw[:, kc, (24 + i) * 128:(25 + i) * 128], hb[:, kc, :]) for kc in range(8)])
                    P.copy(zl[:, i, 1:TB + 1], ps, eng="act")
                else:
                    P.mm_group(ps[0:32, :], [(wvr[:, kc, :], hb[:, kc, :]) for kc in range(8)])
                    P.copy(zl[0:32, i, 1:TB + 1], ps[0:32, :], eng="act")
                P.copy(zl[:, i, 0:1], prev[:, 24 + i:25 + i])
                P.copy(prev[:, 24 + i:25 + i], zl[:, i, TB:TB + 1])
            P.tt(dl[:, 0:nlo, :], zl[:, 0:nlo, 0:TB], zl[:, 0:nlo, 1:TB + 1], ALU.subtract)
            P.stt(dl[:, 0, :], dl[:, 0, :], mu[:, 24:25], zl[:, 0, 1:TB + 1], ALU.mult, ALU.add)
            P.stt(dl[:, 1, :], dl[:, 1, :], mu[:, 25:26], zl[:, 1, 1:TB + 1], ALU.mult, ALU.add)
            P.act(twa[0:64, :], dl[0:64, 0, :], AF.Tanh)
            P.copy(twa[64:128, :], dl[64:128, 0, :], eng="act")
            P.act(sgl, dl[:, 1, :], AF.Sigmoid)
            if l == 1:
                P.stt(vl, dl[0:32, 2, :], muv[0:32, 0:1], zl[0:32, 2, 1:TB + 1], ALU.mult, ALU.add)
            for j in range(8):
                js = slice(j * 128, (j + 1) * 128)
                (kk, sq, t1, kmod, bb, rk, bon, sgw, a_, cum, G, Gi, Gp, gg) = W_
                for i in range(3):
                    ch = i * 8 + j
                    ps = psA[i % 2]
                    P.mm_group(ps, [(wrw[:, kc, ch * 128:(ch + 1) * 128], hb[:, kc, :]) for kc in range(8)])
                    P.copy(z3[:, i, 1:TB + 1], ps, eng="act")
                    P.copy(z3[:, i, 0:1], prev[:, ch:ch + 1])
                    P.copy(prev[:, ch:ch + 1], z3[:, i, TB:TB + 1])
                P.tt(d3, z3[:, :, 0:TB], z3[:, :, 1:TB + 1], ALU.subtract)
                for i in range(3):
                    ch = i * 8 + j
                    P.stt(sh3[:, i, :], d3[:, i, :], mu[:, ch:ch + 1], z3[:, i, 1:TB + 1], ALU.mult, ALU.add)
                r, k, v = sh3[:, 0, :], sh3[:, 1, :], sh3[:, 2, :]
                pw, pa, pg = psL
                P.mm_group(pw, [(wa2[0:64, js], twa[0:64, :])])
                P.mm_group(pa, [(wa2[64:128, js], twa[64:128, :])])
                P.mm_group(pg, [(g2[:, js], sgl)])
                P.act(sgw, pw, AF.Sigmoid, bias=w0[:, j:j + 1])
                P.act(a_, pa, AF.Sigmoid, bias=a0[:, j:j + 1])
                P.copy(gg, pg, eng="act")
                P.dma(K.rw_gT.v(tb * TB, (tb + 1) * TB)[js, :], gg)
                if l == 1:
                    pv = psT[0]
                    P.mm_group(pv, [(v2[0:32, js], vl[0:32, :])])
                    P.act(t1, pv, AF.Sigmoid, bias=v0[:, j:j + 1])
                    P.dma(vfb, K.rw_vfT.v(tb * TB, (tb + 1) * TB)[js, :])
                    P.tt(vfb, vfb, v, ALU.subtract)
                    P.tt(vfb, vfb, t1, ALU.mult)
                    P.tt(v, v, vfb, ALU.add)
                else:
                    P.dma(K.rw_vfT.v(tb * TB, (tb + 1) * TB)[js, :], v)
                P.ts(kk, k, k_k[:, j:j + 1], ALU.mult, eng="pool")
                P.act(sq, kk, AF.Square)
                pn = psT[1]
                P.mm_group(pn, [(K.blk64, sq)])
                P.act(sq, pn, AF.Sqrt)
                P.ts(sq, sq, 1e-12, ALU.max)
                P.recip(sq, sq)
                P.tt(kk, kk, sq, ALU.mult)
                P.ts(t1, a_, k_a[:, j:j + 1], ALU.mult, omka[:, j:j + 1], ALU.add)
                P.tt(kmod, k, t1, ALU.mult, eng="pool")
                P.tt(bb, kk, a_, ALU.mult)
                P.stt(rk, r, r_k[:, j:j + 1], kmod, ALU.mult, ALU.mult)
                pb_ = psT[0]
                P.mm_group(pb_, [(K.blk64, rk)])
                P.tt(bon, pb_, v, ALU.mult)
                P.dma(K.rw_bonT.v(tb * TB, (tb + 1) * TB)[js, :], bon)
                P.scan(cum, K.rmask, sgw, 0.0, ALU.mult, ALU.add)
                P.act(G, cum, AF.Exp, scale=-WSC)
                P.act(Gi, cum, AF.Exp, scale=WSC)
                P.tt(t1, cum, sgw, ALU.subtract, eng="pool")
                P.act(Gp, t1, AF.Exp, scale=-WSC)
                kapb, rhob, betb, ktlb, vb, bhb, khb = ob
                P.tt(kapb, kk, Gp, ALU.mult)
                P.tt(rhob, r, G, ALU.mult, eng="pool")
                P.tt(bb, bb, Gi, ALU.mult)
                P.tt(kmod, kmod, Gi, ALU.mult, eng="pool")
                P.copy(betb, bb, eng="act")
                P.copy(ktlb, kmod, eng="act")
                P.copy(vb, v, eng="act")
                P.copy(gam, G.re("p (n l) -> p n l", l=LRW)[:, :, LRW - 1])
                gb = gam.un(2).bc([128, 8, LRW])
                P.tt(bhb.re("p (n l) -> p n l", l=LRW), bb.re("p (n l) -> p n l", l=LRW), gb, ALU.mult)
                P.tt(khb.re("p (n l) -> p n l", l=LRW), kmod.re("p (n l) -> p n l", l=LRW), gb, ALU.mult,
                     eng="pool")
                for (src, dst) in ((kapb, K.rw_kapT), (rhob, K.rw_rhoT), (betb, K.rw_betT), (ktlb, K.rw_ktlT)):
                    P.dma(dst.v(tb * TB, (tb + 1) * TB)[js, :], src)
                P.dma(K.rw_gamT.v(tb * 8, tb * 8 + 8)[js, :], gam)
                for xi, X in enumerate((vb, bhb, khb)):
                    px = psX[0]
                    for tt_ in range(4):
                        P.transpose(px[:, tt_ * 128:(tt_ + 1) * 128], X[:, tt_ * 128:(tt_ + 1) * 128], K.identb)
                    if xi == 1:
                        P.copy(stg[xi][:, :, js], px[:, 0:TB].re("p (a b) -> p a b", a=4), eng="act")
                    else:
                        P.copy(stg[xi][:, :, js], px[:, 0:TB].re("p (a b) -> p a b", a=4))
            for xi, dst in enumerate((K.rw_vtok, K.rw_bhat, K.rw_khat)):
                P.dma(dst.v(tb * TB, (tb + 1) * TB).re("(a t) e -> t a e", t=128), stg[xi])


def stage_rw_rec_old(K, l, post):
    P = K.P
    NCH = T // LRW
    CB = 8
    L = LRW
    with P.scope():
        mask12 = P.sb([64, 128])
        P.copy(mask12[:, 0:64], K.tris[0:64, 0:64])
        P.copy(mask12[:, 64:128], K.tri[0:64, 0:64])
        lows = K.lows[0:64, 0:64]
        id64 = K.ident[0:64, 0:64]
        KRs = [P.sb([64, 4, CB, 2, L]) for _ in range(2)]
        BTs = [P.sb([64, 4, CB * L]) for _ in range(2)]
        KTs = [P.sb([64, 4, CB * L]) for _ in range(2)]
        gms = [P.sb([64, 4, CB]) for _ in range(2)]
        vts = [P.sb([64, CB, 256]) for _ in range(2)]
        bhs = [P.sb([64, CB, 256]) for _ in range(2)]
        khs = [P.sb([64, CB, 256]) for _ in range(2)]
        ysts = [P.sb([64, CB, 256]) for _ in range(2)]
        S = P.sb([64, 4, L])
        M1 = P.sb([64, 4, 128])
        M2 = P.sb([64, 4, 128])
        Pm = [P.sb([64, 4, L]) for _ in range(2)]
        Qm = [P.sb([64, 4, L]) for _ in range(2)]
        TT = P.sb([64, 4, L])
        Rn = P.sb([64, 4, L])
        U = P.sb([64, 4, L])
        A1 = P.ps([64, 4, 128])
        A2 = P.ps([64, 4, 128])
        A3 = P.ps([64, 4, L])
        PP = P.ps([64, 4, L])
        QQ = P.ps([64, 4, L])
        TP = P.ps([64, 4, L])
        RU = P.ps([64, 2, 4, L])
        YS = P.ps([64, 2, 4, L])
        RP, UP = RU[:, 0], RU[:, 1]
        YP, SP = YS[:, 0], YS[:, 1]
        it = 0
        import os as _os
        lim = [2, 4, 4]
        VAR = _os.environ.get("RW_VAR", "")
        gmall = P.sb([64, 4, NCH])
        for b in range(lim[0]):
            T0 = b * T
            for hg in range(lim[1]):
                P.memset(S, 0.0)
                for cb in range(lim[2]):
                    t0 = T0 + cb * CB * L
                    t1 = t0 + CB * L
                    KR, BT, KT, gm = KRs[it % 2], BTs[it % 2], KTs[it % 2], gms[it % 2]
                    vt, bh, kh, yst = vts[it % 2], bhs[it % 2], khs[it % 2], ysts[it % 2]
                    it += 1
                    for hh in range(4):
                        ch = slice(hg * 256 + hh * 64, hg * 256 + hh * 64 + 64)
                        P.dma(KR[:, hh, :, 0, :], K.rw_kapT.v(t0, t1)[ch, :].re("p (c l) -> p c l", l=L))
                        P.dma(KR[:, hh, :, 1, :], K.rw_rhoT.v(t0, t1)[ch, :].re("p (c l) -> p c l", l=L))
                        P.dma(BT[:, hh, :], K.rw_betT.v(t0, t1)[ch, :])
                        P.dma(KT[:, hh, :], K.rw_ktlT.v(t0, t1)[ch, :])
                        g0 = b * NCH + cb * CB
                        P.dma(gm[:, hh, :], K.rw_gamT.v(g0, g0 + CB)[ch, :])
                        if "gmall" in VAR and cb == 0:
                            P.dma(gmall[:, hh, :], K.rw_gamT.v(b * NCH, (b + 1) * NCH)[ch, :])
                    es = slice(hg * 256, hg * 256 + 256)
                    P.dma(vt, K.rw_vtok.v(t0, t1)[:, es].re("(c t) e -> t c e", t=L))
                    P.dma(bh, K.rw_bhat.v(t0, t1)[:, es].re("(c t) e -> t c e", t=L))
                    P.dma(kh, K.rw_khat.v(t0, t1)[:, es].re("(c t) e -> t c e", t=L))
                    for ci in range(CB):
                        cs = slice(ci * L, (ci + 1) * L)
                        for hh in range(4):
                            krr = KR[:, hh, ci, :, :].re("p a l -> p (a l)")
                            P.mm_group(A1[:, hh, :], [(BT[:, hh, cs], krr)])
                            P.mm_group(A2[:, hh, :], [(KT[:, hh, cs], krr)])
                            P.mm_group(A3[:, hh, :], [(KR[:, hh, ci, 0, :], BT[:, hh, cs])])
                        P.tt(M1, A1, mask12.un(1).bc([64, 4, 128]), ALU.mult)
                        P.tt(M2, A2, mask12.un(1).bc([64, 4, 128]), ALU.mult)
                        P.tt(Pm[0], A3, lows.un(1).bc([64, 4, L]), ALU.mult)
                        Pc = Pm[0]
                        Qc = M1[:, :, 0:L]
                        ArbT = M1[:, :, L:2 * L]
                        AkkT = M2[:, :, 0:L]
                        ArkT = M2[:, :, L:2 * L]
                        P.tt(TT, id64.un(1).bc([64, 4, L]), Qc, ALU.subtract)
                        for lev in range(1, 6):
                            Pn = Pm[lev % 2]
                            Qn = Qm[lev % 2]
                            for hh in range(4):
                                P.mm_group(PP[:, hh, :], [(Qc[:, hh, :], Pc[:, hh, :])])
                            if lev < 5:
                                for hh in range(4):
                                    P.mm_group(QQ[:, hh, :], [(Pc[:, hh, :], Qc[:, hh, :])])
                            P.copy(Pn, PP)
                            if lev < 5:
                                P.copy(Qn, QQ, eng="act")
                            for hh in range(4):
                                P.mm_group(TP[:, hh, :], [(Pn[:, hh, :], TT[:, hh, :])])
                            P.tt(TT, TT, TP, ALU.add)
                            Pc, Qc = Pn, Qn
                        for hh in range(4):
                            vs = slice(hh * 64, hh * 64 + 64)
                            P.mm_group(RP[:, hh, :], [(KR[:, hh, ci, 0, :], S[:, hh, :]), (AkkT[:, hh, :], vt[:, ci, vs])])
                        P.ts(Rn, RP, -1.0, ALU.mult)
                        for hh in range(4):
                            P.mm_group(UP[:, hh, :], [(TT[:, hh, :], Rn[:, hh, :])])
                        P.copy(U, UP)
                        for hh in range(4):
                            vs = slice(hh * 64, hh * 64 + 64)
                            P.mm_group(YP[:, hh, :], [(KR[:, hh, ci, 1, :], S[:, hh, :]), (ArbT[:, hh, :], U[:, hh, :]),
                                                      (ArkT[:, hh, :], vt[:, ci, vs])])
                        if "reorder" not in VAR:
                            P.copy(yst[:, ci, :].re("p (h e) -> p h e", h=4), YP, eng="act")
                        for hh in range(4):
                            vs = slice(hh * 64, hh * 64 + 64)
                            P.mm_group(SP[:, hh, :], [(bh[:, ci, vs], U[:, hh, :]), (kh[:, ci, vs], vt[:, ci, vs])])
                        if "reorder" in VAR:
                            P.copy(yst[:, ci, :].re("p (h e) -> p h e", h=4), YP, eng="act")
                        if "gmall" in VAR:
                            cc = cb * CB + ci
                            P.tt(S, S, gmall[:, :, cc:cc + 1].bc([64, 4, L]), ALU.mult)
                        else:
                            P.tt(S, S, gm[:, :, ci:ci + 1].bc([64, 4, L]), ALU.mult)
                        P.tt(S, S, SP, ALU.add)
                    P.dma(K.rw_ytok.v(t0, t1)[:, es].re("(c t) e -> t c e", t=L), yst)


def lockstep(gens):
    gens = list(gens)
    while gens:
        for g in list(gens):
            try:
                next(g)
            except StopIteration:
                gens.remove(g)


def stage_rw_rec(K, l):
    P = K.P
    import os as _os
    if _os.environ.get("RW_OLD"):
        stage_rw_rec_old(K, l, None)
        return
    NCH = T // LRW
    CB = int(_os.environ.get("RW_CB", "4"))
    NBUF = int(_os.environ.get("RW_NBUF", "2"))
    L = LRW
    with P.scope():
        mask12 = P.sb([64, 128])
        P.copy(mask12[:, 0:64], K.tris[0:64, 0:64])
        P.copy(mask12[:, 64:128], K.tri[0:64, 0:64])
        lows = K.lows[0:64, 0:64]
        id64 = K.ident[0:64, 0:64]

        def stream(b):
            KRs = [P.sb([64, 4, CB, 2, L], BF16) for _ in range(NBUF)]
            BTs = [P.sb([64, 4, CB * L], BF16) for _ in range(NBUF)]
            KTs = [P.sb([64, 4, CB * L], BF16) for _ in range(NBUF)]
            gms = [None, None]
            gmall = P.sb([64, 4, NCH])
            vts = [P.sb([64, CB, 256], BF16) for _ in range(NBUF)]
            bhs = [P.sb([64, CB, 256], BF16) for _ in range(NBUF)]
            khs = [P.sb([64, CB, 256], BF16) for _ in range(NBUF)]
            ysts = [P.sb([64, CB, 256]) for _ in range(NBUF)]
            S = P.sb([64, 4, L])
            Sb = P.sb([64, 4, L], BF16)
            M1 = P.sb([64, 4, 128], BF16)
            M2 = P.sb([64, 4, 128], BF16)
            Pm = [P.sb([64, 4, L], BF16) for _ in range(2)]
            Qm = [P.sb([64, 4, L], BF16) for _ in range(2)]
            TT = P.sb([64, 4, L], BF16)
            Rn = P.sb([64, 4, L], BF16)
            U = P.sb([64, 4, L], BF16)
            import os as _os
            if _os.environ.get("RW_PS") == "old":
                if not hasattr(K, "_rwps"):
                    K._rwps = dict(A1=P.ps([64, 4, 128]), A2=P.ps([64, 4, 128]), A3=P.ps([64, 4, L]), PP=P.ps([64, 4, L]),
                                   QQ=P.ps([64, 4, L]), TP=P.ps([64, 4, L]), RU=P.ps([64, 2, 4, L]), YS=P.ps([64, 2, 4, L]))
                d_ = K._rwps
                A1, A2, A3, PP, QQ, TP = d_["A1"], d_["A2"], d_["A3"], d_["PP"], d_["QQ"], d_["TP"]
                RP, UP = d_["RU"][:, 0], d_["RU"][:, 1]
                YP, SP = d_["YS"][:, 0], d_["YS"][:, 1]
            else:
                B0 = P.ps([64, 4, 128])
                B1 = P.ps([64, 4, 128])
                B2 = P.ps([64, 2, 4, L])
                B3 = P.ps([64, 2, 4, L])
                A1, A2 = B0, B1
                A3, PP = B2[:, 0], B2[:, 1]
                QQ, TP = B3[:, 0], B3[:, 1]
                RP, UP = B0[:, :, 0:L], B0[:, :, L:2 * L]
                YP, SP = B1[:, :, 0:L], B1[:, :, L:2 * L]
            it = 0
            yield
            T0 = b * T
            for hg in range(4):
                P.memset(S, 0.0)
                P.memset(Sb, 0.0)
                for cb in range(NCH // CB):
                    t0 = T0 + cb * CB * L
                    t1 = t0 + CB * L
                    KR, BT, KT = KRs[it % NBUF], BTs[it % NBUF], KTs[it % NBUF]
                    vt, bh, kh, yst = vts[it % NBUF], bhs[it % NBUF], khs[it % NBUF], ysts[it % NBUF]
                    it += 1
                    for hh in range(4):
                        ch = slice(hg * 256 + hh * 64, hg * 256 + hh * 64 + 64)
                        P.dma(KR[:, hh, :, 0, :], K.rw_kapT.v(t0, t1)[ch, :].re("p (c l) -> p c l", l=L))
                        P.dma(KR[:, hh, :, 1, :], K.rw_rhoT.v(t0, t1)[ch, :].re("p (c l) -> p c l", l=L))
                        P.dma(BT[:, hh, :], K.rw_betT.v(t0, t1)[ch, :])
                        P.dma(KT[:, hh, :], K.rw_ktlT.v(t0, t1)[ch, :])
                        if cb == 0:
                            P.dma(gmall[:, hh, :], K.rw_gamT.v(b * NCH, (b + 1) * NCH)[ch, :])
                    es = slice(hg * 256, hg * 256 + 256)
                    P.dma(vt, K.rw_vtok.v(t0, t1)[:, es].re("(c t) e -> t c e", t=L))
                    P.dma(bh, K.rw_bhat.v(t0, t1)[:, es].re("(c t) e -> t c e", t=L))
                    P.dma(kh, K.rw_khat.v(t0, t1)[:, es].re("(c t) e -> t c e", t=L))
                    yield
                    for ci in range(CB):
                        cs = slice(ci * L, (ci + 1) * L)
                        for hh in range(4):
                            krr = KR[:, hh, ci, :, :].re("p a l -> p (a l)")
                            P.mm_group(A1[:, hh, :], [(BT[:, hh, cs], krr)])
                            P.mm_group(A2[:, hh, :], [(KT[:, hh, cs], krr)])
                            P.mm_group(A3[:, hh, :], [(KR[:, hh, ci, 0, :], BT[:, hh, cs])])
                        yield
                        P.tt(M1, A1, mask12.un(1).bc([64, 4, 128]), ALU.mult)
                        P.tt(Pm[0], A3, lows.un(1).bc([64, 4, L]), ALU.mult)
                        P.tt(M2, A2, mask12.un(1).bc([64, 4, 128]), ALU.mult, eng="pool") if False else P.tt(M2, A2, mask12.un(1).bc([64, 4, 128]), ALU.mult)
                        Pc = Pm[0]
                        Qc = M1[:, :, 0:L]
                        ArbT = M1[:, :, L:2 * L]
                        AkkT = M2[:, :, 0:L]
                        ArkT = M2[:, :, L:2 * L]
                        P.tt(TT, id64.un(1).bc([64, 4, L]), Qc, ALU.subtract)
                        yield
                        for lev in range(1, 6):
                            Pn = Pm[lev % 2]
                            Qn = Qm[lev % 2]
                            for hh in range(4):
                                P.mm_group(PP[:, hh, :], [(Qc[:, hh, :], Pc[:, hh, :])])
                            if lev < 5:
                                for hh in range(4):
                                    P.mm_group(QQ[:, hh, :], [(Pc[:, hh, :], Qc[:, hh, :])])
                            yield
                            P.copy(Pn, PP)
                            if lev < 5:
                                P.copy(Qn, QQ, eng="act")
                            yield
                            for hh in range(4):
                                P.mm_group(TP[:, hh, :], [(Pn[:, hh, :], TT[:, hh, :])])
                            yield
                            P.tt(TT, TT, TP, ALU.add)
                            yield
                            Pc, Qc = Pn, Qn
                        for hh in range(4):
                            vs = slice(hh * 64, hh * 64 + 64)
                            P.mm_group(RP[:, hh, :], [(KR[:, hh, ci, 0, :], Sb[:, hh, :]), (AkkT[:, hh, :], vt[:, ci, vs])])
                        yield
                        P.ts(Rn, RP, -1.0, ALU.mult)
                        yield
                        for hh in range(4):
                            P.mm_group(UP[:, hh, :], [(TT[:, hh, :], Rn[:, hh, :])])
                        yield
                        P.copy(U, UP)
                        yield
                        for hh in range(4):
                            vs = slice(hh * 64, hh * 64 + 64)
                            P.mm_group(YP[:, hh, :], [(KR[:, hh, ci, 1, :], Sb[:, hh, :]), (ArbT[:, hh, :], U[:, hh, :]),
                                                      (ArkT[:, hh, :], vt[:, ci, vs])])
                        yield
                        P.copy(yst[:, ci, :].re("p (h e) -> p h e", h=4), YP, eng="act")
                        yield
                        for hh in range(4):
                            vs = slice(hh * 64, hh * 64 + 64)
                            P.mm_group(SP[:, hh, :], [(bh[:, ci, vs], U[:, hh, :]), (kh[:, ci, vs], vt[:, ci, vs])])
                        yield
                        cc = cb * CB + ci
                        P.tt(S, S, gmall[:, :, cc:cc + 1].bc([64, 4, L]), ALU.mult)
                        P.tt(S, S, SP, ALU.add)
                        P.copy(Sb, S, eng="act")
                        yield
                    P.dma(K.rw_ytok.v(t0, t1)[:, es].re("(c t) e -> t c e", t=L), yst)
        import os as _os
        if _os.environ.get("RW_SEQ"):
            for b in range(NB):
                lockstep([stream(b)])
        else:
            lockstep([stream(b) for b in range(NB)])
    with P.scope():
        lnw, lnb = K.vec("rw_ln_w%d" % l), K.vec("rw_ln_b%d" % l)
        ys = [P.sb([128, 16, 64]) for _ in range(2)]
        cen = P.sb([128, 16, 64])
        sq = P.sb([128, 16, 64])
        sm = P.sb([128, 48])
        bons = [P.sb([128, 8, 128]) for _ in range(2)]
        ggs = [P.sb([128, 8, 128]) for _ in range(2)]
        tmp = P.sb([128, 8, 128])
        yo = [P.sb([128, 8, 128], BF16) for _ in range(2)]
        px = [P.ps([128, 4, 128]) for _ in range(2)]
        for tt_ in range(TOK // 128):
            t0 = tt_ * 128
            y, bon, gg, o = ys[tt_ % 2], bons[tt_ % 2], ggs[tt_ % 2], yo[tt_ % 2]
            P.dma(y, K.rw_ytok.v(t0, t0 + 128).re("t (h e) -> t h e", e=64))
            P.dma(bon, K.rw_bonT.v(t0, t0 + 128).re("(c p) t -> p c t", p=128))
            P.dma(gg, K.rw_gT.v(t0, t0 + 128).re("(c p) t -> p c t", p=128))
            P.reduce(sm[:, 0:16], y, ALU.add)
            P.ts(sm[:, 0:16], sm[:, 0:16], 1.0 / 64.0, ALU.mult)
            P.tt(cen, y, sm[:, 0:16].un(2).bc([128, 16, 64]), ALU.subtract)
            P.tt(sq, cen, cen, ALU.mult, eng="pool")
            P.reduce(sm[:, 16:32], sq, ALU.add)
            P.act(sm[:, 32:48], sm[:, 16:32], AF.Sqrt, bias=K.epsb[:, 1:2], scale=1.0 / 64.0)
            P.recip(sm[:, 32:48], sm[:, 32:48])
            P.tt(cen, cen, sm[:, 32:48].un(2).bc([128, 16, 64]), ALU.mult)
            cf = cen.re("p h e -> p (h e)")
            for j in range(8):
                P.transpose(px[j // 4][:, j % 4, :], cf[:, j * 128:(j + 1) * 128], K.ident)
            for j in range(8):
                P.ts(tmp[:, j, :], px[j // 4][:, j % 4, :], lnw[:, j:j + 1], ALU.mult, lnb[:, j:j + 1], ALU.add)
            P.tt(tmp, tmp, bon, ALU.add, eng="pool")
            P.tt(o, tmp, gg, ALU.mult)
            P.dma(K.yT[1].v(t0, t0 + 128).re("(c p) t -> p c t", p=128), o)


def stage_mix(K, l):
    P = K.P
    with P.scope():
        wgt = P.sb([128, 8, 3072], BF16)
        load_w_cols(P, wgt, K.w_in[l], C_GATE, C_GATE + 3072)
        wbr = P.sb([128, 3, 8, D], BF16)
        P.dma(wbr, K.w_branch[l], eng="pool")
        wo = P.sb([128, 8, D], BF16)
        P.dma(wo, K.w_out[l], eng="pool")
        hbs = [P.sb([128, 8, TB], BF16)] * 2
        yb = [P.sb([128, 8, TB], BF16) for _ in range(3)]
        xb = P.sb([128, 8, TB])
        xo = P.sb([128, 8, TB])
        mixb = P.sb([128, 8, TB], BF16)
        gts = [P.sb([128, TB]) for _ in range(2)]
        tmp = P.sb([128, TB])
        acc = P.sb([128, TB])
        psG = [P.ps([128, TB]) for _ in range(2)]
        psY = [P.ps([128, TB]) for _ in range(2)]
        psO = [P.ps([128, TB]) for _ in range(2)]
        for tb in range(NTB):
            b = tb // 4
            hb = hbs[tb % 2]
            P.dma(hb, xview(K.hT, tb))
            for br in range(3):
                P.dma(yb[br], xview(K.yT[br], tb))
            P.dma(xb, xview(K.xs, tb))
            i = 0
            for n in range(8):
                ns = slice(n * 128, (n + 1) * 128)
                for br in range(3):
                    pg, py, g = psG[i % 2], psY[i % 2], gts[i % 2]
                    i += 1
                    P.mm_group(pg, [(wgt[:, kc, br * 1024 + n * 128:br * 1024 + (n + 1) * 128], hb[:, kc, :]) for kc in range(8)])
                    P.mm_group(py, [(wbr[:, br, kc, ns], yb[br][:, kc, :]) for kc in range(8)])
                    P.act(g, pg, AF.Sigmoid)
                    if br == 0:
                        P.tt(acc, g, py, ALU.mult)
                    elif br == 1:
                        P.tt(tmp, g, py, ALU.mult)
                        P.tt(acc, acc, tmp, ALU.add, eng="pool")
                    else:
                        P.tt(tmp, g, py, ALU.mult)
                        P.tt(mixb[:, n, :], acc, tmp, ALU.add, eng="pool")
            for m in range(8):
                po = psO[m % 2]
                P.mm_group(po, [(wo[:, kc, m * 128:(m + 1) * 128], mixb[:, kc, :]) for kc in range(8)])
                P.stt(xo[:, m, :], po, K.gt[l][1][:, m, b:b + 1], xb[:, m, :], ALU.mult, ALU.add)
            P.dma(xview(K.xs, tb), xo)
def prep_core_inputs(inp, vecs, W, core):
    b0 = core * NB
    x = np.asarray(inp["x"], np.float32)[b0:b0 + NB].reshape(TOK, D)
    c = np.asarray(inp["c"], np.float32)[b0:b0 + NB]
    m = dict(W)
    m["xT"] = np.ascontiguousarray(x.T)
    m["cT"] = np.ascontiguousarray(c.reshape(NB, KC, 128).transpose(2, 1, 0))
    m["vecs"] = vecs
    return m


_CACHE = {}


def kernel(**inputs):
    from concourse.bass_utils import run_bass_kernel_spmd
    vp = pack_vecs(inputs)
    vecs = vp.build()
    W = host_weights(inputs)
    key = "main"
    if key not in _CACHE:
        _CACHE[key] = build_program(vp.off, vp.n)
    nc = _CACHE[key]
    in_maps = [prep_core_inputs(inputs, vecs, W, c) for c in range(8)]
    res = run_bass_kernel_spmd(nc, in_maps, core_ids=list(range(8)))
    out = np.empty((16, T, D), np.float32)
    for c in range(8):
        o = np.asarray(res.results[c]["outT"])
        out[c * NB:(c + 1) * NB] = o.T.reshape(NB, T, D)
    return out
```

```python
import contextlib
import numpy as np
import concourse.bass as bass
import concourse.mybir as mybir

F32 = mybir.dt.float32
BF16 = mybir.dt.bfloat16
ALU = mybir.AluOpType
AF = mybir.ActivationFunctionType
AX = mybir.AxisListType

ENGS = ("pe", "act", "dve", "pool", "sp")


class Tok:
    __slots__ = ("w", "r", "name")

    def __init__(self, name=""):
        self.w = None
        self.r = {}
        self.name = name


class V:
    __slots__ = ("ap", "toks")

    def __init__(self, ap, toks):
        self.ap = ap
        self.toks = toks

    def __getitem__(self, idx):
        return V(self.ap[idx], self.toks)

    def re(self, s, **kw):
        return V(self.ap.rearrange(s, **kw), self.toks)

    def bc(self, shape):
        return V(self.ap.broadcast_to(shape), self.toks)

    def un(self, axis):
        return V(self.ap.unsqueeze(axis), self.toks)

    def bitcast(self, dt):
        return V(self.ap.bitcast(dt), self.toks)

    @property
    def shape(self):
        return self.ap.shape


class Prog:
    def __init__(self, nc, es, n_dma_sems=40):
        self.nc = nc
        self.es = es
        self.q = {e: [] for e in ENGS}
        self.cnt = {e: 0 for e in ENGS}
        self.sem = {e: es.enter_context(nc.semaphore("s_" + e)) for e in ENGS}
        self.semobj = dict(self.sem)
        self.waited = {e: {} for e in ENGS}
        self.dsems = []
        self.n_ring = n_dma_sems
        for i in range(n_dma_sems):
            k = "d%d" % i
            self.semobj[k] = es.enter_context(nc.semaphore(k))
            self.dsems.append([k, 0, None])
        self.dnext = 0
        self.uid = 0
        self.psems = []
        for i in range(48):
            k = "q%d" % i
            self.semobj[k] = es.enter_context(nc.semaphore(k))
            self.psems.append(k)
        self.pnext = 0

    def sb(self, shape, dt=F32, name=None):
        self.uid += 1
        name = name or ("t%d" % self.uid)
        t = self.es.enter_context(self.nc.sbuf_tensor(name, list(shape), dt))
        return V(t[:], (Tok(name),))

    def ps(self, shape, dt=F32, name=None):
        self.uid += 1
        name = name or ("p%d" % self.uid)
        shape = list(shape)
        n = 1
        for d_ in shape[1:]:
            n *= d_
        nb = (n + 511) // 512
        t = self.es.enter_context(self.nc.psum_tensor(name, [shape[0], nb * 512], dt))
        ap = t[:][:, 0:n]
        if len(shape) == 3:
            ap = ap.rearrange("p (a b) -> p a b", a=shape[1])
        elif len(shape) == 4:
            ap = ap.rearrange("p (a b c) -> p a b c", a=shape[1], b=shape[2])
        return V(ap, (Tok(name),))

    def dram(self, name, shape, dt=F32, kind="Internal"):
        t = self.nc.dram_tensor(name, list(shape), dt, kind=kind)
        return V(t.ap(), (Tok(name),))

    def sub(self, v, name=""):
        return V(v.ap, (Tok(name),))

    def _needs(self, reads, writes):
        needs = {}

        def need(k, val):
            if val > needs.get(k, 0):
                needs[k] = val
        for v in reads:
            for t in v.toks:
                if t.w is not None:
                    need(*t.w)
        for v in writes:
            for t in v.toks:
                if t.w is not None:
                    need(*t.w)
                for k, val in t.r.items():
                    need(k, val)
        return needs

    def issue(self, eng, fn, reads, writes, inc=True, pe_group=False):
        needs = self._needs(reads, writes)
        waits = []
        wd = self.waited[eng]
        for k, val in needs.items():
            if k == eng and eng == "pe":
                continue
            if wd.get(k, 0) < val:
                wd[k] = val
                waits.append((k, val))
        n = self.cnt[eng] + 1
        if inc:
            self.cnt[eng] = n
        self.q[eng].append((waits, fn, inc))
        for v in reads:
            for t in v.toks:
                if t.r.get(eng, 0) < n:
                    t.r[eng] = n
        for v in writes:
            for t in v.toks:
                t.w = (eng, n)
                t.r = {}
        return n

    def dma(self, out, in_, eng="sp", **kw):
        if eng == "pool":
            slot = [self.psems[self.pnext], 0, None]
            self.pnext += 1
            self.dsems.append(slot)
        else:
            slot = self.dsems[self.dnext]
            self.dnext = (self.dnext + 1) % self.n_ring
        key, cur, _ = slot
        needs = self._needs([in_], [out])
        wd = self.waited[eng]
        waits = []
        if cur > 0:
            needs[key] = max(needs.get(key, 0), cur)
        for k, val in needs.items():
            if wd.get(k, 0) < val:
                wd[k] = val
                waits.append((k, val))
        newv = cur + 16
        slot[1] = newv
        oap, iap = out.ap, in_.ap

        def fn(e, oap=oap, iap=iap, kw=kw):
            return e.dma_start(out=oap, in_=iap, **kw)
        self.q[eng].append((waits, fn, ("dma", key)))
        for t in in_.toks:
            if t.r.get(key, 0) < newv:
                t.r[key] = newv
        for t in out.toks:
            t.w = (key, newv)
            t.r = {}

    def emit(self, final_waits=()):
        nc = self.nc
        engmap = {"pe": "tensor", "act": "scalar", "dve": "vector", "pool": "gpsimd", "sp": "sync"}
        with nc.Block() as block:
            for e in ENGS:
                lst = self.q[e]

                def body(eh, lst=lst, e=e):
                    for waits, fn, inc in lst:
                        for k, val in waits:
                            eh.wait_ge(self.semobj[k], val)
                        if fn is None:
                            continue
                        ins = fn(eh)
                        if inc is True:
                            ins.then_inc(self.semobj[e], 1)
                        elif inc:
                            ins.then_inc(self.semobj[inc[1]], 16)
                    if e == "sp":
                        for k, val in final_waits:
                            eh.wait_ge(self.semobj[k], val)
                getattr(block, engmap[e])(body)

    def mm(self, out, lhsT, rhs, start=True, stop=True):
        o, l, r = out.ap, lhsT.ap, rhs.ap

        def fn(e):
            return e.matmul(o, l, r, start=start, stop=stop)
        if stop:
            self.issue("pe", fn, [lhsT, rhs], [out], inc=True)
        else:
            self.issue("pe", fn, [lhsT, rhs], [], inc=False)
        return

    def mm_group(self, out, pairs):
        n = len(pairs)
        for i, (l, r) in enumerate(pairs):
            o, la, ra = out.ap, l.ap, r.ap
            st, sp_ = (i == 0), (i == n - 1)

            def fn(e, o=o, la=la, ra=ra, st=st, sp_=sp_):
                return e.matmul(o, la, ra, start=st, stop=sp_)
            if sp_ and n == 1:
                self.issue("pe", fn, [l, r], [out], inc=True)
            elif st:
                needs_v = V(out.ap, out.toks)
                self.issue_pe_first(fn, [l, r], needs_v)
            elif sp_:
                self.issue("pe", fn, [l, r], [out], inc=True)
            else:
                self.issue("pe", fn, [l, r], [], inc=False)

    def issue_pe_first(self, fn, reads, outv):
        eng = "pe"
        needs = self._needs(reads, [outv])
        waits = []
        wd = self.waited[eng]
        for k, val in needs.items():
            if k == eng:
                continue
            if wd.get(k, 0) < val:
                wd[k] = val
                waits.append((k, val))
        n = self.cnt[eng] + 1
        self.q[eng].append((waits, fn, False))
        for v in reads:
            for t in v.toks:
                if t.r.get(eng, 0) < n:
                    t.r[eng] = n

    def transpose(self, out, in_, ident):
        o, i, d = out.ap, in_.ap, ident.ap

        def fn(e):
            return e.transpose(o, i, d)
        self.issue("pe", fn, [in_, ident], [out])

    def act(self, out, in_, func, bias=None, scale=None, accum=None, eng="act"):
        o, i = out.ap, in_.ap
        reads = [in_]
        kw = {}
        if bias is not None:
            if isinstance(bias, V):
                reads.append(bias)
                kw["bias"] = bias.ap
            else:
                kw["bias"] = bias
        if scale is not None:
            if isinstance(scale, V):
                reads.append(scale)
                kw["scale"] = scale.ap
            else:
                kw["scale"] = scale
        writes = [out]
        if accum is not None:
            kw["accum_out"] = accum.ap
            writes.append(accum)

        def fn(e):
            return e.activation(o, i, func, **kw)
        self.issue("act", fn, reads, writes)

    def tt(self, out, a, b, op, eng="dve"):
        o, x, y = out.ap, a.ap, b.ap

        def fn(e):
            return e.tensor_tensor(o, x, y, op)
        self.issue(eng, fn, [a, b], [out])

    def ts(self, out, a, s1, op0, s2=None, op1=None, eng="dve", accum=None):
        o, x = out.ap, a.ap
        reads = [a]

        def cv(s):
            if isinstance(s, V):
                reads.append(s)
                return s.ap
            return s
        s1a = cv(s1)
        s2a = cv(s2)
        writes = [out]
        kw = {}
        if accum is not None:
            kw["accum_out"] = accum.ap
            writes.append(accum)

        def fn(e):
            if op1 is None:
                return e.tensor_scalar(o, x, s1a, None, op0, **kw)
            return e.tensor_scalar(o, x, s1a, s2a, op0, op1, **kw)
        self.issue(eng, fn, reads, writes)

    def stt(self, out, a, s, b, op0, op1, eng="dve"):
        o, x, y = out.ap, a.ap, b.ap
        reads = [a, b]
        if isinstance(s, V):
            reads.append(s)
            sa = s.ap
        else:
            sa = s

        def fn(e):
            return e.scalar_tensor_tensor(o, x, sa, y, op0, op1)
        self.issue(eng, fn, reads, [out])

    def copy(self, out, in_, eng="dve"):
        o, i = out.ap, in_.ap
        if eng == "act":
            def fn(e):
                return e.copy(o, i)
        else:
            def fn(e):
                return e.tensor_copy(o, i)
        self.issue(eng, fn, [in_], [out])

    def memset(self, out, val, eng="dve"):
        o = out.ap

        def fn(e):
            return e.memset(o, val)
        self.issue(eng, fn, [], [out])

    def recip(self, out, in_, eng="dve"):
        o, i = out.ap, in_.ap

        def fn(e):
            return e.reciprocal(o, i)
        self.issue(eng, fn, [in_], [out])

    def scan(self, out, d0, d1, init, op0, op1):
        o, a, b = out.ap, d0.ap, d1.ap
        reads = [d0, d1]
        if isinstance(init, V):
            reads.append(init)
            ia = init.ap
        else:
            ia = init

        def fn(e):
            return e.tensor_tensor_scan(o, a, b, ia, op0, op1)
        self.issue("dve", fn, reads, [out])

    def reduce(self, out, in_, op, axis=AX.X, eng="dve"):
        o, i = out.ap, in_.ap

        def fn(e):
            return e.tensor_reduce(o, i, axis, op)
        self.issue(eng, fn, [in_], [out])


def _prog_barrier(self):
    targets = {e: self.cnt[e] for e in ENGS if self.cnt[e] > 0}
    for s in self.dsems:
        if s[1] > 0:
            targets[s[0]] = s[1]
    for e in ENGS:
        wd = self.waited[e]
        waits = []
        for k, val in targets.items():
            if wd.get(k, 0) < val:
                wd[k] = val
                waits.append((k, val))
        if waits:
            self.q[e].append((waits, None, False))


@contextlib.contextmanager
def _prog_scope(self):
    old = self.es
    with contextlib.ExitStack() as es2:
        self.es = es2
        try:
            yield
        finally:
            self.barrier()
            self.es = old


Prog.barrier = _prog_barrier
Prog.scope = _prog_scope
D = 1024; T = 2048; NB = 2; TOK = NB * T; TB = 512; NTB = TOK // TB; KC = 8
DFF = 2816; NJ = 22; NIN = 12552
C_MLX, C_MLO, C_MLG = 0, 1024, 2048
C_RW = 2056; C_HG = 5384; C_GATE = 9480
EPS = 1e-6; RW_LN_EPS = 64e-5
LRW = 64; LHG = 64; LML = 128
WSC = 0.6065306597126334


def fm(v):
    v = np.asarray(v, np.float32).reshape(-1, 128)
    return np.ascontiguousarray(v.T)


def kxn(W):
    K, N = W.shape
    return np.ascontiguousarray(W.reshape(K // 128, 128, N).transpose(1, 0, 2))


class VecPack:
    def __init__(self):
        self.cols = []
        self.off = {}
        self.n = 0

    def add(self, name, arr):
        arr = np.asarray(arr, np.float32)
        assert arr.shape[0] == 128
        self.off[name] = (self.n, arr.shape[1])
        self.cols.append(arr)
        self.n += arr.shape[1]

    def build(self):
        return np.ascontiguousarray(np.concatenate(self.cols, axis=1))


def pack_vecs(inp):
    vp = VecPack()
    rep = lambda v: np.tile(np.asarray(v, np.float32)[None, :], (128, 1))
    for l in range(2):
        for s in range(3):
            vp.add("normw%d%d" % (l, s), fm(inp["norm_w"][l, s]))
        vp.add("adab%d" % l, fm(inp["ada_b"][l]))
        for tp in range(4):
            vp.add("convw%d%d" % (l, tp), fm(inp["ml_conv_w"][l, tp]))
        vp.add("convb%d" % l, fm(inp["ml_conv_b"][l]))
        vp.add("mlnw%d" % l, fm(inp["ml_norm_w"][l]))
        vp.add("mlskip%d" % l, fm(inp["ml_skip"][l]))
        vp.add("ib%d" % l, rep(inp["ml_i_b"][l]))
        vp.add("fb%d" % l, rep(inp["ml_f_b"][l]))
        vp.add("rwmu%d" % l, fm(inp["rw_mu"][l]))
        for nm in ("rw_w0", "rw_a0", "rw_k_k", "rw_k_a", "rw_ln_w", "rw_ln_b"):
            vp.add(nm + str(l), fm(inp[nm][l]))
        vp.add("rw_r_k%d" % l, fm(inp["rw_r_k"][l].reshape(-1)))
        vp.add("hglb%d" % l, fm(inp["hg_lb_logits"][l]))
        vp.add("hgnw%d" % l, fm(inp["hg_norm_w"][l]))
    vp.add("finw", fm(inp["final_norm_w"]))
    vp.add("rw_v0", fm(inp["rw_v0"][0]))
    mv = np.zeros((128, 1), np.float32)
    mv[:32, 0] = inp["rw_mu_vres"][0]
    vp.add("muvres", mv)
    return vp


def host_weights(inp):
    w = {}
    w["ada_w"] = np.ascontiguousarray(
        np.asarray(inp["ada_w"], np.float32).reshape(2, 8, 128, 18, 512).transpose(0, 3, 2, 1, 4))
    w["ffn_up"] = np.ascontiguousarray(
        np.asarray(inp["ffn_up"], np.float32).reshape(2, 2, 8, 128, 2 * DFF).transpose(0, 1, 3, 2, 4))
    w["ffn_down"] = np.ascontiguousarray(
        np.asarray(inp["ffn_down"], np.float32).reshape(2, 2, NJ, 128, D).transpose(0, 1, 3, 2, 4))
    w["w_in"] = np.ascontiguousarray(
        np.asarray(inp["w_in"], np.float32).reshape(2, 8, 128, NIN).transpose(0, 2, 1, 3))
    w["w_vres"] = np.ascontiguousarray(
        np.asarray(inp["w_in_vres"], np.float32).reshape(8, 128, 32).transpose(1, 0, 2))
    w["wq"] = np.ascontiguousarray(
        np.asarray(inp["ml_wq"], np.float32).reshape(2, 4, 2, 128, 256).transpose(0, 3, 1, 2, 4))
    w["wk"] = np.ascontiguousarray(
        np.asarray(inp["ml_wk"], np.float32).reshape(2, 4, 2, 128, 256).transpose(0, 3, 1, 2, 4))
    w["rw_wa2"] = np.ascontiguousarray(np.concatenate(
        [np.asarray(inp["rw_w2"], np.float32), np.asarray(inp["rw_a2"], np.float32)], axis=1))
    w["rw_g2"] = np.ascontiguousarray(np.asarray(inp["rw_g2"], np.float32))
    w["rw_v2"] = np.ascontiguousarray(np.asarray(inp["rw_v2"], np.float32)[0])
    w["w_branch"] = np.ascontiguousarray(
        np.asarray(inp["w_branch"], np.float32).reshape(2, 3, 8, 128, D).transpose(0, 3, 1, 2, 4))
    w["w_out"] = np.ascontiguousarray(
        np.asarray(inp["w_out"], np.float32).reshape(2, 8, 128, D).transpose(0, 2, 1, 3))
    return w


class DT:
    def __init__(self, P, name, shape, dt, tok_axis, kind="Internal", blk=TB):
        self.t = P.nc.dram_tensor(name, list(shape), dt, kind=kind)
        self.ap = self.t.ap()
        self.tok_axis = tok_axis
        self.blk = blk
        n = shape[tok_axis] // blk
        self.toks = [Tok("%s_%d" % (name, i)) for i in range(max(n, 1))]

    def v(self, t0, t1):
        i0, i1 = t0 // self.blk, (t1 - 1) // self.blk
        toks = tuple(self.toks[i0:i1 + 1])
        if self.tok_axis == 0:
            return V(self.ap[t0:t1], toks)
        return V(self.ap[:, t0:t1], toks)

    def all(self):
        return V(self.ap, tuple(self.toks))
class Ctx:
    pass


STAGE_LOG = []


def build_program(vec_off, nvec, stop_after=None, debug_outs=(), only=None):
    nc = bass.Bass("TRN2", target_bir_lowering=False)
    es = contextlib.ExitStack()
    with es:
        P = Prog(nc, es)
        K = Ctx()
        K.P = P
        K.debug = {}

        def din(name, shape, dt=F32):
            return P.dram(name, shape, dt, kind="ExternalInput")
        K.xT_in = din("xT", [D, TOK])
        K.cT = din("cT", [128, KC, NB])
        K.vecs_d = din("vecs", [128, nvec])
        K.ada_w = din("ada_w", [2, 18, 128, 8, 512])
        K.ffn_up = din("ffn_up", [2, 2, 128, 8, 2 * DFF])
        K.ffn_down = din("ffn_down", [2, 2, 128, NJ, D])
        K.w_in = din("w_in", [2, 128, 8, NIN])
        K.w_vres = din("w_vres", [128, 8, 32])
        K.wq = din("wq", [2, 128, 4, 2, 256])
        K.wk = din("wk", [2, 128, 4, 2, 256])
        K.rw_wa2 = din("rw_wa2", [2, 128, D])
        K.rw_g2 = din("rw_g2", [2, 128, D])
        K.rw_v2 = din("rw_v2", [32, D])
        K.w_branch = din("w_branch", [2, 128, 3, 8, D])
        K.w_out = din("w_out", [2, 128, 8, D])
        K.outT = DT(P, "outT", [D, TOK], F32, 1, kind="ExternalOutput")

        def scr(name, shape, dt, tok_axis):
            kind = "ExternalOutput" if name in debug_outs else "Internal"
            return DT(P, name, shape, dt, tok_axis, kind=kind)
        K.xs = scr("xs", [D, TOK], F32, 1)
        K.act = scr("actT", [DFF, TOK], BF16, 1)
        K.hT = scr("hT", [D, TOK], BF16, 1)
        K.yT = [scr("yT%d" % i, [D, TOK], BF16, 1) for i in range(3)]
        K.ml_qT = scr("ml_qT", [D, TOK], F32, 1)
        K.ml_kT = scr("ml_kT", [D, TOK], F32, 1)
        K.ml_skx = scr("ml_skx", [D, TOK], F32, 1)
        K.ml_ktok = scr("ml_ktok", [TOK, D], F32, 0)
        K.ml_vo = scr("ml_vo", [TOK, 2056], F32, 0)
        K.hg_qT = scr("hg_qT", [D, TOK], F32, 1)
        K.hg_kT = scr("hg_kT", [D, TOK], F32, 1)
        K.hg_dec = scr("hg_dec", [D, TOK // LHG], F32, 1) if False else None
        K.hg_decT = DT(P, "hg_decT", [D, TOK // LHG], F32, 1, blk=TB // LHG)
        K.hg_vtok = scr("hg_vtok", [TOK, D], F32, 0)
        K.hg_khat = scr("hg_khat", [TOK, D], F32, 0)
        K.hg_gtok = scr("hg_gtok", [TOK, D], F32, 0)
        for nm in ("rw_kapT", "rw_rhoT", "rw_betT", "rw_ktlT"):
            setattr(K, nm, scr(nm, [D, TOK], BF16, 1))
        for nm in ("rw_bonT", "rw_gT", "rw_vfT"):
            setattr(K, nm, scr(nm, [D, TOK], F32, 1))
        K.rw_gamT = DT(P, "rw_gamT", [D, TOK // LRW], F32, 1, blk=TB // LRW)
        for nm in ("rw_vtok", "rw_bhat", "rw_khat"):
            setattr(K, nm, scr(nm, [TOK, D], BF16, 0))
        K.rw_ytok = scr("rw_ytok", [TOK, D], F32, 0)

        K.vecs = P.sb([128, nvec], F32, "vecs_sb")
        P.dma(K.vecs, K.vecs_d)

        def vec(name):
            o, n = vec_off[name]
            return K.vecs[:, o:o + n]
        K.vec = vec
        K.epsb = P.sb([128, 4], F32, "epsb")
        P.memset(K.epsb[:, 0:1], EPS)
        P.memset(K.epsb[:, 1:2], RW_LN_EPS)
        P.memset(K.epsb[:, 2:3], 1.0)
        P.memset(K.epsb[:, 3:4], 0.0)
        K.ones = P.sb([128, 128], F32, "ones")
        P.memset(K.ones, 1.0)
        K.ident = P.sb([128, 128], F32, "ident")
        P.memset(K.ident, 0.0)
        io = P.sb([128, 128], F32, "iota_f")
        ip = P.sb([128, 1], F32, "iota_p")
        io_i = P.sb([128, 128], mybir.dt.int32, "iota_fi")
        ip_i = P.sb([128, 1], mybir.dt.int32, "iota_pi")

        def iota(out, pattern, cm):
            o = out.ap

            def fn(e):
                return e.iota(o, pattern, base=0, channel_multiplier=cm)
            P.issue("pool", fn, [], [out])
        iota(io_i, [[1, 128]], 0)
        iota(ip_i, [[0, 1]], 1)
        P.copy(io, io_i)
        P.copy(ip, ip_i)
        K.iota_f = io
        K.iota_p = ip
        P.ts(K.ident, io, ip[:, 0:1], ALU.is_equal)
        K.identb = P.sb([128, 128], BF16, "identb")
        P.copy(K.identb, K.ident)
        K.tri = P.sb([128, 128], F32, "tri")
        P.ts(K.tri, io, ip[:, 0:1], ALU.is_ge)
        K.tris = P.sb([128, 128], F32, "tris")
        P.ts(K.tris, io, ip[:, 0:1], ALU.is_gt)
        K.lows = P.sb([128, 128], F32, "lows")
        P.ts(K.lows, io, ip[:, 0:1], ALU.is_lt)
        K.blk64 = P.sb([128, 128], F32, "blk64")
        P.memset(K.blk64, 0.0)
        P.memset(K.blk64[0:64, 0:64], 1.0)
        P.memset(K.blk64[64:128, 64:128], 1.0)
        K.rmask = P.sb([128, TB], F32, "rmask")
        P.memset(K.rmask, 1.0)
        P.memset(K.rmask.re("p (c l) -> p c l", l=64)[:, :, 0:1], 0.0)

        K.modT = [P.sb([128, 72, NB], F32, "modT%d" % l) for l in range(2)]
        K.sc1 = [[P.sb([128, 8, NB], F32) for s in range(3)] for l in range(2)]
        K.gt = [[P.sb([128, 8, NB], F32) for s in range(3)] for l in range(2)]
        stage_mod(K)
        stages = [
            ("copyx", lambda: stage_copyx(K)),
        ]
        for l in range(2):
            stages += [
                ("ffn%d0" % l, lambda l=l: stage_ffn(K, l, 0)),
                ("normmix%d" % l, lambda l=l: stage_normmix(K, l)),
                ("inml%d" % l, lambda l=l: stage_in_ml(K, l)),
                ("mlrec%d" % l, lambda l=l: stage_ml_rec(K, l)),
                ("inhg%d" % l, lambda l=l: stage_in_hg(K, l)),
                ("hgrec%d" % l, lambda l=l: stage_hg_rec(K, l)),
                ("inrw%d" % l, lambda l=l: stage_in_rw(K, l)),
                ("rwrec%d" % l, lambda l=l: stage_rw_rec(K, l)),
                ("mix%d" % l, lambda l=l: stage_mix(K, l)),
                ("ffn%d1" % l, lambda l=l: stage_ffn(K, l, 1)),
            ]
        stages.append(("final", lambda: stage_final(K)))
        for name, fn in stages:
            if only is not None and name not in only:
                continue
            fn()
            STAGE_LOG.append((name, dict(P.cnt)))
            if stop_after == name:
                break
        P.barrier()
        fin = [(k, v) for k, v, _ in P.dsems if v > 0]
        P.emit(final_waits=fin)
    return nc


def stage_mod(K):
    P = K.P
    with P.scope():
        cT = P.sb([128, KC, NB])
        P.dma(cT, K.cT)
        cond = P.sb([128, KC, NB])
        P.act(cond, cT, AF.Silu)
        wb = [P.sb([128, 8, 512]) for _ in range(2)]
        for l in range(2):
            ps = P.ps([128, 72, NB])
            for pc in range(18):
                wt = wb[pc % 2]
                P.dma(wt, K.ada_w[l, pc])
                for jj in range(4):
                    j = pc * 4 + jj
                    P.mm_group(ps[:, j, :], [(wt[:, kc, jj * 128:(jj + 1) * 128], cond[:, kc, :]) for kc in range(KC)])
            P.tt(K.modT[l], ps, K.vec("adab%d" % l).un(2).bc([128, 72, NB]), ALU.add)
            for s in range(3):
                m = K.modT[l]
                nw = K.vec("normw%d%d" % (l, s))
                P.ts(K.sc1[l][s], m[:, s * 24 + 8:s * 24 + 16, :], 1.0, ALU.add)
                P.tt(K.sc1[l][s], K.sc1[l][s], nw.un(2).bc([128, 8, NB]), ALU.mult)
                f = 1.0 if s == 1 else 0.5
                P.ts(K.gt[l][s], m[:, s * 24 + 16:s * 24 + 24, :], 1.0, ALU.add, f, ALU.mult)


def stage_copyx(K):
    P = K.P
    for tb in range(NTB):
        P.dma(K.xs.v(tb * TB, (tb + 1) * TB), K.xT_in[:, tb * TB:(tb + 1) * TB])


def norm_block(K, xb, hb, l, s, b, sq, ssp, sd, rstd, hn, plain_w=None, out_f32=None):
    P = K.P
    P.act(sq, xb, AF.Square)
    P.mm_group(ssp, [(K.ones, sq[:, dc, :]) for dc in range(8)])
    P.act(sd, ssp, AF.Sqrt, bias=K.epsb[:, 0:1], scale=1.0 / D)
    P.recip(rstd, sd)
    P.tt(hn, xb, rstd.un(1).bc([128, 8, TB]), ALU.mult)
    if plain_w is not None:
        P.tt(out_f32, hn, plain_w.un(2).bc([128, 8, TB]), ALU.mult)
        return
    sh = K.modT[l][:, s * 24:s * 24 + 8, :]
    for dc in range(8):
        P.ts(hb[:, dc, :], hn[:, dc, :], K.sc1[l][s][:, dc, b:b + 1], ALU.mult, sh[:, dc, b:b + 1], ALU.add)


def alloc_norm_tmps(K):
    P = K.P
    if not hasattr(K, "epsb"):
        pass
    sq = P.sb([128, 8, TB])
    sd = P.sb([128, TB])
    return dict(sq=sq, ssp=P.ps([128, TB]), sd=sd, rstd=sd, hn=sq)


def xview(dt_, tb):
    return dt_.v(tb * TB, (tb + 1) * TB).re("(c p) t -> p c t", p=128)


def stage_ffn(K, l, f):
    P = K.P
    s = 0 if f == 0 else 2
    with P.scope():
        wup = P.sb([128, 8, 2 * DFF], BF16)
        P.dma(wup.re("p k (a n) -> p k a n", a=4), K.ffn_up[l, f].re("p k (a n) -> p k a n", a=4), eng="pool")
        tm = alloc_norm_tmps(K)
        xbs = [P.sb([128, 8, TB]) for _ in range(2)]
        hbs = [P.sb([128, 8, TB], BF16)] * 2
        acts = [P.sb([128, NJ, TB], BF16) for _ in range(2)]
        sgs = [P.sb([128, TB]) for _ in range(2)]
        pas = [P.ps([128, TB]) for _ in range(2)]
        pbs = [P.ps([128, TB]) for _ in range(2)]
        for tb in range(NTB):
            b = tb // 4
            xb, hb, ab = xbs[tb % 2], hbs[tb % 2], acts[tb % 2]
            P.dma(xb, xview(K.xs, tb))
            norm_block(K, xb, hb, l, s, b, **tm)
            for j in range(NJ):
                pa, pb, sg = pas[j % 2], pbs[j % 2], sgs[j % 2]
                P.mm_group(pa, [(wup[:, kc, j * 128:(j + 1) * 128], hb[:, kc, :]) for kc in range(8)])
                P.mm_group(pb, [(wup[:, kc, DFF + j * 128:DFF + (j + 1) * 128], hb[:, kc, :]) for kc in range(8)])
                P.act(sg, pa, AF.Silu)
                P.tt(ab[:, j, :], sg, pb, ALU.mult)
            P.dma(xview(K.act, tb), ab)
    with P.scope():
        wdn = P.sb([128, NJ, D], BF16)
        P.dma(wdn, K.ffn_down[l, f], eng="pool")
        xbs = [P.sb([128, 8, TB]) for _ in range(2)]
        xos = [P.sb([128, 8, TB]) for _ in range(2)]
        abs_ = [P.sb([128, NJ, TB], BF16) for _ in range(2)]
        pos = [P.ps([128, TB]) for _ in range(2)]
        for tb in range(NTB):
            b = tb // 4
            xb, xo, ab = xbs[tb % 2], xos[tb % 2], abs_[tb % 2]
            P.dma(ab, xview(K.act, tb))
            P.dma(xb, xview(K.xs, tb))
            for m in range(8):
                po = pos[m % 2]
                P.mm_group(po, [(wdn[:, kc, m * 128:(m + 1) * 128], ab[:, kc, :]) for kc in range(NJ)])
                P.stt(xo[:, m, :], po, K.gt[l][s][:, m, b:b + 1], xb[:, m, :], ALU.mult, ALU.add)
            P.dma(xview(K.xs, tb), xo)


def stage_final(K):
    P = K.P
    with P.scope():
        tm = alloc_norm_tmps(K)
        xbs = [P.sb([128, 8, TB]) for _ in range(2)]
        obs = [P.sb([128, 8, TB]) for _ in range(2)]
        for tb in range(NTB):
            xb, ob = xbs[tb % 2], obs[tb % 2]
            P.dma(xb, xview(K.xs, tb))
            norm_block(K, xb, None, 0, 0, 0, plain_w=K.vec("finw"), out_f32=ob, **tm)
            P.dma(xview(K.outT, tb), ob)


def stage_normmix(K, l):
    P = K.P
    with P.scope():
        tm = alloc_norm_tmps(K)
        xbs = [P.sb([128, 8, TB]) for _ in range(2)]
        hbs = [P.sb([128, 8, TB], BF16) for _ in range(2)]
        for tb in range(NTB):
            xb, hb = xbs[tb % 2], hbs[tb % 2]
            P.dma(xb, xview(K.xs, tb))
            norm_block(K, xb, hb, l, 1, tb // 4, **tm)
            P.dma(xview(K.hT, tb), hb)
def load_w_cols(P, dst, src, c0, c1, eng="pool"):
    n = c1 - c0
    h = n // 2
    P.dma(dst[:, :, 0:h], src[:, :, c0:c0 + h], eng=eng)
    P.dma(dst[:, :, h:n], src[:, :, c0 + h:c1], eng=eng)


def tmview(dt_, t0, n):
    return dt_.v(t0, t0 + n)


def stage_in_ml(K, l):
    P = K.P
    with P.scope():
        wml = P.sb([128, 8, 2056], BF16)
        load_w_cols(P, wml, K.w_in[l], 0, 2056)
        wq = P.sb([128, 4, 2, 256], BF16)
        wk = P.sb([128, 4, 2, 256], BF16)
        P.dma(wq, K.wq[l], eng="pool")
        P.dma(wk, K.wk[l], eng="pool")
        cw = [K.vec("convw%d%d" % (l, tp)) for tp in range(4)]
        cb = K.vec("convb%d" % l)
        skip = K.vec("mlskip%d" % l)
        hbs = [P.sb([128, 8, TB], BF16) for _ in range(2)]
        xms = [P.sb([128, 8, TB + 3]) for _ in range(2)]
        acc = P.sb([128, 8, TB])
        xc = P.sb([128, 8, TB])
        xcb = P.sb([128, 8, TB], BF16)
        qst = P.sb([128, 8, TB])
        kst = P.sb([128, 8, TB])
        ktk = [P.sb([128, D]) for _ in range(2)]
        vos = [P.sb([128, 2056]) for _ in range(2)]
        psA = [P.ps([128, TB]) for _ in range(2)]
        psB = [P.ps([128, 2, 512]) for _ in range(2)]
        psC = P.ps([128, 8])
        for tb in range(NTB):
            hb, xm = hbs[tb % 2], xms[tb % 2]
            xmp = xms[(tb + 1) % 2]
            P.dma(hb, xview(K.hT, tb))
            if tb % 4 == 0:
                P.memset(xm[:, :, 0:3], 0.0)
            else:
                P.copy(xm[:, :, 0:3], xmp[:, :, TB:TB + 3])
            for dc in range(8):
                ps = psA[dc % 2]
                P.mm_group(ps, [(wml[:, kc, dc * 128:(dc + 1) * 128], hb[:, kc, :]) for kc in range(8)])
                P.copy(xm[:, dc, 3:TB + 3], ps, eng="act")
            for dc in range(8):
                P.ts(acc[:, dc, :], xm[:, dc, 0:TB], cw[0][:, dc:dc + 1], ALU.mult, cb[:, dc:dc + 1], ALU.add)
                for tp in range(1, 4):
                    P.stt(acc[:, dc, :], xm[:, dc, tp:tp + TB], cw[tp][:, dc:dc + 1], acc[:, dc, :], ALU.mult, ALU.add)
            P.act(xc, acc, AF.Silu)
            P.act(xcb, acc, AF.Silu)
            P.tt(acc, xc, skip.un(2).bc([128, 8, TB]), ALU.mult, eng="pool")
            P.dma(xview(K.ml_skx, tb), acc)
            for (w_, st_, dst) in ((wq, qst, K.ml_qT), (wk, kst, K.ml_kT)):
                for h in range(4):
                    for ec in range(2):
                        ps = psA[ec % 2]
                        P.mm_group(ps, [(w_[:, h, dck, ec * 128:(ec + 1) * 128], xcb[:, 2 * h + dck, :]) for dck in range(2)])
                        if ec == 0:
                            P.copy(st_[:, 2 * h + ec, :], ps, eng="act")
                        else:
                            P.copy(st_[:, 2 * h + ec, :], ps, eng="dve")
                P.dma(xview(dst, tb), st_)
            for tt_ in range(4):
                t0 = tb * TB + tt_ * 128
                tsl = slice(tt_ * 128, (tt_ + 1) * 128)
                kt = ktk[tt_ % 2]
                pb = psB[0]
                for h in range(4):
                    P.mm_group(pb[:, h // 2, (h % 2) * 256:(h % 2) * 256 + 256],
                               [(xcb[:, 2 * h + dck, tsl], wk[:, h, dck, :]) for dck in range(2)])
                P.copy(kt, pb.re("p a b -> p (a b)"), eng="act")
                P.dma(tmview(K.ml_ktok, t0, 128), kt)
                vo = vos[tt_ % 2]
                for half in range(2):
                    pb2 = psB[1] if half == 0 else psB[0]
                    for q in range(2):
                        c0 = half * 1024 + q * 512
                        P.mm_group(pb2[:, q, :], [(hb[:, kc, tsl], wml[:, kc, c0:c0 + 512]) for kc in range(8)])
                    if half == 0:
                        P.copy(vo[:, 0:1024], pb2.re("p a b -> p (a b)"), eng="dve")
                    else:
                        P.copy(vo[:, 1024:2048], pb2.re("p a b -> p (a b)"), eng="act")
                P.mm_group(psC, [(hb[:, kc, tsl], wml[:, kc, 2048:2056]) for kc in range(8)])
                P.copy(vo[:, 2048:2056], psC, eng="dve")
                P.dma(tmview(K.ml_vo, t0, 128), vo)


def stage_ml_rec(K, l):
    P = K.P
    NCH = T // LML
    with P.scope():
        ib = K.vec("ib%d" % l)
        fb = K.vec("fb%d" % l)
        mlnw = K.vec("mlnw%d" % l)
        gin = P.sb([128, NCH, 8])
        sp = P.sb([128, NCH, 4])
        li = P.sb([128, NCH, 4])
        et = P.sb([128, NCH, 4])
        wv = P.sb([128, NCH, 4])
        dec = P.sb([128, NCH, 4])
        wend = P.sb([128, NCH, 4])
        tmpg = P.sb([128, NCH, 4])
        C = P.sb([128, 4, 2, 257])
        qTs = [P.sb([128, 8, LML]) for _ in range(2)]
        kTs = [P.sb([128, 8, LML]) for _ in range(2)]
        kts = [P.sb([128, D]) for _ in range(2)]
        vxs = [P.sb([128, 4, 257]) for _ in range(2)]
        for vx in vxs:
            P.memset(vx[:, :, 256:257], 1.0)
        osb = [P.sb([128, D]) for _ in range(2)]
        sko = [P.sb([128, 8, LML]) for _ in range(2)]
        sgo = P.sb([128, D])
        sts = [P.sb([128, 128]) for _ in range(2)]
        hh = P.sb([128, 4, 256])
        cen = P.sb([128, 4, 256])
        sq = P.sb([128, 4, 256])
        sm = P.sb([128, 16])
        kw = P.sb([128, 4, 256])
        yst = [P.sb([128, 8, LML], BF16) for _ in range(2)]
        stp = [P.ps([128, 128]) for _ in range(2)]
        nps = P.ps([128, 4, 512])
        npsv = [P.sub(nps[:, h, 0:257]) for h in range(4)]
        psX = [P.ps([128, 512]) for _ in range(2)]
        for b in range(NB):
            T0 = b * T
            P.dma(gin, K.ml_vo.v(T0, T0 + T)[:, 2048:2056].re("(c t) g -> t c g", t=LML))
            P.tt(tmpg, gin[:, :, 4:8], fb.un(1).bc([128, NCH, 4]), ALU.add)
            P.act(tmpg, tmpg, AF.Exp, scale=-1.0)
            P.act(sp, tmpg, AF.Ln, bias=K.epsb[:, 2:3])
            P.tt(li, gin[:, :, 0:4], ib.un(1).bc([128, NCH, 4]), ALU.add)
            bps = psX[0][:, 0:NCH * 4]
            tps = psX[1][:, 0:NCH * 4]
            P.mm_group(bps, [(K.tri, sp.re("p c h -> p (c h)"))])
            P.mm_group(tps, [(K.ones, sp.re("p c h -> p (c h)"))])
            P.act(et.re("p c h -> p (c h)"), bps, AF.Exp, scale=-1.0)
            P.tt(tmpg.re("p c h -> p (c h)"), li.re("p c h -> p (c h)"), bps, ALU.add)
            P.act(wv, tmpg, AF.Exp)
            P.ts(wv, wv, 1.0 / 16.0, ALU.mult)
            P.act(dec.re("p c h -> p (c h)"), tps, AF.Exp, scale=-1.0)
            P.tt(wend, wv, dec, ALU.mult)
            P.memset(C, 0.0)
            for c in range(NCH):
                t0 = T0 + c * LML
                qT, kT, kt, vx, ob, sk = qTs[c % 2], kTs[c % 2], kts[c % 2], vxs[c % 2], osb[c % 2], sko[c % 2]
                P.dma(qT, K.ml_qT.v(t0, t0 + LML).re("(c p) t -> p c t", p=128))
                P.dma(kT, K.ml_kT.v(t0, t0 + LML).re("(c p) t -> p c t", p=128))
                P.dma(kt, tmview(K.ml_ktok, t0, LML))
                vo = K.ml_vo.v(t0, t0 + LML)
                P.dma(vx[:, :, 0:256], vo[:, 0:1024].re("t (h e) -> t h e", h=4))
                P.dma(ob, vo[:, 1024:2048])
                P.dma(sk, K.ml_skx.v(t0, t0 + LML).re("(c p) t -> p c t", p=128))
                P.act(sgo, ob, AF.Sigmoid)
                for h in range(4):
                    sp_ = stp[h % 2]
                    st = sts[h % 2]
                    P.mm_group(sp_, [(kT[:, 2 * h + dck, :], qT[:, 2 * h + dck, :]) for dck in range(2)])
                    P.stt(st, sp_, wv[:, c, h:h + 1], K.tri, ALU.mult, ALU.mult)
                    P.mm_group(npsv[h], [(st, vx[:, h, :]),
                                         (qT[:, 2 * h, :], C[:, h, 0, :]),
                                         (qT[:, 2 * h + 1, :], C[:, h, 1, :])])
                for h in range(4):
                    P.tt(sm[:, h:h + 1], npsv[h][:, 256:257], et[:, c, h:h + 1], ALU.mult)
                P.ts(sm[:, 4:8], sm[:, 0:4], -1.0, ALU.mult)
                P.tt(sm[:, 4:8], sm[:, 4:8], sm[:, 0:4], ALU.max)
                P.ts(sm[:, 4:8], sm[:, 4:8], 1.0, ALU.max)
                P.recip(sm[:, 8:12], sm[:, 4:8])
                P.tt(sm[:, 12:16], sm[:, 8:12], et[:, c, :], ALU.mult)
                for h in range(4):
                    P.stt(hh[:, h, :], npsv[h][:, 0:256], sm[:, 12 + h:13 + h], sgo[:, h * 256:(h + 1) * 256],
                          ALU.mult, ALU.mult)
                P.reduce(sm[:, 0:4], hh, ALU.add)
                P.ts(sm[:, 0:4], sm[:, 0:4], 1.0 / 256.0, ALU.mult)
                P.tt(cen, hh, sm[:, 0:4].un(2).bc([128, 4, 256]), ALU.subtract)
                P.tt(sq, cen, cen, ALU.mult, eng="pool")
                P.reduce(sm[:, 4:8], sq, ALU.add)
                P.act(sm[:, 8:12], sm[:, 4:8], AF.Sqrt, bias=K.epsb[:, 0:1], scale=1.0 / 256.0)
                P.recip(sm[:, 8:12], sm[:, 8:12])
                P.tt(cen, cen, sm[:, 8:12].un(2).bc([128, 4, 256]), ALU.mult)
                cenf = cen.re("p h e -> p (h e)")
                ys = yst[c % 2]
                for half in range(2):
                    px = psX[half]
                    for jj in range(4):
                        j = half * 4 + jj
                        P.transpose(px[:, jj * 128:(jj + 1) * 128], cenf[:, j * 128:(j + 1) * 128], K.ident)
                    for jj in range(4):
                        j = half * 4 + jj
                        P.stt(ys[:, j, :], px[:, jj * 128:(jj + 1) * 128], mlnw[:, j:j + 1], sk[:, j, :],
                              ALU.mult, ALU.add)
                P.dma(K.yT[0].v(t0, t0 + LML).re("(c p) t -> p c t", p=128), ys)
                for h in range(4):
                    P.ts(kw[:, h, :], kt[:, h * 256:(h + 1) * 256], wend[:, c, h:h + 1], ALU.mult, eng="pool")
                for h in range(4):
                    for dck in range(2):
                        px = psX[dck]
                        P.mm_group(px[:, 0:257], [(kw[:, h, dck * 128:(dck + 1) * 128], vx[:, h, :])])
                        P.stt(C[:, h, dck, :], C[:, h, dck, :], dec[:, c, h:h + 1], px[:, 0:257], ALU.mult, ALU.add)


def stage_in_hg(K, l):
    P = K.P
    with P.scope():
        whg = P.sb([128, 8, 4096], BF16)
        load_w_cols(P, whg, K.w_in[l], C_HG, C_HG + 4096)
        lb = P.sb([128, 8])
        oml = P.sb([128, 8])
        noml = P.sb([128, 8])
        if l == 0:
            P.memset(lb, 0.0)
        else:
            P.tt(lb, K.vec("hglb1"), K.vec("hglb0"), ALU.subtract)
            P.act(lb, lb, AF.Sigmoid)
        P.ts(oml, lb, -1.0, ALU.mult, 1.0, ALU.add)
        P.ts(noml, oml, -1.0, ALU.mult)
        hbs = [P.sb([128, 8, TB], BF16) for _ in range(2)]
        q = P.sb([128, 8, TB])
        sg = P.sb([128, 8, TB])
        kk = P.sb([128, 8, TB])
        bc_ = P.sb([128, 8, TB])
        eb = P.sb([128, 8, TB])
        enb = P.sb([128, 8, TB])
        dec = P.sb([128, 8, 8])
        kst = [P.sb([128, D])] * 2
        vst = [P.sb([128, D])] * 2
        gst = [P.sb([128, D])] * 2
        psA = [P.ps([128, TB]) for _ in range(2)]
        psF = [P.ps([128, TB]) for _ in range(2)]
        psT = [P.ps([128, 2, 512]) for _ in range(2)]
        for tb in range(NTB):
            hb = hbs[tb % 2]
            P.dma(hb, xview(K.hT, tb))
            for dc in range(8):
                pq, pf = psA[dc % 2], psF[dc % 2]
                P.mm_group(pq, [(whg[:, kc, dc * 128:(dc + 1) * 128], hb[:, kc, :]) for kc in range(8)])
                P.mm_group(pf, [(whg[:, kc, 1024 + dc * 128:1024 + (dc + 1) * 128], hb[:, kc, :]) for kc in range(8)])
                P.act(q[:, dc, :], pq, AF.Silu)
                P.act(sg[:, dc, :], pf, AF.Sigmoid)
                P.ts(kk[:, dc, :], sg[:, dc, :], noml[:, dc:dc + 1], ALU.mult, oml[:, dc:dc + 1], ALU.add, eng="pool")
                P.ts(sg[:, dc, :], sg[:, dc, :], oml[:, dc:dc + 1], ALU.mult, lb[:, dc:dc + 1], ALU.add)
            P.ts(sg, sg, 1e-30, ALU.max)
            P.act(sg, sg, AF.Ln)
            for dc in range(8):
                P.scan(bc_[:, dc, :], K.rmask, sg[:, dc, :], 0.0, ALU.mult, ALU.add)
            P.act(eb, bc_, AF.Exp)
            P.act(enb, bc_, AF.Exp, scale=-1.0)
            P.tt(q, q, eb, ALU.mult)
            P.tt(kk, kk, enb, ALU.mult, eng="pool")
            P.copy(dec, eb.re("p c (n l) -> p c n l", l=LHG)[:, :, :, LHG - 1])
            P.tt(enb.re("p c (n l) -> p c n l", l=LHG), kk.re("p c (n l) -> p c n l", l=LHG),
                 dec.un(3).bc([128, 8, 8, LHG]), ALU.mult)
            P.dma(xview(K.hg_qT, tb), q)
            P.dma(xview(K.hg_kT, tb), kk)
            P.dma(K.hg_decT.v(tb * 8, tb * 8 + 8).re("(c p) n -> p c n", p=128), dec)
            for tt_ in range(4):
                t0 = tb * TB + tt_ * 128
                tsl = slice(tt_ * 128, (tt_ + 1) * 128)
                ks, vs, gs = kst[tt_ % 2], vst[tt_ % 2], gst[tt_ % 2]
                px = psT[0]
                for dc in range(8):
                    P.transpose(px[:, dc // 4, (dc % 4) * 128:(dc % 4) * 128 + 128], enb[:, dc, tsl], K.ident)
                P.copy(ks, px.re("p a b -> p (a b)"), eng="act")
                P.dma(tmview(K.hg_khat, t0, 128), ks)
                pv = psT[1]
                for qq in range(2):
                    c0 = 2048 + qq * 512
                    P.mm_group(pv[:, qq, :], [(hb[:, kc, tsl], whg[:, kc, c0:c0 + 512]) for kc in range(8)])
                P.copy(vs, pv.re("p a b -> p (a b)"), eng="dve")
                P.dma(tmview(K.hg_vtok, t0, 128), vs)
                pg = psT[0]
                for qq in range(2):
                    c0 = 3072 + qq * 512
                    P.mm_group(pg[:, qq, :], [(hb[:, kc, tsl], whg[:, kc, c0:c0 + 512]) for kc in range(8)])
                P.act(gs, pg.re("p a b -> p (a b)"), AF.Sigmoid)
                P.dma(tmview(K.hg_gtok, t0, 128), gs)


def stage_hg_rec(K, l):
    P = K.P
    NCH = T // LHG
    CG = 4
    with P.scope():
        hgnw = K.vec("hgnw%d" % l)
        S = P.sb([128, 8, 128])
        decall = P.sb([128, 8, NCH])
        qts = [P.sb([128, 8, CG * LHG]) for _ in range(2)]
        kts = [P.sb([128, 8, CG * LHG]) for _ in range(2)]
        vts = [P.sb([64, CG, D]) for _ in range(2)]
        khs = [P.sb([64, CG, D]) for _ in range(2)]
        gts = [P.sb([64, CG, D]) for _ in range(2)]
        at = P.sb([64, 8, 64])
        sq = P.sb([64, 8, 128])
        on = P.sb([64, 8, 128])
        sm = P.sb([64, 16])
        yst = [P.sb([128, 8, CG * LHG], BF16) for _ in range(2)]
        atp = P.ps([64, 8, 64])
        ops = P.ps([64, 2, 512])
        tp = P.ps([128, 8, 64])
        sps = P.ps([128, 2, 512])
        tri64 = K.tri[0:64, 0:64]
        id64 = K.ident[0:64, 0:64]
        for b in range(NB):
            T0 = b * T
            P.memset(S, 0.0)
            P.dma(decall, K.hg_decT.v(b * NCH, (b + 1) * NCH).re("(c p) n -> p c n", p=128))
            for cg in range(NCH // CG):
                t0 = T0 + cg * CG * LHG
                t1 = t0 + CG * LHG
                qt, kt, vt, kh, gt, ys = qts[cg % 2], kts[cg % 2], vts[cg % 2], khs[cg % 2], gts[cg % 2], yst[cg % 2]
                P.dma(qt, K.hg_qT.v(t0, t1).re("(c p) t -> p c t", p=128))
                P.dma(kt, K.hg_kT.v(t0, t1).re("(c p) t -> p c t", p=128))
                P.dma(vt, K.hg_vtok.v(t0, t1).re("(c t) e -> t c e", t=LHG))
                P.dma(kh, K.hg_khat.v(t0, t1).re("(c t) e -> t c e", t=LHG))
                P.dma(gt, K.hg_gtok.v(t0, t1).re("(c t) e -> t c e", t=LHG))
                for ci in range(CG):
                    c = cg * CG + ci
                    cs = slice(ci * LHG, (ci + 1) * LHG)
                    for h in range(8):
                        P.mm_group(atp[:, h, :], [(kt[:, h, cs], qt[:, h, cs])])
                    P.tt(at, atp, tri64.un(1).bc([64, 8, 64]), ALU.mult)
                    for h in range(8):
                        hs = slice(h * 128, (h + 1) * 128)
                        P.mm_group(ops[:, h // 4, (h % 4) * 128:(h % 4) * 128 + 128],
                                   [(at[:, h, :], vt[:, ci, hs]), (qt[:, h, cs], S[:, h, :])])
                    opf = ops.re("p a (h e) -> p (a h) e", e=128)
                    P.act(sq, opf, AF.Square)
                    P.reduce(sm[:, 0:8], sq, ALU.add)
                    P.act(sm[:, 8:16], sm[:, 0:8], AF.Sqrt, bias=K.epsb[0:64, 0:1], scale=1.0 / 128.0)
                    P.recip(sm[:, 8:16], sm[:, 8:16])
                    P.tt(on, opf, sm[:, 8:16].un(2).bc([64, 8, 128]), ALU.mult)
                    P.tt(on, on, gt[:, ci, :].re("p (h e) -> p h e", e=128), ALU.mult, eng="pool")
                    for h in range(8):
                        P.transpose(tp[:, h, :], on[:, h, :], id64)
                    P.tt(ys[:, :, cs], tp, hgnw.un(2).bc([128, 8, 64]), ALU.mult)
                    for h in range(8):
                        hs = slice(h * 128, (h + 1) * 128)
                        P.mm_group(sps[:, h // 4, (h % 4) * 128:(h % 4) * 128 + 128], [(kh[:, ci, hs], vt[:, ci, hs])])
                    P.tt(S, S, decall[:, :, c:c + 1].bc([128, 8, 128]), ALU.mult)
                    P.tt(S, S, sps.re("p a (h e) -> p (a h) e", e=128), ALU.add)
                P.dma(K.yT[2].v(t0, t1).re("(c p) t -> p c t", p=128), ys)


def stage_in_rw(K, l):
    P = K.P
    NZ = 3328
    with P.scope():
        wrw = P.sb([128, 8, NZ], BF16)
        load_w_cols(P, wrw, K.w_in[l], C_RW, C_RW + NZ)
        wa2 = P.sb([128, D])
        g2 = P.sb([128, D])
        P.dma(wa2, K.rw_wa2[l])
        P.dma(g2, K.rw_g2[l])
        if l == 1:
            wvr = P.sb([128, 8, 32], BF16)
            P.dma(wvr, K.w_vres, eng="pool")
            v2 = P.sb([32, D])
            P.dma(v2, K.rw_v2)
            v0 = K.vec("rw_v0")
            muv = K.vec("muvres")
        mu = K.vec("rwmu%d" % l)
        w0, a0 = K.vec("rw_w0%d" % l), K.vec("rw_a0%d" % l)
        k_k, k_a, r_k = K.vec("rw_k_k%d" % l), K.vec("rw_k_a%d" % l), K.vec("rw_r_k%d" % l)
        omka = P.sb([128, 8])
        P.ts(omka, k_a, -1.0, ALU.mult, 1.0, ALU.add)
        prevs = [P.sb([128, 4]) for _ in range(9)]
        hb = P.sb([128, 8, TB], BF16)
        zl = P.sb([128, 3, TB + 1])
        P.memset(zl, 0.0)
        dl = P.sb([128, 3, TB])
        twa = P.sb([128, TB])
        sgl = P.sb([128, TB])
        vl = P.sb([32, TB])
        class SB:
            pass
        sets = []
        for si in range(2):
            s_ = SB()
            s_.z3 = P.sb([128, 3, TB + 1])
            s_.d3 = P.sb([128, 3, TB])
            s_.W = [P.sb([128, TB]) for _ in range(12)]
            s_.ob = [P.sb([128, TB], BF16) for _ in range(7)]
            s_.gam = P.sb([128, 8])
            s_.vfb = P.sb([128, TB])
            s_.stg = [P.sb([128, 4, 128], BF16) for _ in range(3)]
            s_.psZ = P.ps([128, TB])
            s_.psL = P.ps([128, TB])
            s_.psT = P.ps([128, TB])
            s_.psX = P.ps([128, 2 * TB], BF16)
            sets.append(s_)
        psA0 = sets[0].psZ

        def jstream(tb, j, s_):
            js = slice(j * 128, (j + 1) * 128)
            z3, d3 = s_.z3, s_.d3
            (kk, sq, t1, kmod, bb, rk, sgw, a_, cum, G, Gi, Gp) = s_.W
            bon, gg = sq, rk
            prev = prevs[j]
            ps = s_.psZ
            for i in range(3):
                ch = i * 8 + j
                P.mm_group(ps, [(wrw[:, kc, ch * 128:(ch + 1) * 128], hb[:, kc, :]) for kc in range(8)])
                P.copy(z3[:, i, 1:TB + 1], ps, eng="act")
                yield
            P.copy(z3[:, :, 0:1], prev[:, 0:3].un(2))
            P.copy(prev[:, 0:3].un(2), z3[:, :, TB:TB + 1])
            P.tt(d3, z3[:, :, 0:TB], z3[:, :, 1:TB + 1], ALU.subtract)
            yield
            for i in range(3):
                ch = i * 8 + j
                P.stt(d3[:, i, :], d3[:, i, :], mu[:, ch:ch + 1], z3[:, i, 1:TB + 1], ALU.mult, ALU.add)
            yield
            r, k, v = d3[:, 0, :], d3[:, 1, :], d3[:, 2, :]
            pl = s_.psL
            P.mm_group(pl, [(g2[:, js], sgl)])
            yield
            P.copy(gg, pl, eng="act")
            P.dma(K.rw_gT.v(tb * TB, (tb + 1) * TB)[js, :], gg)
            yield
            P.mm_group(pl, [(wa2[0:64, js], twa[0:64, :])])
            yield
            P.act(sgw, pl, AF.Sigmoid, bias=w0[:, j:j + 1])
            yield
            P.mm_group(pl, [(wa2[64:128, js], twa[64:128, :])])
            yield
            P.act(a_, pl, AF.Sigmoid, bias=a0[:, j:j + 1])
            yield
            if l == 1:
                P.mm_group(pl, [(v2[0:32, js], vl[0:32, :])])
                yield
                P.act(t1, pl, AF.Sigmoid, bias=v0[:, j:j + 1])
                P.dma(s_.vfb, K.rw_vfT.v(tb * TB, (tb + 1) * TB)[js, :])
                yield
                P.tt(s_.vfb, s_.vfb, v, ALU.subtract)
                P.tt(s_.vfb, s_.vfb, t1, ALU.mult)
                P.tt(v, v, s_.vfb, ALU.add)
                yield
            else:
                P.dma(K.rw_vfT.v(tb * TB, (tb + 1) * TB)[js, :], v)
            P.ts(kk, k, k_k[:, j:j + 1], ALU.mult, eng="pool")
            yield
            P.act(sq, kk, AF.Square)
            yield
            pn = s_.psT
            P.mm_group(pn, [(K.blk64, sq)])
            yield
            P.act(sq, pn, AF.Sqrt)
            yield
            P.ts(sq, sq, 1e-12, ALU.max)
            P.recip(sq, sq)
            P.tt(kk, kk, sq, ALU.mult)
            P.ts(t1, a_, k_a[:, j:j + 1], ALU.mult, omka[:, j:j + 1], ALU.add)
            yield
            P.tt(kmod, k, t1, ALU.mult, eng="pool")
            P.tt(bb, kk, a_, ALU.mult)
            yield
            P.stt(rk, r, r_k[:, j:j + 1], kmod, ALU.mult, ALU.mult)
            yield
            P.mm_group(pn, [(K.blk64, rk)])
            yield
            P.tt(bon, pn, v, ALU.mult)
            P.dma(K.rw_bonT.v(tb * TB, (tb + 1) * TB)[js, :], bon)
            P.scan(cum, K.rmask, sgw, 0.0, ALU.mult, ALU.add)
            yield
            P.act(G, cum, AF.Exp, scale=-WSC)
            P.act(Gi, cum, AF.Exp, scale=WSC)
            P.tt(t1, cum, sgw, ALU.subtract, eng="pool")
            yield
            P.act(Gp, t1, AF.Exp, scale=-WSC)
            yield
            kapb, rhob, betb, ktlb, vb, bhb, khb = s_.ob
            P.tt(kapb, kk, Gp, ALU.mult)
            P.tt(rhob, r, G, ALU.mult, eng="pool")
            P.tt(bb, bb, Gi, ALU.mult)
            P.tt(kmod, kmod, Gi, ALU.mult, eng="pool")
            yield
            P.copy(betb, bb, eng="act")
            P.copy(ktlb, kmod, eng="act")
            P.copy(vb, v, eng="act")
            P.copy(s_.gam, G.re("p (n l) -> p n l", l=LRW)[:, :, LRW - 1])
            gb = s_.gam.un(2).bc([128, 8, LRW])
            P.tt(bhb.re("p (n l) -> p n l", l=LRW), bb.re("p (n l) -> p n l", l=LRW), gb, ALU.mult)
            P.tt(khb.re("p (n l) -> p n l", l=LRW), kmod.re("p (n l) -> p n l", l=LRW), gb, ALU.mult,
                 eng="pool")
            yield
            for (src, dst) in ((kapb, K.rw_kapT), (rhob, K.rw_rhoT), (betb, K.rw_betT), (ktlb, K.rw_ktlT)):
                P.dma(dst.v(tb * TB, (tb + 1) * TB)[js, :], src)
            P.dma(K.rw_gamT.v(tb * 8, tb * 8 + 8)[js, :], s_.gam)
            for xi, (X, dst) in enumerate(((vb, K.rw_vtok), (bhb, K.rw_bhat), (khb, K.rw_khat))):
                px = s_.psX
                for tt_ in range(4):
                    P.transpose(px[:, tt_ * 128:(tt_ + 1) * 128], X[:, tt_ * 128:(tt_ + 1) * 128], K.identb)
                yield
                if xi == 1:
                    P.copy(s_.stg[xi], px[:, 0:TB].re("p (a b) -> p a b", a=4), eng="act")
                else:
                    P.copy(s_.stg[xi], px[:, 0:TB].re("p (a b) -> p a b", a=4))
                P.dma(dst.v(tb * TB, (tb + 1) * TB)[:, js].re("(a t) e -> t a e", t=128), s_.stg[xi])
                yield

        for tb in range(NTB):
            first = (tb % 4 == 0)
            P.dma(hb, xview(K.hT, tb))
            if first:
                for pv_ in prevs:
                    P.memset(pv_, 0.0)
            nlo = 3 if l == 1 else 2
            prev = prevs[8]
            for i in range(nlo):
                ps = psA0
                if i < 2:
                    P.mm_group(ps, [(wrw[:, kc, (24 + i) * 128:(25 + i) * 128], hb[:, kc, :]) for kc in range(8)])
                    P.copy(zl[:, i, 1:TB + 1], ps, eng="act")
                else:
                    P.mm_group(ps[0:32, :], [(wvr[:, kc, :], hb[:, kc, :]) for kc in range(8)])
                    P.copy(zl[0:32, i, 1:TB + 1], ps[0:32, :], eng="act")
                P.copy(zl[:, i, 0:1], prev[:, i:i + 1])
                P.copy(prev[:, i:i + 1], zl[:, i, TB:TB + 1])
            P.tt(dl[:, 0:nlo, :], zl[:, 0:nlo, 0:TB], zl[:, 0:nlo, 1:TB + 1], ALU.subtract)
            P.stt(dl[:, 0, :], dl[:, 0, :], mu[:, 24:25], zl[:, 0, 1:TB + 1], ALU.mult, ALU.add)
            P.stt(dl[:, 1, :], dl[:, 1, :], mu[:, 25:26], zl[:, 1, 1:TB + 1], ALU.mult, ALU.add)
            P.act(twa[0:64, :], dl[0:64, 0, :], AF.Tanh)
            P.copy(twa[64:128, :], dl[64:128, 0, :], eng="act")
            P.act(sgl, dl[:, 1, :], AF.Sigmoid)
            if l == 1:
                P.stt(vl, dl[0:32, 2, :], muv[0:32, 0:1], zl[0:32, 2, 1:TB + 1], ALU.mult, ALU.add)
            for j0 in range(0, 8, 2):
                lockstep([jstream(tb, j0, sets[0]), jstream(tb, j0 + 1, sets[1])])


def stage_rw_rec_old(K, l, post):
    P = K.P
    NCH = T // LRW
    CB = 8
    L = LRW
    with P.scope():
        mask12 = P.sb([64, 128])
        P.copy(mask12[:, 0:64], K.tris[0:64, 0:64])
        P.copy(mask12[:, 64:128], K.tri[0:64, 0:64])
        lows = K.lows[0:64, 0:64]
        id64 = K.ident[0:64, 0:64]
        KRs = [P.sb([64, 4, CB, 2, L]) for _ in range(2)]
        BTs = [P.sb([64, 4, CB * L]) for _ in range(2)]
        KTs = [P.sb([64, 4, CB * L]) for _ in range(2)]
        gms = [P.sb([64, 4, CB]) for _ in range(2)]
        vts = [P.sb([64, CB, 256]) for _ in range(2)]
        bhs = [P.sb([64, CB, 256]) for _ in range(2)]
        khs = [P.sb([64, CB, 256]) for _ in range(2)]
        ysts = [P.sb([64, CB, 256]) for _ in range(2)]
        S = P.sb([64, 4, L])
        M1 = P.sb([64, 4, 128])
        M2 = P.sb([64, 4, 128])
        Pm = [P.sb([64, 4, L]) for _ in range(2)]
        Qm = [P.sb([64, 4, L]) for _ in range(2)]
        TT = P.sb([64, 4, L])
        Rn = P.sb([64, 4, L])
        U = P.sb([64, 4, L])
        A1 = P.ps([64, 4, 128])
        A2 = P.ps([64, 4, 128])
        A3 = P.ps([64, 4, L])
        PP = P.ps([64, 4, L])
        QQ = P.ps([64, 4, L])
        TP = P.ps([64, 4, L])
        RU = P.ps([64, 2, 4, L])
        YS = P.ps([64, 2, 4, L])
        RP, UP = RU[:, 0], RU[:, 1]
        YP, SP = YS[:, 0], YS[:, 1]
        it = 0
        import os as _os
        lim = [2, 4, 4]
        VAR = _os.environ.get("RW_VAR", "")
        gmall = P.sb([64, 4, NCH])
        for b in range(lim[0]):
            T0 = b * T
            for hg in range(lim[1]):
                P.memset(S, 0.0)
                for cb in range(lim[2]):
                    t0 = T0 + cb * CB * L
                    t1 = t0 + CB * L
                    KR, BT, KT, gm = KRs[it % 2], BTs[it % 2], KTs[it % 2], gms[it % 2]
                    vt, bh, kh, yst = vts[it % 2], bhs[it % 2], khs[it % 2], ysts[it % 2]
                    it += 1
                    for hh in range(4):
                        ch = slice(hg * 256 + hh * 64, hg * 256 + hh * 64 + 64)
                        P.dma(KR[:, hh, :, 0, :], K.rw_kapT.v(t0, t1)[ch, :].re("p (c l) -> p c l", l=L))
                        P.dma(KR[:, hh, :, 1, :], K.rw_rhoT.v(t0, t1)[ch, :].re("p (c l) -> p c l", l=L))
                        P.dma(BT[:, hh, :], K.rw_betT.v(t0, t1)[ch, :])
                        P.dma(KT[:, hh, :], K.rw_ktlT.v(t0, t1)[ch, :])
                        g0 = b * NCH + cb * CB
                        P.dma(gm[:, hh, :], K.rw_gamT.v(g0, g0 + CB)[ch, :])
                        if "gmall" in VAR and cb == 0:
                            P.dma(gmall[:, hh, :], K.rw_gamT.v(b * NCH, (b + 1) * NCH)[ch, :])
                    es = slice(hg * 256, hg * 256 + 256)
                    P.dma(vt, K.rw_vtok.v(t0, t1)[:, es].re("(c t) e -> t c e", t=L))
                    P.dma(bh, K.rw_bhat.v(t0, t1)[:, es].re("(c t) e -> t c e", t=L))
                    P.dma(kh, K.rw_khat.v(t0, t1)[:, es].re("(c t) e -> t c e", t=L))
                    for ci in range(CB):
                        cs = slice(ci * L, (ci + 1) * L)
                        for hh in range(4):
                            krr = KR[:, hh, ci, :, :].re("p a l -> p (a l)")
                            P.mm_group(A1[:, hh, :], [(BT[:, hh, cs], krr)])
                            P.mm_group(A2[:, hh, :], [(KT[:, hh, cs], krr)])
                            P.mm_group(A3[:, hh, :], [(KR[:, hh, ci, 0, :], BT[:, hh, cs])])
                        P.tt(M1, A1, mask12.un(1).bc([64, 4, 128]), ALU.mult)
                        P.tt(M2, A2, mask12.un(1).bc([64, 4, 128]), ALU.mult)
                        P.tt(Pm[0], A3, lows.un(1).bc([64, 4, L]), ALU.mult)
                        Pc = Pm[0]
                        Qc = M1[:, :, 0:L]
                        ArbT = M1[:, :, L:2 * L]
                        AkkT = M2[:, :, 0:L]
                        ArkT = M2[:, :, L:2 * L]
                        P.tt(TT, id64.un(1).bc([64, 4, L]), Qc, ALU.subtract)
                        for lev in range(1, 6):
                            Pn = Pm[lev % 2]
                            Qn = Qm[lev % 2]
                            for hh in range(4):
                                P.mm_group(PP[:, hh, :], [(Qc[:, hh, :], Pc[:, hh, :])])
                            if lev < 5:
                                for hh in range(4):
                                    P.mm_group(QQ[:, hh, :], [(Pc[:, hh, :], Qc[:, hh, :])])
                            P.copy(Pn, PP)
                            if lev < 5:
                                P.copy(Qn, QQ, eng="act")
                            for hh in range(4):
                                P.mm_group(TP[:, hh, :], [(Pn[:, hh, :], TT[:, hh, :])])
                            P.tt(TT, TT, TP, ALU.add)
                            Pc, Qc = Pn, Qn
                        for hh in range(4):
                            vs = slice(hh * 64, hh * 64 + 64)
                            P.mm_group(RP[:, hh, :], [(KR[:, hh, ci, 0, :], S[:, hh, :]), (AkkT[:, hh, :], vt[:, ci, vs])])
                        P.ts(Rn, RP, -1.0, ALU.mult)
                        for hh in range(4):
                            P.mm_group(UP[:, hh, :], [(TT[:, hh, :], Rn[:, hh, :])])
                        P.copy(U, UP)
                        for hh in range(4):
                            vs = slice(hh * 64, hh * 64 + 64)
                            P.mm_group(YP[:, hh, :], [(KR[:, hh, ci, 1, :], S[:, hh, :]), (ArbT[:, hh, :], U[:, hh, :]),
                                                      (ArkT[:, hh, :], vt[:, ci, vs])])
                        if "reorder" not in VAR:
                            P.copy(yst[:, ci, :].re("p (h e) -> p h e", h=4), YP, eng="act")
                        for hh in range(4):
                            vs = slice(hh * 64, hh * 64 + 64)
                            P.mm_group(SP[:, hh, :], [(bh[:, ci, vs], U[:, hh, :]), (kh[:, ci, vs], vt[:, ci, vs])])
                        if "reorder" in VAR:
                            P.copy(yst[:, ci, :].re("p (h e) -> p h e", h=4), YP, eng="act")
                        if "gmall" in VAR:
                            cc = cb * CB + ci
                            P.tt(S, S, gmall[:, :, cc:cc + 1].bc([64, 4, L]), ALU.mult)
                        else:
                            P.tt(S, S, gm[:, :, ci:ci + 1].bc([64, 4, L]), ALU.mult)
                        P.tt(S, S, SP, ALU.add)
                    P.dma(K.rw_ytok.v(t0, t1)[:, es].re("(c t) e -> t c e", t=L), yst)


def lockstep(gens):
    gens = list(gens)
    while gens:
        for g in list(gens):
            try:
                next(g)
            except StopIteration:
                gens.remove(g)


def stage_rw_rec(K, l):
    P = K.P
    import os as _os
    if _os.environ.get("RW_OLD"):
        stage_rw_rec_old(K, l, None)
        return
    NCH = T // LRW
    CB = int(_os.environ.get("RW_CB", "4"))
    NBUF = int(_os.environ.get("RW_NBUF", "2"))
    L = LRW
    with P.scope():
        mask12 = P.sb([64, 128])
        P.copy(mask12[:, 0:64], K.tris[0:64, 0:64])
        P.copy(mask12[:, 64:128], K.tri[0:64, 0:64])
        lows = K.lows[0:64, 0:64]
        id64 = K.ident[0:64, 0:64]

        def stream(b):
            KRs = [P.sb([64, 4, CB, 2, L], BF16) for _ in range(NBUF)]
            BTs = [P.sb([64, 4, CB * L], BF16) for _ in range(NBUF)]
            KTs = [P.sb([64, 4, CB * L], BF16) for _ in range(NBUF)]
            gms = [None, None]
            gmall = P.sb([64, 4, NCH])
            vts = [P.sb([64, CB, 256], BF16) for _ in range(NBUF)]
            bhs = [P.sb([64, CB, 256], BF16) for _ in range(NBUF)]
            khs = [P.sb([64, CB, 256], BF16) for _ in range(NBUF)]
            ysts = [P.sb([64, CB, 256]) for _ in range(NBUF)]
            S = P.sb([64, 4, L])
            Sb = P.sb([64, 4, L], BF16)
            M1 = P.sb([64, 4, 128], BF16)
            M2 = P.sb([64, 4, 128], BF16)
            Pm = [P.sb([64, 4, L], BF16) for _ in range(2)]
            Qm = [P.sb([64, 4, L], BF16) for _ in range(2)]
            TT = P.sb([64, 4, L], BF16)
            Rn = P.sb([64, 4, L], BF16)
            U = P.sb([64, 4, L], BF16)
            import os as _os
            if _os.environ.get("RW_PS") == "old":
                if not hasattr(K, "_rwps"):
                    K._rwps = dict(A1=P.ps([64, 4, 128]), A2=P.ps([64, 4, 128]), A3=P.ps([64, 4, L]), PP=P.ps([64, 4, L]),
                                   QQ=P.ps([64, 4, L]), TP=P.ps([64, 4, L]), RU=P.ps([64, 2, 4, L]), YS=P.ps([64, 2, 4, L]))
                d_ = K._rwps
                A1, A2, A3, PP, QQ, TP = d_["A1"], d_["A2"], d_["A3"], d_["PP"], d_["QQ"], d_["TP"]
                RP, UP = d_["RU"][:, 0], d_["RU"][:, 1]
                YP, SP = d_["YS"][:, 0], d_["YS"][:, 1]
            else:
                B0 = P.ps([64, 4, 128])
                B1 = P.ps([64, 4, 128])
                B2 = P.ps([64, 2, 4, L])
                B3 = P.ps([64, 2, 4, L])
                A1, A2 = B0, B1
                A3, PP = B2[:, 0], B2[:, 1]
                QQ, TP = B3[:, 0], B3[:, 1]
                RP, UP = B0[:, :, 0:L], B0[:, :, L:2 * L]
                YP, SP = B1[:, :, 0:L], B1[:, :, L:2 * L]
            it = 0
            yield
            T0 = b * T
            for hg in range(4):
                P.memset(S, 0.0)
                P.memset(Sb, 0.0)
                for cb in range(NCH // CB):
                    t0 = T0 + cb * CB * L
                    t1 = t0 + CB * L
                    KR, BT, KT = KRs[it % NBUF], BTs[it % NBUF], KTs[it % NBUF]
                    vt, bh, kh, yst = vts[it % NBUF], bhs[it % NBUF], khs[it % NBUF], ysts[it % NBUF]
                    it += 1
                    for hh in range(4):
                        ch = slice(hg * 256 + hh * 64, hg * 256 + hh * 64 + 64)
                        P.dma(KR[:, hh, :, 0, :], K.rw_kapT.v(t0, t1)[ch, :].re("p (c l) -> p c l", l=L))
                        P.dma(KR[:, hh, :, 1, :], K.rw_rhoT.v(t0, t1)[ch, :].re("p (c l) -> p c l", l=L))
                        P.dma(BT[:, hh, :], K.rw_betT.v(t0, t1)[ch, :])
                        P.dma(KT[:, hh, :], K.rw_ktlT.v(t0, t1)[ch, :])
                        if cb == 0:
                            P.dma(gmall[:, hh, :], K.rw_gamT.v(b * NCH, (b + 1) * NCH)[ch, :])
                    es = slice(hg * 256, hg * 256 + 256)
                    P.dma(vt, K.rw_vtok.v(t0, t1)[:, es].re("(c t) e -> t c e", t=L))
                    P.dma(bh, K.rw_bhat.v(t0, t1)[:, es].re("(c t) e -> t c e", t=L))
                    P.dma(kh, K.rw_khat.v(t0, t1)[:, es].re("(c t) e -> t c e", t=L))
                    yield
                    for ci in range(CB):
                        cs = slice(ci * L, (ci + 1) * L)
                        for hh in range(4):
                            krr = KR[:, hh, ci, :, :].re("p a l -> p (a l)")
                            P.mm_group(A1[:, hh, :], [(BT[:, hh, cs], krr)])
                            P.mm_group(A2[:, hh, :], [(KT[:, hh, cs], krr)])
                            P.mm_group(A3[:, hh, :], [(KR[:, hh, ci, 0, :], BT[:, hh, cs])])
                        yield
                        P.tt(M1, A1, mask12.un(1).bc([64, 4, 128]), ALU.mult)
                        P.tt(Pm[0], A3, lows.un(1).bc([64, 4, L]), ALU.mult)
                        P.tt(M2, A2, mask12.un(1).bc([64, 4, 128]), ALU.mult, eng="pool") if False else P.tt(M2, A2, mask12.un(1).bc([64, 4, 128]), ALU.mult)
                        Pc = Pm[0]
                        Qc = M1[:, :, 0:L]
                        ArbT = M1[:, :, L:2 * L]
                        AkkT = M2[:, :, 0:L]
                        ArkT = M2[:, :, L:2 * L]
                        P.tt(TT, id64.un(1).bc([64, 4, L]), Qc, ALU.subtract)
                        yield
                        for lev in range(1, 6):
                            Pn = Pm[lev % 2]
                            Qn = Qm[lev % 2]
                            for hh in range(4):
                                P.mm_group(PP[:, hh, :], [(Qc[:, hh, :], Pc[:, hh, :])])
                            if lev < 5:
                                for hh in range(4):
                                    P.mm_group(QQ[:, hh, :], [(Pc[:, hh, :], Qc[:, hh, :])])
                            yield
                            P.copy(Pn, PP)
                            if lev < 5:
                                P.copy(Qn, QQ, eng="act")
                            yield
                            for hh in range(4):
                                P.mm_group(TP[:, hh, :], [(Pn[:, hh, :], TT[:, hh, :])])
                            yield
                            P.tt(TT, TT, TP, ALU.add)
                            yield
                            Pc, Qc = Pn, Qn
                        for hh in range(4):
                            vs = slice(hh * 64, hh * 64 + 64)
                            P.mm_group(RP[:, hh, :], [(KR[:, hh, ci, 0, :], Sb[:, hh, :]), (AkkT[:, hh, :], vt[:, ci, vs])])
                        yield
                        P.ts(Rn, RP, -1.0, ALU.mult)
                        yield
                        for hh in range(4):
                            P.mm_group(UP[:, hh, :], [(TT[:, hh, :], Rn[:, hh, :])])
                        yield
                        P.copy(U, UP)
                        yield
                        for hh in range(4):
                            vs = slice(hh * 64, hh * 64 + 64)
                            P.mm_group(YP[:, hh, :], [(KR[:, hh, ci, 1, :], Sb[:, hh, :]), (ArbT[:, hh, :], U[:, hh, :]),
                                                      (ArkT[:, hh, :], vt[:, ci, vs])])
                        yield
                        P.copy(yst[:, ci, :].re("p (h e) -> p h e", h=4), YP, eng="act")
                        yield
                        for hh in range(4):
                            vs = slice(hh * 64, hh * 64 + 64)
                            P.mm_group(SP[:, hh, :], [(bh[:, ci, vs], U[:, hh, :]), (kh[:, ci, vs], vt[:, ci, vs])])
                        yield
                        cc = cb * CB + ci
                        P.tt(S, S, gmall[:, :, cc:cc + 1].bc([64, 4, L]), ALU.mult)
                        P.tt(S, S, SP, ALU.add)
                        P.copy(Sb, S, eng="act")
                        yield
                    P.dma(K.rw_ytok.v(t0, t1)[:, es].re("(c t) e -> t c e", t=L), yst)
        import os as _os
        if _os.environ.get("RW_SEQ"):
            for b in range(NB):
                lockstep([stream(b)])
        else:
            lockstep([stream(b) for b in range(NB)])
    with P.scope():
        lnw, lnb = K.vec("rw_ln_w%d" % l), K.vec("rw_ln_b%d" % l)
        ys = [P.sb([128, 16, 64]) for _ in range(2)]
        cen = P.sb([128, 16, 64])
        sq = P.sb([128, 16, 64])
        sm = P.sb([128, 48])
        bons = [P.sb([128, 8, 128]) for _ in range(2)]
        ggs = [P.sb([128, 8, 128]) for _ in range(2)]
        tmp = P.sb([128, 8, 128])
        yo = [P.sb([128, 8, 128], BF16) for _ in range(2)]
        px = [P.ps([128, 4, 128]) for _ in range(2)]
        for tt_ in range(TOK // 128):
            t0 = tt_ * 128
            y, bon, gg, o = ys[tt_ % 2], bons[tt_ % 2], ggs[tt_ % 2], yo[tt_ % 2]
            P.dma(y, K.rw_ytok.v(t0, t0 + 128).re("t (h e) -> t h e", e=64))
            P.dma(bon, K.rw_bonT.v(t0, t0 + 128).re("(c p) t -> p c t", p=128))
            P.dma(gg, K.rw_gT.v(t0, t0 + 128).re("(c p) t -> p c t", p=128))
            P.reduce(sm[:, 0:16], y, ALU.add)
            P.ts(sm[:, 0:16], sm[:, 0:16], 1.0 / 64.0, ALU.mult)
            P.tt(cen, y, sm[:, 0:16].un(2).bc([128, 16, 64]), ALU.subtract)
            P.tt(sq, cen, cen, ALU.mult, eng="pool")
            P.reduce(sm[:, 16:32], sq, ALU.add)
            P.act(sm[:, 32:48], sm[:, 16:32], AF.Sqrt, bias=K.epsb[:, 1:2], scale=1.0 / 64.0)
            P.recip(sm[:, 32:48], sm[:, 32:48])
            P.tt(cen, cen, sm[:, 32:48].un(2).bc([128, 16, 64]), ALU.mult)
            cf = cen.re("p h e -> p (h e)")
            for j in range(8):
                P.transpose(px[j // 4][:, j % 4, :], cf[:, j * 128:(j + 1) * 128], K.ident)
            for j in range(8):
                P.ts(tmp[:, j, :], px[j // 4][:, j % 4, :], lnw[:, j:j + 1], ALU.mult, lnb[:, j:j + 1], ALU.add)
            P.tt(tmp, tmp, bon, ALU.add, eng="pool")
            P.tt(o, tmp, gg, ALU.mult)
            P.dma(K.yT[1].v(t0, t0 + 128).re("(c p) t -> p c t", p=128), o)


def stage_mix(K, l):
    P = K.P
    with P.scope():
        wgt = P.sb([128, 8, 3072], BF16)
        load_w_cols(P, wgt, K.w_in[l], C_GATE, C_GATE + 3072)
        wbr = P.sb([128, 3, 8, D], BF16)
        P.dma(wbr, K.w_branch[l], eng="pool")
        wo = P.sb([128, 8, D], BF16)
        P.dma(wo, K.w_out[l], eng="pool")
        hbs = [P.sb([128, 8, TB], BF16)] * 2
        yb = [P.sb([128, 8, TB], BF16) for _ in range(3)]
        xb = P.sb([128, 8, TB])
        xo = P.sb([128, 8, TB])
        mixb = P.sb([128, 8, TB], BF16)
        gts = [P.sb([128, TB]) for _ in range(2)]
        tmp = P.sb([128, TB])
        acc = P.sb([128, TB])
        psG = [P.ps([128, TB]) for _ in range(2)]
        psY = [P.ps([128, TB]) for _ in range(2)]
        psO = [P.ps([128, TB]) for _ in range(2)]
        for tb in range(NTB):
            b = tb // 4
            hb = hbs[tb % 2]
            P.dma(hb, xview(K.hT, tb))
            for br in range(3):
                P.dma(yb[br], xview(K.yT[br], tb))
            P.dma(xb, xview(K.xs, tb))
            i = 0
            for n in range(8):
                ns = slice(n * 128, (n + 1) * 128)
                for br in range(3):
                    pg, py, g = psG[i % 2], psY[i % 2], gts[i % 2]
                    i += 1
                    P.mm_group(pg, [(wgt[:, kc, br * 1024 + n * 128:br * 1024 + (n + 1) * 128], hb[:, kc, :]) for kc in range(8)])
                    P.mm_group(py, [(wbr[:, br, kc, ns], yb[br][:, kc, :]) for kc in range(8)])
                    P.act(g, pg, AF.Sigmoid)
                    if br == 0:
                        P.tt(acc, g, py, ALU.mult)
                    elif br == 1:
                        P.tt(tmp, g, py, ALU.mult)
                        P.tt(acc, acc, tmp, ALU.add, eng="pool")
                    else:
                        P.tt(tmp, g, py, ALU.mult)
                        P.tt(mixb[:, n, :], acc, tmp, ALU.add, eng="pool")
            for m in range(8):
                po = psO[m % 2]
                P.mm_group(po, [(wo[:, kc, m * 128:(m + 1) * 128], mixb[:, kc, :]) for kc in range(8)])
                P.stt(xo[:, m, :], po, K.gt[l][1][:, m, b:b + 1], xb[:, m, :], ALU.mult, ALU.add)
            P.dma(xview(K.xs, tb), xo)
def prep_core_inputs(inp, vecs, W, core):
    b0 = core * NB
    x = np.asarray(inp["x"], np.float32)[b0:b0 + NB].reshape(TOK, D)
    c = np.asarray(inp["c"], np.float32)[b0:b0 + NB]
    m = dict(W)
    m["xT"] = np.ascontiguousarray(x.T)
    m["cT"] = np.ascontiguousarray(c.reshape(NB, KC, 128).transpose(2, 1, 0))
    m["vecs"] = vecs
    return m


_CACHE = {}


def kernel(**inputs):
    from concourse.bass_utils import run_bass_kernel_spmd
    vp = pack_vecs(inputs)
    vecs = vp.build()
    W = host_weights(inputs)
    key = "main"
    if key not in _CACHE:
        _CACHE[key] = build_program(vp.off, vp.n)
    nc = _CACHE[key]
    in_maps = [prep_core_inputs(inputs, vecs, W, c) for c in range(8)]
    res = run_bass_kernel_spmd(nc, in_maps, core_ids=list(range(8)))
    out = np.empty((16, T, D), np.float32)
    for c in range(8):
        o = np.asarray(res.results[c]["outT"])
        out[c * NB:(c + 1) * NB] = o.T.reshape(NB, T, D)
    return out
```

```python
import contextlib
import numpy as np
import concourse.bass as bass
import concourse.mybir as mybir

F32 = mybir.dt.float32
BF16 = mybir.dt.bfloat16
ALU = mybir.AluOpType
AF = mybir.ActivationFunctionType
AX = mybir.AxisListType

ENGS = ("pe", "act", "dve", "pool", "sp")


class Tok:
    __slots__ = ("w", "r", "name")

    def __init__(self, name=""):
        self.w = None
        self.r = {}
        self.name = name


class V:
    __slots__ = ("ap", "toks")

    def __init__(self, ap, toks):
        self.ap = ap
        self.toks = toks

    def __getitem__(self, idx):
        return V(self.ap[idx], self.toks)

    def re(self, s, **kw):
        return V(self.ap.rearrange(s, **kw), self.toks)

    def bc(self, shape):
        return V(self.ap.broadcast_to(shape), self.toks)

    def un(self, axis):
        return V(self.ap.unsqueeze(axis), self.toks)

    def bitcast(self, dt):
        return V(self.ap.bitcast(dt), self.toks)

    @property
    def shape(self):
        return self.ap.shape


class Prog:
    def __init__(self, nc, es, n_dma_sems=40):
        self.nc = nc
        self.es = es
        self.q = {e: [] for e in ENGS}
        self.cnt = {e: 0 for e in ENGS}
        self.sem = {e: es.enter_context(nc.semaphore("s_" + e)) for e in ENGS}
        self.semobj = dict(self.sem)
        self.waited = {e: {} for e in ENGS}
        self.dsems = []
        self.n_ring = n_dma_sems
        for i in range(n_dma_sems):
            k = "d%d" % i
            self.semobj[k] = es.enter_context(nc.semaphore(k))
            self.dsems.append([k, 0, None])
        self.dnext = 0
        self.uid = 0
        self.psems = []
        for i in range(48):
            k = "q%d" % i
            self.semobj[k] = es.enter_context(nc.semaphore(k))
            self.psems.append(k)
        self.pnext = 0

    def sb(self, shape, dt=F32, name=None):
        self.uid += 1
        name = name or ("t%d" % self.uid)
        t = self.es.enter_context(self.nc.sbuf_tensor(name, list(shape), dt))
        return V(t[:], (Tok(name),))

    def ps(self, shape, dt=F32, name=None):
        self.uid += 1
        name = name or ("p%d" % self.uid)
        shape = list(shape)
        n = 1
        for d_ in shape[1:]:
            n *= d_
        nb = (n + 511) // 512
        t = self.es.enter_context(self.nc.psum_tensor(name, [shape[0], nb * 512], dt))
        ap = t[:][:, 0:n]
        if len(shape) == 3:
            ap = ap.rearrange("p (a b) -> p a b", a=shape[1])
        elif len(shape) == 4:
            ap = ap.rearrange("p (a b c) -> p a b c", a=shape[1], b=shape[2])
        return V(ap, (Tok(name),))

    def dram(self, name, shape, dt=F32, kind="Internal"):
        t = self.nc.dram_tensor(name, list(shape), dt, kind=kind)
        return V(t.ap(), (Tok(name),))

    def sub(self, v, name=""):
        return V(v.ap, (Tok(name),))

    def _needs(self, reads, writes):
        needs = {}

        def need(k, val):
            if val > needs.get(k, 0):
                needs[k] = val
        for v in reads:
            for t in v.toks:
                if t.w is not None:
                    need(*t.w)
        for v in writes:
            for t in v.toks:
                if t.w is not None:
                    need(*t.w)
                for k, val in t.r.items():
                    need(k, val)
        return needs

    def issue(self, eng, fn, reads, writes, inc=True, pe_group=False):
        needs = self._needs(reads, writes)
        waits = []
        wd = self.waited[eng]
        for k, val in needs.items():
            if k == eng and eng == "pe":
                continue
            if wd.get(k, 0) < val:
                wd[k] = val
                waits.append((k, val))
        n = self.cnt[eng] + 1
        if inc:
            self.cnt[eng] = n
        self.q[eng].append((waits, fn, inc))
        for v in reads:
            for t in v.toks:
                if t.r.get(eng, 0) < n:
                    t.r[eng] = n
        for v in writes:
            for t in v.toks:
                t.w = (eng, n)
                t.r = {}
        return n

    def dma(self, out, in_, eng="sp", **kw):
        if eng == "pool":
            slot = [self.psems[self.pnext], 0, None]
            self.pnext += 1
            self.dsems.append(slot)
        else:
            slot = self.dsems[self.dnext]
            self.dnext = (self.dnext + 1) % self.n_ring
        key, cur, _ = slot
        needs = self._needs([in_], [out])
        wd = self.waited[eng]
        waits = []
        if cur > 0:
            needs[key] = max(needs.get(key, 0), cur)
        for k, val in needs.items():
            if wd.get(k, 0) < val:
                wd[k] = val
                waits.append((k, val))
        newv = cur + 16
        slot[1] = newv
        oap, iap = out.ap, in_.ap

        def fn(e, oap=oap, iap=iap, kw=kw):
            return e.dma_start(out=oap, in_=iap, **kw)
        self.q[eng].append((waits, fn, ("dma", key)))
        for t in in_.toks:
            if t.r.get(key, 0) < newv:
                t.r[key] = newv
        for t in out.toks:
            t.w = (key, newv)
            t.r = {}

    def emit(self, final_waits=()):
        nc = self.nc
        engmap = {"pe": "tensor", "act": "scalar", "dve": "vector", "pool": "gpsimd", "sp": "sync"}
        with nc.Block() as block:
            for e in ENGS:
                lst = self.q[e]

                def body(eh, lst=lst, e=e):
                    for waits, fn, inc in lst:
                        for k, val in waits:
                            eh.wait_ge(self.semobj[k], val)
                        if fn is None:
                            continue
                        ins = fn(eh)
                        if inc is True:
                            ins.then_inc(self.semobj[e], 1)
                        elif inc:
                            ins.then_inc(self.semobj[inc[1]], 16)
                    if e == "sp":
                        for k, val in final_waits:
                            eh.wait_ge(self.semobj[k], val)
                getattr(block, engmap[e])(body)

    def mm(self, out, lhsT, rhs, start=True, stop=True):
        o, l, r = out.ap, lhsT.ap, rhs.ap

        def fn(e):
            return e.matmul(o, l, r, start=start, stop=stop)
        if stop:
            self.issue("pe", fn, [lhsT, rhs], [out], inc=True)
        else:
            self.issue("pe", fn, [lhsT, rhs], [], inc=False)
        return

    def mm_group(self, out, pairs):
        n = len(pairs)
        for i, (l, r) in enumerate(pairs):
            o, la, ra = out.ap, l.ap, r.ap
            st, sp_ = (i == 0), (i == n - 1)

            def fn(e, o=o, la=la, ra=ra, st=st, sp_=sp_):
                return e.matmul(o, la, ra, start=st, stop=sp_)
            if sp_ and n == 1:
                self.issue("pe", fn, [l, r], [out], inc=True)
            elif st:
                needs_v = V(out.ap, out.toks)
                self.issue_pe_first(fn, [l, r], needs_v)
            elif sp_:
                self.issue("pe", fn, [l, r], [out], inc=True)
            else:
                self.issue("pe", fn, [l, r], [], inc=False)

    def issue_pe_first(self, fn, reads, outv):
        eng = "pe"
        needs = self._needs(reads, [outv])
        waits = []
        wd = self.waited[eng]
        for k, val in needs.items():
            if k == eng:
                continue
            if wd.get(k, 0) < val:
                wd[k] = val
                waits.append((k, val))
        n = self.cnt[eng] + 1
        self.q[eng].append((waits, fn, False))
        for v in reads:
            for t in v.toks:
                if t.r.get(eng, 0) < n:
                    t.r[eng] = n

    def transpose(self, out, in_, ident):
        o, i, d = out.ap, in_.ap, ident.ap

        def fn(e):
            return e.transpose(o, i, d)
        self.issue("pe", fn, [in_, ident], [out])

    def act(self, out, in_, func, bias=None, scale=None, accum=None, eng="act"):
        o, i = out.ap, in_.ap
        reads = [in_]
        kw = {}
        if bias is not None:
            if isinstance(bias, V):
                reads.append(bias)
                kw["bias"] = bias.ap
            else:
                kw["bias"] = bias
        if scale is not None:
            if isinstance(scale, V):
                reads.append(scale)
                kw["scale"] = scale.ap
            else:
                kw["scale"] = scale
        writes = [out]
        if accum is not None:
            kw["accum_out"] = accum.ap
            writes.append(accum)

        def fn(e):
            return e.activation(o, i, func, **kw)
        self.issue("act", fn, reads, writes)

    def tt(self, out, a, b, op, eng="dve"):
        o, x, y = out.ap, a.ap, b.ap

        def fn(e):
            return e.tensor_tensor(o, x, y, op)
        self.issue(eng, fn, [a, b], [out])

    def ts(self, out, a, s1, op0, s2=None, op1=None, eng="dve", accum=None):
        o, x = out.ap, a.ap
        reads = [a]

        def cv(s):
            if isinstance(s, V):
                reads.append(s)
                return s.ap
            return s
        s1a = cv(s1)
        s2a = cv(s2)
        writes = [out]
        kw = {}
        if accum is not None:
            kw["accum_out"] = accum.ap
            writes.append(accum)

        def fn(e):
            if op1 is None:
                return e.tensor_scalar(o, x, s1a, None, op0, **kw)
            return e.tensor_scalar(o, x, s1a, s2a, op0, op1, **kw)
        self.issue(eng, fn, reads, writes)

    def stt(self, out, a, s, b, op0, op1, eng="dve"):
        o, x, y = out.ap, a.ap, b.ap
        reads = [a, b]
        if isinstance(s, V):
            reads.append(s)
            sa = s.ap
        else:
            sa = s

        def fn(e):
            return e.scalar_tensor_tensor(o, x, sa, y, op0, op1)
        self.issue(eng, fn, reads, [out])

    def copy(self, out, in_, eng="dve"):
        o, i = out.ap, in_.ap
        if eng == "act":
            def fn(e):
                return e.copy(o, i)
        else:
            def fn(e):
                return e.tensor_copy(o, i)
        self.issue(eng, fn, [in_], [out])

    def memset(self, out, val, eng="dve"):
        o = out.ap

        def fn(e):
            return e.memset(o, val)
        self.issue(eng, fn, [], [out])

    def recip(self, out, in_, eng="dve"):
        o, i = out.ap, in_.ap

        def fn(e):
            return e.reciprocal(o, i)
        self.issue(eng, fn, [in_], [out])

    def scan(self, out, d0, d1, init, op0, op1):
        o, a, b = out.ap, d0.ap, d1.ap
        reads = [d0, d1]
        if isinstance(init, V):
            reads.append(init)
            ia = init.ap
        else:
            ia = init

        def fn(e):
            return e.tensor_tensor_scan(o, a, b, ia, op0, op1)
        self.issue("dve", fn, reads, [out])

    def reduce(self, out, in_, op, axis=AX.X, eng="dve"):
        o, i = out.ap, in_.ap

        def fn(e):
            return e.tensor_reduce(o, i, axis, op)
        self.issue(eng, fn, [in_], [out])


def _prog_barrier(self):
    targets = {e: self.cnt[e] for e in ENGS if self.cnt[e] > 0}
    for s in self.dsems:
        if s[1] > 0:
            targets[s[0]] = s[1]
    for e in ENGS:
        wd = self.waited[e]
        waits = []
        for k, val in targets.items():
            if wd.get(k, 0) < val:
                wd[k] = val
                waits.append((k, val))
        if waits:
            self.q[e].append((waits, None, False))


@contextlib.contextmanager
def _prog_scope(self):
    old = self.es
    with contextlib.ExitStack() as es2:
        self.es = es2
        try:
            yield
        finally:
            self.barrier()
            self.es = old


Prog.barrier = _prog_barrier
Prog.scope = _prog_scope
D = 1024; T = 2048; NB = 2; TOK = NB * T; TB = 512; NTB = TOK // TB; KC = 8
DFF = 2816; NJ = 22; NIN = 12552
C_MLX, C_MLO, C_MLG = 0, 1024, 2048
C_RW = 2056; C_HG = 5384; C_GATE = 9480
EPS = 1e-6; RW_LN_EPS = 64e-5
LRW = 64; LHG = 64; LML = 128
WSC = 0.6065306597126334


def fm(v):
    v = np.asarray(v, np.float32).reshape(-1, 128)
    return np.ascontiguousarray(v.T)


def kxn(W):
    K, N = W.shape
    return np.ascontiguousarray(W.reshape(K // 128, 128, N).transpose(1, 0, 2))


class VecPack:
    def __init__(self):
        self.cols = []
        self.off = {}
        self.n = 0

    def add(self, name, arr):
        arr = np.asarray(arr, np.float32)
        assert arr.shape[0] == 128
        self.off[name] = (self.n, arr.shape[1])
        self.cols.append(arr)
        self.n += arr.shape[1]

    def build(self):
        return np.ascontiguousarray(np.concatenate(self.cols, axis=1))


def pack_vecs(inp):
    vp = VecPack()
    rep = lambda v: np.tile(np.asarray(v, np.float32)[None, :], (128, 1))
    for l in range(2):
        for s in range(3):
            vp.add("normw%d%d" % (l, s), fm(inp["norm_w"][l, s]))
        vp.add("adab%d" % l, fm(inp["ada_b"][l]))
        for tp in range(4):
            vp.add("convw%d%d" % (l, tp), fm(inp["ml_conv_w"][l, tp]))
        vp.add("convb%d" % l, fm(inp["ml_conv_b"][l]))
        vp.add("mlnw%d" % l, fm(inp["ml_norm_w"][l]))
        vp.add("mlskip%d" % l, fm(inp["ml_skip"][l]))
        vp.add("ib%d" % l, rep(inp["ml_i_b"][l]))
        vp.add("fb%d" % l, rep(inp["ml_f_b"][l]))
        vp.add("rwmu%d" % l, fm(inp["rw_mu"][l]))
        for nm in ("rw_w0", "rw_a0", "rw_k_k", "rw_k_a", "rw_ln_w", "rw_ln_b"):
            vp.add(nm + str(l), fm(inp[nm][l]))
        vp.add("rw_r_k%d" % l, fm(inp["rw_r_k"][l].reshape(-1)))
        vp.add("hglb%d" % l, fm(inp["hg_lb_logits"][l]))
        vp.add("hgnw%d" % l, fm(inp["hg_norm_w"][l]))
    vp.add("finw", fm(inp["final_norm_w"]))
    vp.add("rw_v0", fm(inp["rw_v0"][0]))
    mv = np.zeros((128, 1), np.float32)
    mv[:32, 0] = inp["rw_mu_vres"][0]
    vp.add("muvres", mv)
    return vp


def host_weights(inp):
    w = {}
    w["ada_w"] = np.ascontiguousarray(
        np.asarray(inp["ada_w"], np.float32).reshape(2, 8, 128, 18, 512).transpose(0, 3, 2, 1, 4))
    w["ffn_up"] = np.ascontiguousarray(
        np.asarray(inp["ffn_up"], np.float32).reshape(2, 2, 8, 128, 2 * DFF).transpose(0, 1, 3, 2, 4))
    w["ffn_down"] = np.ascontiguousarray(
        np.asarray(inp["ffn_down"], np.float32).reshape(2, 2, NJ, 128, D).transpose(0, 1, 3, 2, 4))
    w["w_in"] = np.ascontiguousarray(
        np.asarray(inp["w_in"], np.float32).reshape(2, 8, 128, NIN).transpose(0, 2, 1, 3))
    w["w_vres"] = np.ascontiguousarray(
        np.asarray(inp["w_in_vres"], np.float32).reshape(8, 128, 32).transpose(1, 0, 2))
    w["wq"] = np.ascontiguousarray(
        np.asarray(inp["ml_wq"], np.float32).reshape(2, 4, 2, 128, 256).transpose(0, 3, 1, 2, 4))
    w["wk"] = np.ascontiguousarray(
        np.asarray(inp["ml_wk"], np.float32).reshape(2, 4, 2, 128, 256).transpose(0, 3, 1, 2, 4))
    w["rw_wa2"] = np.ascontiguousarray(np.concatenate(
        [np.asarray(inp["rw_w2"], np.float32), np.asarray(inp["rw_a2"], np.float32)], axis=1))
    w["rw_g2"] = np.ascontiguousarray(np.asarray(inp["rw_g2"], np.float32))
    w["rw_v2"] = np.ascontiguousarray(np.asarray(inp["rw_v2"], np.float32)[0])
    w["w_branch"] = np.ascontiguousarray(
        np.asarray(inp["w_branch"], np.float32).reshape(2, 3, 8, 128, D).transpose(0, 3, 1, 2, 4))
    w["w_out"] = np.ascontiguousarray(
        np.asarray(inp["w_out"], np.float32).reshape(2, 8, 128, D).transpose(0, 2, 1, 3))
    return w


class DT:
    def __init__(self, P, name, shape, dt, tok_axis, kind="Internal", blk=TB):
        self.t = P.nc.dram_tensor(name, list(shape), dt, kind=kind)
        self.ap = self.t.ap()
        self.tok_axis = tok_axis
        self.blk = blk
        n = shape[tok_axis] // blk
        self.toks = [Tok("%s_%d" % (name, i)) for i in range(max(n, 1))]

    def v(self, t0, t1):
        i0, i1 = t0 // self.blk, (t1 - 1) // self.blk
        toks = tuple(self.toks[i0:i1 + 1])
        if self.tok_axis == 0:
            return V(self.ap[t0:t1], toks)
        return V(self.ap[:, t0:t1], toks)

    def all(self):
        return V(self.ap, tuple(self.toks))
class Ctx:
    pass


STAGE_LOG = []


def build_program(vec_off, nvec, stop_after=None, debug_outs=(), only=None):
    nc = bass.Bass("TRN2", target_bir_lowering=False)
    es = contextlib.ExitStack()
    with es:
        P = Prog(nc, es)
        K = Ctx()
        K.P = P
        K.debug = {}

        def din(name, shape, dt=F32):
            return P.dram(name, shape, dt, kind="ExternalInput")
        K.xT_in = din("xT", [D, TOK])
        K.cT = din("cT", [128, KC, NB])
        K.vecs_d = din("vecs", [128, nvec])
        K.ada_w = din("ada_w", [2, 18, 128, 8, 512])
        K.ffn_up = din("ffn_up", [2, 2, 128, 8, 2 * DFF])
        K.ffn_down = din("ffn_down", [2, 2, 128, NJ, D])
        K.w_in = din("w_in", [2, 128, 8, NIN])
        K.w_vres = din("w_vres", [128, 8, 32])
        K.wq = din("wq", [2, 128, 4, 2, 256])
        K.wk = din("wk", [2, 128, 4, 2, 256])
        K.rw_wa2 = din("rw_wa2", [2, 128, D])
        K.rw_g2 = din("rw_g2", [2, 128, D])
        K.rw_v2 = din("rw_v2", [32, D])
        K.w_branch = din("w_branch", [2, 128, 3, 8, D])
        K.w_out = din("w_out", [2, 128, 8, D])
        K.outT = DT(P, "outT", [D, TOK], F32, 1, kind="ExternalOutput")

        def scr(name, shape, dt, tok_axis):
            kind = "ExternalOutput" if name in debug_outs else "Internal"
            return DT(P, name, shape, dt, tok_axis, kind=kind)
        K.xs = scr("xs", [D, TOK], F32, 1)
        K.act = scr("actT", [DFF, TOK], BF16, 1)
        K.hT = scr("hT", [D, TOK], BF16, 1)
        K.yT = [scr("yT%d" % i, [D, TOK], BF16, 1) for i in range(3)]
        K.ml_qT = scr("ml_qT", [D, TOK], F32, 1)
        K.ml_kT = scr("ml_kT", [D, TOK], F32, 1)
        K.ml_skx = scr("ml_skx", [D, TOK], F32, 1)
        K.ml_ktok = scr("ml_ktok", [TOK, D], F32, 0)
        K.ml_vo = scr("ml_vo", [TOK, 2056], F32, 0)
        K.hg_qT = scr("hg_qT", [D, TOK], F32, 1)
        K.hg_kT = scr("hg_kT", [D, TOK], F32, 1)
        K.hg_dec = scr("hg_dec", [D, TOK // LHG], F32, 1) if False else None
        K.hg_decT = DT(P, "hg_decT", [D, TOK // LHG], F32, 1, blk=TB // LHG)
        K.hg_vtok = scr("hg_vtok", [TOK, D], F32, 0)
        K.hg_khat = scr("hg_khat", [TOK, D], F32, 0)
        K.hg_gtok = scr("hg_gtok", [TOK, D], F32, 0)
        for nm in ("rw_kapT", "rw_rhoT", "rw_betT", "rw_ktlT"):
            setattr(K, nm, scr(nm, [D, TOK], BF16, 1))
        for nm in ("rw_bonT", "rw_gT", "rw_vfT"):
            setattr(K, nm, scr(nm, [D, TOK], F32, 1))
        K.rw_gamT = DT(P, "rw_gamT", [D, TOK // LRW], F32, 1, blk=TB // LRW)
        for nm in ("rw_vtok", "rw_bhat", "rw_khat"):
            setattr(K, nm, scr(nm, [TOK, D], BF16, 0))
        K.rw_ytok = scr("rw_ytok", [TOK, D], F32, 0)

        K.vecs = P.sb([128, nvec], F32, "vecs_sb")
        P.dma(K.vecs, K.vecs_d)

        def vec(name):
            o, n = vec_off[name]
            return K.vecs[:, o:o + n]
        K.vec = vec
        K.epsb = P.sb([128, 4], F32, "epsb")
        P.memset(K.epsb[:, 0:1], EPS)
        P.memset(K.epsb[:, 1:2], RW_LN_EPS)
        P.memset(K.epsb[:, 2:3], 1.0)
        P.memset(K.epsb[:, 3:4], 0.0)
        K.ones = P.sb([128, 128], F32, "ones")
        P.memset(K.ones, 1.0)
        K.ident = P.sb([128, 128], F32, "ident")
        P.memset(K.ident, 0.0)
        io = P.sb([128, 128], F32, "iota_f")
        ip = P.sb([128, 1], F32, "iota_p")
        io_i = P.sb([128, 128], mybir.dt.int32, "iota_fi")
        ip_i = P.sb([128, 1], mybir.dt.int32, "iota_pi")

        def iota(out, pattern, cm):
            o = out.ap

            def fn(e):
                return e.iota(o, pattern, base=0, channel_multiplier=cm)
            P.issue("pool", fn, [], [out])
        iota(io_i, [[1, 128]], 0)
        iota(ip_i, [[0, 1]], 1)
        P.copy(io, io_i)
        P.copy(ip, ip_i)
        K.iota_f = io
        K.iota_p = ip
        P.ts(K.ident, io, ip[:, 0:1], ALU.is_equal)
        K.identb = P.sb([128, 128], BF16, "identb")
        P.copy(K.identb, K.ident)
        K.tri = P.sb([128, 128], F32, "tri")
        P.ts(K.tri, io, ip[:, 0:1], ALU.is_ge)
        K.tris = P.sb([128, 128], F32, "tris")
        P.ts(K.tris, io, ip[:, 0:1], ALU.is_gt)
        K.lows = P.sb([128, 128], F32, "lows")
        P.ts(K.lows, io, ip[:, 0:1], ALU.is_lt)
        K.blk64 = P.sb([128, 128], F32, "blk64")
        P.memset(K.blk64, 0.0)
        P.memset(K.blk64[0:64, 0:64], 1.0)
        P.memset(K.blk64[64:128, 64:128], 1.0)
        K.rmask = P.sb([128, TB], F32, "rmask")
        P.memset(K.rmask, 1.0)
        P.memset(K.rmask.re("p (c l) -> p c l", l=64)[:, :, 0:1], 0.0)

        K.modT = [P.sb([128, 72, NB], F32, "modT%d" % l) for l in range(2)]
        K.sc1 = [[P.sb([128, 8, NB], F32) for s in range(3)] for l in range(2)]
        K.gt = [[P.sb([128, 8, NB], F32) for s in range(3)] for l in range(2)]
        stage_mod(K)
        stages = [
            ("copyx", lambda: stage_copyx(K)),
        ]
        for l in range(2):
            stages += [
                ("ffn%d0" % l, lambda l=l: stage_ffn(K, l, 0)),
                ("normmix%d" % l, lambda l=l: stage_normmix(K, l)),
                ("inml%d" % l, lambda l=l: stage_in_ml(K, l)),
                ("mlrec%d" % l, lambda l=l: stage_ml_rec(K, l)),
                ("inhg%d" % l, lambda l=l: stage_in_hg(K, l)),
                ("hgrec%d" % l, lambda l=l: stage_hg_rec(K, l)),
                ("inrw%d" % l, lambda l=l: stage_in_rw(K, l)),
                ("rwrec%d" % l, lambda l=l: stage_rw_rec(K, l)),
                ("mix%d" % l, lambda l=l: stage_mix(K, l)),
                ("ffn%d1" % l, lambda l=l: stage_ffn(K, l, 1)),
            ]
        stages.append(("final", lambda: stage_final(K)))
        for name, fn in stages:
            if only is not None and name not in only:
                continue
            fn()
            STAGE_LOG.append((name, dict(P.cnt)))
            if stop_after == name:
                break
        P.barrier()
        fin = [(k, v) for k, v, _ in P.dsems if v > 0]
        P.emit(final_waits=fin)
    return nc


def stage_mod(K):
    P = K.P
    with P.scope():
        cT = P.sb([128, KC, NB])
        P.dma(cT, K.cT)
        cond = P.sb([128, KC, NB])
        P.act(cond, cT, AF.Silu)
        wb = [P.sb([128, 8, 512]) for _ in range(2)]
        for l in range(2):
            ps = P.ps([128, 72, NB])
            for pc in range(18):
                wt = wb[pc % 2]
                P.dma(wt, K.ada_w[l, pc])
                for jj in range(4):
                    j = pc * 4 + jj
                    P.mm_group(ps[:, j, :], [(wt[:, kc, jj * 128:(jj + 1) * 128], cond[:, kc, :]) for kc in range(KC)])
            P.tt(K.modT[l], ps, K.vec("adab%d" % l).un(2).bc([128, 72, NB]), ALU.add)
            for s in range(3):
                m = K.modT[l]
                nw = K.vec("normw%d%d" % (l, s))
                P.ts(K.sc1[l][s], m[:, s * 24 + 8:s * 24 + 16, :], 1.0, ALU.add)
                P.tt(K.sc1[l][s], K.sc1[l][s], nw.un(2).bc([128, 8, NB]), ALU.mult)
                f = 1.0 if s == 1 else 0.5
                P.ts(K.gt[l][s], m[:, s * 24 + 16:s * 24 + 24, :], 1.0, ALU.add, f, ALU.mult)


def stage_copyx(K):
    P = K.P
    for tb in range(NTB):
        P.dma(K.xs.v(tb * TB, (tb + 1) * TB), K.xT_in[:, tb * TB:(tb + 1) * TB])


def norm_block(K, xb, hb, l, s, b, sq, ssp, sd, rstd, hn, plain_w=None, out_f32=None):
    P = K.P
    P.act(sq, xb, AF.Square)
    P.mm_group(ssp, [(K.ones, sq[:, dc, :]) for dc in range(8)])
    P.act(sd, ssp, AF.Sqrt, bias=K.epsb[:, 0:1], scale=1.0 / D)
    P.recip(rstd, sd)
    P.tt(hn, xb, rstd.un(1).bc([128, 8, TB]), ALU.mult)
    if plain_w is not None:
        P.tt(out_f32, hn, plain_w.un(2).bc([128, 8, TB]), ALU.mult)
        return
    sh = K.modT[l][:, s * 24:s * 24 + 8, :]
    for dc in range(8):
        P.ts(hb[:, dc, :], hn[:, dc, :], K.sc1[l][s][:, dc, b:b + 1], ALU.mult, sh[:, dc, b:b + 1], ALU.add)


def alloc_norm_tmps(K):
    P = K.P
    if not hasattr(K, "epsb"):
        pass
    sq = P.sb([128, 8, TB])
    sd = P.sb([128, TB])
    return dict(sq=sq, ssp=P.ps([128, TB]), sd=sd, rstd=sd, hn=sq)


def xview(dt_, tb):
    return dt_.v(tb * TB, (tb + 1) * TB).re("(c p) t -> p c t", p=128)


def stage_ffn(K, l, f):
    P = K.P
    s = 0 if f == 0 else 2
    with P.scope():
        wup = P.sb([128, 8, 2 * DFF], BF16)
        HW_ = DFF // 2
        wa = [P.sub(wup[:, :, g * HW_:(g + 1) * HW_]) for g in range(2)]
        wb = [P.sub(wup[:, :, DFF + g * HW_:DFF + (g + 1) * HW_]) for g in range(2)]
        for g in range(2):
            P.dma(wa[g], K.ffn_up[l, f][:, :, g * HW_:(g + 1) * HW_], eng="pool")
            P.dma(wb[g], K.ffn_up[l, f][:, :, DFF + g * HW_:DFF + (g + 1) * HW_], eng="pool")
        tm = alloc_norm_tmps(K)
        xbs = [P.sb([128, 8, TB]) for _ in range(2)]
        hbs = [P.sb([128, 8, TB], BF16)] * 2
        acts = [P.sb([128, NJ, TB], BF16) for _ in range(2)]
        sgs = [P.sb([128, TB]) for _ in range(2)]
        pas = [P.ps([128, TB]) for _ in range(2)]
        pbs = [P.ps([128, TB]) for _ in range(2)]
        for tb in range(NTB):
            b = tb // 4
            xb, hb, ab = xbs[tb % 2], hbs[tb % 2], acts[tb % 2]
            P.dma(xb, xview(K.xs, tb))
            norm_block(K, xb, hb, l, s, b, **tm)
            for j in range(NJ):
                pa, pb, sg = pas[j % 2], pbs[j % 2], sgs[j % 2]
                g_, jj = j // 11, j % 11
                P.mm_group(pa, [(wa[g_][:, kc, jj * 128:(jj + 1) * 128], hb[:, kc, :]) for kc in range(8)])
                P.mm_group(pb, [(wb[g_][:, kc, jj * 128:(jj + 1) * 128], hb[:, kc, :]) for kc in range(8)])
                P.act(sg, pa, AF.Silu)
                P.tt(ab[:, j, :], sg, pb, ALU.mult)
            P.dma(xview(K.act, tb), ab)
    with P.scope():
        wdn = P.sb([128, NJ, D], BF16)
        P.dma(wdn, K.ffn_down[l, f], eng="pool")
        xbs = [P.sb([128, 8, TB]) for _ in range(2)]
        xos = [P.sb([128, 8, TB]) for _ in range(2)]
        abs_ = [P.sb([128, NJ, TB], BF16) for _ in range(2)]
        pos = [P.ps([128, TB]) for _ in range(2)]
        for tb in range(NTB):
            b = tb // 4
            xb, xo, ab = xbs[tb % 2], xos[tb % 2], abs_[tb % 2]
            P.dma(ab, xview(K.act, tb))
            P.dma(xb, xview(K.xs, tb))
            for m in range(8):
                po = pos[m % 2]
                P.mm_group(po, [(wdn[:, kc, m * 128:(m + 1) * 128], ab[:, kc, :]) for kc in range(NJ)])
                P.stt(xo[:, m, :], po, K.gt[l][s][:, m, b:b + 1], xb[:, m, :], ALU.mult, ALU.add)
            P.dma(xview(K.xs, tb), xo)


def stage_final(K):
    P = K.P
    with P.scope():
        tm = alloc_norm_tmps(K)
        xbs = [P.sb([128, 8, TB]) for _ in range(2)]
        obs = [P.sb([128, 8, TB]) for _ in range(2)]
        for tb in range(NTB):
            xb, ob = xbs[tb % 2], obs[tb % 2]
            P.dma(xb, xview(K.xs, tb))
            norm_block(K, xb, None, 0, 0, 0, plain_w=K.vec("finw"), out_f32=ob, **tm)
            P.dma(xview(K.outT, tb), ob)


def stage_normmix(K, l):
    P = K.P
    with P.scope():
        tm = alloc_norm_tmps(K)
        xbs = [P.sb([128, 8, TB]) for _ in range(2)]
        hbs = [P.sb([128, 8, TB], BF16) for _ in range(2)]
        for tb in range(NTB):
            xb, hb = xbs[tb % 2], hbs[tb % 2]
            P.dma(xb, xview(K.xs, tb))
            norm_block(K, xb, hb, l, 1, tb // 4, **tm)
            P.dma(xview(K.hT, tb), hb)
def load_w_cols(P, dst, src, c0, c1, eng="pool"):
    n = c1 - c0
    h = n // 2
    P.dma(dst[:, :, 0:h], src[:, :, c0:c0 + h], eng=eng)
    P.dma(dst[:, :, h:n], src[:, :, c0 + h:c1], eng=eng)


def tmview(dt_, t0, n):
    return dt_.v(t0, t0 + n)


def stage_in_ml(K, l):
    P = K.P
    with P.scope():
        wml = P.sb([128, 8, 2056], BF16)
        load_w_cols(P, wml, K.w_in[l], 0, 2056)
        wq = P.sb([128, 4, 2, 256], BF16)
        wk = P.sb([128, 4, 2, 256], BF16)
        P.dma(wq, K.wq[l], eng="pool")
        P.dma(wk, K.wk[l], eng="pool")
        cw = [K.vec("convw%d%d" % (l, tp)) for tp in range(4)]
        cb = K.vec("convb%d" % l)
        skip = K.vec("mlskip%d" % l)
        hbs = [P.sb([128, 8, TB], BF16) for _ in range(2)]
        xms = [P.sb([128, 8, TB + 3]) for _ in range(2)]
        acc = P.sb([128, 8, TB])
        xc = P.sb([128, 8, TB])
        xcb = P.sb([128, 8, TB], BF16)
        qst = P.sb([128, 8, TB])
        kst = P.sb([128, 8, TB])
        ktk = [P.sb([128, D]) for _ in range(2)]
        vos = [P.sb([128, 2056]) for _ in range(2)]
        psA = [P.ps([128, TB]) for _ in range(2)]
        psB = [P.ps([128, 2, 512]) for _ in range(2)]
        psC = P.ps([128, 8])
        for tb in range(NTB):
            hb, xm = hbs[tb % 2], xms[tb % 2]
            xmp = xms[(tb + 1) % 2]
            P.dma(hb, xview(K.hT, tb))
            if tb % 4 == 0:
                P.memset(xm[:, :, 0:3], 0.0)
            else:
                P.copy(xm[:, :, 0:3], xmp[:, :, TB:TB + 3])
            for dc in range(8):
                ps = psA[dc % 2]
                P.mm_group(ps, [(wml[:, kc, dc * 128:(dc + 1) * 128], hb[:, kc, :]) for kc in range(8)])
                P.copy(xm[:, dc, 3:TB + 3], ps, eng="act")
            for dc in range(8):
                P.ts(acc[:, dc, :], xm[:, dc, 0:TB], cw[0][:, dc:dc + 1], ALU.mult, cb[:, dc:dc + 1], ALU.add)
                for tp in range(1, 4):
                    P.stt(acc[:, dc, :], xm[:, dc, tp:tp + TB], cw[tp][:, dc:dc + 1], acc[:, dc, :], ALU.mult, ALU.add)
            P.act(xc, acc, AF.Silu)
            P.act(xcb, acc, AF.Silu)
            P.tt(acc, xc, skip.un(2).bc([128, 8, TB]), ALU.mult, eng="pool")
            P.dma(xview(K.ml_skx, tb), acc)
            for (w_, st_, dst) in ((wq, qst, K.ml_qT), (wk, kst, K.ml_kT)):
                for h in range(4):
                    for ec in range(2):
                        ps = psA[ec % 2]
                        P.mm_group(ps, [(w_[:, h, dck, ec * 128:(ec + 1) * 128], xcb[:, 2 * h + dck, :]) for dck in range(2)])
                        if ec == 0:
                            P.copy(st_[:, 2 * h + ec, :], ps, eng="act")
                        else:
                            P.copy(st_[:, 2 * h + ec, :], ps, eng="dve")
                P.dma(xview(dst, tb), st_)
            for tt_ in range(4):
                t0 = tb * TB + tt_ * 128
                tsl = slice(tt_ * 128, (tt_ + 1) * 128)
                kt = ktk[tt_ % 2]
                pb = psB[0]
                for h in range(4):
                    P.mm_group(pb[:, h // 2, (h % 2) * 256:(h % 2) * 256 + 256],
                               [(xcb[:, 2 * h + dck, tsl], wk[:, h, dck, :]) for dck in range(2)])
                P.copy(kt, pb.re("p a b -> p (a b)"), eng="act")
                P.dma(tmview(K.ml_ktok, t0, 128), kt)
                vo = vos[tt_ % 2]
                for half in range(2):
                    pb2 = psB[1] if half == 0 else psB[0]
                    for q in range(2):
                        c0 = half * 1024 + q * 512
                        P.mm_group(pb2[:, q, :], [(hb[:, kc, tsl], wml[:, kc, c0:c0 + 512]) for kc in range(8)])
                    if half == 0:
                        P.copy(vo[:, 0:1024], pb2.re("p a b -> p (a b)"), eng="dve")
                    else:
                        P.copy(vo[:, 1024:2048], pb2.re("p a b -> p (a b)"), eng="act")
                P.mm_group(psC, [(hb[:, kc, tsl], wml[:, kc, 2048:2056]) for kc in range(8)])
                P.copy(vo[:, 2048:2056], psC, eng="dve")
                P.dma(tmview(K.ml_vo, t0, 128), vo)


def stage_ml_rec(K, l):
    P = K.P
    NCH = T // LML
    with P.scope():
        ib = K.vec("ib%d" % l)
        fb = K.vec("fb%d" % l)
        mlnw = K.vec("mlnw%d" % l)
        gin = P.sb([128, NCH, 8])
        sp = P.sb([128, NCH, 4])
        li = P.sb([128, NCH, 4])
        et = P.sb([128, NCH, 4])
        wv = P.sb([128, NCH, 4])
        dec = P.sb([128, NCH, 4])
        wend = P.sb([128, NCH, 4])
        tmpg = P.sb([128, NCH, 4])
        C = P.sb([128, 4, 2, 257])
        qTs = [P.sb([128, 8, LML]) for _ in range(2)]
        kTs = [P.sb([128, 8, LML]) for _ in range(2)]
        kts = [P.sb([128, D]) for _ in range(2)]
        vxs = [P.sb([128, 4, 257]) for _ in range(2)]
        for vx in vxs:
            P.memset(vx[:, :, 256:257], 1.0)
        osb = [P.sb([128, D]) for _ in range(2)]
        sko = [P.sb([128, 8, LML]) for _ in range(2)]
        sgo = P.sb([128, D])
        sts = [P.sb([128, 128]) for _ in range(2)]
        hh = P.sb([128, 4, 256])
        cen = P.sb([128, 4, 256])
        sq = P.sb([128, 4, 256])
        sm = P.sb([128, 16])
        kw = P.sb([128, 4, 256])
        yst = [P.sb([128, 8, LML], BF16) for _ in range(2)]
        stp = [P.ps([128, 128]) for _ in range(2)]
        nps = P.ps([128, 4, 512])
        npsv = [P.sub(nps[:, h, 0:257]) for h in range(4)]
        psX = [P.ps([128, 512]) for _ in range(2)]
        for b in range(NB):
            T0 = b * T
            P.dma(gin, K.ml_vo.v(T0, T0 + T)[:, 2048:2056].re("(c t) g -> t c g", t=LML))
            P.tt(tmpg, gin[:, :, 4:8], fb.un(1).bc([128, NCH, 4]), ALU.add)
            P.act(tmpg, tmpg, AF.Exp, scale=-1.0)
            P.act(sp, tmpg, AF.Ln, bias=K.epsb[:, 2:3])
            P.tt(li, gin[:, :, 0:4], ib.un(1).bc([128, NCH, 4]), ALU.add)
            bps = psX[0][:, 0:NCH * 4]
            tps = psX[1][:, 0:NCH * 4]
            P.mm_group(bps, [(K.tri, sp.re("p c h -> p (c h)"))])
            P.mm_group(tps, [(K.ones, sp.re("p c h -> p (c h)"))])
            P.act(et.re("p c h -> p (c h)"), bps, AF.Exp, scale=-1.0)
            P.tt(tmpg.re("p c h -> p (c h)"), li.re("p c h -> p (c h)"), bps, ALU.add)
            P.act(wv, tmpg, AF.Exp)
            P.ts(wv, wv, 1.0 / 16.0, ALU.mult)
            P.act(dec.re("p c h -> p (c h)"), tps, AF.Exp, scale=-1.0)
            P.tt(wend, wv, dec, ALU.mult)
            P.memset(C, 0.0)
            for c in range(NCH):
                t0 = T0 + c * LML
                qT, kT, kt, vx, ob, sk = qTs[c % 2], kTs[c % 2], kts[c % 2], vxs[c % 2], osb[c % 2], sko[c % 2]
                P.dma(qT, K.ml_qT.v(t0, t0 + LML).re("(c p) t -> p c t", p=128))
                P.dma(kT, K.ml_kT.v(t0, t0 + LML).re("(c p) t -> p c t", p=128))
                P.dma(kt, tmview(K.ml_ktok, t0, LML))
                vo = K.ml_vo.v(t0, t0 + LML)
                P.dma(vx[:, :, 0:256], vo[:, 0:1024].re("t (h e) -> t h e", h=4))
                P.dma(ob, vo[:, 1024:2048])
                P.dma(sk, K.ml_skx.v(t0, t0 + LML).re("(c p) t -> p c t", p=128))
                P.act(sgo, ob, AF.Sigmoid)
                for h in range(4):
                    sp_ = stp[h % 2]
                    st = sts[h % 2]
                    P.mm_group(sp_, [(kT[:, 2 * h + dck, :], qT[:, 2 * h + dck, :]) for dck in range(2)])
                    P.stt(st, sp_, wv[:, c, h:h + 1], K.tri, ALU.mult, ALU.mult)
                    P.mm_group(npsv[h], [(st, vx[:, h, :]),
                                         (qT[:, 2 * h, :], C[:, h, 0, :]),
                                         (qT[:, 2 * h + 1, :], C[:, h, 1, :])])
                for h in range(4):
                    P.tt(sm[:, h:h + 1], npsv[h][:, 256:257], et[:, c, h:h + 1], ALU.mult)
                P.ts(sm[:, 4:8], sm[:, 0:4], -1.0, ALU.mult)
                P.tt(sm[:, 4:8], sm[:, 4:8], sm[:, 0:4], ALU.max)
                P.ts(sm[:, 4:8], sm[:, 4:8], 1.0, ALU.max)
                P.recip(sm[:, 8:12], sm[:, 4:8])
                P.tt(sm[:, 12:16], sm[:, 8:12], et[:, c, :], ALU.mult)
                for h in range(4):
                    P.stt(hh[:, h, :], npsv[h][:, 0:256], sm[:, 12 + h:13 + h], sgo[:, h * 256:(h + 1) * 256],
                          ALU.mult, ALU.mult)
                P.reduce(sm[:, 0:4], hh, ALU.add)
                P.ts(sm[:, 0:4], sm[:, 0:4], 1.0 / 256.0, ALU.mult)
                P.tt(cen, hh, sm[:, 0:4].un(2).bc([128, 4, 256]), ALU.subtract)
                P.tt(sq, cen, cen, ALU.mult, eng="pool")
                P.reduce(sm[:, 4:8], sq, ALU.add)
                P.act(sm[:, 8:12], sm[:, 4:8], AF.Sqrt, bias=K.epsb[:, 0:1], scale=1.0 / 256.0)
                P.recip(sm[:, 8:12], sm[:, 8:12])
                P.tt(cen, cen, sm[:, 8:12].un(2).bc([128, 4, 256]), ALU.mult)
                cenf = cen.re("p h e -> p (h e)")
                ys = yst[c % 2]
                for half in range(2):
                    px = psX[half]
                    for jj in range(4):
                        j = half * 4 + jj
                        P.transpose(px[:, jj * 128:(jj + 1) * 128], cenf[:, j * 128:(j + 1) * 128], K.ident)
                    for jj in range(4):
                        j = half * 4 + jj
                        P.stt(ys[:, j, :], px[:, jj * 128:(jj + 1) * 128], mlnw[:, j:j + 1], sk[:, j, :],
                              ALU.mult, ALU.add)
                P.dma(K.yT[0].v(t0, t0 + LML).re("(c p) t -> p c t", p=128), ys)
                for h in range(4):
                    P.ts(kw[:, h, :], kt[:, h * 256:(h + 1) * 256], wend[:, c, h:h + 1], ALU.mult, eng="pool")
                for h in range(4):
                    for dck in range(2):
                        px = psX[dck]
                        P.mm_group(px[:, 0:257], [(kw[:, h, dck * 128:(dck + 1) * 128], vx[:, h, :])])
                        P.stt(C[:, h, dck, :], C[:, h, dck, :], dec[:, c, h:h + 1], px[:, 0:257], ALU.mult, ALU.add)


def stage_in_hg(K, l):
    P = K.P
    with P.scope():
        whg = P.sb([128, 8, 4096], BF16)
        load_w_cols(P, whg, K.w_in[l], C_HG, C_HG + 4096)
        lb = P.sb([128, 8])
        oml = P.sb([128, 8])
        noml = P.sb([128, 8])
        if l == 0:
            P.memset(lb, 0.0)
        else:
            P.tt(lb, K.vec("hglb1"), K.vec("hglb0"), ALU.subtract)
            P.act(lb, lb, AF.Sigmoid)
        P.ts(oml, lb, -1.0, ALU.mult, 1.0, ALU.add)
        P.ts(noml, oml, -1.0, ALU.mult)
        hbs = [P.sb([128, 8, TB], BF16) for _ in range(2)]
        q = P.sb([128, 8, TB])
        sg = P.sb([128, 8, TB])
        kk = P.sb([128, 8, TB])
        bc_ = P.sb([128, 8, TB])
        eb = P.sb([128, 8, TB])
        enb = P.sb([128, 8, TB])
        dec = P.sb([128, 8, 8])
        kst = [P.sb([128, D])] * 2
        vst = [P.sb([128, D])] * 2
        gst = [P.sb([128, D])] * 2
        psA = [P.ps([128, TB]) for _ in range(2)]
        psF = [P.ps([128, TB]) for _ in range(2)]
        psT = [P.ps([128, 2, 512]) for _ in range(2)]
        for tb in range(NTB):
            hb = hbs[tb % 2]
            P.dma(hb, xview(K.hT, tb))
            for dc in range(8):
                pq, pf = psA[dc % 2], psF[dc % 2]
                P.mm_group(pq, [(whg[:, kc, dc * 128:(dc + 1) * 128], hb[:, kc, :]) for kc in range(8)])
                P.mm_group(pf, [(whg[:, kc, 1024 + dc * 128:1024 + (dc + 1) * 128], hb[:, kc, :]) for kc in range(8)])
                P.act(q[:, dc, :], pq, AF.Silu)
                P.act(sg[:, dc, :], pf, AF.Sigmoid)
                P.ts(kk[:, dc, :], sg[:, dc, :], noml[:, dc:dc + 1], ALU.mult, oml[:, dc:dc + 1], ALU.add, eng="pool")
                P.ts(sg[:, dc, :], sg[:, dc, :], oml[:, dc:dc + 1], ALU.mult, lb[:, dc:dc + 1], ALU.add)
            P.ts(sg, sg, 1e-30, ALU.max)
            P.act(sg, sg, AF.Ln)
            for dc in range(8):
                P.scan(bc_[:, dc, :], K.rmask, sg[:, dc, :], 0.0, ALU.mult, ALU.add)
            P.act(eb, bc_, AF.Exp)
            P.act(enb, bc_, AF.Exp, scale=-1.0)
            P.tt(q, q, eb, ALU.mult)
            P.tt(kk, kk, enb, ALU.mult, eng="pool")
            P.copy(dec, eb.re("p c (n l) -> p c n l", l=LHG)[:, :, :, LHG - 1])
            P.tt(enb.re("p c (n l) -> p c n l", l=LHG), kk.re("p c (n l) -> p c n l", l=LHG),
                 dec.un(3).bc([128, 8, 8, LHG]), ALU.mult)
            P.dma(xview(K.hg_qT, tb), q)
            P.dma(xview(K.hg_kT, tb), kk)
            P.dma(K.hg_decT.v(tb * 8, tb * 8 + 8).re("(c p) n -> p c n", p=128), dec)
            for tt_ in range(4):
                t0 = tb * TB + tt_ * 128
                tsl = slice(tt_ * 128, (tt_ + 1) * 128)
                ks, vs, gs = kst[tt_ % 2], vst[tt_ % 2], gst[tt_ % 2]
                px = psT[0]
                for dc in range(8):
                    P.transpose(px[:, dc // 4, (dc % 4) * 128:(dc % 4) * 128 + 128], enb[:, dc, tsl], K.ident)
                P.copy(ks, px.re("p a b -> p (a b)"), eng="act")
                P.dma(tmview(K.hg_khat, t0, 128), ks)
                pv = psT[1]
                for qq in range(2):
                    c0 = 2048 + qq * 512
                    P.mm_group(pv[:, qq, :], [(hb[:, kc, tsl], whg[:, kc, c0:c0 + 512]) for kc in range(8)])
                P.copy(vs, pv.re("p a b -> p (a b)"), eng="dve")
                P.dma(tmview(K.hg_vtok, t0, 128), vs)
                pg = psT[0]
                for qq in range(2):
                    c0 = 3072 + qq * 512
                    P.mm_group(pg[:, qq, :], [(hb[:, kc, tsl], whg[:, kc, c0:c0 + 512]) for kc in range(8)])
                P.act(gs, pg.re("p a b -> p (a b)"), AF.Sigmoid)
                P.dma(tmview(K.hg_gtok, t0, 128), gs)


def stage_hg_rec(K, l):
    P = K.P
    NCH = T // LHG
    CG = 4
    with P.scope():
        hgnw = K.vec("hgnw%d" % l)
        S = P.sb([128, 8, 128])
        decall = P.sb([128, 8, NCH])
        qts = [P.sb([128, 8, CG * LHG]) for _ in range(2)]
        kts = [P.sb([128, 8, CG * LHG]) for _ in range(2)]
        vts = [P.sb([64, CG, D]) for _ in range(2)]
        khs = [P.sb([64, CG, D]) for _ in range(2)]
        gts = [P.sb([64, CG, D]) for _ in range(2)]
        at = P.sb([64, 8, 64])
        sq = P.sb([64, 8, 128])
        on = P.sb([64, 8, 128])
        sm = P.sb([64, 16])
        yst = [P.sb([128, 8, CG * LHG], BF16) for _ in range(2)]
        atp = P.ps([64, 8, 64])
        ops = P.ps([64, 2, 512])
        tp = P.ps([128, 8, 64])
        sps = P.ps([128, 2, 512])
        tri64 = K.tri[0:64, 0:64]
        id64 = K.ident[0:64, 0:64]
        for b in range(NB):
            T0 = b * T
            P.memset(S, 0.0)
            P.dma(decall, K.hg_decT.v(b * NCH, (b + 1) * NCH).re("(c p) n -> p c n", p=128))
            for cg in range(NCH // CG):
                t0 = T0 + cg * CG * LHG
                t1 = t0 + CG * LHG
                qt, kt, vt, kh, gt, ys = qts[cg % 2], kts[cg % 2], vts[cg % 2], khs[cg % 2], gts[cg % 2], yst[cg % 2]
                P.dma(qt, K.hg_qT.v(t0, t1).re("(c p) t -> p c t", p=128))
                P.dma(kt, K.hg_kT.v(t0, t1).re("(c p) t -> p c t", p=128))
                P.dma(vt, K.hg_vtok.v(t0, t1).re("(c t) e -> t c e", t=LHG))
                P.dma(kh, K.hg_khat.v(t0, t1).re("(c t) e -> t c e", t=LHG))
                P.dma(gt, K.hg_gtok.v(t0, t1).re("(c t) e -> t c e", t=LHG))
                for ci in range(CG):
                    c = cg * CG + ci
                    cs = slice(ci * LHG, (ci + 1) * LHG)
                    for h in range(8):
                        P.mm_group(atp[:, h, :], [(kt[:, h, cs], qt[:, h, cs])])
                    P.tt(at, atp, tri64.un(1).bc([64, 8, 64]), ALU.mult)
                    for h in range(8):
                        hs = slice(h * 128, (h + 1) * 128)
                        P.mm_group(ops[:, h // 4, (h % 4) * 128:(h % 4) * 128 + 128],
                                   [(at[:, h, :], vt[:, ci, hs]), (qt[:, h, cs], S[:, h, :])])
                    opf = ops.re("p a (h e) -> p (a h) e", e=128)
                    P.act(sq, opf, AF.Square)
                    P.reduce(sm[:, 0:8], sq, ALU.add)
                    P.act(sm[:, 8:16], sm[:, 0:8], AF.Sqrt, bias=K.epsb[0:64, 0:1], scale=1.0 / 128.0)
                    P.recip(sm[:, 8:16], sm[:, 8:16])
                    P.tt(on, opf, sm[:, 8:16].un(2).bc([64, 8, 128]), ALU.mult)
                    P.tt(on, on, gt[:, ci, :].re("p (h e) -> p h e", e=128), ALU.mult, eng="pool")
                    for h in range(8):
                        P.transpose(tp[:, h, :], on[:, h, :], id64)
                    P.tt(ys[:, :, cs], tp, hgnw.un(2).bc([128, 8, 64]), ALU.mult)
                    for h in range(8):
                        hs = slice(h * 128, (h + 1) * 128)
                        P.mm_group(sps[:, h // 4, (h % 4) * 128:(h % 4) * 128 + 128], [(kh[:, ci, hs], vt[:, ci, hs])])
                    P.tt(S, S, decall[:, :, c:c + 1].bc([128, 8, 128]), ALU.mult)
                    P.tt(S, S, sps.re("p a (h e) -> p (a h) e", e=128), ALU.add)
                P.dma(K.yT[2].v(t0, t1).re("(c p) t -> p c t", p=128), ys)


def stage_in_rw(K, l):
    P = K.P
    NZ = 3328
    with P.scope():
        wrw = P.sb([128, 8, NZ], BF16)
        load_w_cols(P, wrw, K.w_in[l], C_RW, C_RW + NZ)
        wa2 = P.sb([128, D])
        g2 = P.sb([128, D])
        P.dma(wa2, K.rw_wa2[l])
        P.dma(g2, K.rw_g2[l])
        if l == 1:
            wvr = P.sb([128, 8, 32], BF16)
            P.dma(wvr, K.w_vres, eng="pool")
            v2 = P.sb([32, D])
            P.dma(v2, K.rw_v2)
            v0 = K.vec("rw_v0")
            muv = K.vec("muvres")
        mu = K.vec("rwmu%d" % l)
        w0, a0 = K.vec("rw_w0%d" % l), K.vec("rw_a0%d" % l)
        k_k, k_a, r_k = K.vec("rw_k_k%d" % l), K.vec("rw_k_a%d" % l), K.vec("rw_r_k%d" % l)
        omka = P.sb([128, 8])
        P.ts(omka, k_a, -1.0, ALU.mult, 1.0, ALU.add)
        prevs = [P.sb([128, 4]) for _ in range(9)]
        hb = P.sb([128, 8, TB], BF16)
        zl = P.sb([128, 3, TB + 1])
        P.memset(zl, 0.0)
        dl = P.sb([128, 3, TB])
        twa = P.sb([128, TB])
        sgl = P.sb([128, TB])
        vl = P.sb([32, TB])
        class SB:
            pass
        sets = []
        for si in range(2):
            s_ = SB()
            s_.z3 = P.sb([128, 3, TB + 1])
            s_.d3 = P.sb([128, 3, TB])
            s_.W = [P.sb([128, TB]) for _ in range(12)]
            s_.ob = [P.sb([128, TB], BF16) for _ in range(7)]
            s_.gam = P.sb([128, 8])
            s_.vfb = P.sb([128, TB])
            s_.stg = [P.sb([128, 4, 128], BF16) for _ in range(3)]
            s_.psZ = P.ps([128, TB])
            s_.psL = P.ps([128, TB])
            s_.psT = P.ps([128, TB])
            s_.psX = P.ps([128, 2 * TB], BF16)
            sets.append(s_)
        psA0 = sets[0].psZ

        def jstream(tb, j, s_):
            js = slice(j * 128, (j + 1) * 128)
            z3, d3 = s_.z3, s_.d3
            (kk, sq, t1, kmod, bb, rk, sgw, a_, cum, G, Gi, Gp) = s_.W
            bon, gg = sq, rk
            prev = prevs[j]
            ps = s_.psZ
            for i in range(3):
                ch = i * 8 + j
                P.mm_group(ps, [(wrw[:, kc, ch * 128:(ch + 1) * 128], hb[:, kc, :]) for kc in range(8)])
                P.copy(z3[:, i, 1:TB + 1], ps, eng="act")
                yield
            P.copy(z3[:, :, 0:1], prev[:, 0:3].un(2))
            P.copy(prev[:, 0:3].un(2), z3[:, :, TB:TB + 1])
            P.tt(d3, z3[:, :, 0:TB], z3[:, :, 1:TB + 1], ALU.subtract)
            yield
            for i in range(3):
                ch = i * 8 + j
                P.stt(d3[:, i, :], d3[:, i, :], mu[:, ch:ch + 1], z3[:, i, 1:TB + 1], ALU.mult, ALU.add)
            yield
            r, k, v = d3[:, 0, :], d3[:, 1, :], d3[:, 2, :]
            pl = s_.psL
            P.mm_group(pl, [(g2[:, js], sgl)])
            yield
            P.copy(gg, pl, eng="act")
            P.dma(K.rw_gT.v(tb * TB, (tb + 1) * TB)[js, :], gg)
            yield
            P.mm_group(pl, [(wa2[0:64, js], twa[0:64, :])])
            yield
            P.act(sgw, pl, AF.Sigmoid, bias=w0[:, j:j + 1])
            yield
            P.mm_group(pl, [(wa2[64:128, js], twa[64:128, :])])
            yield
            P.act(a_, pl, AF.Sigmoid, bias=a0[:, j:j + 1])
            yield
            if l == 1:
                P.mm_group(pl, [(v2[0:32, js], vl[0:32, :])])
                yield
                P.act(t1, pl, AF.Sigmoid, bias=v0[:, j:j + 1])
                P.dma(s_.vfb, K.rw_vfT.v(tb * TB, (tb + 1) * TB)[js, :])
                yield
                P.tt(s_.vfb, s_.vfb, v, ALU.subtract)
                P.tt(s_.vfb, s_.vfb, t1, ALU.mult)
                P.tt(v, v, s_.vfb, ALU.add)
                yield
            else:
                P.dma(K.rw_vfT.v(tb * TB, (tb + 1) * TB)[js, :], v)
            P.ts(kk, k, k_k[:, j:j + 1], ALU.mult, eng="pool")
            yield
            P.act(sq, kk, AF.Square)
            yield
            pn = s_.psT
            P.mm_group(pn, [(K.blk64, sq)])
            yield
            P.act(sq, pn, AF.Sqrt)
            yield
            P.ts(sq, sq, 1e-12, ALU.max)
            P.recip(sq, sq)
            P.tt(kk, kk, sq, ALU.mult)
            P.ts(t1, a_, k_a[:, j:j + 1], ALU.mult, omka[:, j:j + 1], ALU.add)
            yield
            P.tt(kmod, k, t1, ALU.mult, eng="pool")
            P.tt(bb, kk, a_, ALU.mult)
            yield
            P.stt(rk, r, r_k[:, j:j + 1], kmod, ALU.mult, ALU.mult)
            yield
            P.mm_group(pn, [(K.blk64, rk)])
            yield
            P.tt(bon, pn, v, ALU.mult)
            P.dma(K.rw_bonT.v(tb * TB, (tb + 1) * TB)[js, :], bon)
            P.scan(cum, K.rmask, sgw, 0.0, ALU.mult, ALU.add)
            yield
            P.act(G, cum, AF.Exp, scale=-WSC)
            P.act(Gi, cum, AF.Exp, scale=WSC)
            P.tt(t1, cum, sgw, ALU.subtract, eng="pool")
            yield
            P.act(Gp, t1, AF.Exp, scale=-WSC)
            yield
            kapb, rhob, betb, ktlb, vb, bhb, khb = s_.ob
            P.tt(kapb, kk, Gp, ALU.mult)
            P.tt(rhob, r, G, ALU.mult, eng="pool")
            P.tt(bb, bb, Gi, ALU.mult)
            P.tt(kmod, kmod, Gi, ALU.mult, eng="pool")
            yield
            P.copy(betb, bb, eng="act")
            P.copy(ktlb, kmod, eng="act")
            P.copy(vb, v, eng="act")
            P.copy(s_.gam, G.re("p (n l) -> p n l", l=LRW)[:, :, LRW - 1])
            gb = s_.gam.un(2).bc([128, 8, LRW])
            P.tt(bhb.re("p (n l) -> p n l", l=LRW), bb.re("p (n l) -> p n l", l=LRW), gb, ALU.mult)
            P.tt(khb.re("p (n l) -> p n l", l=LRW), kmod.re("p (n l) -> p n l", l=LRW), gb, ALU.mult,
                 eng="pool")
            yield
            for (src, dst) in ((kapb, K.rw_kapT), (rhob, K.rw_rhoT), (betb, K.rw_betT), (ktlb, K.rw_ktlT)):
                P.dma(dst.v(tb * TB, (tb + 1) * TB)[js, :], src)
            P.dma(K.rw_gamT.v(tb * 8, tb * 8 + 8)[js, :], s_.gam)
            for xi, (X, dst) in enumerate(((vb, K.rw_vtok), (bhb, K.rw_bhat), (khb, K.rw_khat))):
                px = s_.psX
                for tt_ in range(4):
                    P.transpose(px[:, tt_ * 128:(tt_ + 1) * 128], X[:, tt_ * 128:(tt_ + 1) * 128], K.identb)
                yield
                if xi == 1:
                    P.copy(s_.stg[xi], px[:, 0:TB].re("p (a b) -> p a b", a=4), eng="act")
                else:
                    P.copy(s_.stg[xi], px[:, 0:TB].re("p (a b) -> p a b", a=4))
                P.dma(dst.v(tb * TB, (tb + 1) * TB)[:, js].re("(a t) e -> t a e", t=128), s_.stg[xi])
                yield

        for tb in range(NTB):
            first = (tb % 4 == 0)
            P.dma(hb, xview(K.hT, tb))
            if first:
                for pv_ in prevs:
                    P.memset(pv_, 0.0)
            nlo = 3 if l == 1 else 2
            prev = prevs[8]
            for i in range(nlo):
                ps = psA0
                if i < 2:
                    P.mm_group(ps, [(wrw[:, kc, (24 + i) * 128:(25 + i) * 128], hb[:, kc, :]) for kc in range(8)])
                    P.copy(zl[:, i, 1:TB + 1], ps, eng="act")
                else:
                    P.mm_group(ps[0:32, :], [(wvr[:, kc, :], hb[:, kc, :]) for kc in range(8)])
                    P.copy(zl[0:32, i, 1:TB + 1], ps[0:32, :], eng="act")
                P.copy(zl[:, i, 0:1], prev[:, i:i + 1])
                P.copy(prev[:, i:i + 1], zl[:, i, TB:TB + 1])
            P.tt(dl[:, 0:nlo, :], zl[:, 0:nlo, 0:TB], zl[:, 0:nlo, 1:TB + 1], ALU.subtract)
            P.stt(dl[:, 0, :], dl[:, 0, :], mu[:, 24:25], zl[:, 0, 1:TB + 1], ALU.mult, ALU.add)
            P.stt(dl[:, 1, :], dl[:, 1, :], mu[:, 25:26], zl[:, 1, 1:TB + 1], ALU.mult, ALU.add)
            P.act(twa[0:64, :], dl[0:64, 0, :], AF.Tanh)
            P.copy(twa[64:128, :], dl[64:128, 0, :], eng="act")
            P.act(sgl, dl[:, 1, :], AF.Sigmoid)
            if l == 1:
                P.stt(vl, dl[0:32, 2, :], muv[0:32, 0:1], zl[0:32, 2, 1:TB + 1], ALU.mult, ALU.add)
            for j0 in range(0, 8, 2):
                lockstep([jstream(tb, j0, sets[0]), jstream(tb, j0 + 1, sets[1])])


def stage_rw_rec_old(K, l, post):
    P = K.P
    NCH = T // LRW
    CB = 8
    L = LRW
    with P.scope():
        mask12 = P.sb([64, 128])
        P.copy(mask12[:, 0:64], K.tris[0:64, 0:64])
        P.copy(mask12[:, 64:128], K.tri[0:64, 0:64])
        lows = K.lows[0:64, 0:64]
        id64 = K.ident[0:64, 0:64]
        KRs = [P.sb([64, 4, CB, 2, L]) for _ in range(2)]
        BTs = [P.sb([64, 4, CB * L]) for _ in range(2)]
        KTs = [P.sb([64, 4, CB * L]) for _ in range(2)]
        gms = [P.sb([64, 4, CB]) for _ in range(2)]
        vts = [P.sb([64, CB, 256]) for _ in range(2)]
        bhs = [P.sb([64, CB, 256]) for _ in range(2)]
        khs = [P.sb([64, CB, 256]) for _ in range(2)]
        ysts = [P.sb([64, CB, 256]) for _ in range(2)]
        S = P.sb([64, 4, L])
        M1 = P.sb([64, 4, 128])
        M2 = P.sb([64, 4, 128])
        Pm = [P.sb([64, 4, L]) for _ in range(2)]
        Qm = [P.sb([64, 4, L]) for _ in range(2)]
        TT = P.sb([64, 4, L])
        Rn = P.sb([64, 4, L])
        U = P.sb([64, 4, L])
        A1 = P.ps([64, 4, 128])
        A2 = P.ps([64, 4, 128])
        A3 = P.ps([64, 4, L])
        PP = P.ps([64, 4, L])
        QQ = P.ps([64, 4, L])
        TP = P.ps([64, 4, L])
        RU = P.ps([64, 2, 4, L])
        YS = P.ps([64, 2, 4, L])
        RP, UP = RU[:, 0], RU[:, 1]
        YP, SP = YS[:, 0], YS[:, 1]
        it = 0
        import os as _os
        lim = [2, 4, 4]
        VAR = _os.environ.get("RW_VAR", "")
        gmall = P.sb([64, 4, NCH])
        for b in range(lim[0]):
            T0 = b * T
            for hg in range(lim[1]):
                P.memset(S, 0.0)
                for cb in range(lim[2]):
                    t0 = T0 + cb * CB * L
                    t1 = t0 + CB * L
                    KR, BT, KT, gm = KRs[it % 2], BTs[it % 2], KTs[it % 2], gms[it % 2]
                    vt, bh, kh, yst = vts[it % 2], bhs[it % 2], khs[it % 2], ysts[it % 2]
                    it += 1
                    for hh in range(4):
                        ch = slice(hg * 256 + hh * 64, hg * 256 + hh * 64 + 64)
                        P.dma(KR[:, hh, :, 0, :], K.rw_kapT.v(t0, t1)[ch, :].re("p (c l) -> p c l", l=L))
                        P.dma(KR[:, hh, :, 1, :], K.rw_rhoT.v(t0, t1)[ch, :].re("p (c l) -> p c l", l=L))
                        P.dma(BT[:, hh, :], K.rw_betT.v(t0, t1)[ch, :])
                        P.dma(KT[:, hh, :], K.rw_ktlT.v(t0, t1)[ch, :])
                        g0 = b * NCH + cb * CB
                        P.dma(gm[:, hh, :], K.rw_gamT.v(g0, g0 + CB)[ch, :])
                        if "gmall" in VAR and cb == 0:
                            P.dma(gmall[:, hh, :], K.rw_gamT.v(b * NCH, (b + 1) * NCH)[ch, :])
                    es = slice(hg * 256, hg * 256 + 256)
                    P.dma(vt, K.rw_vtok.v(t0, t1)[:, es].re("(c t) e -> t c e", t=L))
                    P.dma(bh, K.rw_bhat.v(t0, t1)[:, es].re("(c t) e -> t c e", t=L))
                    P.dma(kh, K.rw_khat.v(t0, t1)[:, es].re("(c t) e -> t c e", t=L))
                    for ci in range(CB):
                        cs = slice(ci * L, (ci + 1) * L)
                        for hh in range(4):
                            krr = KR[:, hh, ci, :, :].re("p a l -> p (a l)")
                            P.mm_group(A1[:, hh, :], [(BT[:, hh, cs], krr)])
                            P.mm_group(A2[:, hh, :], [(KT[:, hh, cs], krr)])
                            P.mm_group(A3[:, hh, :], [(KR[:, hh, ci, 0, :], BT[:, hh, cs])])
                        P.tt(M1, A1, mask12.un(1).bc([64, 4, 128]), ALU.mult)
                        P.tt(M2, A2, mask12.un(1).bc([64, 4, 128]), ALU.mult)
                        P.tt(Pm[0], A3, lows.un(1).bc([64, 4, L]), ALU.mult)
                        Pc = Pm[0]
                        Qc = M1[:, :, 0:L]
                        ArbT = M1[:, :, L:2 * L]
                        AkkT = M2[:, :, 0:L]
                        ArkT = M2[:, :, L:2 * L]
                        P.tt(TT, id64.un(1).bc([64, 4, L]), Qc, ALU.subtract)
                        for lev in range(1, 6):
                            Pn = Pm[lev % 2]
                            Qn = Qm[lev % 2]
                            for hh in range(4):
                                P.mm_group(PP[:, hh, :], [(Qc[:, hh, :], Pc[:, hh, :])])
                            if lev < 5:
                                for hh in range(4):
                                    P.mm_group(QQ[:, hh, :], [(Pc[:, hh, :], Qc[:, hh, :])])
                            P.copy(Pn, PP)
                            if lev < 5:
                                P.copy(Qn, QQ, eng="act")
                            for hh in range(4):
                                P.mm_group(TP[:, hh, :], [(Pn[:, hh, :], TT[:, hh, :])])
                            P.tt(TT, TT, TP, ALU.add)
                            Pc, Qc = Pn, Qn
                        for hh in range(4):
                            vs = slice(hh * 64, hh * 64 + 64)
                            P.mm_group(RP[:, hh, :], [(KR[:, hh, ci, 0, :], S[:, hh, :]), (AkkT[:, hh, :], vt[:, ci, vs])])
                        P.ts(Rn, RP, -1.0, ALU.mult)
                        for hh in range(4):
                            P.mm_group(UP[:, hh, :], [(TT[:, hh, :], Rn[:, hh, :])])
                        P.copy(U, UP)
                        for hh in range(4):
                            vs = slice(hh * 64, hh * 64 + 64)
                            P.mm_group(YP[:, hh, :], [(KR[:, hh, ci, 1, :], S[:, hh, :]), (ArbT[:, hh, :], U[:, hh, :]),
                                                      (ArkT[:, hh, :], vt[:, ci, vs])])
                        if "reorder" not in VAR:
                            P.copy(yst[:, ci, :].re("p (h e) -> p h e", h=4), YP, eng="act")
                        for hh in range(4):
                            vs = slice(hh * 64, hh * 64 + 64)
                            P.mm_group(SP[:, hh, :], [(bh[:, ci, vs], U[:, hh, :]), (kh[:, ci, vs], vt[:, ci, vs])])
                        if "reorder" in VAR:
                            P.copy(yst[:, ci, :].re("p (h e) -> p h e", h=4), YP, eng="act")
                        if "gmall" in VAR:
                            cc = cb * CB + ci
                            P.tt(S, S, gmall[:, :, cc:cc + 1].bc([64, 4, L]), ALU.mult)
                        else:
                            P.tt(S, S, gm[:, :, ci:ci + 1].bc([64, 4, L]), ALU.mult)
                        P.tt(S, S, SP, ALU.add)
                    P.dma(K.rw_ytok.v(t0, t1)[:, es].re("(c t) e -> t c e", t=L), yst)


def lockstep(gens):
    gens = list(gens)
    while gens:
        for g in list(gens):
            try:
                next(g)
            except StopIteration:
                gens.remove(g)


def stage_rw_rec(K, l):
    P = K.P
    import os as _os
    if _os.environ.get("RW_OLD"):
        stage_rw_rec_old(K, l, None)
        return
    NCH = T // LRW
    CB = int(_os.environ.get("RW_CB", "4"))
    NBUF = int(_os.environ.get("RW_NBUF", "2"))
    L = LRW
    with P.scope():
        mask12 = P.sb([64, 128])
        P.copy(mask12[:, 0:64], K.tris[0:64, 0:64])
        P.copy(mask12[:, 64:128], K.tri[0:64, 0:64])
        lows = K.lows[0:64, 0:64]
        id64 = K.ident[0:64, 0:64]

        def stream(b):
            KRs = [P.sb([64, 4, CB, 2, L], BF16) for _ in range(NBUF)]
            BTs = [P.sb([64, 4, CB * L], BF16) for _ in range(NBUF)]
            KTs = [P.sb([64, 4, CB * L], BF16) for _ in range(NBUF)]
            gms = [None, None]
            gmall = P.sb([64, 4, NCH])
            vts = [P.sb([64, CB, 256], BF16) for _ in range(NBUF)]
            bhs = [P.sb([64, CB, 256], BF16) for _ in range(NBUF)]
            khs = [P.sb([64, CB, 256], BF16) for _ in range(NBUF)]
            ysts = [P.sb([64, CB, 256]) for _ in range(NBUF)]
            S = P.sb([64, 4, L])
            Sb = P.sb([64, 4, L], BF16)
            M1 = P.sb([64, 4, 128], BF16)
            M2 = P.sb([64, 4, 128], BF16)
            Pm = [P.sb([64, 4, L], BF16) for _ in range(2)]
            Qm = [P.sb([64, 4, L], BF16) for _ in range(2)]
            TT = P.sb([64, 4, L], BF16)
            Rn = P.sb([64, 4, L], BF16)
            U = P.sb([64, 4, L], BF16)
            import os as _os
            if _os.environ.get("RW_PS") == "old":
                if not hasattr(K, "_rwps"):
                    K._rwps = dict(A1=P.ps([64, 4, 128]), A2=P.ps([64, 4, 128]), A3=P.ps([64, 4, L]), PP=P.ps([64, 4, L]),
                                   QQ=P.ps([64, 4, L]), TP=P.ps([64, 4, L]), RU=P.ps([64, 2, 4, L]), YS=P.ps([64, 2, 4, L]))
                d_ = K._rwps
                A1, A2, A3, PP, QQ, TP = d_["A1"], d_["A2"], d_["A3"], d_["PP"], d_["QQ"], d_["TP"]
                RP, UP = d_["RU"][:, 0], d_["RU"][:, 1]
                YP, SP = d_["YS"][:, 0], d_["YS"][:, 1]
            else:
                B0 = P.ps([64, 4, 128])
                B1 = P.ps([64, 4, 128])
                B2 = P.ps([64, 2, 4, L])
                B3 = P.ps([64, 2, 4, L])
                A1, A2 = B0, B1
                A3, PP = B2[:, 0], B2[:, 1]
                QQ, TP = B3[:, 0], B3[:, 1]
                RP, UP = B0[:, :, 0:L], B0[:, :, L:2 * L]
                YP, SP = B1[:, :, 0:L], B1[:, :, L:2 * L]
            it = 0
            yield
            T0 = b * T
            for hg in range(4):
                P.memset(S, 0.0)
                P.memset(Sb, 0.0)
                for cb in range(NCH // CB):
                    t0 = T0 + cb * CB * L
                    t1 = t0 + CB * L
                    KR, BT, KT = KRs[it % NBUF], BTs[it % NBUF], KTs[it % NBUF]
                    vt, bh, kh, yst = vts[it % NBUF], bhs[it % NBUF], khs[it % NBUF], ysts[it % NBUF]
                    it += 1
                    for hh in range(4):
                        ch = slice(hg * 256 + hh * 64, hg * 256 + hh * 64 + 64)
                        P.dma(KR[:, hh, :, 0, :], K.rw_kapT.v(t0, t1)[ch, :].re("p (c l) -> p c l", l=L))
                        P.dma(KR[:, hh, :, 1, :], K.rw_rhoT.v(t0, t1)[ch, :].re("p (c l) -> p c l", l=L))
                        P.dma(BT[:, hh, :], K.rw_betT.v(t0, t1)[ch, :])
                        P.dma(KT[:, hh, :], K.rw_ktlT.v(t0, t1)[ch, :])
                        if cb == 0:
                            P.dma(gmall[:, hh, :], K.rw_gamT.v(b * NCH, (b + 1) * NCH)[ch, :])
                    es = slice(hg * 256, hg * 256 + 256)
                    P.dma(vt, K.rw_vtok.v(t0, t1)[:, es].re("(c t) e -> t c e", t=L))
                    P.dma(bh, K.rw_bhat.v(t0, t1)[:, es].re("(c t) e -> t c e", t=L))
                    P.dma(kh, K.rw_khat.v(t0, t1)[:, es].re("(c t) e -> t c e", t=L))
                    yield
                    for ci in range(CB):
                        cs = slice(ci * L, (ci + 1) * L)
                        for hh in range(4):
                            krr = KR[:, hh, ci, :, :].re("p a l -> p (a l)")
                            P.mm_group(A1[:, hh, :], [(BT[:, hh, cs], krr)])
                            P.mm_group(A2[:, hh, :], [(KT[:, hh, cs], krr)])
                            P.mm_group(A3[:, hh, :], [(KR[:, hh, ci, 0, :], BT[:, hh, cs])])
                        yield
                        P.tt(M1, A1, mask12.un(1).bc([64, 4, 128]), ALU.mult)
                        P.tt(Pm[0], A3, lows.un(1).bc([64, 4, L]), ALU.mult)
                        P.tt(M2, A2, mask12.un(1).bc([64, 4, 128]), ALU.mult, eng="pool") if False else P.tt(M2, A2, mask12.un(1).bc([64, 4, 128]), ALU.mult)
                        Pc = Pm[0]
                        Qc = M1[:, :, 0:L]
                        ArbT = M1[:, :, L:2 * L]
                        AkkT = M2[:, :, 0:L]
                        ArkT = M2[:, :, L:2 * L]
                        P.tt(TT, id64.un(1).bc([64, 4, L]), Qc, ALU.subtract)
                        yield
                        for lev in range(1, 6):
                            Pn = Pm[lev % 2]
                            Qn = Qm[lev % 2]
                            for hh in range(4):
                                P.mm_group(PP[:, hh, :], [(Qc[:, hh, :], Pc[:, hh, :])])
                            if lev < 5:
                                for hh in range(4):
                                    P.mm_group(QQ[:, hh, :], [(Pc[:, hh, :], Qc[:, hh, :])])
                            yield
                            P.copy(Pn, PP)
                            if lev < 5:
                                P.copy(Qn, QQ, eng="act")
                            yield
                            for hh in range(4):
                                P.mm_group(TP[:, hh, :], [(Pn[:, hh, :], TT[:, hh, :])])
                            yield
                            P.tt(TT, TT, TP, ALU.add)
                            yield
                            Pc, Qc = Pn, Qn
                        for hh in range(4):
                            vs = slice(hh * 64, hh * 64 + 64)
                            P.mm_group(RP[:, hh, :], [(KR[:, hh, ci, 0, :], Sb[:, hh, :]), (AkkT[:, hh, :], vt[:, ci, vs])])
                        yield
                        P.ts(Rn, RP, -1.0, ALU.mult)
                        yield
                        for hh in range(4):
                            P.mm_group(UP[:, hh, :], [(TT[:, hh, :], Rn[:, hh, :])])
                        yield
                        P.copy(U, UP)
                        yield
                        for hh in range(4):
                            vs = slice(hh * 64, hh * 64 + 64)
                            P.mm_group(YP[:, hh, :], [(KR[:, hh, ci, 1, :], Sb[:, hh, :]), (ArbT[:, hh, :], U[:, hh, :]),
                                                      (ArkT[:, hh, :], vt[:, ci, vs])])
                        yield
                        P.copy(yst[:, ci, :].re("p (h e) -> p h e", h=4), YP, eng="act")
                        yield
                        for hh in range(4):
                            vs = slice(hh * 64, hh * 64 + 64)
                            P.mm_group(SP[:, hh, :], [(bh[:, ci, vs], U[:, hh, :]), (kh[:, ci, vs], vt[:, ci, vs])])
                        yield
                        cc = cb * CB + ci
                        P.tt(S, S, gmall[:, :, cc:cc + 1].bc([64, 4, L]), ALU.mult)
                        P.tt(S, S, SP, ALU.add)
                        P.copy(Sb, S, eng="act")
                        yield
                    P.dma(K.rw_ytok.v(t0, t1)[:, es].re("(c t) e -> t c e", t=L), yst)
        import os as _os
        if _os.environ.get("RW_SEQ"):
            for b in range(NB):
                lockstep([stream(b)])
        else:
            lockstep([stream(b) for b in range(NB)])
    with P.scope():
        lnw, lnb = K.vec("rw_ln_w%d" % l), K.vec("rw_ln_b%d" % l)
        ys = [P.sb([128, 16, 64]) for _ in range(2)]
        cen = P.sb([128, 16, 64])
        sq = P.sb([128, 16, 64])
        sm = P.sb([128, 48])
        bons = [P.sb([128, 8, 128]) for _ in range(2)]
        ggs = [P.sb([128, 8, 128]) for _ in range(2)]
        tmp = P.sb([128, 8, 128])
        yo = [P.sb([128, 8, 128], BF16) for _ in range(2)]
        px = [P.ps([128, 4, 128]) for _ in range(2)]
        for tt_ in range(TOK // 128):
            t0 = tt_ * 128
            y, bon, gg, o = ys[tt_ % 2], bons[tt_ % 2], ggs[tt_ % 2], yo[tt_ % 2]
            P.dma(y, K.rw_ytok.v(t0, t0 + 128).re("t (h e) -> t h e", e=64))
            P.dma(bon, K.rw_bonT.v(t0, t0 + 128).re("(c p) t -> p c t", p=128))
            P.dma(gg, K.rw_gT.v(t0, t0 + 128).re("(c p) t -> p c t", p=128))
            P.reduce(sm[:, 0:16], y, ALU.add)
            P.ts(sm[:, 0:16], sm[:, 0:16], 1.0 / 64.0, ALU.mult)
            P.tt(cen, y, sm[:, 0:16].un(2).bc([128, 16, 64]), ALU.subtract)
            P.tt(sq, cen, cen, ALU.mult, eng="pool")
            P.reduce(sm[:, 16:32], sq, ALU.add)
            P.act(sm[:, 32:48], sm[:, 16:32], AF.Sqrt, bias=K.epsb[:, 1:2], scale=1.0 / 64.0)
            P.recip(sm[:, 32:48], sm[:, 32:48])
            P.tt(cen, cen, sm[:, 32:48].un(2).bc([128, 16, 64]), ALU.mult)
            cf = cen.re("p h e -> p (h e)")
            for j in range(8):
                P.transpose(px[j // 4][:, j % 4, :], cf[:, j * 128:(j + 1) * 128], K.ident)
            for j in range(8):
                P.ts(tmp[:, j, :], px[j // 4][:, j % 4, :], lnw[:, j:j + 1], ALU.mult, lnb[:, j:j + 1], ALU.add)
            P.tt(tmp, tmp, bon, ALU.add, eng="pool")
            P.tt(o, tmp, gg, ALU.mult)
            P.dma(K.yT[1].v(t0, t0 + 128).re("(c p) t -> p c t", p=128), o)


def stage_mix(K, l):
    P = K.P
    with P.scope():
        wgt = P.sb([128, 8, 3072], BF16)
        load_w_cols(P, wgt, K.w_in[l], C_GATE, C_GATE + 3072)
        wbr = P.sb([128, 3, 8, D], BF16)
        P.dma(wbr, K.w_branch[l], eng="pool")
        wo = P.sb([128, 8, D], BF16)
        P.dma(wo, K.w_out[l], eng="pool")
        hbs = [P.sb([128, 8, TB], BF16)] * 2
        yb = [P.sb([128, 8, TB], BF16) for _ in range(3)]
        xb = P.sb([128, 8, TB])
        xo = P.sb([128, 8, TB])
        mixb = P.sb([128, 8, TB], BF16)
        gts = [P.sb([128, TB]) for _ in range(2)]
        tmp = P.sb([128, TB])
        acc = P.sb([128, TB])
        psG = [P.ps([128, TB]) for _ in range(2)]
        psY = [P.ps([128, TB]) for _ in range(2)]
        psO = [P.ps([128, TB]) for _ in range(2)]
        for tb in range(NTB):
            b = tb // 4
            hb = hbs[tb % 2]
            P.dma(hb, xview(K.hT, tb))
            for br in range(3):
                P.dma(yb[br], xview(K.yT[br], tb))
            P.dma(xb, xview(K.xs, tb))
            i = 0
            for n in range(8):
                ns = slice(n * 128, (n + 1) * 128)
                for br in range(3):
                    pg, py, g = psG[i % 2], psY[i % 2], gts[i % 2]
                    i += 1
                    P.mm_group(pg, [(wgt[:, kc, br * 1024 + n * 128:br * 1024 + (n + 1) * 128], hb[:, kc, :]) for kc in range(8)])
                    P.mm_group(py, [(wbr[:, br, kc, ns], yb[br][:, kc, :]) for kc in range(8)])
                    P.act(g, pg, AF.Sigmoid)
                    if br == 0:
                        P.tt(acc, g, py, ALU.mult)
                    elif br == 1:
                        P.tt(tmp, g, py, ALU.mult)
                        P.tt(acc, acc, tmp, ALU.add, eng="pool")
                    else:
                        P.tt(tmp, g, py, ALU.mult)
                        P.tt(mixb[:, n, :], acc, tmp, ALU.add, eng="pool")
            for m in range(8):
                po = psO[m % 2]
                P.mm_group(po, [(wo[:, kc, m * 128:(m + 1) * 128], mixb[:, kc, :]) for kc in range(8)])
                P.stt(xo[:, m, :], po, K.gt[l][1][:, m, b:b + 1], xb[:, m, :], ALU.mult, ALU.add)
            P.dma(xview(K.xs, tb), xo)
def prep_core_inputs(inp, vecs, W, core):
    b0 = core * NB
    x = np.asarray(inp["x"], np.float32)[b0:b0 + NB].reshape(TOK, D)
    c = np.asarray(inp["c"], np.float32)[b0:b0 + NB]
    m = dict(W)
    m["xT"] = np.ascontiguousarray(x.T)
    m["cT"] = np.ascontiguousarray(c.reshape(NB, KC, 128).transpose(2, 1, 0))
    m["vecs"] = vecs
    return m


_CACHE = {}


def kernel(**inputs):
    from concourse.bass_utils import run_bass_kernel_spmd
    vp = pack_vecs(inputs)
    vecs = vp.build()
    W = host_weights(inputs)
    key = "main"
    if key not in _CACHE:
        _CACHE[key] = build_program(vp.off, vp.n)
    nc = _CACHE[key]
    in_maps = [prep_core_inputs(inputs, vecs, W, c) for c in range(8)]
    res = run_bass_kernel_spmd(nc, in_maps, core_ids=list(range(8)))
    out = np.empty((16, T, D), np.float32)
    for c in range(8):
        o = np.asarray(res.results[c]["outT"])
        out[c * NB:(c + 1) * NB] = o.T.reshape(NB, T, D)
    return out
```
